# Optimizing a Trainium2 kernel written in Bass

```python
import math
import jax
import jax.numpy as jnp
from jax import lax
import numpy as np

D_MODEL = 1024
BATCH = 4
SEQ = 8192
DEPTH = 1

N_META = 16
ATT_HEAD_DIM = 64
ATT_HEADS = D_MODEL // 128
ATT_WIDTH = ATT_HEADS * ATT_HEAD_DIM
Q_BLOCK = 128
SSM_WIDTH = D_MODEL // 2
SSM_GROUP_CH = 16
SSM_GROUPS = SSM_WIDTH // SSM_GROUP_CH
SSM_STATE = 64
DT_MIN = 0.001
DT_MAX = 0.1
IN_COLS = 3 * ATT_WIDTH + SSM_WIDTH + 2 * D_MODEL
MOE_GROUPS = 4
EXPERTS_PER_GROUP = 8
N_EXPERTS = MOE_GROUPS * EXPERTS_PER_GROUP
INNER_TOP_K = 2
EXPERT_FF = D_MODEL // 2
MOE_BLOCK = 128

RMS_EPS = 1e-6
NEG_BIG = -1e30

kernel_name = "hybrid_stickbreak_s5_hmoe_block"


def rms_norm(x, g):
    x32 = x.astype(jnp.float32)
    y = x32 * lax.rsqrt(jnp.mean(x32 * x32, axis=-1, keepdims=True) + RMS_EPS)
    return (y * g.astype(jnp.float32)).astype(x.dtype)


def stick_breaking_attention(q, k, v):
    b, l, _ = q.shape
    pad = (-N_META) % Q_BLOCK
    lp = l + pad

    def heads(t):
        t = jnp.pad(t, ((0, 0), (pad, 0), (0, 0)))
        return t.reshape(b, lp, ATT_HEADS, ATT_HEAD_DIM).transpose(0, 2, 1, 3)

    qh, kh, vh = heads(q), heads(k), heads(v)
    n_blocks = lp // Q_BLOCK
    key_pos = jnp.arange(lp)
    scale = ATT_HEAD_DIM ** -0.5

    def one_block(i):
        start = i * Q_BLOCK
        qb = lax.dynamic_slice_in_dim(qh, start, Q_BLOCK, axis=2)
        z = jnp.einsum('bhqd,bhkd->bhqk', qb, kh).astype(jnp.float32) * scale
        q_pos = start + jnp.arange(Q_BLOCK)
        valid = (key_pos[None, :] < q_pos[:, None]) & (key_pos[None, :] >= pad)
        log_beta = jnp.where(valid, jax.nn.log_sigmoid(z), NEG_BIG)
        log_keep = jnp.where(valid, jax.nn.log_sigmoid(-z), 0.0)
        tail = lax.cumsum(log_keep, axis=3, reverse=True)
        log_stick = jnp.concatenate([tail[..., 1:], jnp.zeros_like(tail[..., :1])], axis=-1)
        w = jnp.exp(log_beta + log_stick)
        return jnp.einsum('bhqk,bhkd->bhqd', w.astype(vh.dtype), vh)

    out = lax.map(one_block, jnp.arange(n_blocks))
    out = out.transpose(1, 0, 3, 2, 4).reshape(b, lp, ATT_WIDTH)
    return out[:, pad:]


def s5_ssm(u, lam_re, lam_im, log_dt, b_re, b_im, c_re, c_im, d_skip):
    b, l, _ = u.shape
    f32 = jnp.float32
    ug = u.astype(f32).reshape(b, l, SSM_GROUPS, SSM_GROUP_CH)
    lr, li = lam_re.astype(f32), lam_im.astype(f32)
    dt = jnp.exp(log_dt.astype(f32))[:, None]
    mag = jnp.exp(lr * dt)
    lb_re, lb_im = mag * jnp.cos(li * dt), mag * jnp.sin(li * dt)
    nr, ni = lb_re - 1.0, lb_im
    den = lr * lr + li * li
    f_re = (nr * lr + ni * li) / den
    f_im = (ni * lr - nr * li) / den
    br, bi = b_re.astype(f32), b_im.astype(f32)
    bb_re = f_re[:, :, None] * br - f_im[:, :, None] * bi
    bb_im = f_re[:, :, None] * bi + f_im[:, :, None] * br
    bu_re = jnp.einsum('blgc,gpc->lbgp', ug, bb_re)
    bu_im = jnp.einsum('blgc,gpc->lbgp', ug, bb_im)
    a_re = jnp.broadcast_to(lb_re, (l,) + lb_re.shape)
    a_im = jnp.broadcast_to(lb_im, (l,) + lb_im.shape)

    def combine(e1, e2):
        a1r, a1i, b1r, b1i = e1
        a2r, a2i, b2r, b2i = e2
        ar = a2r * a1r - a2i * a1i
        ai = a2r * a1i + a2i * a1r
        a2r_b, a2i_b = a2r[:, None], a2i[:, None]
        new_br = a2r_b * b1r - a2i_b * b1i + b2r
        new_bi = a2r_b * b1i + a2i_b * b1r + b2i
        return ar, ai, new_br, new_bi

    _, _, x_re, x_im = lax.associative_scan(combine, (a_re, a_im, bu_re, bu_im), axis=0)
    y = (jnp.einsum('lbgp,gcp->blgc', x_re, c_re.astype(f32))
         - jnp.einsum('lbgp,gcp->blgc', x_im, c_im.astype(f32))
         + d_skip.astype(f32).reshape(SSM_GROUPS, SSM_GROUP_CH) * ug)
    return y.reshape(b, l, SSM_WIDTH)


def hierarchical_moe(x, rg_w, rg_b, re_w, re_b, w1, w3, w2):
    b, l, d = x.shape
    t = b * l
    xt = x.reshape(t, d)
    group_probs = jax.nn.softmax((xt @ rg_w).astype(jnp.float32) + rg_b.astype(jnp.float32), axis=-1)
    p_top, g_top = lax.top_k(group_probs, 1)
    e_logits = ((xt @ re_w).astype(jnp.float32) + re_b.astype(jnp.float32)).reshape(t, MOE_GROUPS, EXPERTS_PER_GROUP)
    sel = jnp.take_along_axis(e_logits, g_top[:, :, None], axis=1)[:, 0]
    e_val, e_idx = lax.top_k(sel, INNER_TOP_K)
    gate = jax.nn.softmax(e_val, axis=-1) * p_top
    expert_id = g_top * EXPERTS_PER_GROUP + e_idx

    n = t * INNER_TOP_K
    n_blocks = (n + N_EXPERTS * (MOE_BLOCK - 1)) // MOE_BLOCK + 1
    cap = n_blocks * MOE_BLOCK
    e_flat = expert_id.reshape(n).astype(jnp.int32)
    tok_flat = jnp.repeat(jnp.arange(t, dtype=jnp.int32), INNER_TOP_K)
    w_flat = gate.reshape(n)
    order = jnp.argsort(e_flat)
    e_s, t_s, w_s = e_flat[order], tok_flat[order], w_flat[order]
    counts = jnp.bincount(e_flat, length=N_EXPERTS)
    padded = ((counts + MOE_BLOCK - 1) // MOE_BLOCK) * MOE_BLOCK
    start = jnp.cumsum(counts) - counts
    pend = jnp.cumsum(padded)
    pstart = pend - padded
    dest = pstart[e_s] + (jnp.arange(n, dtype=jnp.int32) - start[e_s])
    slot_tok = jnp.full((cap,), t, dtype=jnp.int32).at[dest].set(t_s)
    slot_w = jnp.zeros((cap,), jnp.float32).at[dest].set(w_s)
    block_e = jnp.minimum(jnp.searchsorted(pend, jnp.arange(n_blocks) * MOE_BLOCK, side='right'), N_EXPERTS - 1)
    x_pad = jnp.concatenate([xt, jnp.zeros((1, d), xt.dtype)], axis=0)
    xb = x_pad[slot_tok].reshape(n_blocks, MOE_BLOCK, d)

    def expert_block(args):
        xi, e = args
        hdn = jax.nn.silu(xi @ w1[e]) * (xi @ w3[e])
        return hdn @ w2[e]

    yb = lax.map(expert_block, (xb, block_e)).reshape(cap, d)
    y = jax.ops.segment_sum(yb * slot_w[:, None].astype(yb.dtype), slot_tok, num_segments=t + 1)[:t]
    return y.reshape(b, l, d)


def setup_inputs(seed: int = 0) -> dict:
    key = jax.random.key(seed)
    ks = jax.random.split(key, 32)
    nrm = jax.random.normal
    f32 = jnp.float32
    D, G, P, C, S = D_MODEL, SSM_GROUPS, SSM_STATE, SSM_GROUP_CH, SSM_WIDTH
    lam_im0 = math.pi * jnp.arange(P, dtype=f32)
    return {
        "x": nrm(ks[0], (BATCH, SEQ, D), f32),
        "meta_tokens": nrm(ks[1], (N_META, D), f32),
        "norm_mix_g": 1.0 + 0.01 * nrm(ks[2], (DEPTH, D), f32),
        "w_in": nrm(ks[3], (DEPTH, D, IN_COLS), f32) * D ** -0.5,
        "ssm_lambda_re": -0.5 + 0.01 * nrm(ks[4], (DEPTH, G, P), f32),
        "ssm_lambda_im": lam_im0 + 0.01 * nrm(ks[5], (DEPTH, G, P), f32),
        "ssm_log_dt": jax.random.uniform(ks[6], (DEPTH, G), f32, math.log(DT_MIN), math.log(DT_MAX)),
        "ssm_b_re": nrm(ks[7], (DEPTH, G, P, C), f32) * (2 * C) ** -0.5,
        "ssm_b_im": nrm(ks[8], (DEPTH, G, P, C), f32) * (2 * C) ** -0.5,
        "ssm_c_re": nrm(ks[9], (DEPTH, G, C, P), f32) * P ** -0.5,
        "ssm_c_im": nrm(ks[10], (DEPTH, G, C, P), f32) * P ** -0.5,
        "ssm_d": nrm(ks[11], (DEPTH, S), f32),
        "ssm_glu_w": nrm(ks[12], (DEPTH, S, S), f32) * S ** -0.5,
        "ssm_glu_b": 0.01 * nrm(ks[13], (DEPTH, S), f32),
        "w_branch_attn": nrm(ks[14], (DEPTH, ATT_WIDTH, D), f32) * ATT_WIDTH ** -0.5,
        "w_branch_ssm": nrm(ks[15], (DEPTH, S, D), f32) * S ** -0.5,
        "w_out": nrm(ks[16], (DEPTH, D, D), f32) * D ** -0.5,
        "norm_ffn_g": 1.0 + 0.01 * nrm(ks[17], (DEPTH, D), f32),
        "router_group_w": nrm(ks[18], (DEPTH, D, MOE_GROUPS), f32) * D ** -0.5,
        "router_group_b": 0.01 * nrm(ks[19], (DEPTH, MOE_GROUPS), f32),
        "router_expert_w": nrm(ks[20], (DEPTH, D, N_EXPERTS), f32) * D ** -0.5,
        "router_expert_b": 0.01 * nrm(ks[21], (DEPTH, N_EXPERTS), f32),
        "expert_w1": nrm(ks[22], (DEPTH, N_EXPERTS, D, EXPERT_FF), f32) * D ** -0.5,
        "expert_w3": nrm(ks[23], (DEPTH, N_EXPERTS, D, EXPERT_FF), f32) * D ** -0.5,
        "expert_w2": nrm(ks[24], (DEPTH, N_EXPERTS, EXPERT_FF, D), f32) * EXPERT_FF ** -0.5,
        "norm_final_g": 1.0 + 0.01 * nrm(ks[25], (D,), f32),
    }


def reference(x, meta_tokens, norm_mix_g, w_in, ssm_lambda_re, ssm_lambda_im, ssm_log_dt,
              ssm_b_re, ssm_b_im, ssm_c_re, ssm_c_im, ssm_d, ssm_glu_w, ssm_glu_b,
              w_branch_attn, w_branch_ssm, w_out, norm_ffn_g, router_group_w, router_group_b,
              router_expert_w, router_expert_b, expert_w1, expert_w3, expert_w2, norm_final_g):
    b = x.shape[0]
    meta = jnp.broadcast_to(meta_tokens.astype(x.dtype)[None], (b, N_META, D_MODEL))
    h = jnp.concatenate([meta, x], axis=1)
    splits = np.cumsum([ATT_WIDTH, ATT_WIDTH, ATT_WIDTH, SSM_WIDTH, D_MODEL]).tolist()
    for layer in range(DEPTH):
        xn = rms_norm(h, norm_mix_g[layer])
        proj = xn @ w_in[layer]
        q, k, v, u, ga, gb = jnp.split(proj, splits, axis=-1)
        attn = stick_breaking_attention(q, k, v)
        y = jax.nn.gelu(s5_ssm(u, ssm_lambda_re[layer], ssm_lambda_im[layer], ssm_log_dt[layer],
                               ssm_b_re[layer], ssm_b_im[layer], ssm_c_re[layer], ssm_c_im[layer],
                               ssm_d[layer]))
        ssm = (y * jax.nn.sigmoid(y @ ssm_glu_w[layer].astype(jnp.float32)
                                  + ssm_glu_b[layer].astype(jnp.float32))).astype(x.dtype)
        merged = (jax.nn.sigmoid(ga) * (attn @ w_branch_attn[layer])
                  + jax.nn.sigmoid(gb) * (ssm @ w_branch_ssm[layer]))
        h = h + merged @ w_out[layer]
        hn = rms_norm(h, norm_ffn_g[layer])
        h = h + hierarchical_moe(hn, router_group_w[layer], router_group_b[layer],
                                 router_expert_w[layer], router_expert_b[layer],
                                 expert_w1[layer], expert_w3[layer], expert_w2[layer])
    return rms_norm(h, norm_final_g)[:, N_META:]
```

```python
import math
from contextlib import ExitStack
import numpy as np
import ml_dtypes
import concourse.bass as bass
import concourse.mybir as mybir
from concourse.bass_utils import run_bass_kernel_spmd

F32 = mybir.dt.float32
BF16 = mybir.dt.bfloat16
I32 = mybir.dt.int32
AF = mybir.ActivationFunctionType
ALU = mybir.AluOpType
AX = mybir.AxisListType

SAME_ENGINE_SYNC = True
NDS = 20
D = 1024
TWO_PI = 2.0 * math.pi


class Buf:
    def __init__(self, name):
        self.name = name
        self.w = None
        self.r = {}


class View:
    def __init__(self, ap, buf):
        self.ap = ap
        self.buf = buf

    def __getitem__(self, k):
        return View(self.ap[k], self.buf)

    def re(self, pat, **kw):
        return View(self.ap.rearrange(pat, **kw), self.buf)

    def bc(self, shape):
        return View(self.ap.broadcast_to(list(shape)), self.buf)

    def un(self, axis):
        return View(self.ap.unsqueeze(axis), self.buf)


class T:
    def __init__(self, h, name):
        self.h = h
        self.buf = Buf(name)

    def __getitem__(self, k):
        return View(self.h[k], self.buf)


class Sched:
    ENG = ["pe", "act", "dve", "pool", "sp"]

    def __init__(self, nc, stack):
        self.nc = nc
        self.stack = stack
        self.epoch = {e: 0 for e in self.ENG}
        self.last_tok = {e: None for e in self.ENG}
        self.sem = {e: stack.enter_context(nc.semaphore("s_" + e)) for e in self.ENG}
        self.cnt = {e: 0 for e in self.ENG}
        self.ops = {e: [] for e in self.ENG}
        self.waited = {e: {} for e in self.ENG}
        self.dsem = {q: [stack.enter_context(nc.semaphore("d_%s%d" % (q, i))) for i in range(NDS)]
                     for q in ("sp", "pool")}
        self.dcnt = {q: [0] * NDS for q in ("sp", "pool")}
        self.drr = {"sp": 0, "pool": 0}

    def _deps(self, reads, writes):
        deps = []
        for b in reads:
            if b.w is not None:
                deps.append(b.w)
        for b in writes:
            if b.w is not None:
                deps.append(b.w)
            deps.extend(b.r.values())
        return deps

    def _waits(self, e, deps):
        need = {}
        for (key, sem, val) in deps:
            if val <= 0:
                continue
            if key[0] == "E" and key[1] == e and (e == "pe" or not SAME_ENGINE_SYNC):
                continue
            if self.waited[e].get(key, 0) >= val:
                continue
            if key not in need or need[key][1] < val:
                need[key] = (sem, val)
        for key, (sem, val) in need.items():
            self.waited[e][key] = val
        return list(need.values())

    def _record(self, tok, reads, writes):
        for b in reads:
            old = b.r.get(tok[0])
            if old is None or old[2] < tok[2]:
                b.r[tok[0]] = tok
        for b in writes:
            b.w = tok
            b.r = {}

    def op(self, e, fn, reads=(), writes=()):
        waits = self._waits(e, self._deps(reads, writes))
        if self.cnt[e] >= 16000:
            self.epoch[e] += 1
            self.sem[e] = self.stack.enter_context(self.nc.semaphore("s_%s_%d" % (e, self.epoch[e])))
            self.cnt[e] = 0
        self.cnt[e] += 1
        tok = (("E", e, self.epoch[e]), self.sem[e], self.cnt[e])
        self.last_tok[e] = tok
        self.ops[e].append((waits, fn, self.sem[e], 1))
        self._record(tok, reads, writes)

    def dma(self, q, fn, reads=(), writes=()):
        deps = self._deps(reads, writes)
        i = self.drr[q]
        self.drr[q] = (i + 1) % NDS
        sem = self.dsem[q][i]
        key = ("D", q, i)
        prev = self.dcnt[q][i]
        deps.append((key, sem, prev))
        waits = self._waits(q, deps)
        self.dcnt[q][i] = prev + 16
        tok = (key, sem, prev + 16)
        self.ops[q].append((waits, fn, sem, 16))
        self._record(tok, reads, writes)

    def barrier(self):
        toks = [self.last_tok[e] for e in self.ENG if self.last_tok[e] is not None]
        for q in ("sp", "pool"):
            for i in range(NDS):
                if self.dcnt[q][i] > 0:
                    toks.append((("D", q, i), self.dsem[q][i], self.dcnt[q][i]))
        for e in self.ENG:
            waits = self._waits(e, toks)
            if waits:
                self.ops[e].append((waits, None, None, 0))

    def replay(self, e, eng):
        for waits, fn, sem, inc in self.ops[e]:
            for (s, v) in waits:
                eng.wait_ge(s, v)
            if fn is not None:
                fn(eng).then_inc(sem, inc)

    def final_waits(self, e, eng, bufs):
        deps = []
        for b in bufs:
            if b.w is not None:
                deps.append(b.w)
        for (s, v) in self._waits(e, deps):
            eng.wait_ge(s, v)


def build(NSLOT, CAPB, debug=(), moe=True, nphase=4):
    NB = 1 + 8 * NSLOT
    NPOS = NB * 128
    TOWN = 512 * NSLOT
    NOB = 4 * NSLOT
    CAP = 128 * CAPB
    NXB = 32 * CAP
    nc = bass.Bass("TRN2", target_bir_lowering=False)
    stack = ExitStack()
    S = Sched(nc, stack)

    def din(name, shape, dt=F32):
        return T(nc.dram_tensor(name, list(shape), dt, kind="ExternalInput").ap(), name)

    def dscr(name, shape, dt=F32):
        kind = "ExternalOutput" if "scr" in debug else "Internal"
        return T(nc.dram_tensor(name, list(shape), dt, kind=kind).ap(), name)

    def dout(name, shape, dt=F32):
        return T(nc.dram_tensor(name, list(shape), dt, kind="ExternalOutput").ap(), name)

    cur = [stack]

    def sb(name, shape, dt=F32):
        return T(cur[0].enter_context(nc.sbuf_tensor(name, list(shape), dt)), name)

    def open_scope():
        st = ExitStack()
        cur.insert(0, st)
        return st

    def close_scope(st):
        assert cur[0] is st
        S.barrier()
        st.close()
        cur.pop(0)

    x_all = din("x_all", [NPOS, D])
    x_own = din("x_own", [TOWN, D])
    w_in = din("w_in", [D, 4096])
    g_mix = din("g_mix", [1, D])
    g_ffn = din("g_ffn", [1, D])
    g_fin = din("g_fin", [1, D])
    lam_re = din("lam_re", [1, 2048])
    lam_im = din("lam_im", [1, 2048])
    log_dt = din("log_dt", [1, 32])
    b_re = din("b_re", [32, 64, 16])
    b_im = din("b_im", [32, 64, 16])
    c_re = din("c_re", [512, 64])
    c_im = din("c_im", [512, 64])
    d_skip = din("d_skip", [1, 512])
    glu_w = din("glu_w", [512, 512])
    glu_b = din("glu_b", [1, 512])
    w_pa = din("w_pa", [512, D])
    w_pb = din("w_pb", [512, D])
    w_o = din("w_o", [D, D])
    w_r = din("w_r", [D, 36])
    b_r = din("b_r", [1, 36])
    NE = 32 if moe else 1
    w1 = din("w1", [NE, D, 512])
    w3 = din("w3", [NE, D, 512])
    w2 = din("w2", [NE, 512, D])
    k_ident = din("k_ident", [128, 128])
    k_triltf = din("k_triltf", [128, 128])
    k_onesf = din("k_onesf", [128, 128])
    k_jrow = din("k_jrow", [1, 128])
    k_maskB = din("k_maskB", [128, 32])
    k_maskC = din("k_maskC", [128, 128])
    k_ecap = din("k_ecap", [128, 32])
    k_maskQ = din("k_maskQ", [128, 2])
    k_maskR = din("k_maskR", [128, 256])
    k_trige = din("k_trige", [128, 128], BF16)
    k_trilt = din("k_trilt", [128, 128], BF16)
    k_trile = din("k_trile", [128, 128], BF16)
    k_mask0 = din("k_mask0", [128, 512], BF16)
    k_maske = din("k_maske", [128, 8, 512], BF16)
    k_masko = din("k_masko", [128, 8, 512], BF16)
    k_sel = din("k_sel", [128, 2 * NSLOT])

    kt_scr = dscr("kt_scr", [512, NPOS], BF16)
    v_scr = dscr("v_scr", [8, 128, NB, 64], BF16)
    ssm_scr = dscr("ssm_scr", [4, 128, NPOS], BF16)
    h_scr = dscr("h_scr", [TOWN, D])
    xb_scr = dscr("xb_scr", [NXB, D])
    yb_scr = dscr("yb_scr", [NXB, D])
    out_own = dout("out_own", [TOWN, D])

    dbg_outs = {}

    def dbg(name, view, shape, dt=F32):
        if name not in debug:
            return
        t = dout("dbg_" + name, shape, dt)
        dbg_outs[name] = t
        dma("sp", t[:], view)

    bcreg = {}

    def bc_reg(e):
        if "r" not in bcreg:
            bcreg["r"] = e.to_reg(NXB - 1)
        return bcreg["r"]

    def mm(out, lhsT, rhs, start=True, stop=True):
        S.op("pe", lambda e: e.matmul(out.ap, lhsT.ap, rhs.ap, start=start, stop=stop),
             reads=[lhsT.buf, rhs.buf], writes=[out.buf])

    def tr(out, in_, ident_v):
        S.op("pe", lambda e: e.transpose(out.ap, in_.ap, ident_v.ap),
             reads=[in_.buf, ident_v.buf], writes=[out.buf])

    def act(out, in_, func, bias=None, scale=None, accum=None, extra_reads=()):
        kw = {}
        reads = [in_.buf] + list(extra_reads)
        writes = [out.buf]
        if bias is not None:
            if isinstance(bias, View):
                kw["bias"] = bias.ap
                reads.append(bias.buf)
            else:
                kw["bias"] = bias
        if scale is not None:
            if isinstance(scale, View):
                kw["scale"] = scale.ap
                reads.append(scale.buf)
            else:
                kw["scale"] = scale
        if accum is not None:
            kw["accum_out"] = accum.ap
            writes.append(accum.buf)
        S.op("act", lambda e: e.activation(out.ap, in_.ap, func, **kw), reads=reads, writes=writes)

    def tt(eng, out, in0, in1, op):
        S.op(eng, lambda e: e.tensor_tensor(out.ap, in0.ap, in1.ap, op),
             reads=[in0.buf, in1.buf], writes=[out.buf])

    def _sc(x, reads):
        if isinstance(x, View):
            reads.append(x.buf)
            return x.ap
        return x

    def ts(eng, out, in0, s1, op0, s2=None, op1=None):
        reads = [in0.buf]
        a1 = _sc(s1, reads)
        a2 = _sc(s2, reads)
        if op1 is None:
            S.op(eng, lambda e: e.tensor_scalar(out.ap, in0.ap, a1, None, op0), reads=reads, writes=[out.buf])
        else:
            S.op(eng, lambda e: e.tensor_scalar(out.ap, in0.ap, a1, a2, op0, op1), reads=reads,
                 writes=[out.buf])

    def stt(out, in0, scalar, in1, op0, op1):
        reads = [in0.buf, in1.buf]
        a = _sc(scalar, reads)
        S.op("dve", lambda e: e.scalar_tensor_tensor(out.ap, in0.ap, a, in1.ap, op0, op1),
             reads=reads, writes=[out.buf])

    def cp(eng, out, in_):
        if eng == "act":
            S.op("act", lambda e: e.copy(out.ap, in_.ap), reads=[in_.buf], writes=[out.buf])
        else:
            S.op(eng, lambda e: e.tensor_copy(out.ap, in_.ap), reads=[in_.buf], writes=[out.buf])

    def dma(q, out, in_, **kw):
        S.dma(q, lambda e: e.dma_start(out=out.ap, in_=in_.ap, **kw), reads=[in_.buf], writes=[out.buf])

    P = [T(stack.enter_context(nc.psum_tensor("ps%d" % i, [128, 512], F32)), "ps%d" % i) for i in range(8)]

    def finish():
        final_bufs = [out_own.buf, kt_scr.buf, v_scr.buf, ssm_scr.buf, h_scr.buf, xb_scr.buf, yb_scr.buf] + [t.buf for t in dbg_outs.values()]
        with nc.Block() as block:
            @block.tensor
            def _(e):
                S.replay("pe", e)

            @block.scalar
            def _(e):
                S.replay("act", e)

            @block.vector
            def _(e):
                S.replay("dve", e)

            @block.gpsimd
            def _(e):
                S.replay("pool", e)

            @block.sync
            def _(e):
                S.replay("sp", e)
                S.final_waits("sp", e, final_bufs)
        stack.close()
        return nc

    ident = sb("ident", [128, 128])
    triltf = sb("triltf", [128, 128])
    onesf = sb("onesf", [128, 128])
    jrow = sb("jrow", [1, 128])
    maskB = sb("maskB", [128, 32])
    maskC = sb("maskC", [128, 128])
    ecap = sb("ecap", [128, 32])
    maskQ = sb("maskQ", [128, 2])
    maskR = sb("maskR", [128, 256])
    trige = sb("trige", [128, 128], BF16)
    trilt = sb("trilt", [128, 128], BF16)
    trile = sb("trile", [128, 128], BF16)
    sel = sb("sel", [128, 2 * NSLOT])
    epsc = sb("epsc", [128, 1])
    onec = sb("onec", [128, 1])
    grep_mix = sb("grep_mix", [128, D])
    grep_ffn = sb("grep_ffn", [128, D])
    grep_fin = sb("grep_fin", [128, D])
    brrep = sb("brrep", [128, 36])
    destall = sb("destall", [128, NOB, 2], I32)
    gwall = sb("gwall", [128, NOB, 2])
    scopeA = open_scope()
    for dst, src in ((ident, k_ident), (triltf, k_triltf), (onesf, k_onesf), (jrow, k_jrow), (maskB, k_maskB),
                     (maskC, k_maskC), (ecap, k_ecap), (maskQ, k_maskQ), (maskR, k_maskR), (trige, k_trige), (trilt, k_trilt), (trile, k_trile),
                     (sel, k_sel)):
        dma("sp", dst[:], src[:])

    dcol = sb("dcol", [128, 4])
    gbcol = sb("gbcol", [128, 4])
    T1re = sb("T1re", [128, 2048])
    T1im = sb("T1im", [128, 2048])
    T2re = sb("T2re", [128, 2048])
    T2im = sb("T2im", [128, 2048])
    are = sb("are", [128, 16])
    aim = sb("aim", [128, 16])
    Bp = sb("Bp", [128, 4, 2, 128])
    Cp = sb("Cp", [128, 4, 2, 128])
    Bq = sb("Bq", [128, 4, 2, 2, 128], BF16)
    Cq = sb("Cq", [128, 4, 2, 2, 128], BF16)
    s16 = [sb("s16_%d" % i, [128, 16]) for i in range(6)]
    scope0 = open_scope()
    rowtmp = sb("rowtmp", [1, 2048])

    def replicate_row(dst, src_dram, n):
        dma("sp", rowtmp[0:1, 0:n], src_dram[0:1, 0:n])
        for c0 in range(0, n, 512):
            c1 = min(n, c0 + 512)
            mm(P[0][:, 0:c1 - c0], onesf[0:1, :], rowtmp[0:1, c0:c1])
            cp("dve", dst[:, c0:c1], P[0][:, 0:c1 - c0])

    replicate_row(grep_mix, g_mix, D)
    replicate_row(grep_ffn, g_ffn, D)
    replicate_row(grep_fin, g_fin, D)
    replicate_row(brrep, b_r, 36)
    S.op("dve", lambda e: e.memset(onec[:].ap, 1.0), writes=[onec.buf])

    def row_to_col(dst, src_dram, nchunk):
        dma("sp", rowtmp[0:1, 0:nchunk * 128], src_dram[0:1, 0:nchunk * 128])
        for q in range(nchunk):
            mm(P[0][:, 2 * q:2 * q + 2], rowtmp[0:1, q * 128:(q + 1) * 128], onesf[0:1, 0:2])
        cp("dve", dst[:, 0:nchunk], P[0][:, 0:2 * nchunk].re("p (q t) -> p q t", t=2)[:, :, 0])

    row_to_col(dcol, d_skip, 4)
    row_to_col(gbcol, glu_b, 4)

    lrrow = sb("lrrow", [1, 2048])
    lirow = sb("lirow", [1, 2048])
    dtrow = sb("dtrow", [1, 32])
    rhorow = sb("rhorow", [1, 2048])
    throw = sb("throw", [1, 2048])
    dma("sp", lrrow[:], lam_re[:])
    dma("sp", lirow[:], lam_im[:])
    dma("sp", dtrow[:], log_dt[:])
    act(dtrow[:], dtrow[:], AF.Exp)
    dtb = dtrow[:].un(2).bc([1, 32, 64])
    tt("dve", rhorow[:].re("o (g p) -> o g p", p=64), lrrow[:].re("o (g p) -> o g p", p=64), dtb, ALU.mult)
    tt("dve", throw[:].re("o (g p) -> o g p", p=64), lirow[:].re("o (g p) -> o g p", p=64), dtb, ALU.mult)

    wkA = sb("wkA", [128, 2048])
    wkB = sb("wkB", [128, 2048])
    wkI = sb("wkI", [128, 2048], I32)
    wkM = sb("wkM", [128, 2048])

    def outer_tok(row):
        for k in range(4):
            mm(P[k][:, :], jrow[0:1, :], row[0:1, k * 512:(k + 1) * 512])

    def outer_feat(row):
        for gp in range(16):
            mm(P[gp // 4][:, (gp % 4) * 128:(gp % 4 + 1) * 128], row[0:1, gp * 128:(gp + 1) * 128], jrow[0:1, :])

    def sin_of(dst, shift):
        for k in range(4):
            ts("dve", wkA[:, k * 512:(k + 1) * 512], P[k][:, :], 1.0 / TWO_PI, ALU.mult, 0.5 + shift, ALU.add)
        cp("dve", wkI[:], wkA[:])
        cp("dve", wkB[:], wkI[:])
        for k in range(4):
            stt(wkA[:, k * 512:(k + 1) * 512], wkB[:, k * 512:(k + 1) * 512], -TWO_PI, P[k][:, :],
                ALU.mult, ALU.add)
        if shift != 0.0:
            ts("dve", wkA[:], wkA[:], shift * TWO_PI, ALU.add)
        ts("dve", wkB[:], wkA[:], math.pi, ALU.is_gt)
        stt(wkA[:], wkB[:], -TWO_PI, wkA[:], ALU.mult, ALU.add)
        ts("dve", wkB[:], wkA[:], -math.pi, ALU.is_lt)
        stt(wkA[:], wkB[:], TWO_PI, wkA[:], ALU.mult, ALU.add)
        ts("dve", wkA[:], wkA[:], 3.14159, ALU.min, -3.14159, ALU.max)
        act(dst, wkA[:], AF.Sin)

    def build_table(Tre, Tim, outer_fn, sign):
        outer_fn(rhorow)
        for k in range(4):
            act(wkM[:, k * 512:(k + 1) * 512], P[k][:, :], AF.Exp, scale=float(sign))
        outer_fn(throw)
        sin_of(Tim[:], 0.0)
        sin_of(Tre[:], 0.25)
        tt("dve", Tre[:], Tre[:], wkM[:], ALU.mult)
        stt(Tim[:], Tim[:], float(sign), wkM[:], ALU.mult, ALU.mult)

    build_table(T1re, T1im, outer_tok, -1)
    build_table(T2re, T2im, outer_feat, +1)

    lrc = sb("lrc", [128, 16])
    lic = sb("lic", [128, 16])
    for dst, row in ((lrc, lrrow), (lic, lirow)):
        for gp in range(16):
            mm(P[4][:, 2 * gp:2 * gp + 2], row[0:1, gp * 128:(gp + 1) * 128], onesf[0:1, 0:2])
        cp("dve", dst[:], P[4][:, 0:32].re("p (q t) -> p q t", t=2)[:, :, 0])
    T2re3 = T2re[:].re("p (g j) -> p g j", j=128)
    T2im3 = T2im[:].re("p (g j) -> p g j", j=128)
    cp("dve", are[:], T2re3[:, :, 1])
    cp("dve", aim[:], T2im3[:, :, 1])
    fre = sb("fre", [128, 16])
    fim = sb("fim", [128, 16])
    nr, den, t0, t1, t2, t3 = [s[:] for s in s16]
    ts("dve", nr, are[:], -1.0, ALU.add)
    tt("dve", t0, lrc[:], lrc[:], ALU.mult)
    tt("dve", t1, lic[:], lic[:], ALU.mult)
    tt("dve", den, t0, t1, ALU.add)
    S.op("dve", lambda e: e.reciprocal(den.ap, den.ap), reads=[den.buf], writes=[den.buf])
    tt("dve", t0, nr, lrc[:], ALU.mult)
    tt("dve", t1, aim[:], lic[:], ALU.mult)
    tt("dve", t2, t0, t1, ALU.add)
    tt("dve", fre[:], t2, den, ALU.mult)
    tt("dve", t0, aim[:], lrc[:], ALU.mult)
    tt("dve", t1, nr, lic[:], ALU.mult)
    tt("dve", t2, t0, t1, ALU.subtract)
    tt("dve", fim[:], t2, den, ALU.mult)

    Bsr = sb("Bsr", [128, 16, 16])
    Bsi = sb("Bsi", [128, 16, 16])
    for dst, src in ((Bsr, b_re), (Bsi, b_im)):
        v = src[:].re("(gp g2) p c -> g2 p gp c", g2=2)
        for g2 in range(2):
            dma("sp", dst[g2 * 64:(g2 + 1) * 64, :, :], v[g2])
    Bbr = sb("Bbr", [128, 16, 16])
    Bbi = sb("Bbi", [128, 16, 16])
    tB0 = sb("tB0", [128, 16, 16])
    tB1 = sb("tB1", [128, 16, 16])
    freb = fre[:].un(2).bc([128, 16, 16])
    fimb = fim[:].un(2).bc([128, 16, 16])
    tt("dve", tB0[:], Bsr[:], freb, ALU.mult)
    tt("dve", tB1[:], Bsi[:], fimb, ALU.mult)
    tt("dve", Bbr[:], tB0[:], tB1[:], ALU.subtract)
    tt("dve", tB0[:], Bsi[:], freb, ALU.mult)
    tt("dve", tB1[:], Bsr[:], fimb, ALU.mult)
    tt("dve", Bbi[:], tB0[:], tB1[:], ALU.add)
    dbg("Bbr", Bbr[:], [128, 16, 16])
    Y2 = sb("Y2", [128, 2, 16, 2, 16])
    mBb = maskB[:].re("p (a c) -> p a c", a=2).un(1).bc([128, 16, 2, 16])
    for pl, Bb in ((0, Bbr), (1, Bbi)):
        tt("dve", Y2[:, pl], Bb[:].un(2).bc([128, 16, 2, 16]), mBb, ALU.mult)
    for pl in range(2):
        for Q in range(4):
            tr(P[5][:, Q * 128:(Q + 1) * 128], Y2[:, pl, 4 * Q:4 * Q + 4].re("p a b c -> p (a b c)"), ident[:])
        cp("dve", Bp[:, :, pl, :], P[5][:, :].re("p (q n) -> p q n", n=128))
        for lh in range(2):
            ts("dve", Bq[:, :, lh, pl, :], Bp[:, :, pl, :], maskQ[:, lh:lh + 1], ALU.mult)
    Csr = sb("Csr", [128, 4, 64])
    Csi = sb("Csi", [128, 4, 64])
    dma("sp", Csr[:], c_re[:].re("(q r) p -> r q p", r=128))
    dma("sp", Csi[:], c_im[:].re("(q r) p -> r q p", r=128))
    Xc = sb("Xc", [128, 2, 4, 2, 64])
    mCb = maskC[:].re("r (a p) -> r a p", a=2).un(1).bc([128, 4, 2, 64])
    tt("dve", Xc[:, 0], Csr[:].un(2).bc([128, 4, 2, 64]), mCb, ALU.mult)
    for Q in range(4):
        stt(Xc[:, 1, Q], Csi[:, Q].un(1).bc([128, 2, 64]), -1.0, maskC[:].re("r (a p) -> r a p", a=2),
            ALU.mult, ALU.mult)
    for pl in range(2):
        for Q in range(4):
            tr(P[5][:, Q * 128:(Q + 1) * 128], Xc[:, pl, Q].re("r a p -> r (a p)"), ident[:])
        cp("dve", Cp[:, :, pl, :], P[5][:, :].re("p (q n) -> p q n", n=128))
        for lh in range(2):
            tt("dve", Cq[:, :, pl, lh, :], Cp[:, :, pl, :],
               maskR[:, lh * 128:(lh + 1) * 128].un(1).bc([128, 4, 128]), ALU.mult)

    close_scope(scope0)
    Wkvu = sb("Wkvu", [128, 8, 1536], BF16)
    for kc in range(8):
        dma("pool", Wkvu[:, kc, :], w_in[kc * 128:(kc + 1) * 128, 512:2048])
    Wg = sb("Wg", [128, 4, 512], BF16)
    for kc in range(4):
        dma("pool", Wg[:, kc, :], glu_w[kc * 128:(kc + 1) * 128, :])

    xblk = [sb("xblk%d" % i, [128, D]) for i in range(2)]
    junk = sb("junk", [128, D])
    ssq = sb("ssq", [128, 1])
    rstd = sb("rstd", [128, 1])
    xn = sb("xn", [128, D])
    xnT = sb("xnT", [128, 8, 128], BF16)
    ktb = sb("ktb", [128, 4, 128], BF16)
    vb = sb("vb", [128, 512], BF16)
    uT = sb("uT", [128, 4, 128], BF16)
    zt = sb("zt", [128, 16, 2, 128], BF16)
    tz = [sb("tz%d" % i, [128, 8, 128]) for i in range(4)]
    cg = sb("cg", [128, 8, 2, 128])
    xT = sb("xT", [128, 16, 2, 128], BF16)
    Gre = sb("Gre", [128, 16])
    Gim = sb("Gim", [128, 16])
    xlast = sb("xlast", [128, 16, 2])
    yv = sb("yv", [128, 4, 128])
    y2 = sb("y2", [128, 4, 128])
    y3 = sb("y3", [128, 4, 128])
    yg = sb("yg", [128, 4, 128])
    ygb = sb("ygb", [128, 4, 128], BF16)
    sgl = sb("sgl", [128, 4, 128])
    ssmT = sb("ssmT", [128, 4, 128], BF16)
    S.op("dve", lambda e: e.memset(Gre[:].ap, 0.0), writes=[Gre.buf])
    S.op("dve", lambda e: e.memset(Gim[:].ap, 0.0), writes=[Gim.buf])
    T1re3 = T1re[:].re("p (g n) -> p g n", n=128)
    T1im3 = T1im[:].re("p (g n) -> p g n", n=128)

    def rmsnorm(xv, grep, outv):
        act(junk[:], xv, AF.Square, accum=ssq[:])
        act(rstd[:], ssq[:], AF.Ln, scale=1.0 / D, bias=epsc[:])
        act(rstd[:], rstd[:], AF.Exp, scale=-0.5)
        stt(outv, xv, rstd[:], grep[:], ALU.mult, ALU.mult)

    S.op("dve", lambda e: e.memset(epsc[:].ap, 1e-6), writes=[epsc.buf])

    def transpose8(src, dstT, col0, ncol_total):
        for half in range(2):
            for k in range(4):
                kc = half * 4 + k
                tr(P[6 + half][:, k * 128:(k + 1) * 128], src[:, kc * 128:(kc + 1) * 128], ident[:])
            cp("act" if half == 0 else "dve", dstT[:, half * 4:half * 4 + 4, col0:col0 + 128],
               P[6 + half][:, :].re("p (k n) -> p k n", n=128))

    for blk in range(NB):
        xb_ = xblk[blk % 2]
        dma("sp", xb_[:], x_all[blk * 128:(blk + 1) * 128, :])
        rmsnorm(xb_[:], grep_mix, xn[:])
        transpose8(xn, xnT, 0, 128)
        for m in range(4):
            for kc in range(8):
                mm(P[0][:, m * 128:(m + 1) * 128], Wkvu[:, kc, m * 128:(m + 1) * 128], xnT[:, kc, :],
                   start=(kc == 0), stop=(kc == 7))
        cp("act", ktb[:], P[0][:, :].re("p (m n) -> p m n", n=128))
        dma("sp", kt_scr[:].re("(m q) n -> q m n", q=128)[:, :, blk * 128:(blk + 1) * 128], ktb[:])
        for kc in range(8):
            mm(P[2][:, :], xnT[:, kc, :], Wkvu[:, kc, 512:1024], start=(kc == 0), stop=(kc == 7))
        cp("act", vb[:], P[2][:, :])
        dma("sp", v_scr[:].re("h j b d -> j h b d")[:, :, blk, :], vb[:].re("j (h d) -> j h d", d=64))
        for m in range(4):
            for kc in range(8):
                mm(P[1][:, m * 128:(m + 1) * 128], Wkvu[:, kc, 1024 + m * 128:1024 + (m + 1) * 128],
                   xnT[:, kc, :], start=(kc == 0), stop=(kc == 7))
        cp("act", uT[:], P[1][:, :].re("p (m n) -> p m n", n=128))
        if blk == 1:
            dbg("uT", uT[:], [128, 4, 128], BF16)
        for hf in range(2):
            for b4 in range(4):
                Q, h = hf * 2 + b4 // 2, b4 % 2
                mm(P[2 + b4][:, :], uT[64 * h:64 * h + 64, Q, :],
                   Bq[64 * h:64 * h + 64, Q, :, :, :].re("k l a n -> k (l a n)"))
            def buv(pl):
                vs = []
                for b4 in range(4):
                    vs.append(P[2 + b4][:, :].re("p (g a n) -> p g a n", a=2, n=128)[:, :, pl, :])
                return vs
            bre = buv(0)
            bim = buv(1)
            gs = slice(hf * 8, hf * 8 + 8)
            for b4 in range(4):
                g2s = slice(hf * 8 + 2 * b4, hf * 8 + 2 * b4 + 2)
                l2s = slice(2 * b4, 2 * b4 + 2)
                tt("dve", tz[0][:, l2s, :], bre[b4], T1re3[:, g2s, :], ALU.mult)
                tt("dve", tz[1][:, l2s, :], bim[b4], T1im3[:, g2s, :], ALU.mult)
                tt("dve", tz[2][:, l2s, :], bre[b4], T1im3[:, g2s, :], ALU.mult)
                tt("dve", tz[3][:, l2s, :], bim[b4], T1re3[:, g2s, :], ALU.mult)
            tt("pool", zt[:, gs, 0, :], tz[0][:], tz[1][:], ALU.subtract)
            tt("pool", zt[:, gs, 1, :], tz[2][:], tz[3][:], ALU.add)
        for hf in range(2):
            gs = slice(hf * 8, hf * 8 + 8)
            for g8 in range(8):
                gp = hf * 8 + g8
                for pl in range(2):
                    idx = g8 * 2 + pl
                    mm(P[2 + idx // 4][:, (idx % 4) * 128:(idx % 4 + 1) * 128], zt[:, gp, pl, :], trile[:])
            for b4 in range(4):
                g2s = slice(hf * 8 + 2 * b4, hf * 8 + 2 * b4 + 2)
                l2s = slice(2 * b4, 2 * b4 + 2)
                pv = P[2 + b4][:, :].re("p (g a n) -> p g a n", a=2, n=128)
                tt("dve", cg[:, l2s, 0, :], pv[:, :, 0, :], Gre[:, g2s].un(2).bc([128, 2, 128]), ALU.add)
                tt("dve", cg[:, l2s, 1, :], pv[:, :, 1, :], Gim[:, g2s].un(2).bc([128, 2, 128]), ALU.add)
            tt("dve", tz[0][:], cg[:, :, 0, :], T2re3[:, gs, :], ALU.mult)
            tt("pool", tz[1][:], cg[:, :, 1, :], T2im3[:, gs, :], ALU.mult)
            tt("dve", tz[2][:], cg[:, :, 1, :], T2re3[:, gs, :], ALU.mult)
            tt("pool", tz[3][:], cg[:, :, 0, :], T2im3[:, gs, :], ALU.mult)
            tt("dve", xT[:, gs, 0, :], tz[0][:], tz[1][:], ALU.subtract)
            tt("pool", xT[:, gs, 1, :], tz[2][:], tz[3][:], ALU.add)
            tt("dve", xlast[:, gs, 0], tz[0][:, :, 127], tz[1][:, :, 127], ALU.subtract)
            tt("dve", xlast[:, gs, 1], tz[2][:, :, 127], tz[3][:, :, 127], ALU.add)
        tt("dve", s16[0][:], xlast[:, :, 0], are[:], ALU.mult)
        tt("dve", s16[1][:], xlast[:, :, 1], aim[:], ALU.mult)
        tt("dve", s16[2][:], xlast[:, :, 0], aim[:], ALU.mult)
        tt("dve", s16[3][:], xlast[:, :, 1], are[:], ALU.mult)
        tt("dve", Gre[:], s16[0][:], s16[1][:], ALU.subtract)
        tt("dve", Gim[:], s16[2][:], s16[3][:], ALU.add)
        for Q in range(4):
            for h in range(2):
                i = 0
                for lh in range(2):
                    gp = 4 * Q + 2 * h + lh
                    for pl in range(2):
                        mm(P[1][64 * h:64 * h + 64, Q * 128:(Q + 1) * 128], Cq[:, Q, pl, lh, 64 * h:64 * h + 64],
                           xT[:, gp, pl, :], start=(i == 0), stop=(i == 3))
                        i += 1
        for Q in range(4):
            stt(yv[:, Q, :], uT[:, Q, :], dcol[:, Q:Q + 1], P[1][:, Q * 128:(Q + 1) * 128], ALU.mult, ALU.add)
        if blk == 1:
            dbg("yv", yv[:], [128, 4, 128])
        tt("pool", y2[:], yv[:], yv[:], ALU.mult)
        ts("dve", y2[:], y2[:], 0.044715, ALU.mult, 1.0, ALU.add)
        tt("pool", y3[:], y2[:], yv[:], ALU.mult)
        act(y3[:], y3[:], AF.Sigmoid, scale=1.5957691216057308)
        tt("dve", yg[:], yv[:], y3[:], ALU.mult)
        cp("pool", ygb[:], yg[:])
        for co in range(4):
            for kc in range(4):
                mm(P[0][:, co * 128:(co + 1) * 128], Wg[:, kc, co * 128:(co + 1) * 128], ygb[:, kc, :],
                   start=(kc == 0), stop=(kc == 3))
        for co in range(4):
            act(sgl[:, co, :], P[0][:, co * 128:(co + 1) * 128], AF.Sigmoid, bias=gbcol[:, co:co + 1])
        tt("dve", ssmT[:], yg[:], sgl[:], ALU.mult)
        dma("sp", ssm_scr[:].re("q p n -> p q n")[:, :, blk * 128:(blk + 1) * 128], ssmT[:])

    close_scope(scopeA)
    if nphase < 2:
        while len(cur) > 1:
            cur[0].close()
            cur.pop(0)
        return finish()
    scope2 = open_scope()
    mask0 = sb("mask0", [128, 512], BF16)
    maske = sb("maske", [128, 8, 512], BF16)
    masko = sb("masko", [128, 8, 512], BF16)
    dma("sp", mask0[:], k_mask0[:])
    dma("sp", maske[:], k_maske[:])
    dma("sp", masko[:], k_masko[:])
    Wr = sb("Wr", [128, 8, 36])
    dma("sp", Wr[:], w_r[:].re("(kc p) n -> p kc n", p=128))
    Wpa = sb("Wpa", [64, 8, D], BF16)
    Wpb = sb("Wpb", [128, 4, D], BF16)
    Wo = sb("Wo", [128, 8, D], BF16)
    for h in range(8):
        dma("pool", Wpa[:, h, :], w_pa[h * 64:(h + 1) * 64, :])
    for kc in range(4):
        dma("pool", Wpb[:, kc, :], w_pb[kc * 128:(kc + 1) * 128, :])
    for kc in range(8):
        dma("pool", Wo[:, kc, :], w_o[kc * 128:(kc + 1) * 128, :])
    wring = [sb("wring%d" % i, [128, 8, 512], BF16) for i in range(2)]
    wri = [0]

    def load_wchunk(c0):
        t = wring[wri[0] % 2]
        wri[0] += 1
        for kc in range(8):
            dma("pool", t[:, kc, :], w_in[kc * 128:(kc + 1) * 128, c0:c0 + 512])
        return t

    xo = [sb("xo%d" % i, [128, D]) for i in range(2)]
    xn2 = sb("xn2", [128, D])
    junk2 = sb("junk2", [128, D], BF16)
    ssq2 = sb("ssq2", [128, 1])
    rstd2 = sb("rstd2", [128, 1])
    xoT = sb("xoT", [128, 8, 512], BF16)
    qT = sb("qT", [64, 8, 512], BF16)
    ktc = [[sb("ktc%d_%d" % (a, i), [64, 1024], BF16) for i in range(2)] for a in range(2)]
    vtc = [[sb("vtc%d_%d" % (a, i), [128, 8, 64], BF16) for i in range(2)] for a in range(2)]
    er = [[sb("er%d_%d" % (a, i), [128, 512], BF16) for i in range(2)] for a in range(2)]
    spr = [[sb("spr%d_%d" % (a, i), [128, 512], BF16) for i in range(2)] for a in range(2)]
    tr_ = [sb("tr%d" % a, [128, 512], BF16) for a in range(2)]
    wr_ = [[sb("wr%d_%d" % (a, i), [128, 512], BF16) for i in range(2)] for a in range(2)]
    attnT = sb("attnT", [64, 8, 512], BF16)
    ssA = sb("ssA", [128, 4, 512], BF16)
    ssB = sb("ssB", [128, 4, 512], BF16)
    ssO = ssA
    sg1 = sb("sg1", [128, 512], BF16)
    sg2 = sb("sg2", [128, 512], BF16)
    mg1 = sb("mg1", [128, 512])
    mg2 = sb("mg2", [128, 512])
    mergedT = sb("mergedT", [128, 8, 512], BF16)
    hblk = [sb("hblk0", [128, D])] * 2
    hn = [xn2, xn2]
    hnT = sb("hnT", [128, 8, 128])
    lgt = sb("lgt", [128, 36])
    r1 = [sb("r1_%d" % i, [128, 1]) for i in range(12)]
    ohg = sb("ohg", [128, 4])
    eg = sb("eg", [128, 4])
    selv = sb("selv", [128, 8])
    m8 = sb("m8", [128, 8])
    oh1 = sb("oh1", [128, 8])
    oh2 = sb("oh2", [128, 8])
    M12 = sb("M12", [128, 64])
    pm = sb("pm", [128, 64])
    pmt = sb("pmt", [128, 64])
    base = sb("base", [128, 32])
    destf = sb("destf", [128, 2])
    S.op("dve", lambda e: e.memset(base[:].ap, 0.0), writes=[base.buf])

    def rmsnorm2(xv, grep, outv):
        act(junk2[:], xv, AF.Square, accum=ssq2[:])
        act(rstd2[:], ssq2[:], AF.Ln, scale=1.0 / D, bias=epsc[:])
        act(rstd2[:], rstd2[:], AF.Exp, scale=-0.5)
        stt(outv, xv, rstd2[:], grep[:], ALU.mult, ALU.mult)

    def mmacc(out, lhsT, rhs, start):
        S.op("pe", lambda e: e.matmul(out.ap, lhsT.ap, rhs.ap, start=start, stop=True, skip_group_check=True),
             reads=[lhsT.buf, rhs.buf], writes=[out.buf])

    for n in range(NSLOT):
        pmask = maske if n % 2 == 0 else masko
        for r in range(4):
            xb_ = xo[r % 2]
            dma("sp", xb_[:], x_own[(4 * n + r) * 128:(4 * n + r + 1) * 128, :])
            rmsnorm2(xb_[:], grep_mix, xn2[:])
            transpose8(xn2, xoT, r * 128, 512)
        wq = load_wchunk(0)
        for h in range(8):
            pb = P[4 + h % 4]
            for kc in range(8):
                mm(pb[0:64, :], wq[:, kc, h * 64:(h + 1) * 64], xoT[:, kc, :], start=(kc == 0), stop=(kc == 7))
            cp("act" if h % 2 == 0 else "dve", qT[:, h, :], pb[0:64, :])
        if n == 0:
            dbg("qT", qT[:], [64, 8, 512], BF16)
        nkb = 8 * n + 9
        kbs = [8 * n + 8 - s for s in range(nkb)]
        for hp in range(4):
            heads = (2 * hp, 2 * hp + 1)
            ACC = (P[0], P[1])
            OB = (P[2], P[3])
            ZR = ((P[4], P[5]), (P[6], P[7]))
            cur_chunk = [None, None]
            cur_kt = [None, None]
            cur_vt = [None, None]
            nload = [0, 0]

            def get_kv(a, kb):
                h = heads[a]
                c = -1 if kb == 0 else (kb - 1) // 8
                if cur_chunk[a] != c:
                    i = nload[a] % 2
                    nload[a] += 1
                    kt_, vt_ = ktc[a][i], vtc[a][i]
                    if c < 0:
                        dma("sp", kt_[:, 0:128], kt_scr[h * 64:(h + 1) * 64, 0:128])
                        dma("sp", vt_[:, 0:1, :], v_scr[h, :, 0:1, :])
                    else:
                        dma("sp", kt_[:, :], kt_scr[h * 64:(h + 1) * 64, (8 * c + 1) * 128:(8 * c + 9) * 128])
                        dma("sp", vt_[:, :, :], v_scr[h, :, 8 * c + 1:8 * c + 9, :])
                    cur_chunk[a], cur_kt[a], cur_vt[a] = c, kt_, vt_
                lb = 0 if kb == 0 else (kb - 1) % 8
                return cur_kt[a][:, lb * 128:(lb + 1) * 128], cur_vt[a][:, lb, :]

            vsel = {}

            def front(a, s):
                kb = kbs[s]
                ktv, vtv = get_kv(a, kb)
                vsel[(a, s)] = vtv
                Z = ZR[a][s % 2]
                mm(Z[:, :], ktv, qT[:, heads[a], :])
                e_ = er[a][s % 2]
                act(e_[:], Z[:, :], AF.Exp, scale=0.125)
                if s < 8:
                    tt("dve", e_[:], e_[:], pmask[:, s, :], ALU.mult)
                elif kb == 0:
                    tt("dve", e_[:], e_[:], mask0[:], ALU.mult)
                act(spr[a][s % 2][:], e_[:], AF.Ln, bias=onec[:])

            for a in range(2):
                front(a, 0)
            for s in range(nkb):
                if s + 1 < nkb:
                    for a in range(2):
                        front(a, s + 1)
                for a in range(2):
                    mmacc(ACC[a][:, :], trige[:], spr[a][s % 2][:], start=(s == 0))
                for a in range(2):
                    act(tr_[a][:], ACC[a][:, :], AF.Exp, scale=-1.0)
                for a in range(2):
                    mmacc(ACC[a][:, :], trilt[:], spr[a][s % 2][:], start=False)
                for a in range(2):
                    tt("dve" if a == 0 else "pool", wr_[a][s % 2][:], er[a][s % 2][:], tr_[a][:], ALU.mult)
                for a in range(2):
                    mmacc(OB[a][0:64, :], vsel[(a, s)], wr_[a][s % 2][:], start=(s == 0))
            for a in range(2):
                cp("act" if a == 0 else "dve", attnT[:, heads[a], :], OB[a][0:64, :])
        if n == 0:
            dbg("attnT", attnT[:], [64, 8, 512], BF16)
        posA = (8 * n + 1) * 128
        posB = (8 * n + 5) * 128
        sv = ssm_scr[:].re("q p n -> p q n")
        dma("sp", ssA[:], sv[:, :, posA:posA + 512])
        dma("sp", ssB[:], sv[:, :, posB:posB + 512])
        ts("dve", ssO[:], ssA[:], sel[:, 2 * n:2 * n + 1], ALU.mult)
        stt(ssO[:], ssB[:], sel[:, 2 * n + 1:2 * n + 2], ssO[:], ALU.mult, ALU.add)
        for c2 in range(2):
            wga = load_wchunk(2048 + c2 * 512)
            wgb = load_wchunk(3072 + c2 * 512)
            for c4 in range(4):
                co = c2 * 4 + c4
                for kc in range(8):
                    mm(P[4][:, :], wga[:, kc, c4 * 128:(c4 + 1) * 128], xoT[:, kc, :], start=(kc == 0), stop=(kc == 7))
                for kc in range(8):
                    mm(P[5][:, :], wgb[:, kc, c4 * 128:(c4 + 1) * 128], xoT[:, kc, :], start=(kc == 0), stop=(kc == 7))
                for h in range(8):
                    mm(P[6][:, :], Wpa[:, h, co * 128:(co + 1) * 128], attnT[:, h, :], start=(h == 0), stop=(h == 7))
                for kc in range(4):
                    mm(P[7][:, :], Wpb[:, kc, co * 128:(co + 1) * 128], ssO[:, kc, :], start=(kc == 0), stop=(kc == 3))
                act(sg1[:], P[4][:, :], AF.Sigmoid)
                act(sg2[:], P[5][:, :], AF.Sigmoid)
                tt("dve", mg1[:], P[6][:, :], sg1[:], ALU.mult)
                tt("dve", mg2[:], P[7][:, :], sg2[:], ALU.mult)
                tt("pool", mergedT[:, co, :], mg1[:], mg2[:], ALU.add)
        if n == 0:
            dbg("mergedT", mergedT[:], [128, 8, 512], BF16)
        for r in range(4):
            ob = 4 * n + r
            xb_ = xo[r % 2]
            hb_ = hblk[r % 2]
            hn_ = hn[r % 2]
            dma("sp", xb_[:], x_own[ob * 128:(ob + 1) * 128, :])
            for half in range(2):
                for kc in range(8):
                    mm(P[half][:, :], mergedT[:, kc, r * 128:(r + 1) * 128], Wo[:, kc, half * 512:(half + 1) * 512],
                       start=(kc == 0), stop=(kc == 7))
                tt("dve", hb_[:, half * 512:(half + 1) * 512], P[half][:, :], xb_[:, half * 512:(half + 1) * 512],
                   ALU.add)
            dma("sp", h_scr[ob * 128:(ob + 1) * 128, :], hb_[:])
            rmsnorm2(hb_[:], grep_ffn, hn_[:])
            for half in range(2):
                for k in range(4):
                    kc = half * 4 + k
                    tr(P[6 + half][:, k * 128:(k + 1) * 128], hn_[:, kc * 128:(kc + 1) * 128], ident[:])
                cp("act" if half == 0 else "dve", hnT[:, half * 4:half * 4 + 4, :],
                   P[6 + half][:, :].re("p (k n) -> p k n", n=128))
            for kc in range(8):
                mm(P[2][:, 0:36], hnT[:, kc, :], Wr[:, kc, :], start=(kc == 0), stop=(kc == 7))
            tt("dve", lgt[:], P[2][:, 0:36], brrep[:], ALU.add)
            gmax, ngmax, gsum, ptop, dv, ex, g1, g2, p1loc, p2loc, ov1, ov2 = [t_[:] for t_ in r1]
            S.op("dve", lambda e: e.tensor_reduce(gmax.ap, lgt[:, 0:4].ap, AX.X, ALU.max),
                 reads=[lgt.buf], writes=[gmax.buf])
            ts("dve", ohg[:], lgt[:, 0:4], gmax, ALU.is_equal)
            ts("dve", ngmax, gmax, -1.0, ALU.mult)
            act(eg[:], lgt[:, 0:4], AF.Exp, bias=ngmax, accum=gsum)
            S.op("dve", lambda e: e.reciprocal(ptop.ap, gsum.ap), reads=[gsum.buf], writes=[ptop.buf])
            ts("dve", selv[:], lgt[:, 4:12], ohg[:, 0:1], ALU.mult)
            for g in range(1, 4):
                stt(selv[:], lgt[:, 4 + 8 * g:12 + 8 * g], ohg[:, g:g + 1], selv[:], ALU.mult, ALU.add)
            S.op("dve", lambda e: e.max(m8[:].ap, selv[:].ap), reads=[selv.buf], writes=[m8.buf])
            ts("dve", oh1[:], selv[:], m8[:, 0:1], ALU.is_equal)
            ts("dve", oh2[:], selv[:], m8[:, 1:2], ALU.is_equal)
            tt("dve", dv, m8[:, 1:2], m8[:, 0:1], ALU.subtract)
            act(ex, dv, AF.Exp)
            ts("dve", g1, ex, 1.0, ALU.add)
            S.op("dve", lambda e: e.reciprocal(g1.ap, g1.ap), reads=[g1.buf], writes=[g1.buf])
            tt("dve", g2, ex, g1, ALU.mult)
            tt("dve", gwall[:, ob, 0:1], g1, ptop, ALU.mult)
            tt("dve", gwall[:, ob, 1:2], g2, ptop, ALU.mult)
            tt("dve", M12[:, 0:32].re("p (g e) -> p g e", e=8), ohg[:].un(2).bc([128, 4, 8]),
               oh1[:].un(1).bc([128, 4, 8]), ALU.mult)
            tt("dve", M12[:, 32:64].re("p (g e) -> p g e", e=8), ohg[:].un(2).bc([128, 4, 8]),
               oh2[:].un(1).bc([128, 4, 8]), ALU.mult)
            mm(P[3][:, 0:64], triltf[:], M12[:])
            mm(P[3][:, 64:128], onesf[:], M12[:])
            tt("dve", pm[:, 0:32], P[3][:, 0:32], base[:], ALU.add)
            tt("dve", pm[:, 32:64], P[3][:, 32:64], base[:], ALU.add)
            tt("dve", pm[:, 32:64], P[3][:, 64:96], pm[:, 32:64], ALU.add)
            tt("dve", pmt[:], pm[:], M12[:], ALU.mult)
            S.op("dve", lambda e: e.tensor_reduce(p1loc.ap, pmt[:, 0:32].ap, AX.X, ALU.add),
                 reads=[pmt.buf], writes=[p1loc.buf])
            S.op("dve", lambda e: e.tensor_reduce(p2loc.ap, pmt[:, 32:64].ap, AX.X, ALU.add),
                 reads=[pmt.buf], writes=[p2loc.buf])
            tt("dve", pmt[:, 0:32], M12[:, 0:32], ecap[:], ALU.mult)
            tt("dve", pmt[:, 32:64], M12[:, 32:64], ecap[:], ALU.mult)
            S.op("dve", lambda e: e.tensor_reduce(ov1.ap, pmt[:, 0:32].ap, AX.X, ALU.add),
                 reads=[pmt.buf], writes=[ov1.buf])
            S.op("dve", lambda e: e.tensor_reduce(ov2.ap, pmt[:, 32:64].ap, AX.X, ALU.add),
                 reads=[pmt.buf], writes=[ov2.buf])
            tt("dve", destf[:, 0:1], ov1, p1loc, ALU.add)
            tt("dve", destf[:, 1:2], ov2, p2loc, ALU.add)
            ts("dve", ov1, p1loc, float(CAP) - 0.5, ALU.is_gt, float(4 * NXB), ALU.mult)
            ts("dve", ov2, p2loc, float(CAP) - 0.5, ALU.is_gt, float(4 * NXB), ALU.mult)
            tt("dve", destf[:, 0:1], destf[:, 0:1], ov1, ALU.add)
            tt("dve", destf[:, 1:2], destf[:, 1:2], ov2, ALU.add)
            cp("dve", destall[:, ob, :], destf[:])
            tt("dve", base[:], P[3][:, 64:96], base[:], ALU.add)
            tt("dve", base[:], P[3][:, 96:128], base[:], ALU.add)
            for k in range(2 if "noscat" not in debug else 0):
                dv_ = destall[:, ob, k:k + 1]
                S.dma("pool", (lambda dv_, hn_: (lambda e: e.indirect_dma_start(
                    out=xb_scr[:].ap, out_offset=bass.IndirectOffsetOnAxis(ap=dv_.ap, axis=0),
                    in_=hn_[:].ap, in_offset=None, bounds_check=bc_reg(e), oob_is_err=False)))(dv_, hn_),
                    reads=[hn_.buf, destall.buf], writes=[xb_scr.buf])
    dbg("destall", destall[:], [128, NOB, 2], I32)
    dbg("gwall", gwall[:], [128, NOB, 2])
    close_scope(scope2)
    if nphase < 3 or not moe:
        return finish()
    if nphase >= 3 and moe:
        scope3 = open_scope()
        W1 = [sb("W1_%d" % i, [128, 8, 512], BF16) for i in range(2)]
        W3 = [sb("W3_%d" % i, [128, 8, 512], BF16) for i in range(2)]
        W2 = [sb("W2_%d" % i, [128, 4, D], BF16) for i in range(2)]
        xg = [sb("xg%d" % i, [128, D]) for i in range(2)]
        xeT = sb("xeT", [128, 8, CAP], BF16)
        s1 = sb("s1", [128, CAP])
        hgT = sb("hgT", [128, 4, CAP], BF16)
        ybk = [sb("ybk%d" % i, [128, D]) for i in range(2)]
        for ex_ in range(32):
            w1_, w3_, w2_ = W1[ex_ % 2], W3[ex_ % 2], W2[ex_ % 2]
            for kc in range(8):
                dma("pool", w1_[:, kc, :], w1[ex_, kc * 128:(kc + 1) * 128, :])
                dma("pool", w3_[:, kc, :], w3[ex_, kc * 128:(kc + 1) * 128, :])
            for fc in range(4):
                dma("pool", w2_[:, fc, :], w2[ex_, fc * 128:(fc + 1) * 128, :])
            for c in range(CAPB):
                xg_ = xg[c % 2]
                dma("sp", xg_[:], xb_scr[ex_ * CAP + c * 128:ex_ * CAP + (c + 1) * 128, :])
                transpose8(xg_, xeT, c * 128, CAP)
            for fc in range(4):
                for kc in range(8):
                    mm(P[0][:, 0:CAP], w1_[:, kc, fc * 128:(fc + 1) * 128], xeT[:, kc, :], start=(kc == 0), stop=(kc == 7))
                for kc in range(8):
                    mm(P[1][:, 0:CAP], w3_[:, kc, fc * 128:(fc + 1) * 128], xeT[:, kc, :], start=(kc == 0), stop=(kc == 7))
                act(s1[:], P[0][:, 0:CAP], AF.Silu)
                tt("dve", hgT[:, fc, :], s1[:], P[1][:, 0:CAP], ALU.mult)
            for c in range(CAPB):
                yb_ = ybk[c % 2]
                for half in range(2):
                    for fc in range(4):
                        mm(P[2 + half][:, :], hgT[:, fc, c * 128:(c + 1) * 128], w2_[:, fc, half * 512:(half + 1) * 512],
                           start=(fc == 0), stop=(fc == 3))
                    cp("act" if half == 0 else "dve", yb_[:, half * 512:(half + 1) * 512], P[2 + half][:, :])
                dma("sp", yb_scr[ex_ * CAP + c * 128:ex_ * CAP + (c + 1) * 128, :], yb_[:])
        if nphase < 4:
            close_scope(scope3)
            return finish()
        yk = [sb("yk%d" % i, [128, D]) for i in range(2)]
        hb4 = [sb("hb4_%d" % i, [128, D]) for i in range(2)]
        oo = [sb("oo%d" % i, [128, D]) for i in range(2)]
        junk4 = sb("junk4", [128, D], BF16)
        ssq4 = sb("ssq4", [128, 1])
        rstd4 = sb("rstd4", [128, 1])
        for ob in range(NOB):
            hb_ = hb4[ob % 2]
            dma("sp", hb_[:], h_scr[ob * 128:(ob + 1) * 128, :])
            for k in range(2):
                yk_ = yk[k]
                S.op("pool", (lambda yk_: (lambda e: e.memset(yk_[:].ap, 0.0)))(yk_), writes=[yk_.buf])
                dv_ = destall[:, ob, k:k + 1]
                S.dma("pool", (lambda dv_, yk_: (lambda e: e.indirect_dma_start(
                    out=yk_[:].ap, out_offset=None, in_=yb_scr[:].ap,
                    in_offset=bass.IndirectOffsetOnAxis(ap=dv_.ap, axis=0),
                    bounds_check=bc_reg(e), oob_is_err=False)))(dv_, yk_),
                    reads=[yb_scr.buf, destall.buf], writes=[yk_.buf])
                stt(hb_[:], yk_[:], gwall[:, ob, k:k + 1], hb_[:], ALU.mult, ALU.add)
            oo_ = oo[ob % 2]
            act(junk4[:], hb_[:], AF.Square, accum=ssq4[:])
            act(rstd4[:], ssq4[:], AF.Ln, scale=1.0 / D, bias=epsc[:])
            act(rstd4[:], rstd4[:], AF.Exp, scale=-0.5)
            stt(oo_[:], hb_[:], rstd4[:], grep_fin[:], ALU.mult, ALU.mult)
            dma("sp", out_own[ob * 128:(ob + 1) * 128, :], oo_[:])
        close_scope(scope3)

    return finish()


def _consts(NSLOT, CAP):
    bf = ml_dtypes.bfloat16
    r = np.arange(128)[:, None]
    c = np.arange(128)[None, :]
    k = {}
    k["k_ident"] = (r == c).astype(np.float32)
    k["k_triltf"] = (r < c).astype(np.float32)
    k["k_onesf"] = np.ones((128, 128), np.float32)
    k["k_jrow"] = np.arange(128, dtype=np.float32)[None, :]
    mB = np.zeros((128, 2, 16), np.float32)
    for g2 in range(2):
        mB[g2 * 64:(g2 + 1) * 64, g2, :] = 1.0
    k["k_maskB"] = mB.reshape(128, 32)
    mC = np.zeros((128, 2, 64), np.float32)
    for rr in range(128):
        mC[rr, (rr // 16) % 2, :] = 1.0
    k["k_maskC"] = mC.reshape(128, 128)
    mQ = np.zeros((128, 2), np.float32)
    for rr in range(128):
        mQ[rr, (rr // 32) % 2] = 1.0
    k["k_maskQ"] = mQ
    mR = np.zeros((128, 2, 128), np.float32)
    for cc in range(128):
        mR[:, (cc // 32) % 2, cc] = 1.0
    k["k_maskR"] = mR.reshape(128, 256)
    k["k_ecap"] = np.tile((np.arange(32, dtype=np.float32) * CAP)[None, :], (128, 1))
    k["k_trige"] = (r >= c).astype(bf)
    k["k_trilt"] = (r < c).astype(bf)
    k["k_trile"] = (r <= c).astype(bf)
    m0 = np.ones((128, 512), np.float32)
    m0[:112, :] = 0.0
    k["k_mask0"] = m0.astype(bf)
    j = np.arange(128)[:, None]
    t = np.arange(512)[None, :]
    patA = np.zeros((128, 8, 512), np.float32)
    patB = np.zeros((128, 8, 512), np.float32)
    for s in range(8):
        if s < 4:
            rr = 3 - s
            patB[:, s, :] = (rr * 128 + j < t)
        else:
            rr = 7 - s
            patA[:, s, :] = (rr * 128 + j < t)
            patB[:, s, :] = 1.0
    return k, patA.astype(bf), patB.astype(bf)


def own_is_A(n, hf):
    return ((n + hf) % 2) == 0


def make_in_maps(inputs, NSLOT, CAP, n_cores=8, moe=True):
    x = np.asarray(inputs["x"], np.float32)
    NB = 1 + 8 * NSLOT
    NPOS = NB * 128
    meta = np.asarray(inputs["meta_tokens"], np.float32)
    k, patA, patB = _consts(NSLOT, CAP)
    f = lambda a: np.ascontiguousarray(np.asarray(a, np.float32))
    shared = dict(
        w_in=f(inputs["w_in"][0]), g_mix=f(inputs["norm_mix_g"][0:1]), g_ffn=f(inputs["norm_ffn_g"][0:1]),
        g_fin=f(inputs["norm_final_g"]).reshape(1, D),
        lam_re=f(inputs["ssm_lambda_re"][0]).reshape(1, 2048), lam_im=f(inputs["ssm_lambda_im"][0]).reshape(1, 2048),
        log_dt=f(inputs["ssm_log_dt"][0]).reshape(1, 32),
        b_re=f(inputs["ssm_b_re"][0]), b_im=f(inputs["ssm_b_im"][0]),
        c_re=f(inputs["ssm_c_re"][0]).reshape(512, 64), c_im=f(inputs["ssm_c_im"][0]).reshape(512, 64),
        d_skip=f(inputs["ssm_d"][0]).reshape(1, 512), glu_w=f(inputs["ssm_glu_w"][0]),
        glu_b=f(inputs["ssm_glu_b"][0]).reshape(1, 512),
        w_pa=f(inputs["w_branch_attn"][0]), w_pb=f(inputs["w_branch_ssm"][0]), w_o=f(inputs["w_out"][0]),
        w_r=f(np.concatenate([inputs["router_group_w"][0], inputs["router_expert_w"][0]], axis=1)),
        b_r=f(np.concatenate([inputs["router_group_b"][0], inputs["router_expert_b"][0]], axis=0)).reshape(1, 36),
        w1=f(inputs["expert_w1"][0][:(32 if moe else 1)]), w3=f(inputs["expert_w3"][0][:(32 if moe else 1)]),
        w2=f(inputs["expert_w2"][0][:(32 if moe else 1)]),
    )
    shared.update(k)
    maps = []
    for core in range(n_cores):
        b, hf = core // 2, core % 2
        xa = np.zeros((NPOS, D), np.float32)
        xa[112:128] = meta
        xa[128:] = x[b, :NPOS - 128]
        xo = np.zeros((512 * NSLOT, D), np.float32)
        sel = np.zeros((128, 2 * NSLOT), np.float32)
        for n in range(NSLOT):
            a = own_is_A(n, hf)
            r0 = (8 * n + (0 if a else 4)) * 128
            xo[n * 512:(n + 1) * 512] = x[b, r0:r0 + 512]
            sel[:, 2 * n] = 1.0 if a else 0.0
            sel[:, 2 * n + 1] = 0.0 if a else 1.0
        m = dict(shared)
        m["x_all"] = xa
        m["x_own"] = xo
        m["k_sel"] = sel
        m["k_maske"] = patA if own_is_A(0, hf) else patB
        m["k_masko"] = patA if own_is_A(1, hf) else patB
        maps.append(m)
    return maps


def assemble(results, NSLOT, B, SEQ):
    out = np.zeros((B, SEQ, D), np.float32)
    for core, r in enumerate(results):
        b, hf = core // 2, core % 2
        oo = np.asarray(r["out_own"], np.float32)
        for n in range(NSLOT):
            a = own_is_A(n, hf)
            r0 = (8 * n + (0 if a else 4)) * 128
            out[b, r0:r0 + 512] = oo[n * 512:(n + 1) * 512]
    return out


_NC_CACHE = {}


def kernel(**inputs):
    NSLOT, CAPB = 8, 3
    key = (NSLOT, CAPB)
    if key not in _NC_CACHE:
        _NC_CACHE[key] = build(NSLOT, CAPB)
    nc = _NC_CACHE[key]
    maps = make_in_maps(inputs, NSLOT, 128 * CAPB)
    res = run_bass_kernel_spmd(nc, maps, core_ids=list(range(8)))
    return assemble(res.results, NSLOT, 4, 8192)
```

```python
import math
from contextlib import ExitStack
import numpy as np
import ml_dtypes
import concourse.bass as bass
import concourse.mybir as mybir
from concourse.bass_utils import run_bass_kernel_spmd

F32 = mybir.dt.float32
BF16 = mybir.dt.bfloat16
I32 = mybir.dt.int32
AF = mybir.ActivationFunctionType
ALU = mybir.AluOpType
AX = mybir.AxisListType

SAME_ENGINE_SYNC = True
NDS = 20
D = 1024
TWO_PI = 2.0 * math.pi


class Buf:
    def __init__(self, name):
        self.name = name
        self.w = None
        self.r = {}


class View:
    def __init__(self, ap, buf):
        self.ap = ap
        self.buf = buf

    def __getitem__(self, k):
        return View(self.ap[k], self.buf)

    def re(self, pat, **kw):
        return View(self.ap.rearrange(pat, **kw), self.buf)

    def bc(self, shape):
        return View(self.ap.broadcast_to(list(shape)), self.buf)

    def un(self, axis):
        return View(self.ap.unsqueeze(axis), self.buf)


class T:
    def __init__(self, h, name):
        self.h = h
        self.buf = Buf(name)

    def __getitem__(self, k):
        return View(self.h[k], self.buf)


class Sched:
    ENG = ["pe", "act", "dve", "pool", "sp"]

    def __init__(self, nc, stack):
        self.nc = nc
        self.stack = stack
        self.epoch = {e: 0 for e in self.ENG}
        self.last_tok = {e: None for e in self.ENG}
        self.sem = {e: stack.enter_context(nc.semaphore("s_" + e)) for e in self.ENG}
        self.cnt = {e: 0 for e in self.ENG}
        self.ops = {e: [] for e in self.ENG}
        self.waited = {e: {} for e in self.ENG}
        self.dsem = {q: [stack.enter_context(nc.semaphore("d_%s%d" % (q, i))) for i in range(NDS)]
                     for q in ("sp", "pool")}
        self.dcnt = {q: [0] * NDS for q in ("sp", "pool")}
        self.drr = {"sp": 0, "pool": 0}

    def _deps(self, reads, writes):
        deps = []
        for b in reads:
            if b.w is not None:
                deps.append(b.w)
        for b in writes:
            if b.w is not None:
                deps.append(b.w)
            deps.extend(b.r.values())
        return deps

    def _waits(self, e, deps):
        need = {}
        for (key, sem, val) in deps:
            if val <= 0:
                continue
            if key[0] == "E" and key[1] == e and (e == "pe" or not SAME_ENGINE_SYNC):
                continue
            if self.waited[e].get(key, 0) >= val:
                continue
            if key not in need or need[key][1] < val:
                need[key] = (sem, val)
        for key, (sem, val) in need.items():
            self.waited[e][key] = val
        return list(need.values())

    def _record(self, tok, reads, writes):
        for b in reads:
            old = b.r.get(tok[0])
            if old is None or old[2] < tok[2]:
                b.r[tok[0]] = tok
        for b in writes:
            b.w = tok
            b.r = {}

    def op(self, e, fn, reads=(), writes=()):
        waits = self._waits(e, self._deps(reads, writes))
        if self.cnt[e] >= 16000:
            self.epoch[e] += 1
            self.sem[e] = self.stack.enter_context(self.nc.semaphore("s_%s_%d" % (e, self.epoch[e])))
            self.cnt[e] = 0
        self.cnt[e] += 1
        tok = (("E", e, self.epoch[e]), self.sem[e], self.cnt[e])
        self.last_tok[e] = tok
        self.ops[e].append((waits, fn, self.sem[e], 1))
        self._record(tok, reads, writes)

    def dma(self, q, fn, reads=(), writes=()):
        deps = self._deps(reads, writes)
        i = self.drr[q]
        self.drr[q] = (i + 1) % NDS
        sem = self.dsem[q][i]
        key = ("D", q, i)
        prev = self.dcnt[q][i]
        deps.append((key, sem, prev))
        waits = self._waits(q, deps)
        self.dcnt[q][i] = prev + 16
        tok = (key, sem, prev + 16)
        self.ops[q].append((waits, fn, sem, 16))
        self._record(tok, reads, writes)

    def barrier(self):
        toks = [self.last_tok[e] for e in self.ENG if self.last_tok[e] is not None]
        for q in ("sp", "pool"):
            for i in range(NDS):
                if self.dcnt[q][i] > 0:
                    toks.append((("D", q, i), self.dsem[q][i], self.dcnt[q][i]))
        for e in self.ENG:
            waits = self._waits(e, toks)
            if waits:
                self.ops[e].append((waits, None, None, 0))

    def replay(self, e, eng):
        for waits, fn, sem, inc in self.ops[e]:
            for (s, v) in waits:
                eng.wait_ge(s, v)
            if fn is not None:
                fn(eng).then_inc(sem, inc)

    def final_waits(self, e, eng, bufs):
        deps = []
        for b in bufs:
            if b.w is not None:
                deps.append(b.w)
        for (s, v) in self._waits(e, deps):
            eng.wait_ge(s, v)


def build(NSLOT, CAPB, debug=(), moe=True, nphase=4):
    NB = 1 + 8 * NSLOT
    NPOS = NB * 128
    TOWN = 512 * NSLOT
    NOB = 4 * NSLOT
    CAP = 128 * CAPB
    NXB = 32 * CAP
    nc = bass.Bass("TRN2", target_bir_lowering=False)
    stack = ExitStack()
    S = Sched(nc, stack)

    def din(name, shape, dt=F32):
        return T(nc.dram_tensor(name, list(shape), dt, kind="ExternalInput").ap(), name)

    def dscr(name, shape, dt=F32):
        kind = "ExternalOutput" if "scr" in debug else "Internal"
        return T(nc.dram_tensor(name, list(shape), dt, kind=kind).ap(), name)

    def dout(name, shape, dt=F32):
        return T(nc.dram_tensor(name, list(shape), dt, kind="ExternalOutput").ap(), name)

    cur = [stack]

    def sb(name, shape, dt=F32):
        return T(cur[0].enter_context(nc.sbuf_tensor(name, list(shape), dt)), name)

    def open_scope():
        st = ExitStack()
        cur.insert(0, st)
        return st

    def close_scope(st):
        assert cur[0] is st
        S.barrier()
        st.close()
        cur.pop(0)

    x_all = din("x_all", [NPOS, D])
    x_own = din("x_own", [TOWN, D])
    w_in = din("w_in", [D, 4096])
    g_mix = din("g_mix", [1, D])
    g_ffn = din("g_ffn", [1, D])
    g_fin = din("g_fin", [1, D])
    lam_re = din("lam_re", [1, 2048])
    lam_im = din("lam_im", [1, 2048])
    log_dt = din("log_dt", [1, 32])
    b_re = din("b_re", [32, 64, 16])
    b_im = din("b_im", [32, 64, 16])
    c_re = din("c_re", [512, 64])
    c_im = din("c_im", [512, 64])
    d_skip = din("d_skip", [1, 512])
    glu_w = din("glu_w", [512, 512])
    glu_b = din("glu_b", [1, 512])
    w_pa = din("w_pa", [512, D])
    w_pb = din("w_pb", [512, D])
    w_o = din("w_o", [D, D])
    w_r = din("w_r", [D, 36])
    b_r = din("b_r", [1, 36])
    NE = 32 if moe else 1
    w1 = din("w1", [NE, D, 512])
    w3 = din("w3", [NE, D, 512])
    w2 = din("w2", [NE, 512, D])
    k_ident = din("k_ident", [128, 128])
    k_triltf = din("k_triltf", [128, 128])
    k_onesf = din("k_onesf", [128, 128])
    k_jrow = din("k_jrow", [1, 128])
    k_maskB = din("k_maskB", [128, 32])
    k_maskC = din("k_maskC", [128, 128])
    k_ecap = din("k_ecap", [128, 32])
    k_maskQ = din("k_maskQ", [128, 2])
    k_maskR = din("k_maskR", [128, 256])
    k_trige = din("k_trige", [128, 128], BF16)
    k_trilt = din("k_trilt", [128, 128], BF16)
    k_trile = din("k_trile", [128, 128], BF16)
    k_mask0 = din("k_mask0", [128, 512], BF16)
    k_maske = din("k_maske", [128, 8, 512], BF16)
    k_masko = din("k_masko", [128, 8, 512], BF16)
    k_sel = din("k_sel", [128, 2 * NSLOT])

    kt_scr = dscr("kt_scr", [512, NPOS], BF16)
    v_scr = dscr("v_scr", [8, 128, NB, 64], BF16)
    ssm_scr = dscr("ssm_scr", [4, 128, NPOS], BF16)
    h_scr = dscr("h_scr", [TOWN, D])
    xb_scr = dscr("xb_scr", [NXB, D])
    yb_scr = dscr("yb_scr", [NXB, D])
    out_own = dout("out_own", [TOWN, D])

    dbg_outs = {}

    def dbg(name, view, shape, dt=F32):
        if name not in debug:
            return
        t = dout("dbg_" + name, shape, dt)
        dbg_outs[name] = t
        dma("sp", t[:], view)

    bcreg = {}

    def bc_reg(e):
        if "r" not in bcreg:
            bcreg["r"] = e.to_reg(NXB - 1)
        return bcreg["r"]

    def mm(out, lhsT, rhs, start=True, stop=True):
        S.op("pe", lambda e: e.matmul(out.ap, lhsT.ap, rhs.ap, start=start, stop=stop),
             reads=[lhsT.buf, rhs.buf], writes=[out.buf])

    def tr(out, in_, ident_v):
        S.op("pe", lambda e: e.transpose(out.ap, in_.ap, ident_v.ap),
             reads=[in_.buf, ident_v.buf], writes=[out.buf])

    def act(out, in_, func, bias=None, scale=None, accum=None, extra_reads=()):
        kw = {}
        reads = [in_.buf] + list(extra_reads)
        writes = [out.buf]
        if bias is not None:
            if isinstance(bias, View):
                kw["bias"] = bias.ap
                reads.append(bias.buf)
            else:
                kw["bias"] = bias
        if scale is not None:
            if isinstance(scale, View):
                kw["scale"] = scale.ap
                reads.append(scale.buf)
            else:
                kw["scale"] = scale
        if accum is not None:
            kw["accum_out"] = accum.ap
            writes.append(accum.buf)
        S.op("act", lambda e: e.activation(out.ap, in_.ap, func, **kw), reads=reads, writes=writes)

    def tt(eng, out, in0, in1, op):
        S.op(eng, lambda e: e.tensor_tensor(out.ap, in0.ap, in1.ap, op),
             reads=[in0.buf, in1.buf], writes=[out.buf])

    def _sc(x, reads):
        if isinstance(x, View):
            reads.append(x.buf)
            return x.ap
        return x

    def ts(eng, out, in0, s1, op0, s2=None, op1=None):
        reads = [in0.buf]
        a1 = _sc(s1, reads)
        a2 = _sc(s2, reads)
        if op1 is None:
            S.op(eng, lambda e: e.tensor_scalar(out.ap, in0.ap, a1, None, op0), reads=reads, writes=[out.buf])
        else:
            S.op(eng, lambda e: e.tensor_scalar(out.ap, in0.ap, a1, a2, op0, op1), reads=reads,
                 writes=[out.buf])

    def stt(out, in0, scalar, in1, op0, op1):
        reads = [in0.buf, in1.buf]
        a = _sc(scalar, reads)
        S.op("dve", lambda e: e.scalar_tensor_tensor(out.ap, in0.ap, a, in1.ap, op0, op1),
             reads=reads, writes=[out.buf])

    def cp(eng, out, in_):
        if eng == "act":
            S.op("act", lambda e: e.copy(out.ap, in_.ap), reads=[in_.buf], writes=[out.buf])
        else:
            S.op(eng, lambda e: e.tensor_copy(out.ap, in_.ap), reads=[in_.buf], writes=[out.buf])

    def dma(q, out, in_, **kw):
        S.dma(q, lambda e: e.dma_start(out=out.ap, in_=in_.ap, **kw), reads=[in_.buf], writes=[out.buf])

    P = [T(stack.enter_context(nc.psum_tensor("ps%d" % i, [128, 512], F32)), "ps%d" % i) for i in range(8)]

    def finish():
        final_bufs = [out_own.buf, kt_scr.buf, v_scr.buf, ssm_scr.buf, h_scr.buf, xb_scr.buf, yb_scr.buf] + [t.buf for t in dbg_outs.values()]
        with nc.Block() as block:
            @block.tensor
            def _(e):
                S.replay("pe", e)

            @block.scalar
            def _(e):
                S.replay("act", e)

            @block.vector
            def _(e):
                S.replay("dve", e)

            @block.gpsimd
            def _(e):
                S.replay("pool", e)

            @block.sync
            def _(e):
                S.replay("sp", e)
                S.final_waits("sp", e, final_bufs)
        stack.close()
        return nc

    ident = sb("ident", [128, 128])
    triltf = sb("triltf", [128, 128])
    onesf = sb("onesf", [128, 128])
    jrow = sb("jrow", [1, 128])
    maskB = sb("maskB", [128, 32])
    maskC = sb("maskC", [128, 128])
    ecap = sb("ecap", [128, 32])
    maskQ = sb("maskQ", [128, 2])
    maskR = sb("maskR", [128, 256])
    trige = sb("trige", [128, 128], BF16)
    trilt = sb("trilt", [128, 128], BF16)
    trile = sb("trile", [128, 128], BF16)
    sel = sb("sel", [128, 2 * NSLOT])
    epsc = sb("epsc", [128, 1])
    onec = sb("onec", [128, 1])
    grep_mix = sb("grep_mix", [128, D])
    grep_ffn = sb("grep_ffn", [128, D])
    grep_fin = sb("grep_fin", [128, D])
    brrep = sb("brrep", [128, 36])
    destall = sb("destall", [128, NOB, 2], I32)
    gwall = sb("gwall", [128, NOB, 2])
    scopeA = open_scope()
    for dst, src in ((ident, k_ident), (triltf, k_triltf), (onesf, k_onesf), (jrow, k_jrow), (maskB, k_maskB),
                     (maskC, k_maskC), (ecap, k_ecap), (maskQ, k_maskQ), (maskR, k_maskR), (trige, k_trige), (trilt, k_trilt), (trile, k_trile),
                     (sel, k_sel)):
        dma("sp", dst[:], src[:])

    dcol = sb("dcol", [128, 4])
    gbcol = sb("gbcol", [128, 4])
    T1re = sb("T1re", [128, 2048])
    T1im = sb("T1im", [128, 2048])
    T2re = sb("T2re", [128, 2048])
    T2im = sb("T2im", [128, 2048])
    are = sb("are", [128, 16])
    aim = sb("aim", [128, 16])
    Bp = sb("Bp", [128, 4, 2, 128])
    Cp = sb("Cp", [128, 4, 2, 128])
    Bq = sb("Bq", [128, 4, 2, 2, 128], BF16)
    Cq = sb("Cq", [128, 4, 2, 2, 128], BF16)
    s16 = [sb("s16_%d" % i, [128, 16]) for i in range(6)]
    scope0 = open_scope()
    rowtmp = sb("rowtmp", [1, 2048])

    def replicate_row(dst, src_dram, n):
        dma("sp", rowtmp[0:1, 0:n], src_dram[0:1, 0:n])
        for c0 in range(0, n, 512):
            c1 = min(n, c0 + 512)
            mm(P[0][:, 0:c1 - c0], onesf[0:1, :], rowtmp[0:1, c0:c1])
            cp("dve", dst[:, c0:c1], P[0][:, 0:c1 - c0])

    replicate_row(grep_mix, g_mix, D)
    replicate_row(grep_ffn, g_ffn, D)
    replicate_row(grep_fin, g_fin, D)
    replicate_row(brrep, b_r, 36)
    S.op("dve", lambda e: e.memset(onec[:].ap, 1.0), writes=[onec.buf])

    def row_to_col(dst, src_dram, nchunk):
        dma("sp", rowtmp[0:1, 0:nchunk * 128], src_dram[0:1, 0:nchunk * 128])
        for q in range(nchunk):
            mm(P[0][:, 2 * q:2 * q + 2], rowtmp[0:1, q * 128:(q + 1) * 128], onesf[0:1, 0:2])
        cp("dve", dst[:, 0:nchunk], P[0][:, 0:2 * nchunk].re("p (q t) -> p q t", t=2)[:, :, 0])

    row_to_col(dcol, d_skip, 4)
    row_to_col(gbcol, glu_b, 4)

    lrrow = sb("lrrow", [1, 2048])
    lirow = sb("lirow", [1, 2048])
    dtrow = sb("dtrow", [1, 32])
    rhorow = sb("rhorow", [1, 2048])
    throw = sb("throw", [1, 2048])
    dma("sp", lrrow[:], lam_re[:])
    dma("sp", lirow[:], lam_im[:])
    dma("sp", dtrow[:], log_dt[:])
    act(dtrow[:], dtrow[:], AF.Exp)
    dtb = dtrow[:].un(2).bc([1, 32, 64])
    tt("dve", rhorow[:].re("o (g p) -> o g p", p=64), lrrow[:].re("o (g p) -> o g p", p=64), dtb, ALU.mult)
    tt("dve", throw[:].re("o (g p) -> o g p", p=64), lirow[:].re("o (g p) -> o g p", p=64), dtb, ALU.mult)

    wkA = sb("wkA", [128, 2048])
    wkB = sb("wkB", [128, 2048])
    wkI = sb("wkI", [128, 2048], I32)
    wkM = sb("wkM", [128, 2048])

    def outer_tok(row):
        for k in range(4):
            mm(P[k][:, :], jrow[0:1, :], row[0:1, k * 512:(k + 1) * 512])

    def outer_feat(row):
        for gp in range(16):
            mm(P[gp // 4][:, (gp % 4) * 128:(gp % 4 + 1) * 128], row[0:1, gp * 128:(gp + 1) * 128], jrow[0:1, :])

    def sin_of(dst, shift):
        for k in range(4):
            ts("dve", wkA[:, k * 512:(k + 1) * 512], P[k][:, :], 1.0 / TWO_PI, ALU.mult, 0.5 + shift, ALU.add)
        cp("dve", wkI[:], wkA[:])
        cp("dve", wkB[:], wkI[:])
        for k in range(4):
            stt(wkA[:, k * 512:(k + 1) * 512], wkB[:, k * 512:(k + 1) * 512], -TWO_PI, P[k][:, :],
                ALU.mult, ALU.add)
        if shift != 0.0:
            ts("dve", wkA[:], wkA[:], shift * TWO_PI, ALU.add)
        ts("dve", wkB[:], wkA[:], math.pi, ALU.is_gt)
        stt(wkA[:], wkB[:], -TWO_PI, wkA[:], ALU.mult, ALU.add)
        ts("dve", wkB[:], wkA[:], -math.pi, ALU.is_lt)
        stt(wkA[:], wkB[:], TWO_PI, wkA[:], ALU.mult, ALU.add)
        ts("dve", wkA[:], wkA[:], 3.14159, ALU.min, -3.14159, ALU.max)
        act(dst, wkA[:], AF.Sin)

    def build_table(Tre, Tim, outer_fn, sign):
        outer_fn(rhorow)
        for k in range(4):
            act(wkM[:, k * 512:(k + 1) * 512], P[k][:, :], AF.Exp, scale=float(sign))
        outer_fn(throw)
        sin_of(Tim[:], 0.0)
        sin_of(Tre[:], 0.25)
        tt("dve", Tre[:], Tre[:], wkM[:], ALU.mult)
        stt(Tim[:], Tim[:], float(sign), wkM[:], ALU.mult, ALU.mult)

    build_table(T1re, T1im, outer_tok, -1)
    build_table(T2re, T2im, outer_feat, +1)

    lrc = sb("lrc", [128, 16])
    lic = sb("lic", [128, 16])
    for dst, row in ((lrc, lrrow), (lic, lirow)):
        for gp in range(16):
            mm(P[4][:, 2 * gp:2 * gp + 2], row[0:1, gp * 128:(gp + 1) * 128], onesf[0:1, 0:2])
        cp("dve", dst[:], P[4][:, 0:32].re("p (q t) -> p q t", t=2)[:, :, 0])
    T2re3 = T2re[:].re("p (g j) -> p g j", j=128)
    T2im3 = T2im[:].re("p (g j) -> p g j", j=128)
    cp("dve", are[:], T2re3[:, :, 1])
    cp("dve", aim[:], T2im3[:, :, 1])
    fre = sb("fre", [128, 16])
    fim = sb("fim", [128, 16])
    nr, den, t0, t1, t2, t3 = [s[:] for s in s16]
    ts("dve", nr, are[:], -1.0, ALU.add)
    tt("dve", t0, lrc[:], lrc[:], ALU.mult)
    tt("dve", t1, lic[:], lic[:], ALU.mult)
    tt("dve", den, t0, t1, ALU.add)
    S.op("dve", lambda e: e.reciprocal(den.ap, den.ap), reads=[den.buf], writes=[den.buf])
    tt("dve", t0, nr, lrc[:], ALU.mult)
    tt("dve", t1, aim[:], lic[:], ALU.mult)
    tt("dve", t2, t0, t1, ALU.add)
    tt("dve", fre[:], t2, den, ALU.mult)
    tt("dve", t0, aim[:], lrc[:], ALU.mult)
    tt("dve", t1, nr, lic[:], ALU.mult)
    tt("dve", t2, t0, t1, ALU.subtract)
    tt("dve", fim[:], t2, den, ALU.mult)

    Bsr = sb("Bsr", [128, 16, 16])
    Bsi = sb("Bsi", [128, 16, 16])
    for dst, src in ((Bsr, b_re), (Bsi, b_im)):
        v = src[:].re("(gp g2) p c -> g2 p gp c", g2=2)
        for g2 in range(2):
            dma("sp", dst[g2 * 64:(g2 + 1) * 64, :, :], v[g2])
    Bbr = sb("Bbr", [128, 16, 16])
    Bbi = sb("Bbi", [128, 16, 16])
    tB0 = sb("tB0", [128, 16, 16])
    tB1 = sb("tB1", [128, 16, 16])
    freb = fre[:].un(2).bc([128, 16, 16])
    fimb = fim[:].un(2).bc([128, 16, 16])
    tt("dve", tB0[:], Bsr[:], freb, ALU.mult)
    tt("dve", tB1[:], Bsi[:], fimb, ALU.mult)
    tt("dve", Bbr[:], tB0[:], tB1[:], ALU.subtract)
    tt("dve", tB0[:], Bsi[:], freb, ALU.mult)
    tt("dve", tB1[:], Bsr[:], fimb, ALU.mult)
    tt("dve", Bbi[:], tB0[:], tB1[:], ALU.add)
    dbg("Bbr", Bbr[:], [128, 16, 16])
    Y2 = sb("Y2", [128, 2, 16, 2, 16])
    mBb = maskB[:].re("p (a c) -> p a c", a=2).un(1).bc([128, 16, 2, 16])
    for pl, Bb in ((0, Bbr), (1, Bbi)):
        tt("dve", Y2[:, pl], Bb[:].un(2).bc([128, 16, 2, 16]), mBb, ALU.mult)
    for pl in range(2):
        for Q in range(4):
            tr(P[5][:, Q * 128:(Q + 1) * 128], Y2[:, pl, 4 * Q:4 * Q + 4].re("p a b c -> p (a b c)"), ident[:])
        cp("dve", Bp[:, :, pl, :], P[5][:, :].re("p (q n) -> p q n", n=128))
        for lh in range(2):
            ts("dve", Bq[:, :, lh, pl, :], Bp[:, :, pl, :], maskQ[:, lh:lh + 1], ALU.mult)
    Csr = sb("Csr", [128, 4, 64])
    Csi = sb("Csi", [128, 4, 64])
    dma("sp", Csr[:], c_re[:].re("(q r) p -> r q p", r=128))
    dma("sp", Csi[:], c_im[:].re("(q r) p -> r q p", r=128))
    Xc = sb("Xc", [128, 2, 4, 2, 64])
    mCb = maskC[:].re("r (a p) -> r a p", a=2).un(1).bc([128, 4, 2, 64])
    tt("dve", Xc[:, 0], Csr[:].un(2).bc([128, 4, 2, 64]), mCb, ALU.mult)
    for Q in range(4):
        stt(Xc[:, 1, Q], Csi[:, Q].un(1).bc([128, 2, 64]), -1.0, maskC[:].re("r (a p) -> r a p", a=2),
            ALU.mult, ALU.mult)
    for pl in range(2):
        for Q in range(4):
            tr(P[5][:, Q * 128:(Q + 1) * 128], Xc[:, pl, Q].re("r a p -> r (a p)"), ident[:])
        cp("dve", Cp[:, :, pl, :], P[5][:, :].re("p (q n) -> p q n", n=128))
        for lh in range(2):
            tt("dve", Cq[:, :, pl, lh, :], Cp[:, :, pl, :],
               maskR[:, lh * 128:(lh + 1) * 128].un(1).bc([128, 4, 128]), ALU.mult)

    close_scope(scope0)
    Wkvu = sb("Wkvu", [128, 8, 1536], BF16)
    for kc in range(8):
        dma("pool", Wkvu[:, kc, :], w_in[kc * 128:(kc + 1) * 128, 512:2048])
    Wg = sb("Wg", [128, 4, 512], BF16)
    for kc in range(4):
        dma("pool", Wg[:, kc, :], glu_w[kc * 128:(kc + 1) * 128, :])

    xblk = [sb("xblk%d" % i, [128, D]) for i in range(2)]
    junk = sb("junk", [128, D])
    ssq = sb("ssq", [128, 1])
    rstd = sb("rstd", [128, 1])
    xn = sb("xn", [128, D])
    xnT = sb("xnT", [128, 8, 128], BF16)
    ktb = sb("ktb", [128, 4, 128], BF16)
    vb = sb("vb", [128, 512], BF16)
    uT = sb("uT", [128, 4, 128], BF16)
    zt = sb("zt", [128, 16, 2, 128], BF16)
    tz = [sb("tz%d" % i, [128, 8, 128]) for i in range(4)]
    cg = sb("cg", [128, 8, 2, 128])
    xT = sb("xT", [128, 16, 2, 128], BF16)
    Gre = sb("Gre", [128, 16])
    Gim = sb("Gim", [128, 16])
    xlast = sb("xlast", [128, 16, 2])
    yv = sb("yv", [128, 4, 128])
    y2 = sb("y2", [128, 4, 128])
    y3 = sb("y3", [128, 4, 128])
    yg = sb("yg", [128, 4, 128])
    ygb = sb("ygb", [128, 4, 128], BF16)
    sgl = sb("sgl", [128, 4, 128])
    ssmT = sb("ssmT", [128, 4, 128], BF16)
    S.op("dve", lambda e: e.memset(Gre[:].ap, 0.0), writes=[Gre.buf])
    S.op("dve", lambda e: e.memset(Gim[:].ap, 0.0), writes=[Gim.buf])
    T1re3 = T1re[:].re("p (g n) -> p g n", n=128)
    T1im3 = T1im[:].re("p (g n) -> p g n", n=128)

    def rmsnorm(xv, grep, outv):
        act(junk[:], xv, AF.Square, accum=ssq[:])
        act(rstd[:], ssq[:], AF.Ln, scale=1.0 / D, bias=epsc[:])
        act(rstd[:], rstd[:], AF.Exp, scale=-0.5)
        stt(outv, xv, rstd[:], grep[:], ALU.mult, ALU.mult)

    S.op("dve", lambda e: e.memset(epsc[:].ap, 1e-6), writes=[epsc.buf])

    def transpose8(src, dstT, col0, ncol_total):
        for half in range(2):
            for k in range(4):
                kc = half * 4 + k
                tr(P[6 + half][:, k * 128:(k + 1) * 128], src[:, kc * 128:(kc + 1) * 128], ident[:])
            cp("act" if half == 0 else "dve", dstT[:, half * 4:half * 4 + 4, col0:col0 + 128],
               P[6 + half][:, :].re("p (k n) -> p k n", n=128))

    for blk in range(NB):
        xb_ = xblk[blk % 2]
        dma("sp", xb_[:], x_all[blk * 128:(blk + 1) * 128, :])
        rmsnorm(xb_[:], grep_mix, xn[:])
        transpose8(xn, xnT, 0, 128)
        for m in range(4):
            for kc in range(8):
                mm(P[0][:, m * 128:(m + 1) * 128], Wkvu[:, kc, m * 128:(m + 1) * 128], xnT[:, kc, :],
                   start=(kc == 0), stop=(kc == 7))
        cp("act", ktb[:], P[0][:, :].re("p (m n) -> p m n", n=128))
        dma("sp", kt_scr[:].re("(m q) n -> q m n", q=128)[:, :, blk * 128:(blk + 1) * 128], ktb[:])
        for kc in range(8):
            mm(P[2][:, :], xnT[:, kc, :], Wkvu[:, kc, 512:1024], start=(kc == 0), stop=(kc == 7))
        cp("act", vb[:], P[2][:, :])
        dma("sp", v_scr[:].re("h j b d -> j h b d")[:, :, blk, :], vb[:].re("j (h d) -> j h d", d=64))
        for m in range(4):
            for kc in range(8):
                mm(P[1][:, m * 128:(m + 1) * 128], Wkvu[:, kc, 1024 + m * 128:1024 + (m + 1) * 128],
                   xnT[:, kc, :], start=(kc == 0), stop=(kc == 7))
        cp("act", uT[:], P[1][:, :].re("p (m n) -> p m n", n=128))
        if blk == 1:
            dbg("uT", uT[:], [128, 4, 128], BF16)
        for hf in range(2):
            for b4 in range(4):
                Q, h = hf * 2 + b4 // 2, b4 % 2
                mm(P[2 + b4][:, :], uT[64 * h:64 * h + 64, Q, :],
                   Bq[64 * h:64 * h + 64, Q, :, :, :].re("k l a n -> k (l a n)"))
            def buv(pl):
                vs = []
                for b4 in range(4):
                    vs.append(P[2 + b4][:, :].re("p (g a n) -> p g a n", a=2, n=128)[:, :, pl, :])
                return vs
            bre = buv(0)
            bim = buv(1)
            gs = slice(hf * 8, hf * 8 + 8)
            for b4 in range(4):
                g2s = slice(hf * 8 + 2 * b4, hf * 8 + 2 * b4 + 2)
                l2s = slice(2 * b4, 2 * b4 + 2)
                tt("dve", tz[0][:, l2s, :], bre[b4], T1re3[:, g2s, :], ALU.mult)
                tt("dve", tz[1][:, l2s, :], bim[b4], T1im3[:, g2s, :], ALU.mult)
                tt("dve", tz[2][:, l2s, :], bre[b4], T1im3[:, g2s, :], ALU.mult)
                tt("dve", tz[3][:, l2s, :], bim[b4], T1re3[:, g2s, :], ALU.mult)
            tt("pool", zt[:, gs, 0, :], tz[0][:], tz[1][:], ALU.subtract)
            tt("pool", zt[:, gs, 1, :], tz[2][:], tz[3][:], ALU.add)
        for hf in range(2):
            gs = slice(hf * 8, hf * 8 + 8)
            for g8 in range(8):
                gp = hf * 8 + g8
                for pl in range(2):
                    idx = g8 * 2 + pl
                    mm(P[2 + idx // 4][:, (idx % 4) * 128:(idx % 4 + 1) * 128], zt[:, gp, pl, :], trile[:])
            for b4 in range(4):
                g2s = slice(hf * 8 + 2 * b4, hf * 8 + 2 * b4 + 2)
                l2s = slice(2 * b4, 2 * b4 + 2)
                pv = P[2 + b4][:, :].re("p (g a n) -> p g a n", a=2, n=128)
                tt("dve", cg[:, l2s, 0, :], pv[:, :, 0, :], Gre[:, g2s].un(2).bc([128, 2, 128]), ALU.add)
                tt("dve", cg[:, l2s, 1, :], pv[:, :, 1, :], Gim[:, g2s].un(2).bc([128, 2, 128]), ALU.add)
            tt("dve", tz[0][:], cg[:, :, 0, :], T2re3[:, gs, :], ALU.mult)
            tt("pool", tz[1][:], cg[:, :, 1, :], T2im3[:, gs, :], ALU.mult)
            tt("dve", tz[2][:], cg[:, :, 1, :], T2re3[:, gs, :], ALU.mult)
            tt("pool", tz[3][:], cg[:, :, 0, :], T2im3[:, gs, :], ALU.mult)
            tt("dve", xT[:, gs, 0, :], tz[0][:], tz[1][:], ALU.subtract)
            tt("pool", xT[:, gs, 1, :], tz[2][:], tz[3][:], ALU.add)
            tt("dve", xlast[:, gs, 0], tz[0][:, :, 127], tz[1][:, :, 127], ALU.subtract)
            tt("dve", xlast[:, gs, 1], tz[2][:, :, 127], tz[3][:, :, 127], ALU.add)
        tt("dve", s16[0][:], xlast[:, :, 0], are[:], ALU.mult)
        tt("dve", s16[1][:], xlast[:, :, 1], aim[:], ALU.mult)
        tt("dve", s16[2][:], xlast[:, :, 0], aim[:], ALU.mult)
        tt("dve", s16[3][:], xlast[:, :, 1], are[:], ALU.mult)
        tt("dve", Gre[:], s16[0][:], s16[1][:], ALU.subtract)
        tt("dve", Gim[:], s16[2][:], s16[3][:], ALU.add)
        for Q in range(4):
            for h in range(2):
                i = 0
                for lh in range(2):
                    gp = 4 * Q + 2 * h + lh
                    for pl in range(2):
                        mm(P[1][64 * h:64 * h + 64, Q * 128:(Q + 1) * 128], Cq[:, Q, pl, lh, 64 * h:64 * h + 64],
                           xT[:, gp, pl, :], start=(i == 0), stop=(i == 3))
                        i += 1
        for Q in range(4):
            stt(yv[:, Q, :], uT[:, Q, :], dcol[:, Q:Q + 1], P[1][:, Q * 128:(Q + 1) * 128], ALU.mult, ALU.add)
        if blk == 1:
            dbg("yv", yv[:], [128, 4, 128])
        tt("pool", y2[:], yv[:], yv[:], ALU.mult)
        ts("dve", y2[:], y2[:], 0.044715, ALU.mult, 1.0, ALU.add)
        tt("pool", y3[:], y2[:], yv[:], ALU.mult)
        act(y3[:], y3[:], AF.Sigmoid, scale=1.5957691216057308)
        tt("dve", yg[:], yv[:], y3[:], ALU.mult)
        cp("pool", ygb[:], yg[:])
        for co in range(4):
            for kc in range(4):
                mm(P[0][:, co * 128:(co + 1) * 128], Wg[:, kc, co * 128:(co + 1) * 128], ygb[:, kc, :],
                   start=(kc == 0), stop=(kc == 3))
        for co in range(4):
            act(sgl[:, co, :], P[0][:, co * 128:(co + 1) * 128], AF.Sigmoid, bias=gbcol[:, co:co + 1])
        tt("dve", ssmT[:], yg[:], sgl[:], ALU.mult)
        dma("sp", ssm_scr[:].re("q p n -> p q n")[:, :, blk * 128:(blk + 1) * 128], ssmT[:])

    close_scope(scopeA)
    if nphase < 2:
        while len(cur) > 1:
            cur[0].close()
            cur.pop(0)
        return finish()
    scope2 = open_scope()
    mask0 = sb("mask0", [128, 512], BF16)
    maske = sb("maske", [128, 8, 512], BF16)
    masko = sb("masko", [128, 8, 512], BF16)
    dma("sp", mask0[:], k_mask0[:])
    dma("sp", maske[:], k_maske[:])
    dma("sp", masko[:], k_masko[:])
    Wr = sb("Wr", [128, 8, 36])
    dma("sp", Wr[:], w_r[:].re("(kc p) n -> p kc n", p=128))
    Wpa = sb("Wpa", [64, 8, D], BF16)
    Wpb = sb("Wpb", [128, 4, D], BF16)
    Wo = sb("Wo", [128, 8, D], BF16)
    for h in range(8):
        dma("pool", Wpa[:, h, :], w_pa[h * 64:(h + 1) * 64, :])
    for kc in range(4):
        dma("pool", Wpb[:, kc, :], w_pb[kc * 128:(kc + 1) * 128, :])
    for kc in range(8):
        dma("pool", Wo[:, kc, :], w_o[kc * 128:(kc + 1) * 128, :])
    wring = [sb("wring%d" % i, [128, 8, 512], BF16) for i in range(2)]
    wri = [0]

    def load_wchunk(c0):
        t = wring[wri[0] % 2]
        wri[0] += 1
        for kc in range(8):
            dma("pool", t[:, kc, :], w_in[kc * 128:(kc + 1) * 128, c0:c0 + 512])
        return t

    xo = [sb("xo%d" % i, [128, D]) for i in range(2)]
    xn2 = sb("xn2", [128, D])
    junk2 = sb("junk2", [128, D], BF16)
    ssq2 = sb("ssq2", [128, 1])
    rstd2 = sb("rstd2", [128, 1])
    xoT = sb("xoT", [128, 8, 512], BF16)
    qT = sb("qT", [64, 8, 512], BF16)
    ktc = [[sb("ktc%d_%d" % (a, i), [64, 1024], BF16) for i in range(2)] for a in range(2)]
    vtc = [[sb("vtc%d_%d" % (a, i), [128, 8, 64], BF16) for i in range(2)] for a in range(2)]
    er = [[sb("er%d_%d" % (a, i), [128, 512], BF16) for i in range(2)] for a in range(2)]
    spr = [[sb("spr%d_%d" % (a, i), [128, 512], BF16) for i in range(2)] for a in range(2)]
    tr_ = [sb("tr%d" % a, [128, 512], BF16) for a in range(2)]
    wr_ = [[sb("wr%d_%d" % (a, i), [128, 512], BF16) for i in range(2)] for a in range(2)]
    attnT = sb("attnT", [64, 8, 512], BF16)
    ssA = sb("ssA", [128, 4, 512], BF16)
    ssB = sb("ssB", [128, 4, 512], BF16)
    ssO = ssA
    sg1 = sb("sg1", [128, 512], BF16)
    sg2 = sb("sg2", [128, 512], BF16)
    mg1 = sb("mg1", [128, 512])
    mg2 = sb("mg2", [128, 512])
    mergedT = sb("mergedT", [128, 8, 512], BF16)
    hblk = [sb("hblk0", [128, D])] * 2
    hn = [xn2, xn2]
    hnT = sb("hnT", [128, 8, 128])
    lgt = sb("lgt", [128, 36])
    r1 = [sb("r1_%d" % i, [128, 1]) for i in range(12)]
    ohg = sb("ohg", [128, 4])
    eg = sb("eg", [128, 4])
    selv = sb("selv", [128, 8])
    m8 = sb("m8", [128, 8])
    oh1 = sb("oh1", [128, 8])
    oh2 = sb("oh2", [128, 8])
    M12 = sb("M12", [128, 64])
    pm = sb("pm", [128, 64])
    pmt = sb("pmt", [128, 64])
    base = sb("base", [128, 32])
    destf = sb("destf", [128, 2])
    S.op("dve", lambda e: e.memset(base[:].ap, 0.0), writes=[base.buf])

    def rmsnorm2(xv, grep, outv):
        act(junk2[:], xv, AF.Square, accum=ssq2[:])
        act(rstd2[:], ssq2[:], AF.Ln, scale=1.0 / D, bias=epsc[:])
        act(rstd2[:], rstd2[:], AF.Exp, scale=-0.5)
        stt(outv, xv, rstd2[:], grep[:], ALU.mult, ALU.mult)

    def mmacc(out, lhsT, rhs, start):
        S.op("pe", lambda e: e.matmul(out.ap, lhsT.ap, rhs.ap, start=start, stop=True, skip_group_check=True),
             reads=[lhsT.buf, rhs.buf], writes=[out.buf])

    for n in range(NSLOT):
        pmask = maske if n % 2 == 0 else masko
        for r in range(4):
            xb_ = xo[r % 2]
            dma("sp", xb_[:], x_own[(4 * n + r) * 128:(4 * n + r + 1) * 128, :])
            rmsnorm2(xb_[:], grep_mix, xn2[:])
            transpose8(xn2, xoT, r * 128, 512)
        wq = load_wchunk(0)
        for h in range(8):
            pb = P[4 + h % 4]
            for kc in range(8):
                mm(pb[0:64, :], wq[:, kc, h * 64:(h + 1) * 64], xoT[:, kc, :], start=(kc == 0), stop=(kc == 7))
            cp("act" if h % 2 == 0 else "dve", qT[:, h, :], pb[0:64, :])
        if n == 0:
            dbg("qT", qT[:], [64, 8, 512], BF16)
        nkb = 8 * n + 9
        kbs = [8 * n + 8 - s for s in range(nkb)]
        for hp in range(4):
            heads = (2 * hp, 2 * hp + 1)
            ACC = (P[0], P[1])
            OB = (P[2], P[3])
            ZR = ((P[4], P[5]), (P[6], P[7]))
            cur_chunk = [None, None]
            cur_kt = [None, None]
            cur_vt = [None, None]
            nload = [0, 0]

            def get_kv(a, kb):
                h = heads[a]
                c = -1 if kb == 0 else (kb - 1) // 8
                if cur_chunk[a] != c:
                    i = nload[a] % 2
                    nload[a] += 1
                    kt_, vt_ = ktc[a][i], vtc[a][i]
                    if c < 0:
                        dma("sp", kt_[:, 0:128], kt_scr[h * 64:(h + 1) * 64, 0:128])
                        dma("sp", vt_[:, 0:1, :], v_scr[h, :, 0:1, :])
                    else:
                        dma("sp", kt_[:, :], kt_scr[h * 64:(h + 1) * 64, (8 * c + 1) * 128:(8 * c + 9) * 128])
                        dma("sp", vt_[:, :, :], v_scr[h, :, 8 * c + 1:8 * c + 9, :])
                    cur_chunk[a], cur_kt[a], cur_vt[a] = c, kt_, vt_
                lb = 0 if kb == 0 else (kb - 1) % 8
                return cur_kt[a][:, lb * 128:(lb + 1) * 128], cur_vt[a][:, lb, :]

            vsel = {}

            def front(a, s):
                kb = kbs[s]
                ktv, vtv = get_kv(a, kb)
                vsel[(a, s)] = vtv
                Z = ZR[a][s % 2]
                mm(Z[:, :], ktv, qT[:, heads[a], :])
                e_ = er[a][s % 2]
                act(e_[:], Z[:, :], AF.Exp, scale=0.125)
                if s < 8:
                    tt("dve", e_[:], e_[:], pmask[:, s, :], ALU.mult)
                elif kb == 0:
                    tt("dve", e_[:], e_[:], mask0[:], ALU.mult)
                act(spr[a][s % 2][:], e_[:], AF.Ln, bias=onec[:])

            for a in range(2):
                front(a, 0)
            for s in range(nkb):
                for a in range(2):
                    mmacc(ACC[a][:, :], trige[:], spr[a][s % 2][:], start=(s == 0))
                for a in range(2):
                    act(tr_[a][:], ACC[a][:, :], AF.Exp, scale=-1.0)
                if s + 1 < nkb:
                    for a in range(2):
                        front(a, s + 1)
                for a in range(2):
                    mmacc(ACC[a][:, :], trilt[:], spr[a][s % 2][:], start=False)
                for a in range(2):
                    tt("dve" if a == 0 else "pool", wr_[a][s % 2][:], er[a][s % 2][:], tr_[a][:], ALU.mult)
                for a in range(2):
                    mmacc(OB[a][0:64, :], vsel[(a, s)], wr_[a][s % 2][:], start=(s == 0))
            for a in range(2):
                cp("act" if a == 0 else "dve", attnT[:, heads[a], :], OB[a][0:64, :])
        if n == 0:
            dbg("attnT", attnT[:], [64, 8, 512], BF16)
        posA = (8 * n + 1) * 128
        posB = (8 * n + 5) * 128
        sv = ssm_scr[:].re("q p n -> p q n")
        dma("sp", ssA[:], sv[:, :, posA:posA + 512])
        dma("sp", ssB[:], sv[:, :, posB:posB + 512])
        ts("dve", ssO[:], ssA[:], sel[:, 2 * n:2 * n + 1], ALU.mult)
        stt(ssO[:], ssB[:], sel[:, 2 * n + 1:2 * n + 2], ssO[:], ALU.mult, ALU.add)
        for c2 in range(2):
            wga = load_wchunk(2048 + c2 * 512)
            wgb = load_wchunk(3072 + c2 * 512)
            for c4 in range(4):
                co = c2 * 4 + c4
                for kc in range(8):
                    mm(P[4][:, :], wga[:, kc, c4 * 128:(c4 + 1) * 128], xoT[:, kc, :], start=(kc == 0), stop=(kc == 7))
                for kc in range(8):
                    mm(P[5][:, :], wgb[:, kc, c4 * 128:(c4 + 1) * 128], xoT[:, kc, :], start=(kc == 0), stop=(kc == 7))
                for h in range(8):
                    mm(P[6][:, :], Wpa[:, h, co * 128:(co + 1) * 128], attnT[:, h, :], start=(h == 0), stop=(h == 7))
                for kc in range(4):
                    mm(P[7][:, :], Wpb[:, kc, co * 128:(co + 1) * 128], ssO[:, kc, :], start=(kc == 0), stop=(kc == 3))
                act(sg1[:], P[4][:, :], AF.Sigmoid)
                act(sg2[:], P[5][:, :], AF.Sigmoid)
                tt("dve", mg1[:], P[6][:, :], sg1[:], ALU.mult)
                tt("dve", mg2[:], P[7][:, :], sg2[:], ALU.mult)
                tt("pool", mergedT[:, co, :], mg1[:], mg2[:], ALU.add)
        if n == 0:
            dbg("mergedT", mergedT[:], [128, 8, 512], BF16)
        for r in range(4):
            ob = 4 * n + r
            xb_ = xo[r % 2]
            hb_ = hblk[r % 2]
            hn_ = hn[r % 2]
            dma("sp", xb_[:], x_own[ob * 128:(ob + 1) * 128, :])
            for half in range(2):
                for kc in range(8):
                    mm(P[half][:, :], mergedT[:, kc, r * 128:(r + 1) * 128], Wo[:, kc, half * 512:(half + 1) * 512],
                       start=(kc == 0), stop=(kc == 7))
                tt("dve", hb_[:, half * 512:(half + 1) * 512], P[half][:, :], xb_[:, half * 512:(half + 1) * 512],
                   ALU.add)
            dma("sp", h_scr[ob * 128:(ob + 1) * 128, :], hb_[:])
            rmsnorm2(hb_[:], grep_ffn, hn_[:])
            for half in range(2):
                for k in range(4):
                    kc = half * 4 + k
                    tr(P[6 + half][:, k * 128:(k + 1) * 128], hn_[:, kc * 128:(kc + 1) * 128], ident[:])
                cp("act" if half == 0 else "dve", hnT[:, half * 4:half * 4 + 4, :],
                   P[6 + half][:, :].re("p (k n) -> p k n", n=128))
            for kc in range(8):
                mm(P[2][:, 0:36], hnT[:, kc, :], Wr[:, kc, :], start=(kc == 0), stop=(kc == 7))
            tt("dve", lgt[:], P[2][:, 0:36], brrep[:], ALU.add)
            gmax, ngmax, gsum, ptop, dv, ex, g1, g2, p1loc, p2loc, ov1, ov2 = [t_[:] for t_ in r1]
            S.op("dve", lambda e: e.tensor_reduce(gmax.ap, lgt[:, 0:4].ap, AX.X, ALU.max),
                 reads=[lgt.buf], writes=[gmax.buf])
            ts("dve", ohg[:], lgt[:, 0:4], gmax, ALU.is_equal)
            ts("dve", ngmax, gmax, -1.0, ALU.mult)
            act(eg[:], lgt[:, 0:4], AF.Exp, bias=ngmax, accum=gsum)
            S.op("dve", lambda e: e.reciprocal(ptop.ap, gsum.ap), reads=[gsum.buf], writes=[ptop.buf])
            ts("dve", selv[:], lgt[:, 4:12], ohg[:, 0:1], ALU.mult)
            for g in range(1, 4):
                stt(selv[:], lgt[:, 4 + 8 * g:12 + 8 * g], ohg[:, g:g + 1], selv[:], ALU.mult, ALU.add)
            S.op("dve", lambda e: e.max(m8[:].ap, selv[:].ap), reads=[selv.buf], writes=[m8.buf])
            ts("dve", oh1[:], selv[:], m8[:, 0:1], ALU.is_equal)
            ts("dve", oh2[:], selv[:], m8[:, 1:2], ALU.is_equal)
            tt("dve", dv, m8[:, 1:2], m8[:, 0:1], ALU.subtract)
            act(ex, dv, AF.Exp)
            ts("dve", g1, ex, 1.0, ALU.add)
            S.op("dve", lambda e: e.reciprocal(g1.ap, g1.ap), reads=[g1.buf], writes=[g1.buf])
            tt("dve", g2, ex, g1, ALU.mult)
            tt("dve", gwall[:, ob, 0:1], g1, ptop, ALU.mult)
            tt("dve", gwall[:, ob, 1:2], g2, ptop, ALU.mult)
            tt("dve", M12[:, 0:32].re("p (g e) -> p g e", e=8), ohg[:].un(2).bc([128, 4, 8]),
               oh1[:].un(1).bc([128, 4, 8]), ALU.mult)
            tt("dve", M12[:, 32:64].re("p (g e) -> p g e", e=8), ohg[:].un(2).bc([128, 4, 8]),
               oh2[:].un(1).bc([128, 4, 8]), ALU.mult)
            mm(P[3][:, 0:64], triltf[:], M12[:])
            mm(P[3][:, 64:128], onesf[:], M12[:])
            tt("dve", pm[:, 0:32], P[3][:, 0:32], base[:], ALU.add)
            tt("dve", pm[:, 32:64], P[3][:, 32:64], base[:], ALU.add)
            tt("dve", pm[:, 32:64], P[3][:, 64:96], pm[:, 32:64], ALU.add)
            tt("dve", pmt[:], pm[:], M12[:], ALU.mult)
            S.op("dve", lambda e: e.tensor_reduce(p1loc.ap, pmt[:, 0:32].ap, AX.X, ALU.add),
                 reads=[pmt.buf], writes=[p1loc.buf])
            S.op("dve", lambda e: e.tensor_reduce(p2loc.ap, pmt[:, 32:64].ap, AX.X, ALU.add),
                 reads=[pmt.buf], writes=[p2loc.buf])
            tt("dve", pmt[:, 0:32], M12[:, 0:32], ecap[:], ALU.mult)
            tt("dve", pmt[:, 32:64], M12[:, 32:64], ecap[:], ALU.mult)
            S.op("dve", lambda e: e.tensor_reduce(ov1.ap, pmt[:, 0:32].ap, AX.X, ALU.add),
                 reads=[pmt.buf], writes=[ov1.buf])
            S.op("dve", lambda e: e.tensor_reduce(ov2.ap, pmt[:, 32:64].ap, AX.X, ALU.add),
                 reads=[pmt.buf], writes=[ov2.buf])
            tt("dve", destf[:, 0:1], ov1, p1loc, ALU.add)
            tt("dve", destf[:, 1:2], ov2, p2loc, ALU.add)
            ts("dve", ov1, p1loc, float(CAP) - 0.5, ALU.is_gt, float(4 * NXB), ALU.mult)
            ts("dve", ov2, p2loc, float(CAP) - 0.5, ALU.is_gt, float(4 * NXB), ALU.mult)
            tt("dve", destf[:, 0:1], destf[:, 0:1], ov1, ALU.add)
            tt("dve", destf[:, 1:2], destf[:, 1:2], ov2, ALU.add)
            cp("dve", destall[:, ob, :], destf[:])
            tt("dve", base[:], P[3][:, 64:96], base[:], ALU.add)
            tt("dve", base[:], P[3][:, 96:128], base[:], ALU.add)
            for k in range(2 if "noscat" not in debug else 0):
                dv_ = destall[:, ob, k:k + 1]
                S.dma("pool", (lambda dv_, hn_: (lambda e: e.indirect_dma_start(
                    out=xb_scr[:].ap, out_offset=bass.IndirectOffsetOnAxis(ap=dv_.ap, axis=0),
                    in_=hn_[:].ap, in_offset=None, bounds_check=bc_reg(e), oob_is_err=False)))(dv_, hn_),
                    reads=[hn_.buf, destall.buf], writes=[xb_scr.buf])
    dbg("destall", destall[:], [128, NOB, 2], I32)
    dbg("gwall", gwall[:], [128, NOB, 2])
    close_scope(scope2)
    if nphase < 3 or not moe:
        return finish()
    if nphase >= 3 and moe:
        scope3 = open_scope()
        W1 = [sb("W1_%d" % i, [128, 8, 512], BF16) for i in range(2)]
        W3 = [sb("W3_%d" % i, [128, 8, 512], BF16) for i in range(2)]
        W2 = [sb("W2_%d" % i, [128, 4, D], BF16) for i in range(2)]
        xg = [sb("xg%d" % i, [128, D]) for i in range(2)]
        xeT = sb("xeT", [128, 8, CAP], BF16)
        s1 = sb("s1", [128, CAP])
        hgT = sb("hgT", [128, 4, CAP], BF16)
        ybk = [sb("ybk%d" % i, [128, D]) for i in range(2)]
        for ex_ in range(32):
            w1_, w3_, w2_ = W1[ex_ % 2], W3[ex_ % 2], W2[ex_ % 2]
            for kc in range(8):
                dma("pool", w1_[:, kc, :], w1[ex_, kc * 128:(kc + 1) * 128, :])
                dma("pool", w3_[:, kc, :], w3[ex_, kc * 128:(kc + 1) * 128, :])
            for fc in range(4):
                dma("pool", w2_[:, fc, :], w2[ex_, fc * 128:(fc + 1) * 128, :])
            for c in range(CAPB):
                xg_ = xg[c % 2]
                dma("sp", xg_[:], xb_scr[ex_ * CAP + c * 128:ex_ * CAP + (c + 1) * 128, :])
                transpose8(xg_, xeT, c * 128, CAP)
            for fc in range(4):
                for kc in range(8):
                    mm(P[0][:, 0:CAP], w1_[:, kc, fc * 128:(fc + 1) * 128], xeT[:, kc, :], start=(kc == 0), stop=(kc == 7))
                for kc in range(8):
                    mm(P[1][:, 0:CAP], w3_[:, kc, fc * 128:(fc + 1) * 128], xeT[:, kc, :], start=(kc == 0), stop=(kc == 7))
                act(s1[:], P[0][:, 0:CAP], AF.Silu)
                tt("dve", hgT[:, fc, :], s1[:], P[1][:, 0:CAP], ALU.mult)
            for c in range(CAPB):
                yb_ = ybk[c % 2]
                for half in range(2):
                    for fc in range(4):
                        mm(P[2 + half][:, :], hgT[:, fc, c * 128:(c + 1) * 128], w2_[:, fc, half * 512:(half + 1) * 512],
                           start=(fc == 0), stop=(fc == 3))
                    cp("act" if half == 0 else "dve", yb_[:, half * 512:(half + 1) * 512], P[2 + half][:, :])
                dma("sp", yb_scr[ex_ * CAP + c * 128:ex_ * CAP + (c + 1) * 128, :], yb_[:])
        if nphase < 4:
            close_scope(scope3)
            return finish()
        yk = [sb("yk%d" % i, [128, D]) for i in range(2)]
        hb4 = [sb("hb4_%d" % i, [128, D]) for i in range(2)]
        oo = [sb("oo%d" % i, [128, D]) for i in range(2)]
        junk4 = sb("junk4", [128, D], BF16)
        ssq4 = sb("ssq4", [128, 1])
        rstd4 = sb("rstd4", [128, 1])
        for ob in range(NOB):
            hb_ = hb4[ob % 2]
            dma("sp", hb_[:], h_scr[ob * 128:(ob + 1) * 128, :])
            for k in range(2):
                yk_ = yk[k]
                S.op("pool", (lambda yk_: (lambda e: e.memset(yk_[:].ap, 0.0)))(yk_), writes=[yk_.buf])
                dv_ = destall[:, ob, k:k + 1]
                S.dma("pool", (lambda dv_, yk_: (lambda e: e.indirect_dma_start(
                    out=yk_[:].ap, out_offset=None, in_=yb_scr[:].ap,
                    in_offset=bass.IndirectOffsetOnAxis(ap=dv_.ap, axis=0),
                    bounds_check=bc_reg(e), oob_is_err=False)))(dv_, yk_),
                    reads=[yb_scr.buf, destall.buf], writes=[yk_.buf])
                stt(hb_[:], yk_[:], gwall[:, ob, k:k + 1], hb_[:], ALU.mult, ALU.add)
            oo_ = oo[ob % 2]
            act(junk4[:], hb_[:], AF.Square, accum=ssq4[:])
            act(rstd4[:], ssq4[:], AF.Ln, scale=1.0 / D, bias=epsc[:])
            act(rstd4[:], rstd4[:], AF.Exp, scale=-0.5)
            stt(oo_[:], hb_[:], rstd4[:], grep_fin[:], ALU.mult, ALU.mult)
            dma("sp", out_own[ob * 128:(ob + 1) * 128, :], oo_[:])
        close_scope(scope3)

    return finish()


def _consts(NSLOT, CAP):
    bf = ml_dtypes.bfloat16
    r = np.arange(128)[:, None]
    c = np.arange(128)[None, :]
    k = {}
    k["k_ident"] = (r == c).astype(np.float32)
    k["k_triltf"] = (r < c).astype(np.float32)
    k["k_onesf"] = np.ones((128, 128), np.float32)
    k["k_jrow"] = np.arange(128, dtype=np.float32)[None, :]
    mB = np.zeros((128, 2, 16), np.float32)
    for g2 in range(2):
        mB[g2 * 64:(g2 + 1) * 64, g2, :] = 1.0
    k["k_maskB"] = mB.reshape(128, 32)
    mC = np.zeros((128, 2, 64), np.float32)
    for rr in range(128):
        mC[rr, (rr // 16) % 2, :] = 1.0
    k["k_maskC"] = mC.reshape(128, 128)
    mQ = np.zeros((128, 2), np.float32)
    for rr in range(128):
        mQ[rr, (rr // 32) % 2] = 1.0
    k["k_maskQ"] = mQ
    mR = np.zeros((128, 2, 128), np.float32)
    for cc in range(128):
        mR[:, (cc // 32) % 2, cc] = 1.0
    k["k_maskR"] = mR.reshape(128, 256)
    k["k_ecap"] = np.tile((np.arange(32, dtype=np.float32) * CAP)[None, :], (128, 1))
    k["k_trige"] = (r >= c).astype(bf)
    k["k_trilt"] = (r < c).astype(bf)
    k["k_trile"] = (r <= c).astype(bf)
    m0 = np.ones((128, 512), np.float32)
    m0[:112, :] = 0.0
    k["k_mask0"] = m0.astype(bf)
    j = np.arange(128)[:, None]
    t = np.arange(512)[None, :]
    patA = np.zeros((128, 8, 512), np.float32)
    patB = np.zeros((128, 8, 512), np.float32)
    for s in range(8):
        if s < 4:
            rr = 3 - s
            patB[:, s, :] = (rr * 128 + j < t)
        else:
            rr = 7 - s
            patA[:, s, :] = (rr * 128 + j < t)
            patB[:, s, :] = 1.0
    return k, patA.astype(bf), patB.astype(bf)


def own_is_A(n, hf):
    return ((n + hf) % 2) == 0


def make_in_maps(inputs, NSLOT, CAP, n_cores=8, moe=True):
    x = np.asarray(inputs["x"], np.float32)
    NB = 1 + 8 * NSLOT
    NPOS = NB * 128
    meta = np.asarray(inputs["meta_tokens"], np.float32)
    k, patA, patB = _consts(NSLOT, CAP)
    f = lambda a: np.ascontiguousarray(np.asarray(a, np.float32))
    shared = dict(
        w_in=f(inputs["w_in"][0]), g_mix=f(inputs["norm_mix_g"][0:1]), g_ffn=f(inputs["norm_ffn_g"][0:1]),
        g_fin=f(inputs["norm_final_g"]).reshape(1, D),
        lam_re=f(inputs["ssm_lambda_re"][0]).reshape(1, 2048), lam_im=f(inputs["ssm_lambda_im"][0]).reshape(1, 2048),
        log_dt=f(inputs["ssm_log_dt"][0]).reshape(1, 32),
        b_re=f(inputs["ssm_b_re"][0]), b_im=f(inputs["ssm_b_im"][0]),
        c_re=f(inputs["ssm_c_re"][0]).reshape(512, 64), c_im=f(inputs["ssm_c_im"][0]).reshape(512, 64),
        d_skip=f(inputs["ssm_d"][0]).reshape(1, 512), glu_w=f(inputs["ssm_glu_w"][0]),
        glu_b=f(inputs["ssm_glu_b"][0]).reshape(1, 512),
        w_pa=f(inputs["w_branch_attn"][0]), w_pb=f(inputs["w_branch_ssm"][0]), w_o=f(inputs["w_out"][0]),
        w_r=f(np.concatenate([inputs["router_group_w"][0], inputs["router_expert_w"][0]], axis=1)),
        b_r=f(np.concatenate([inputs["router_group_b"][0], inputs["router_expert_b"][0]], axis=0)).reshape(1, 36),
        w1=f(inputs["expert_w1"][0][:(32 if moe else 1)]), w3=f(inputs["expert_w3"][0][:(32 if moe else 1)]),
        w2=f(inputs["expert_w2"][0][:(32 if moe else 1)]),
    )
    shared.update(k)
    maps = []
    for core in range(n_cores):
        b, hf = core // 2, core % 2
        xa = np.zeros((NPOS, D), np.float32)
        xa[112:128] = meta
        xa[128:] = x[b, :NPOS - 128]
        xo = np.zeros((512 * NSLOT, D), np.float32)
        sel = np.zeros((128, 2 * NSLOT), np.float32)
        for n in range(NSLOT):
            a = own_is_A(n, hf)
            r0 = (8 * n + (0 if a else 4)) * 128
            xo[n * 512:(n + 1) * 512] = x[b, r0:r0 + 512]
            sel[:, 2 * n] = 1.0 if a else 0.0
            sel[:, 2 * n + 1] = 0.0 if a else 1.0
        m = dict(shared)
        m["x_all"] = xa
        m["x_own"] = xo
        m["k_sel"] = sel
        m["k_maske"] = patA if own_is_A(0, hf) else patB
        m["k_masko"] = patA if own_is_A(1, hf) else patB
        maps.append(m)
    return maps


def assemble(results, NSLOT, B, SEQ):
    out = np.zeros((B, SEQ, D), np.float32)
    for core, r in enumerate(results):
        b, hf = core // 2, core % 2
        oo = np.asarray(r["out_own"], np.float32)
        for n in range(NSLOT):
            a = own_is_A(n, hf)
            r0 = (8 * n + (0 if a else 4)) * 128
            out[b, r0:r0 + 512] = oo[n * 512:(n + 1) * 512]
    return out


_NC_CACHE = {}


def kernel(**inputs):
    NSLOT, CAPB = 8, 3
    key = (NSLOT, CAPB)
    if key not in _NC_CACHE:
        _NC_CACHE[key] = build(NSLOT, CAPB)
    nc = _NC_CACHE[key]
    maps = make_in_maps(inputs, NSLOT, 128 * CAPB)
    res = run_bass_kernel_spmd(nc, maps, core_ids=list(range(8)))
    return assemble(res.results, NSLOT, 4, 8192)
```

```python
import math
from contextlib import ExitStack
import numpy as np
import ml_dtypes
import concourse.bass as bass
import concourse.mybir as mybir
from concourse.bass_utils import run_bass_kernel_spmd

F32 = mybir.dt.float32
BF16 = mybir.dt.bfloat16
I32 = mybir.dt.int32
AF = mybir.ActivationFunctionType
ALU = mybir.AluOpType
AX = mybir.AxisListType

SAME_ENGINE_SYNC = True
NDS = 20
D = 1024
TWO_PI = 2.0 * math.pi


class Buf:
    def __init__(self, name):
        self.name = name
        self.w = None
        self.r = {}


class View:
    def __init__(self, ap, buf):
        self.ap = ap
        self.buf = buf

    def __getitem__(self, k):
        return View(self.ap[k], self.buf)

    def re(self, pat, **kw):
        return View(self.ap.rearrange(pat, **kw), self.buf)

    def bc(self, shape):
        return View(self.ap.broadcast_to(list(shape)), self.buf)

    def un(self, axis):
        return View(self.ap.unsqueeze(axis), self.buf)


class T:
    def __init__(self, h, name):
        self.h = h
        self.buf = Buf(name)

    def __getitem__(self, k):
        return View(self.h[k], self.buf)


class Sched:
    ENG = ["pe", "act", "dve", "pool", "sp"]

    def __init__(self, nc, stack):
        self.nc = nc
        self.stack = stack
        self.epoch = {e: 0 for e in self.ENG}
        self.last_tok = {e: None for e in self.ENG}
        self.sem = {e: stack.enter_context(nc.semaphore("s_" + e)) for e in self.ENG}
        self.cnt = {e: 0 for e in self.ENG}
        self.ops = {e: [] for e in self.ENG}
        self.waited = {e: {} for e in self.ENG}
        self.dsem = {q: [stack.enter_context(nc.semaphore("d_%s%d" % (q, i))) for i in range(NDS)]
                     for q in ("sp", "pool")}
        self.dcnt = {q: [0] * NDS for q in ("sp", "pool")}
        self.drr = {"sp": 0, "pool": 0}

    @staticmethod
    def _flat(bufs):
        out = []
        for b in bufs:
            if isinstance(b, (tuple, list)):
                out.extend(b)
            else:
                out.append(b)
        return out

    def _deps(self, reads, writes):
        reads = self._flat(reads)
        writes = self._flat(writes)
        deps = []
        for b in reads:
            if b.w is not None:
                deps.append(b.w)
        for b in writes:
            if b.w is not None:
                deps.append(b.w)
            deps.extend(b.r.values())
        return deps

    def _waits(self, e, deps):
        need = {}
        for (key, sem, val) in deps:
            if val <= 0:
                continue
            if key[0] == "E" and key[1] == e and (e == "pe" or not SAME_ENGINE_SYNC):
                continue
            if self.waited[e].get(key, 0) >= val:
                continue
            if key not in need or need[key][1] < val:
                need[key] = (sem, val)
        for key, (sem, val) in need.items():
            self.waited[e][key] = val
        return list(need.values())

    def _record(self, tok, reads, writes):
        reads = self._flat(reads)
        writes = self._flat(writes)
        for b in reads:
            old = b.r.get(tok[0])
            if old is None or old[2] < tok[2]:
                b.r[tok[0]] = tok
        for b in writes:
            b.w = tok
            b.r = {}

    def op(self, e, fn, reads=(), writes=()):
        waits = self._waits(e, self._deps(reads, writes))
        if self.cnt[e] >= 16000:
            self.epoch[e] += 1
            self.sem[e] = self.stack.enter_context(self.nc.semaphore("s_%s_%d" % (e, self.epoch[e])))
            self.cnt[e] = 0
        self.cnt[e] += 1
        tok = (("E", e, self.epoch[e]), self.sem[e], self.cnt[e])
        self.last_tok[e] = tok
        self.ops[e].append((waits, fn, self.sem[e], 1))
        self._record(tok, reads, writes)

    def dma(self, q, fn, reads=(), writes=()):
        deps = self._deps(reads, writes)
        i = self.drr[q]
        self.drr[q] = (i + 1) % NDS
        sem = self.dsem[q][i]
        key = ("D", q, i)
        prev = self.dcnt[q][i]
        deps.append((key, sem, prev))
        waits = self._waits(q, deps)
        self.dcnt[q][i] = prev + 16
        tok = (key, sem, prev + 16)
        self.ops[q].append((waits, fn, sem, 16))
        self._record(tok, reads, writes)

    def barrier(self):
        toks = [self.last_tok[e] for e in self.ENG if self.last_tok[e] is not None]
        for q in ("sp", "pool"):
            for i in range(NDS):
                if self.dcnt[q][i] > 0:
                    toks.append((("D", q, i), self.dsem[q][i], self.dcnt[q][i]))
        for e in self.ENG:
            waits = self._waits(e, toks)
            if waits:
                self.ops[e].append((waits, None, None, 0))

    def replay(self, e, eng):
        for waits, fn, sem, inc in self.ops[e]:
            for (s, v) in waits:
                eng.wait_ge(s, v)
            if fn is not None:
                fn(eng).then_inc(sem, inc)

    def final_waits(self, e, eng, bufs):
        deps = []
        for b in self._flat(bufs):
            if b.w is not None:
                deps.append(b.w)
        for (s, v) in self._waits(e, deps):
            eng.wait_ge(s, v)


def build(NSLOT, CAPB, debug=(), moe=True, nphase=4):
    NB = 1 + 8 * NSLOT
    NPOS = NB * 128
    TOWN = 512 * NSLOT
    NOB = 4 * NSLOT
    CAP = 128 * CAPB
    NXB = 32 * CAP
    nc = bass.Bass("TRN2", target_bir_lowering=False)
    stack = ExitStack()
    S = Sched(nc, stack)

    def din(name, shape, dt=F32):
        return T(nc.dram_tensor(name, list(shape), dt, kind="ExternalInput").ap(), name)

    def dscr(name, shape, dt=F32):
        kind = "ExternalOutput" if "scr" in debug else "Internal"
        return T(nc.dram_tensor(name, list(shape), dt, kind=kind).ap(), name)

    def dout(name, shape, dt=F32):
        return T(nc.dram_tensor(name, list(shape), dt, kind="ExternalOutput").ap(), name)

    cur = [stack]

    def sb(name, shape, dt=F32):
        return T(cur[0].enter_context(nc.sbuf_tensor(name, list(shape), dt)), name)

    def open_scope():
        st = ExitStack()
        cur.insert(0, st)
        return st

    def close_scope(st):
        assert cur[0] is st
        S.barrier()
        st.close()
        cur.pop(0)

    x_all = din("x_all", [NPOS, D])
    x_own = din("x_own", [TOWN, D])
    w_in = din("w_in", [D, 4096])
    g_mix = din("g_mix", [1, D])
    g_ffn = din("g_ffn", [1, D])
    g_fin = din("g_fin", [1, D])
    lam_re = din("lam_re", [1, 2048])
    lam_im = din("lam_im", [1, 2048])
    log_dt = din("log_dt", [1, 32])
    b_re = din("b_re", [32, 64, 16])
    b_im = din("b_im", [32, 64, 16])
    c_re = din("c_re", [512, 64])
    c_im = din("c_im", [512, 64])
    d_skip = din("d_skip", [1, 512])
    glu_w = din("glu_w", [512, 512])
    glu_b = din("glu_b", [1, 512])
    w_pa = din("w_pa", [512, D])
    w_pb = din("w_pb", [512, D])
    w_o = din("w_o", [D, D])
    w_r = din("w_r", [D, 36])
    b_r = din("b_r", [1, 36])
    NE = 32 if moe else 1
    w1 = din("w1", [NE, D, 512])
    w3 = din("w3", [NE, D, 512])
    w2 = din("w2", [NE, 512, D])
    k_ident = din("k_ident", [128, 128])
    k_triltf = din("k_triltf", [128, 128])
    k_onesf = din("k_onesf", [128, 128])
    k_jrow = din("k_jrow", [1, 128])
    k_maskB = din("k_maskB", [128, 32])
    k_maskC = din("k_maskC", [128, 128])
    k_ecap = din("k_ecap", [128, 32])
    k_maskQ = din("k_maskQ", [128, 2])
    k_maskR = din("k_maskR", [128, 256])
    k_trige = din("k_trige", [128, 128], BF16)
    k_trilt = din("k_trilt", [128, 128], BF16)
    k_trile = din("k_trile", [128, 128], BF16)
    k_mask0 = din("k_mask0", [128, 512], BF16)
    k_maske = din("k_maske", [128, 8, 512], BF16)
    k_masko = din("k_masko", [128, 8, 512], BF16)
    k_sel = din("k_sel", [128, 2 * NSLOT])

    kt_scr = dscr("kt_scr", [512, NPOS], BF16)
    v_scr = dscr("v_scr", [8, 128, NB, 64], BF16)
    ssm_scr = dscr("ssm_scr", [4, 128, NPOS], BF16)
    h_scr = dscr("h_scr", [TOWN, D])
    xb_scr = dscr("xb_scr", [NXB, D])
    yb_scr = dscr("yb_scr", [NXB, D])
    out_own = dout("out_own", [TOWN, D])

    dbg_outs = {}

    def dbg(name, view, shape, dt=F32):
        if name not in debug:
            return
        t = dout("dbg_" + name, shape, dt)
        dbg_outs[name] = t
        dma("sp", t[:], view)

    bcreg = {}

    def bc_reg(e):
        if "r" not in bcreg:
            bcreg["r"] = e.to_reg(NXB - 1)
        return bcreg["r"]

    def mm(out, lhsT, rhs, start=True, stop=True):
        S.op("pe", lambda e: e.matmul(out.ap, lhsT.ap, rhs.ap, start=start, stop=stop),
             reads=[lhsT.buf, rhs.buf], writes=[out.buf])

    def tr(out, in_, ident_v):
        S.op("pe", lambda e: e.transpose(out.ap, in_.ap, ident_v.ap),
             reads=[in_.buf, ident_v.buf], writes=[out.buf])

    def act(out, in_, func, bias=None, scale=None, accum=None, extra_reads=()):
        kw = {}
        reads = [in_.buf] + list(extra_reads)
        writes = [out.buf]
        if bias is not None:
            if isinstance(bias, View):
                kw["bias"] = bias.ap
                reads.append(bias.buf)
            else:
                kw["bias"] = bias
        if scale is not None:
            if isinstance(scale, View):
                kw["scale"] = scale.ap
                reads.append(scale.buf)
            else:
                kw["scale"] = scale
        if accum is not None:
            kw["accum_out"] = accum.ap
            writes.append(accum.buf)
        S.op("act", lambda e: e.activation(out.ap, in_.ap, func, **kw), reads=reads, writes=writes)

    def tt(eng, out, in0, in1, op):
        S.op(eng, lambda e: e.tensor_tensor(out.ap, in0.ap, in1.ap, op),
             reads=[in0.buf, in1.buf], writes=[out.buf])

    def _sc(x, reads):
        if isinstance(x, View):
            reads.append(x.buf)
            return x.ap
        return x

    def ts(eng, out, in0, s1, op0, s2=None, op1=None):
        reads = [in0.buf]
        a1 = _sc(s1, reads)
        a2 = _sc(s2, reads)
        if op1 is None:
            S.op(eng, lambda e: e.tensor_scalar(out.ap, in0.ap, a1, None, op0), reads=reads, writes=[out.buf])
        else:
            S.op(eng, lambda e: e.tensor_scalar(out.ap, in0.ap, a1, a2, op0, op1), reads=reads,
                 writes=[out.buf])

    def stt(out, in0, scalar, in1, op0, op1):
        reads = [in0.buf, in1.buf]
        a = _sc(scalar, reads)
        S.op("dve", lambda e: e.scalar_tensor_tensor(out.ap, in0.ap, a, in1.ap, op0, op1),
             reads=reads, writes=[out.buf])

    def cp(eng, out, in_):
        if eng == "act":
            S.op("act", lambda e: e.copy(out.ap, in_.ap), reads=[in_.buf], writes=[out.buf])
        else:
            S.op(eng, lambda e: e.tensor_copy(out.ap, in_.ap), reads=[in_.buf], writes=[out.buf])

    def dma(q, out, in_, **kw):
        S.dma(q, lambda e: e.dma_start(out=out.ap, in_=in_.ap, **kw), reads=[in_.buf], writes=[out.buf])

    Pall_h = stack.enter_context(nc.psum_tensor("psall", [128, 4096], F32))

    class PB:
        def __init__(self, i0, nb):
            self.i0, self.nb = i0, nb
            self.buf = tuple(pbufs[i0:i0 + nb]) if nb > 1 else pbufs[i0]

        def __getitem__(self, k):
            return View(Pall_h[:, self.i0 * 512:(self.i0 + self.nb) * 512][k], self.buf)

    pbufs = [Buf("ps%d" % i) for i in range(8)]
    P = [PB(i, 1) for i in range(8)]
    PQ = PB(2, 4)
    P01 = PB(0, 2)
    P45 = PB(4, 2)
    P67 = PB(6, 2)

    def finish():
        final_bufs = [out_own.buf, kt_scr.buf, v_scr.buf, ssm_scr.buf, h_scr.buf, xb_scr.buf, yb_scr.buf] + [t.buf for t in dbg_outs.values()]
        with nc.Block() as block:
            @block.tensor
            def _(e):
                S.replay("pe", e)

            @block.scalar
            def _(e):
                S.replay("act", e)

            @block.vector
            def _(e):
                S.replay("dve", e)

            @block.gpsimd
            def _(e):
                S.replay("pool", e)

            @block.sync
            def _(e):
                S.replay("sp", e)
                S.final_waits("sp", e, final_bufs)
        stack.close()
        return nc

    ident = sb("ident", [128, 128])
    triltf = sb("triltf", [128, 128])
    onesf = sb("onesf", [128, 128])
    jrow = sb("jrow", [1, 128])
    maskB = sb("maskB", [128, 32])
    maskC = sb("maskC", [128, 128])
    ecap = sb("ecap", [128, 32])
    maskQ = sb("maskQ", [128, 2])
    maskR = sb("maskR", [128, 256])
    trige = sb("trige", [128, 128], BF16)
    trilt = sb("trilt", [128, 128], BF16)
    trile = sb("trile", [128, 128], BF16)
    sel = sb("sel", [128, 2 * NSLOT])
    epsc = sb("epsc", [128, 1])
    onec = sb("onec", [128, 1])
    grep_mix = sb("grep_mix", [128, D])
    grep_ffn = sb("grep_ffn", [128, D])
    grep_fin = sb("grep_fin", [128, D])
    brrep = sb("brrep", [128, 36])
    destall = sb("destall", [128, NOB, 2], I32)
    gwall = sb("gwall", [128, NOB, 2])
    scopeA = open_scope()
    for dst, src in ((ident, k_ident), (triltf, k_triltf), (onesf, k_onesf), (jrow, k_jrow), (maskB, k_maskB),
                     (maskC, k_maskC), (ecap, k_ecap), (maskQ, k_maskQ), (maskR, k_maskR), (trige, k_trige), (trilt, k_trilt), (trile, k_trile),
                     (sel, k_sel)):
        dma("sp", dst[:], src[:])

    dcol = sb("dcol", [128, 4])
    gbcol = sb("gbcol", [128, 4])
    T1re = sb("T1re", [128, 2048])
    T1im = sb("T1im", [128, 2048])
    T2re = sb("T2re", [128, 2048])
    T2im = sb("T2im", [128, 2048])
    are = sb("are", [128, 16])
    aim = sb("aim", [128, 16])
    Bp = sb("Bp", [128, 4, 2, 128])
    Cp = sb("Cp", [128, 4, 2, 128])
    Bq = sb("Bq", [128, 4, 2, 2, 128], BF16)
    Cq = sb("Cq", [128, 4, 2, 2, 128], BF16)
    s16 = [sb("s16_%d" % i, [128, 16]) for i in range(6)]
    scope0 = open_scope()
    rowtmp = sb("rowtmp", [1, 2048])

    def replicate_row(dst, src_dram, n):
        dma("sp", rowtmp[0:1, 0:n], src_dram[0:1, 0:n])
        for c0 in range(0, n, 512):
            c1 = min(n, c0 + 512)
            mm(P[0][:, 0:c1 - c0], onesf[0:1, :], rowtmp[0:1, c0:c1])
            cp("dve", dst[:, c0:c1], P[0][:, 0:c1 - c0])

    replicate_row(grep_mix, g_mix, D)
    replicate_row(grep_ffn, g_ffn, D)
    replicate_row(grep_fin, g_fin, D)
    replicate_row(brrep, b_r, 36)
    S.op("dve", lambda e: e.memset(onec[:].ap, 1.0), writes=[onec.buf])

    def row_to_col(dst, src_dram, nchunk):
        dma("sp", rowtmp[0:1, 0:nchunk * 128], src_dram[0:1, 0:nchunk * 128])
        for q in range(nchunk):
            mm(P[0][:, 2 * q:2 * q + 2], rowtmp[0:1, q * 128:(q + 1) * 128], onesf[0:1, 0:2])
        cp("dve", dst[:, 0:nchunk], P[0][:, 0:2 * nchunk].re("p (q t) -> p q t", t=2)[:, :, 0])

    row_to_col(dcol, d_skip, 4)
    row_to_col(gbcol, glu_b, 4)

    lrrow = sb("lrrow", [1, 2048])
    lirow = sb("lirow", [1, 2048])
    dtrow = sb("dtrow", [1, 32])
    rhorow = sb("rhorow", [1, 2048])
    throw = sb("throw", [1, 2048])
    dma("sp", lrrow[:], lam_re[:])
    dma("sp", lirow[:], lam_im[:])
    dma("sp", dtrow[:], log_dt[:])
    act(dtrow[:], dtrow[:], AF.Exp)
    dtb = dtrow[:].un(2).bc([1, 32, 64])
    tt("dve", rhorow[:].re("o (g p) -> o g p", p=64), lrrow[:].re("o (g p) -> o g p", p=64), dtb, ALU.mult)
    tt("dve", throw[:].re("o (g p) -> o g p", p=64), lirow[:].re("o (g p) -> o g p", p=64), dtb, ALU.mult)

    wkA = sb("wkA", [128, 2048])
    wkB = sb("wkB", [128, 2048])
    wkI = sb("wkI", [128, 2048], I32)
    wkM = sb("wkM", [128, 2048])

    def outer_tok(row):
        for k in range(4):
            mm(P[k][:, :], jrow[0:1, :], row[0:1, k * 512:(k + 1) * 512])

    def outer_feat(row):
        for gp in range(16):
            mm(P[gp // 4][:, (gp % 4) * 128:(gp % 4 + 1) * 128], row[0:1, gp * 128:(gp + 1) * 128], jrow[0:1, :])

    def sin_of(dst, shift):
        for k in range(4):
            ts("dve", wkA[:, k * 512:(k + 1) * 512], P[k][:, :], 1.0 / TWO_PI, ALU.mult, 0.5 + shift, ALU.add)
        cp("dve", wkI[:], wkA[:])
        cp("dve", wkB[:], wkI[:])
        for k in range(4):
            stt(wkA[:, k * 512:(k + 1) * 512], wkB[:, k * 512:(k + 1) * 512], -TWO_PI, P[k][:, :],
                ALU.mult, ALU.add)
        if shift != 0.0:
            ts("dve", wkA[:], wkA[:], shift * TWO_PI, ALU.add)
        ts("dve", wkB[:], wkA[:], math.pi, ALU.is_gt)
        stt(wkA[:], wkB[:], -TWO_PI, wkA[:], ALU.mult, ALU.add)
        ts("dve", wkB[:], wkA[:], -math.pi, ALU.is_lt)
        stt(wkA[:], wkB[:], TWO_PI, wkA[:], ALU.mult, ALU.add)
        ts("dve", wkA[:], wkA[:], 3.14159, ALU.min, -3.14159, ALU.max)
        act(dst, wkA[:], AF.Sin)

    def build_table(Tre, Tim, outer_fn, sign):
        outer_fn(rhorow)
        for k in range(4):
            act(wkM[:, k * 512:(k + 1) * 512], P[k][:, :], AF.Exp, scale=float(sign))
        outer_fn(throw)
        sin_of(Tim[:], 0.0)
        sin_of(Tre[:], 0.25)
        tt("dve", Tre[:], Tre[:], wkM[:], ALU.mult)
        stt(Tim[:], Tim[:], float(sign), wkM[:], ALU.mult, ALU.mult)

    build_table(T1re, T1im, outer_tok, -1)
    build_table(T2re, T2im, outer_feat, +1)

    lrc = sb("lrc", [128, 16])
    lic = sb("lic", [128, 16])
    for dst, row in ((lrc, lrrow), (lic, lirow)):
        for gp in range(16):
            mm(P[4][:, 2 * gp:2 * gp + 2], row[0:1, gp * 128:(gp + 1) * 128], onesf[0:1, 0:2])
        cp("dve", dst[:], P[4][:, 0:32].re("p (q t) -> p q t", t=2)[:, :, 0])
    T2re3 = T2re[:].re("p (g j) -> p g j", j=128)
    T2im3 = T2im[:].re("p (g j) -> p g j", j=128)
    cp("dve", are[:], T2re3[:, :, 1])
    cp("dve", aim[:], T2im3[:, :, 1])
    fre = sb("fre", [128, 16])
    fim = sb("fim", [128, 16])
    nr, den, t0, t1, t2, t3 = [s[:] for s in s16]
    ts("dve", nr, are[:], -1.0, ALU.add)
    tt("dve", t0, lrc[:], lrc[:], ALU.mult)
    tt("dve", t1, lic[:], lic[:], ALU.mult)
    tt("dve", den, t0, t1, ALU.add)
    S.op("dve", lambda e: e.reciprocal(den.ap, den.ap), reads=[den.buf], writes=[den.buf])
    tt("dve", t0, nr, lrc[:], ALU.mult)
    tt("dve", t1, aim[:], lic[:], ALU.mult)
    tt("dve", t2, t0, t1, ALU.add)
    tt("dve", fre[:], t2, den, ALU.mult)
    tt("dve", t0, aim[:], lrc[:], ALU.mult)
    tt("dve", t1, nr, lic[:], ALU.mult)
    tt("dve", t2, t0, t1, ALU.subtract)
    tt("dve", fim[:], t2, den, ALU.mult)

    Bsr = sb("Bsr", [128, 16, 16])
    Bsi = sb("Bsi", [128, 16, 16])
    for dst, src in ((Bsr, b_re), (Bsi, b_im)):
        v = src[:].re("(gp g2) p c -> g2 p gp c", g2=2)
        for g2 in range(2):
            dma("sp", dst[g2 * 64:(g2 + 1) * 64, :, :], v[g2])
    Bbr = sb("Bbr", [128, 16, 16])
    Bbi = sb("Bbi", [128, 16, 16])
    tB0 = sb("tB0", [128, 16, 16])
    tB1 = sb("tB1", [128, 16, 16])
    freb = fre[:].un(2).bc([128, 16, 16])
    fimb = fim[:].un(2).bc([128, 16, 16])
    tt("dve", tB0[:], Bsr[:], freb, ALU.mult)
    tt("dve", tB1[:], Bsi[:], fimb, ALU.mult)
    tt("dve", Bbr[:], tB0[:], tB1[:], ALU.subtract)
    tt("dve", tB0[:], Bsi[:], freb, ALU.mult)
    tt("dve", tB1[:], Bsr[:], fimb, ALU.mult)
    tt("dve", Bbi[:], tB0[:], tB1[:], ALU.add)
    dbg("Bbr", Bbr[:], [128, 16, 16])
    Y2 = sb("Y2", [128, 2, 16, 2, 16])
    mBb = maskB[:].re("p (a c) -> p a c", a=2).un(1).bc([128, 16, 2, 16])
    for pl, Bb in ((0, Bbr), (1, Bbi)):
        tt("dve", Y2[:, pl], Bb[:].un(2).bc([128, 16, 2, 16]), mBb, ALU.mult)
    for pl in range(2):
        for Q in range(4):
            tr(P[5][:, Q * 128:(Q + 1) * 128], Y2[:, pl, 4 * Q:4 * Q + 4].re("p a b c -> p (a b c)"), ident[:])
        cp("dve", Bp[:, :, pl, :], P[5][:, :].re("p (q n) -> p q n", n=128))
        for lh in range(2):
            ts("dve", Bq[:, :, lh, pl, :], Bp[:, :, pl, :], maskQ[:, lh:lh + 1], ALU.mult)
    Csr = sb("Csr", [128, 4, 64])
    Csi = sb("Csi", [128, 4, 64])
    dma("sp", Csr[:], c_re[:].re("(q r) p -> r q p", r=128))
    dma("sp", Csi[:], c_im[:].re("(q r) p -> r q p", r=128))
    Xc = sb("Xc", [128, 2, 4, 2, 64])
    mCb = maskC[:].re("r (a p) -> r a p", a=2).un(1).bc([128, 4, 2, 64])
    tt("dve", Xc[:, 0], Csr[:].un(2).bc([128, 4, 2, 64]), mCb, ALU.mult)
    for Q in range(4):
        stt(Xc[:, 1, Q], Csi[:, Q].un(1).bc([128, 2, 64]), -1.0, maskC[:].re("r (a p) -> r a p", a=2),
            ALU.mult, ALU.mult)
    for pl in range(2):
        for Q in range(4):
            tr(P[5][:, Q * 128:(Q + 1) * 128], Xc[:, pl, Q].re("r a p -> r (a p)"), ident[:])
        cp("dve", Cp[:, :, pl, :], P[5][:, :].re("p (q n) -> p q n", n=128))
        for lh in range(2):
            tt("dve", Cq[:, :, pl, lh, :], Cp[:, :, pl, :],
               maskR[:, lh * 128:(lh + 1) * 128].un(1).bc([128, 4, 128]), ALU.mult)

    close_scope(scope0)
    Wkvu = sb("Wkvu", [128, 8, 1536], BF16)
    for kc in range(8):
        dma("pool", Wkvu[:, kc, :], w_in[kc * 128:(kc + 1) * 128, 512:2048])
    Wg = sb("Wg", [128, 4, 512], BF16)
    for kc in range(4):
        dma("pool", Wg[:, kc, :], glu_w[kc * 128:(kc + 1) * 128, :])

    xblk = [sb("xblk%d" % i, [128, D]) for i in range(2)]
    junk = sb("junk", [128, D])
    ssq = sb("ssq", [128, 1])
    rstd = sb("rstd", [128, 1])
    xn = sb("xn", [128, D])
    xnT = sb("xnT", [128, 8, 128], BF16)
    ktb = sb("ktb", [128, 4, 128], BF16)
    vb = sb("vb", [128, 512], BF16)
    uT = sb("uT", [128, 4, 128], BF16)
    zt = sb("zt", [128, 16, 2, 128], BF16)
    tz = [sb("tz%d" % i, [128, 8, 128]) for i in range(4)]
    cg = sb("cg", [128, 8, 2, 128])
    xT = sb("xT", [128, 16, 2, 128], BF16)
    Gre = sb("Gre", [128, 16])
    Gim = sb("Gim", [128, 16])
    xlast = sb("xlast", [128, 16, 2])
    yv = sb("yv", [128, 4, 128])
    y2 = sb("y2", [128, 4, 128])
    y3 = sb("y3", [128, 4, 128])
    yg = sb("yg", [128, 4, 128])
    ygb = sb("ygb", [128, 4, 128], BF16)
    sgl = sb("sgl", [128, 4, 128])
    ssmT = sb("ssmT", [128, 4, 128], BF16)
    S.op("dve", lambda e: e.memset(Gre[:].ap, 0.0), writes=[Gre.buf])
    S.op("dve", lambda e: e.memset(Gim[:].ap, 0.0), writes=[Gim.buf])
    T1re3 = T1re[:].re("p (g n) -> p g n", n=128)
    T1im3 = T1im[:].re("p (g n) -> p g n", n=128)

    def rmsnorm(xv, grep, outv):
        act(junk[:], xv, AF.Square, accum=ssq[:])
        act(rstd[:], ssq[:], AF.Ln, scale=1.0 / D, bias=epsc[:])
        act(rstd[:], rstd[:], AF.Exp, scale=-0.5)
        stt(outv, xv, rstd[:], grep[:], ALU.mult, ALU.mult)

    S.op("dve", lambda e: e.memset(epsc[:].ap, 1e-6), writes=[epsc.buf])

    def transpose8(src, dstT, col0, ncol_total):
        for half in range(2):
            for k in range(4):
                kc = half * 4 + k
                tr(P[6 + half][:, k * 128:(k + 1) * 128], src[:, kc * 128:(kc + 1) * 128], ident[:])
            cp("act" if half == 0 else "dve", dstT[:, half * 4:half * 4 + 4, col0:col0 + 128],
               P[6 + half][:, :].re("p (k n) -> p k n", n=128))

    for blk in range(NB):
        xb_ = xblk[blk % 2]
        dma("sp", xb_[:], x_all[blk * 128:(blk + 1) * 128, :])
        rmsnorm(xb_[:], grep_mix, xn[:])
        transpose8(xn, xnT, 0, 128)
        for m in range(4):
            for kc in range(8):
                mm(P[0][:, m * 128:(m + 1) * 128], Wkvu[:, kc, m * 128:(m + 1) * 128], xnT[:, kc, :],
                   start=(kc == 0), stop=(kc == 7))
        cp("act", ktb[:], P[0][:, :].re("p (m n) -> p m n", n=128))
        dma("sp", kt_scr[:].re("(m q) n -> q m n", q=128)[:, :, blk * 128:(blk + 1) * 128], ktb[:])
        for kc in range(8):
            mm(P[2][:, :], xnT[:, kc, :], Wkvu[:, kc, 512:1024], start=(kc == 0), stop=(kc == 7))
        cp("act", vb[:], P[2][:, :])
        dma("sp", v_scr[:].re("h j b d -> j h b d")[:, :, blk, :], vb[:].re("j (h d) -> j h d", d=64))
        for m in range(4):
            for kc in range(8):
                mm(P[1][:, m * 128:(m + 1) * 128], Wkvu[:, kc, 1024 + m * 128:1024 + (m + 1) * 128],
                   xnT[:, kc, :], start=(kc == 0), stop=(kc == 7))
        cp("act", uT[:], P[1][:, :].re("p (m n) -> p m n", n=128))
        if blk == 1:
            dbg("uT", uT[:], [128, 4, 128], BF16)
        for hf in range(2):
            for b4 in range(4):
                Q, h = hf * 2 + b4 // 2, b4 % 2
                mm(P[2 + b4][:, :], uT[64 * h:64 * h + 64, Q, :],
                   Bq[64 * h:64 * h + 64, Q, :, :, :].re("k l a n -> k (l a n)"))
            def buv(pl):
                vs = []
                for b4 in range(4):
                    vs.append(P[2 + b4][:, :].re("p (g a n) -> p g a n", a=2, n=128)[:, :, pl, :])
                return vs
            bre = buv(0)
            bim = buv(1)
            gs = slice(hf * 8, hf * 8 + 8)
            for b4 in range(4):
                g2s = slice(hf * 8 + 2 * b4, hf * 8 + 2 * b4 + 2)
                l2s = slice(2 * b4, 2 * b4 + 2)
                tt("dve", tz[0][:, l2s, :], bre[b4], T1re3[:, g2s, :], ALU.mult)
                tt("dve", tz[1][:, l2s, :], bim[b4], T1im3[:, g2s, :], ALU.mult)
                tt("dve", tz[2][:, l2s, :], bre[b4], T1im3[:, g2s, :], ALU.mult)
                tt("dve", tz[3][:, l2s, :], bim[b4], T1re3[:, g2s, :], ALU.mult)
            tt("pool", zt[:, gs, 0, :], tz[0][:], tz[1][:], ALU.subtract)
            tt("pool", zt[:, gs, 1, :], tz[2][:], tz[3][:], ALU.add)
        for hf in range(2):
            gs = slice(hf * 8, hf * 8 + 8)
            for g8 in range(8):
                gp = hf * 8 + g8
                for pl in range(2):
                    idx = g8 * 2 + pl
                    mm(P[2 + idx // 4][:, (idx % 4) * 128:(idx % 4 + 1) * 128], zt[:, gp, pl, :], trile[:])
            for b4 in range(4):
                g2s = slice(hf * 8 + 2 * b4, hf * 8 + 2 * b4 + 2)
                l2s = slice(2 * b4, 2 * b4 + 2)
                pv = P[2 + b4][:, :].re("p (g a n) -> p g a n", a=2, n=128)
                tt("dve", cg[:, l2s, 0, :], pv[:, :, 0, :], Gre[:, g2s].un(2).bc([128, 2, 128]), ALU.add)
                tt("dve", cg[:, l2s, 1, :], pv[:, :, 1, :], Gim[:, g2s].un(2).bc([128, 2, 128]), ALU.add)
            tt("dve", tz[0][:], cg[:, :, 0, :], T2re3[:, gs, :], ALU.mult)
            tt("pool", tz[1][:], cg[:, :, 1, :], T2im3[:, gs, :], ALU.mult)
            tt("dve", tz[2][:], cg[:, :, 1, :], T2re3[:, gs, :], ALU.mult)
            tt("pool", tz[3][:], cg[:, :, 0, :], T2im3[:, gs, :], ALU.mult)
            tt("dve", xT[:, gs, 0, :], tz[0][:], tz[1][:], ALU.subtract)
            tt("pool", xT[:, gs, 1, :], tz[2][:], tz[3][:], ALU.add)
            tt("dve", xlast[:, gs, 0], tz[0][:, :, 127], tz[1][:, :, 127], ALU.subtract)
            tt("dve", xlast[:, gs, 1], tz[2][:, :, 127], tz[3][:, :, 127], ALU.add)
        tt("dve", s16[0][:], xlast[:, :, 0], are[:], ALU.mult)
        tt("dve", s16[1][:], xlast[:, :, 1], aim[:], ALU.mult)
        tt("dve", s16[2][:], xlast[:, :, 0], aim[:], ALU.mult)
        tt("dve", s16[3][:], xlast[:, :, 1], are[:], ALU.mult)
        tt("dve", Gre[:], s16[0][:], s16[1][:], ALU.subtract)
        tt("dve", Gim[:], s16[2][:], s16[3][:], ALU.add)
        for Q in range(4):
            for h in range(2):
                i = 0
                for lh in range(2):
                    gp = 4 * Q + 2 * h + lh
                    for pl in range(2):
                        mm(P[1][64 * h:64 * h + 64, Q * 128:(Q + 1) * 128], Cq[:, Q, pl, lh, 64 * h:64 * h + 64],
                           xT[:, gp, pl, :], start=(i == 0), stop=(i == 3))
                        i += 1
        for Q in range(4):
            stt(yv[:, Q, :], uT[:, Q, :], dcol[:, Q:Q + 1], P[1][:, Q * 128:(Q + 1) * 128], ALU.mult, ALU.add)
        if blk == 1:
            dbg("yv", yv[:], [128, 4, 128])
        tt("pool", y2[:], yv[:], yv[:], ALU.mult)
        ts("dve", y2[:], y2[:], 0.044715, ALU.mult, 1.0, ALU.add)
        tt("pool", y3[:], y2[:], yv[:], ALU.mult)
        act(y3[:], y3[:], AF.Sigmoid, scale=1.5957691216057308)
        tt("dve", yg[:], yv[:], y3[:], ALU.mult)
        cp("pool", ygb[:], yg[:])
        for co in range(4):
            for kc in range(4):
                mm(P[0][:, co * 128:(co + 1) * 128], Wg[:, kc, co * 128:(co + 1) * 128], ygb[:, kc, :],
                   start=(kc == 0), stop=(kc == 3))
        for co in range(4):
            act(sgl[:, co, :], P[0][:, co * 128:(co + 1) * 128], AF.Sigmoid, bias=gbcol[:, co:co + 1])
        tt("dve", ssmT[:], yg[:], sgl[:], ALU.mult)
        dma("sp", ssm_scr[:].re("q p n -> p q n")[:, :, blk * 128:(blk + 1) * 128], ssmT[:])

    close_scope(scopeA)
    if nphase < 2:
        while len(cur) > 1:
            cur[0].close()
            cur.pop(0)
        return finish()
    scope2 = open_scope()
    mask0 = sb("mask0", [128, 512], BF16)
    maske = sb("maske", [128, 8, 512], BF16)
    masko = sb("masko", [128, 8, 512], BF16)
    dma("sp", mask0[:], k_mask0[:])
    dma("sp", maske[:], k_maske[:])
    dma("sp", masko[:], k_masko[:])
    Wr = sb("Wr", [128, 8, 36])
    dma("sp", Wr[:], w_r[:].re("(kc p) n -> p kc n", p=128))
    Wpa = sb("Wpa", [64, 8, D], BF16)
    Wpb = sb("Wpb", [128, 4, D], BF16)
    Wo = sb("Wo", [128, 8, D], BF16)
    for h in range(8):
        dma("pool", Wpa[:, h, :], w_pa[h * 64:(h + 1) * 64, :])
    for kc in range(4):
        dma("pool", Wpb[:, kc, :], w_pb[kc * 128:(kc + 1) * 128, :])
    for kc in range(8):
        dma("pool", Wo[:, kc, :], w_o[kc * 128:(kc + 1) * 128, :])
    wring = [sb("wring%d" % i, [128, 8, 512], BF16) for i in range(2)]
    wri = [0]

    def load_wchunk(c0):
        t = wring[wri[0] % 2]
        wri[0] += 1
        for kc in range(8):
            dma("pool", t[:, kc, :], w_in[kc * 128:(kc + 1) * 128, c0:c0 + 512])
        return t

    xo = [sb("xo%d" % i, [128, D]) for i in range(2)]
    xn2 = sb("xn2", [128, D])
    junk2 = sb("junk2", [128, D], BF16)
    ssq2 = sb("ssq2", [128, 1])
    rstd2 = sb("rstd2", [128, 1])
    xoT = sb("xoT", [128, 8, 512], BF16)
    qT = sb("qT", [64, 8, 512], BF16)
    ktc = [[sb("ktc%d_%d" % (a, i), [64, 1024], BF16) for i in range(2)] for a in range(2)]
    vtc = [[sb("vtc%d_%d" % (a, i), [128, 8, 64], BF16) for i in range(2)] for a in range(2)]
    erw = [sb("erw%d" % i, [128, 1024], BF16) for i in range(2)]
    sprw = [sb("sprw%d" % i, [128, 1024], BF16) for i in range(2)]
    trw = sb("trw", [128, 1024], BF16)
    wrw = [sb("wrw%d" % i, [128, 1024], BF16) for i in range(2)]
    attnT = sb("attnT", [64, 8, 512], BF16)
    ssA = sb("ssA", [128, 4, 512], BF16)
    ssB = sb("ssB", [128, 4, 512], BF16)
    ssO = ssA
    sg1 = sb("sg1", [128, 512], BF16)
    sg2 = sb("sg2", [128, 512], BF16)
    mg1 = sb("mg1", [128, 512])
    mg2 = sb("mg2", [128, 512])
    mergedT = sb("mergedT", [128, 8, 512], BF16)
    hblk = [sb("hblk0", [128, D])] * 2
    hn = [xn2, xn2]
    hnT = sb("hnT", [128, 8, 128])
    lgt = sb("lgt", [128, 36])
    r1 = [sb("r1_%d" % i, [128, 1]) for i in range(12)]
    ohg = sb("ohg", [128, 4])
    eg = sb("eg", [128, 4])
    selv = sb("selv", [128, 8])
    m8 = sb("m8", [128, 8])
    oh1 = sb("oh1", [128, 8])
    oh2 = sb("oh2", [128, 8])
    M12 = sb("M12", [128, 64])
    pm = sb("pm", [128, 64])
    pmt = sb("pmt", [128, 64])
    base = sb("base", [128, 32])
    destf = sb("destf", [128, 2])
    S.op("dve", lambda e: e.memset(base[:].ap, 0.0), writes=[base.buf])

    def rmsnorm2(xv, grep, outv):
        act(junk2[:], xv, AF.Square, accum=ssq2[:])
        act(rstd2[:], ssq2[:], AF.Ln, scale=1.0 / D, bias=epsc[:])
        act(rstd2[:], rstd2[:], AF.Exp, scale=-0.5)
        stt(outv, xv, rstd2[:], grep[:], ALU.mult, ALU.mult)

    def mmacc(out, lhsT, rhs, start):
        S.op("pe", lambda e: e.matmul(out.ap, lhsT.ap, rhs.ap, start=start, stop=True, skip_group_check=True),
             reads=[lhsT.buf, rhs.buf], writes=[out.buf])

    for n in range(NSLOT):
        pmask = maske if n % 2 == 0 else masko
        for r in range(4):
            xb_ = xo[r % 2]
            dma("sp", xb_[:], x_own[(4 * n + r) * 128:(4 * n + r + 1) * 128, :])
            rmsnorm2(xb_[:], grep_mix, xn2[:])
            transpose8(xn2, xoT, r * 128, 512)
        wq = load_wchunk(0)
        for h in range(8):
            pb = P[4 + h % 4]
            for kc in range(8):
                mm(pb[0:64, :], wq[:, kc, h * 64:(h + 1) * 64], xoT[:, kc, :], start=(kc == 0), stop=(kc == 7))
            cp("act" if h % 2 == 0 else "dve", qT[:, h, :], pb[0:64, :])
        if n == 0:
            dbg("qT", qT[:], [64, 8, 512], BF16)
        nkb = 8 * n + 9
        kbs = [8 * n + 8 - s for s in range(nkb)]
        for hp in range(4):
            heads = (2 * hp, 2 * hp + 1)
            OB = (P[2], P[3])
            ZW = (P45, P67)
            cur_chunk = [None, None]
            cur_kt = [None, None]
            cur_vt = [None, None]
            nload = [0, 0]

            def get_kv(a, kb):
                h = heads[a]
                c = -1 if kb == 0 else (kb - 1) // 8
                if cur_chunk[a] != c:
                    i = nload[a] % 2
                    nload[a] += 1
                    kt_, vt_ = ktc[a][i], vtc[a][i]
                    if c < 0:
                        dma("sp", kt_[:, 0:128], kt_scr[h * 64:(h + 1) * 64, 0:128])
                        dma("sp", vt_[:, 0:1, :], v_scr[h, :, 0:1, :])
                    else:
                        dma("sp", kt_[:, :], kt_scr[h * 64:(h + 1) * 64, (8 * c + 1) * 128:(8 * c + 9) * 128])
                        dma("sp", vt_[:, :, :], v_scr[h, :, 8 * c + 1:8 * c + 9, :])
                    cur_chunk[a], cur_kt[a], cur_vt[a] = c, kt_, vt_
                lb = 0 if kb == 0 else (kb - 1) % 8
                return cur_kt[a][:, lb * 128:(lb + 1) * 128], cur_vt[a][:, lb, :]

            vsel = {}

            def front(s):
                kb = kbs[s]
                Z = ZW[s % 2]
                for a in range(2):
                    ktv, vtv = get_kv(a, kb)
                    vsel[(a, s)] = vtv
                    mm(Z[:, a * 512:(a + 1) * 512], ktv, qT[:, heads[a], :])
                e_ = erw[s % 2]
                act(e_[:], Z[:, :], AF.Exp, scale=0.125)
                e3 = e_[:].re("p (a n) -> p a n", a=2)
                if s < 8:
                    tt("dve", e3, e3, pmask[:, s, :].un(1).bc([128, 2, 512]), ALU.mult)
                elif kb == 0:
                    tt("dve", e3, e3, mask0[:].un(1).bc([128, 2, 512]), ALU.mult)
                act(sprw[s % 2][:], e_[:], AF.Ln, bias=onec[:])

            front(0)
            for s in range(nkb):
                sp_ = sprw[s % 2]
                for a in range(2):
                    mmacc(P01[:, a * 512:(a + 1) * 512], trige[:], sp_[:, a * 512:(a + 1) * 512], start=(s == 0))
                act(trw[:], P01[:, :], AF.Exp, scale=-1.0)
                if s + 1 < nkb:
                    front(s + 1)
                for a in range(2):
                    mmacc(P01[:, a * 512:(a + 1) * 512], trilt[:], sp_[:, a * 512:(a + 1) * 512], start=False)
                w_ = wrw[s % 2]
                tt("dve", w_[:], erw[s % 2][:], trw[:], ALU.mult)
                for a in range(2):
                    mmacc(OB[a][0:64, :], vsel[(a, s)], w_[:, a * 512:(a + 1) * 512], start=(s == 0))
            for a in range(2):
                cp("act" if a == 0 else "dve", attnT[:, heads[a], :], OB[a][0:64, :])
        if n == 0:
            dbg("attnT", attnT[:], [64, 8, 512], BF16)
        posA = (8 * n + 1) * 128
        posB = (8 * n + 5) * 128
        sv = ssm_scr[:].re("q p n -> p q n")
        dma("sp", ssA[:], sv[:, :, posA:posA + 512])
        dma("sp", ssB[:], sv[:, :, posB:posB + 512])
        ts("dve", ssO[:], ssA[:], sel[:, 2 * n:2 * n + 1], ALU.mult)
        stt(ssO[:], ssB[:], sel[:, 2 * n + 1:2 * n + 2], ssO[:], ALU.mult, ALU.add)
        for c2 in range(2):
            wga = load_wchunk(2048 + c2 * 512)
            wgb = load_wchunk(3072 + c2 * 512)
            for c4 in range(4):
                co = c2 * 4 + c4
                for kc in range(8):
                    mm(P[4][:, :], wga[:, kc, c4 * 128:(c4 + 1) * 128], xoT[:, kc, :], start=(kc == 0), stop=(kc == 7))
                for kc in range(8):
                    mm(P[5][:, :], wgb[:, kc, c4 * 128:(c4 + 1) * 128], xoT[:, kc, :], start=(kc == 0), stop=(kc == 7))
                for h in range(8):
                    mm(P[6][:, :], Wpa[:, h, co * 128:(co + 1) * 128], attnT[:, h, :], start=(h == 0), stop=(h == 7))
                for kc in range(4):
                    mm(P[7][:, :], Wpb[:, kc, co * 128:(co + 1) * 128], ssO[:, kc, :], start=(kc == 0), stop=(kc == 3))
                act(sg1[:], P[4][:, :], AF.Sigmoid)
                act(sg2[:], P[5][:, :], AF.Sigmoid)
                tt("dve", mg1[:], P[6][:, :], sg1[:], ALU.mult)
                tt("dve", mg2[:], P[7][:, :], sg2[:], ALU.mult)
                tt("pool", mergedT[:, co, :], mg1[:], mg2[:], ALU.add)
        if n == 0:
            dbg("mergedT", mergedT[:], [128, 8, 512], BF16)
        for r in range(4):
            ob = 4 * n + r
            xb_ = xo[r % 2]
            hb_ = hblk[r % 2]
            hn_ = hn[r % 2]
            dma("sp", xb_[:], x_own[ob * 128:(ob + 1) * 128, :])
            for half in range(2):
                for kc in range(8):
                    mm(P[half][:, :], mergedT[:, kc, r * 128:(r + 1) * 128], Wo[:, kc, half * 512:(half + 1) * 512],
                       start=(kc == 0), stop=(kc == 7))
                tt("dve", hb_[:, half * 512:(half + 1) * 512], P[half][:, :], xb_[:, half * 512:(half + 1) * 512],
                   ALU.add)
            dma("sp", h_scr[ob * 128:(ob + 1) * 128, :], hb_[:])
            rmsnorm2(hb_[:], grep_ffn, hn_[:])
            for half in range(2):
                for k in range(4):
                    kc = half * 4 + k
                    tr(P[6 + half][:, k * 128:(k + 1) * 128], hn_[:, kc * 128:(kc + 1) * 128], ident[:])
                cp("act" if half == 0 else "dve", hnT[:, half * 4:half * 4 + 4, :],
                   P[6 + half][:, :].re("p (k n) -> p k n", n=128))
            for kc in range(8):
                mm(P[2][:, 0:36], hnT[:, kc, :], Wr[:, kc, :], start=(kc == 0), stop=(kc == 7))
            tt("dve", lgt[:], P[2][:, 0:36], brrep[:], ALU.add)
            gmax, ngmax, gsum, ptop, dv, ex, g1, g2, p1loc, p2loc, ov1, ov2 = [t_[:] for t_ in r1]
            S.op("dve", lambda e: e.tensor_reduce(gmax.ap, lgt[:, 0:4].ap, AX.X, ALU.max),
                 reads=[lgt.buf], writes=[gmax.buf])
            ts("dve", ohg[:], lgt[:, 0:4], gmax, ALU.is_equal)
            ts("dve", ngmax, gmax, -1.0, ALU.mult)
            act(eg[:], lgt[:, 0:4], AF.Exp, bias=ngmax, accum=gsum)
            S.op("dve", lambda e: e.reciprocal(ptop.ap, gsum.ap), reads=[gsum.buf], writes=[ptop.buf])
            ts("dve", selv[:], lgt[:, 4:12], ohg[:, 0:1], ALU.mult)
            for g in range(1, 4):
                stt(selv[:], lgt[:, 4 + 8 * g:12 + 8 * g], ohg[:, g:g + 1], selv[:], ALU.mult, ALU.add)
            S.op("dve", lambda e: e.max(m8[:].ap, selv[:].ap), reads=[selv.buf], writes=[m8.buf])
            ts("dve", oh1[:], selv[:], m8[:, 0:1], ALU.is_equal)
            ts("dve", oh2[:], selv[:], m8[:, 1:2], ALU.is_equal)
            tt("dve", dv, m8[:, 1:2], m8[:, 0:1], ALU.subtract)
            act(ex, dv, AF.Exp)
            ts("dve", g1, ex, 1.0, ALU.add)
            S.op("dve", lambda e: e.reciprocal(g1.ap, g1.ap), reads=[g1.buf], writes=[g1.buf])
            tt("dve", g2, ex, g1, ALU.mult)
            tt("dve", gwall[:, ob, 0:1], g1, ptop, ALU.mult)
            tt("dve", gwall[:, ob, 1:2], g2, ptop, ALU.mult)
            tt("dve", M12[:, 0:32].re("p (g e) -> p g e", e=8), ohg[:].un(2).bc([128, 4, 8]),
               oh1[:].un(1).bc([128, 4, 8]), ALU.mult)
            tt("dve", M12[:, 32:64].re("p (g e) -> p g e", e=8), ohg[:].un(2).bc([128, 4, 8]),
               oh2[:].un(1).bc([128, 4, 8]), ALU.mult)
            mm(P[3][:, 0:64], triltf[:], M12[:])
            mm(P[3][:, 64:128], onesf[:], M12[:])
            tt("dve", pm[:, 0:32], P[3][:, 0:32], base[:], ALU.add)
            tt("dve", pm[:, 32:64], P[3][:, 32:64], base[:], ALU.add)
            tt("dve", pm[:, 32:64], P[3][:, 64:96], pm[:, 32:64], ALU.add)
            tt("dve", pmt[:], pm[:], M12[:], ALU.mult)
            S.op("dve", lambda e: e.tensor_reduce(p1loc.ap, pmt[:, 0:32].ap, AX.X, ALU.add),
                 reads=[pmt.buf], writes=[p1loc.buf])
            S.op("dve", lambda e: e.tensor_reduce(p2loc.ap, pmt[:, 32:64].ap, AX.X, ALU.add),
                 reads=[pmt.buf], writes=[p2loc.buf])
            tt("dve", pmt[:, 0:32], M12[:, 0:32], ecap[:], ALU.mult)
            tt("dve", pmt[:, 32:64], M12[:, 32:64], ecap[:], ALU.mult)
            S.op("dve", lambda e: e.tensor_reduce(ov1.ap, pmt[:, 0:32].ap, AX.X, ALU.add),
                 reads=[pmt.buf], writes=[ov1.buf])
            S.op("dve", lambda e: e.tensor_reduce(ov2.ap, pmt[:, 32:64].ap, AX.X, ALU.add),
                 reads=[pmt.buf], writes=[ov2.buf])
            tt("dve", destf[:, 0:1], ov1, p1loc, ALU.add)
            tt("dve", destf[:, 1:2], ov2, p2loc, ALU.add)
            ts("dve", ov1, p1loc, float(CAP) - 0.5, ALU.is_gt, float(4 * NXB), ALU.mult)
            ts("dve", ov2, p2loc, float(CAP) - 0.5, ALU.is_gt, float(4 * NXB), ALU.mult)
            tt("dve", destf[:, 0:1], destf[:, 0:1], ov1, ALU.add)
            tt("dve", destf[:, 1:2], destf[:, 1:2], ov2, ALU.add)
            cp("dve", destall[:, ob, :], destf[:])
            tt("dve", base[:], P[3][:, 64:96], base[:], ALU.add)
            tt("dve", base[:], P[3][:, 96:128], base[:], ALU.add)
            for k in range(2 if "noscat" not in debug else 0):
                dv_ = destall[:, ob, k:k + 1]
                S.dma("pool", (lambda dv_, hn_: (lambda e: e.indirect_dma_start(
                    out=xb_scr[:].ap, out_offset=bass.IndirectOffsetOnAxis(ap=dv_.ap, axis=0),
                    in_=hn_[:].ap, in_offset=None, bounds_check=bc_reg(e), oob_is_err=False)))(dv_, hn_),
                    reads=[hn_.buf, destall.buf], writes=[xb_scr.buf])
    dbg("destall", destall[:], [128, NOB, 2], I32)
    dbg("gwall", gwall[:], [128, NOB, 2])
    close_scope(scope2)
    if nphase < 3 or not moe:
        return finish()
    if nphase >= 3 and moe:
        scope3 = open_scope()
        W1 = [sb("W1_%d" % i, [128, 8, 512], BF16) for i in range(2)]
        W3 = [sb("W3_%d" % i, [128, 8, 512], BF16) for i in range(2)]
        W2 = [sb("W2_%d" % i, [128, 4, D], BF16) for i in range(2)]
        xg = [sb("xg%d" % i, [128, D]) for i in range(2)]
        xeT = sb("xeT", [128, 8, CAP], BF16)
        s1 = sb("s1", [128, CAP])
        hgT = sb("hgT", [128, 4, CAP], BF16)
        ybk = [sb("ybk%d" % i, [128, D]) for i in range(2)]
        for ex_ in range(32):
            w1_, w3_, w2_ = W1[ex_ % 2], W3[ex_ % 2], W2[ex_ % 2]
            for kc in range(8):
                dma("pool", w1_[:, kc, :], w1[ex_, kc * 128:(kc + 1) * 128, :])
                dma("pool", w3_[:, kc, :], w3[ex_, kc * 128:(kc + 1) * 128, :])
            for fc in range(4):
                dma("pool", w2_[:, fc, :], w2[ex_, fc * 128:(fc + 1) * 128, :])
            for c in range(CAPB):
                xg_ = xg[c % 2]
                dma("sp", xg_[:], xb_scr[ex_ * CAP + c * 128:ex_ * CAP + (c + 1) * 128, :])
                transpose8(xg_, xeT, c * 128, CAP)
            for fc in range(4):
                for kc in range(8):
                    mm(P[0][:, 0:CAP], w1_[:, kc, fc * 128:(fc + 1) * 128], xeT[:, kc, :], start=(kc == 0), stop=(kc == 7))
                for kc in range(8):
                    mm(P[1][:, 0:CAP], w3_[:, kc, fc * 128:(fc + 1) * 128], xeT[:, kc, :], start=(kc == 0), stop=(kc == 7))
                act(s1[:], P[0][:, 0:CAP], AF.Silu)
                tt("dve", hgT[:, fc, :], s1[:], P[1][:, 0:CAP], ALU.mult)
            for c in range(CAPB):
                yb_ = ybk[c % 2]
                for half in range(2):
                    for fc in range(4):
                        mm(P[2 + half][:, :], hgT[:, fc, c * 128:(c + 1) * 128], w2_[:, fc, half * 512:(half + 1) * 512],
                           start=(fc == 0), stop=(fc == 3))
                    cp("act" if half == 0 else "dve", yb_[:, half * 512:(half + 1) * 512], P[2 + half][:, :])
                dma("sp", yb_scr[ex_ * CAP + c * 128:ex_ * CAP + (c + 1) * 128, :], yb_[:])
        if nphase < 4:
            close_scope(scope3)
            return finish()
        yk = [sb("yk%d" % i, [128, D]) for i in range(2)]
        hb4 = [sb("hb4_%d" % i, [128, D]) for i in range(2)]
        oo = [sb("oo%d" % i, [128, D]) for i in range(2)]
        junk4 = sb("junk4", [128, D], BF16)
        ssq4 = sb("ssq4", [128, 1])
        rstd4 = sb("rstd4", [128, 1])
        for ob in range(NOB):
            hb_ = hb4[ob % 2]
            dma("sp", hb_[:], h_scr[ob * 128:(ob + 1) * 128, :])
            for k in range(2):
                yk_ = yk[k]
                S.op("pool", (lambda yk_: (lambda e: e.memset(yk_[:].ap, 0.0)))(yk_), writes=[yk_.buf])
                dv_ = destall[:, ob, k:k + 1]
                S.dma("pool", (lambda dv_, yk_: (lambda e: e.indirect_dma_start(
                    out=yk_[:].ap, out_offset=None, in_=yb_scr[:].ap,
                    in_offset=bass.IndirectOffsetOnAxis(ap=dv_.ap, axis=0),
                    bounds_check=bc_reg(e), oob_is_err=False)))(dv_, yk_),
                    reads=[yb_scr.buf, destall.buf], writes=[yk_.buf])
                stt(hb_[:], yk_[:], gwall[:, ob, k:k + 1], hb_[:], ALU.mult, ALU.add)
            oo_ = oo[ob % 2]
            act(junk4[:], hb_[:], AF.Square, accum=ssq4[:])
            act(rstd4[:], ssq4[:], AF.Ln, scale=1.0 / D, bias=epsc[:])
            act(rstd4[:], rstd4[:], AF.Exp, scale=-0.5)
            stt(oo_[:], hb_[:], rstd4[:], grep_fin[:], ALU.mult, ALU.mult)
            dma("sp", out_own[ob * 128:(ob + 1) * 128, :], oo_[:])
        close_scope(scope3)

    return finish()


def _consts(NSLOT, CAP):
    bf = ml_dtypes.bfloat16
    r = np.arange(128)[:, None]
    c = np.arange(128)[None, :]
    k = {}
    k["k_ident"] = (r == c).astype(np.float32)
    k["k_triltf"] = (r < c).astype(np.float32)
    k["k_onesf"] = np.ones((128, 128), np.float32)
    k["k_jrow"] = np.arange(128, dtype=np.float32)[None, :]
    mB = np.zeros((128, 2, 16), np.float32)
    for g2 in range(2):
        mB[g2 * 64:(g2 + 1) * 64, g2, :] = 1.0
    k["k_maskB"] = mB.reshape(128, 32)
    mC = np.zeros((128, 2, 64), np.float32)
    for rr in range(128):
        mC[rr, (rr // 16) % 2, :] = 1.0
    k["k_maskC"] = mC.reshape(128, 128)
    mQ = np.zeros((128, 2), np.float32)
    for rr in range(128):
        mQ[rr, (rr // 32) % 2] = 1.0
    k["k_maskQ"] = mQ
    mR = np.zeros((128, 2, 128), np.float32)
    for cc in range(128):
        mR[:, (cc // 32) % 2, cc] = 1.0
    k["k_maskR"] = mR.reshape(128, 256)
    k["k_ecap"] = np.tile((np.arange(32, dtype=np.float32) * CAP)[None, :], (128, 1))
    k["k_trige"] = (r >= c).astype(bf)
    k["k_trilt"] = (r < c).astype(bf)
    k["k_trile"] = (r <= c).astype(bf)
    m0 = np.ones((128, 512), np.float32)
    m0[:112, :] = 0.0
    k["k_mask0"] = m0.astype(bf)
    j = np.arange(128)[:, None]
    t = np.arange(512)[None, :]
    patA = np.zeros((128, 8, 512), np.float32)
    patB = np.zeros((128, 8, 512), np.float32)
    for s in range(8):
        if s < 4:
            rr = 3 - s
            patB[:, s, :] = (rr * 128 + j < t)
        else:
            rr = 7 - s
            patA[:, s, :] = (rr * 128 + j < t)
            patB[:, s, :] = 1.0
    return k, patA.astype(bf), patB.astype(bf)


def own_is_A(n, hf):
    return ((n + hf) % 2) == 0


def make_in_maps(inputs, NSLOT, CAP, n_cores=8, moe=True):
    x = np.asarray(inputs["x"], np.float32)
    NB = 1 + 8 * NSLOT
    NPOS = NB * 128
    meta = np.asarray(inputs["meta_tokens"], np.float32)
    k, patA, patB = _consts(NSLOT, CAP)
    f = lambda a: np.ascontiguousarray(np.asarray(a, np.float32))
    shared = dict(
        w_in=f(inputs["w_in"][0]), g_mix=f(inputs["norm_mix_g"][0:1]), g_ffn=f(inputs["norm_ffn_g"][0:1]),
        g_fin=f(inputs["norm_final_g"]).reshape(1, D),
        lam_re=f(inputs["ssm_lambda_re"][0]).reshape(1, 2048), lam_im=f(inputs["ssm_lambda_im"][0]).reshape(1, 2048),
        log_dt=f(inputs["ssm_log_dt"][0]).reshape(1, 32),
        b_re=f(inputs["ssm_b_re"][0]), b_im=f(inputs["ssm_b_im"][0]),
        c_re=f(inputs["ssm_c_re"][0]).reshape(512, 64), c_im=f(inputs["ssm_c_im"][0]).reshape(512, 64),
        d_skip=f(inputs["ssm_d"][0]).reshape(1, 512), glu_w=f(inputs["ssm_glu_w"][0]),
        glu_b=f(inputs["ssm_glu_b"][0]).reshape(1, 512),
        w_pa=f(inputs["w_branch_attn"][0]), w_pb=f(inputs["w_branch_ssm"][0]), w_o=f(inputs["w_out"][0]),
        w_r=f(np.concatenate([inputs["router_group_w"][0], inputs["router_expert_w"][0]], axis=1)),
        b_r=f(np.concatenate([inputs["router_group_b"][0], inputs["router_expert_b"][0]], axis=0)).reshape(1, 36),
        w1=f(inputs["expert_w1"][0][:(32 if moe else 1)]), w3=f(inputs["expert_w3"][0][:(32 if moe else 1)]),
        w2=f(inputs["expert_w2"][0][:(32 if moe else 1)]),
    )
    shared.update(k)
    maps = []
    for core in range(n_cores):
        b, hf = core // 2, core % 2
        xa = np.zeros((NPOS, D), np.float32)
        xa[112:128] = meta
        xa[128:] = x[b, :NPOS - 128]
        xo = np.zeros((512 * NSLOT, D), np.float32)
        sel = np.zeros((128, 2 * NSLOT), np.float32)
        for n in range(NSLOT):
            a = own_is_A(n, hf)
            r0 = (8 * n + (0 if a else 4)) * 128
            xo[n * 512:(n + 1) * 512] = x[b, r0:r0 + 512]
            sel[:, 2 * n] = 1.0 if a else 0.0
            sel[:, 2 * n + 1] = 0.0 if a else 1.0
        m = dict(shared)
        m["x_all"] = xa
        m["x_own"] = xo
        m["k_sel"] = sel
        m["k_maske"] = patA if own_is_A(0, hf) else patB
        m["k_masko"] = patA if own_is_A(1, hf) else patB
        maps.append(m)
    return maps


def assemble(results, NSLOT, B, SEQ):
    out = np.zeros((B, SEQ, D), np.float32)
    for core, r in enumerate(results):
        b, hf = core // 2, core % 2
        oo = np.asarray(r["out_own"], np.float32)
        for n in range(NSLOT):
            a = own_is_A(n, hf)
            r0 = (8 * n + (0 if a else 4)) * 128
            out[b, r0:r0 + 512] = oo[n * 512:(n + 1) * 512]
    return out


_NC_CACHE = {}


def kernel(**inputs):
    NSLOT, CAPB = 8, 3
    key = (NSLOT, CAPB)
    if key not in _NC_CACHE:
        _NC_CACHE[key] = build(NSLOT, CAPB)
    nc = _NC_CACHE[key]
    maps = make_in_maps(inputs, NSLOT, 128 * CAPB)
    res = run_bass_kernel_spmd(nc, maps, core_ids=list(range(8)))
    return assemble(res.results, NSLOT, 4, 8192)
```

```python
import math
from contextlib import ExitStack
import numpy as np
import ml_dtypes
import concourse.bass as bass
import concourse.mybir as mybir
from concourse.bass_utils import run_bass_kernel_spmd

F32 = mybir.dt.float32
BF16 = mybir.dt.bfloat16
I32 = mybir.dt.int32
AF = mybir.ActivationFunctionType
ALU = mybir.AluOpType
AX = mybir.AxisListType

SAME_ENGINE_SYNC = True
NDS = 20
D = 1024
TWO_PI = 2.0 * math.pi


class Buf:
    def __init__(self, name):
        self.name = name
        self.w = None
        self.r = {}


class View:
    def __init__(self, ap, buf):
        self.ap = ap
        self.buf = buf

    def __getitem__(self, k):
        return View(self.ap[k], self.buf)

    def re(self, pat, **kw):
        return View(self.ap.rearrange(pat, **kw), self.buf)

    def bc(self, shape):
        return View(self.ap.broadcast_to(list(shape)), self.buf)

    def un(self, axis):
        return View(self.ap.unsqueeze(axis), self.buf)


class T:
    def __init__(self, h, name):
        self.h = h
        self.buf = Buf(name)

    def __getitem__(self, k):
        return View(self.h[k], self.buf)


class Sched:
    ENG = ["pe", "act", "dve", "pool", "sp"]

    def __init__(self, nc, stack):
        self.nc = nc
        self.stack = stack
        self.epoch = {e: 0 for e in self.ENG}
        self.last_tok = {e: None for e in self.ENG}
        self.sem = {e: stack.enter_context(nc.semaphore("s_" + e)) for e in self.ENG}
        self.cnt = {e: 0 for e in self.ENG}
        self.ops = {e: [] for e in self.ENG}
        self.waited = {e: {} for e in self.ENG}
        self.dsem = {q: [stack.enter_context(nc.semaphore("d_%s%d" % (q, i))) for i in range(NDS)]
                     for q in ("sp", "pool")}
        self.dcnt = {q: [0] * NDS for q in ("sp", "pool")}
        self.drr = {"sp": 0, "pool": 0}

    @staticmethod
    def _flat(bufs):
        out = []
        for b in bufs:
            if isinstance(b, (tuple, list)):
                out.extend(b)
            else:
                out.append(b)
        return out

    def _deps(self, reads, writes):
        reads = self._flat(reads)
        writes = self._flat(writes)
        deps = []
        for b in reads:
            if b.w is not None:
                deps.append(b.w)
        for b in writes:
            if b.w is not None:
                deps.append(b.w)
            deps.extend(b.r.values())
        return deps

    def _waits(self, e, deps):
        need = {}
        for (key, sem, val) in deps:
            if val <= 0:
                continue
            if key[0] == "E" and key[1] == e and (e == "pe" or not SAME_ENGINE_SYNC):
                continue
            if self.waited[e].get(key, 0) >= val:
                continue
            if key not in need or need[key][1] < val:
                need[key] = (sem, val)
        for key, (sem, val) in need.items():
            self.waited[e][key] = val
        return list(need.values())

    def _record(self, tok, reads, writes):
        reads = self._flat(reads)
        writes = self._flat(writes)
        for b in reads:
            old = b.r.get(tok[0])
            if old is None or old[2] < tok[2]:
                b.r[tok[0]] = tok
        for b in writes:
            b.w = tok
            b.r = {}

    def op(self, e, fn, reads=(), writes=()):
        waits = self._waits(e, self._deps(reads, writes))
        if self.cnt[e] >= 16000:
            self.epoch[e] += 1
            self.sem[e] = self.stack.enter_context(self.nc.semaphore("s_%s_%d" % (e, self.epoch[e])))
            self.cnt[e] = 0
        self.cnt[e] += 1
        tok = (("E", e, self.epoch[e]), self.sem[e], self.cnt[e])
        self.last_tok[e] = tok
        self.ops[e].append((waits, fn, self.sem[e], 1))
        self._record(tok, reads, writes)

    def dma(self, q, fn, reads=(), writes=()):
        deps = self._deps(reads, writes)
        i = self.drr[q]
        self.drr[q] = (i + 1) % NDS
        sem = self.dsem[q][i]
        key = ("D", q, i)
        prev = self.dcnt[q][i]
        deps.append((key, sem, prev))
        waits = self._waits(q, deps)
        self.dcnt[q][i] = prev + 16
        tok = (key, sem, prev + 16)
        self.ops[q].append((waits, fn, sem, 16))
        self._record(tok, reads, writes)

    def barrier(self):
        toks = [self.last_tok[e] for e in self.ENG if self.last_tok[e] is not None]
        for q in ("sp", "pool"):
            for i in range(NDS):
                if self.dcnt[q][i] > 0:
                    toks.append((("D", q, i), self.dsem[q][i], self.dcnt[q][i]))
        for e in self.ENG:
            waits = self._waits(e, toks)
            if waits:
                self.ops[e].append((waits, None, None, 0))

    def replay(self, e, eng):
        for waits, fn, sem, inc in self.ops[e]:
            for (s, v) in waits:
                eng.wait_ge(s, v)
            if fn is not None:
                fn(eng).then_inc(sem, inc)

    def final_waits(self, e, eng, bufs):
        deps = []
        for b in self._flat(bufs):
            if b.w is not None:
                deps.append(b.w)
        for (s, v) in self._waits(e, deps):
            eng.wait_ge(s, v)


def build(NSLOT, CAPB, debug=(), moe=True, nphase=4):
    NB = 1 + 8 * NSLOT
    NPOS = NB * 128
    TOWN = 512 * NSLOT
    NOB = 4 * NSLOT
    CAP = 128 * CAPB
    NXB = 32 * CAP
    nc = bass.Bass("TRN2", target_bir_lowering=False)
    stack = ExitStack()
    S = Sched(nc, stack)

    def din(name, shape, dt=F32):
        return T(nc.dram_tensor(name, list(shape), dt, kind="ExternalInput").ap(), name)

    def dscr(name, shape, dt=F32):
        kind = "ExternalOutput" if "scr" in debug else "Internal"
        return T(nc.dram_tensor(name, list(shape), dt, kind=kind).ap(), name)

    def dout(name, shape, dt=F32):
        return T(nc.dram_tensor(name, list(shape), dt, kind="ExternalOutput").ap(), name)

    cur = [stack]

    def sb(name, shape, dt=F32):
        return T(cur[0].enter_context(nc.sbuf_tensor(name, list(shape), dt)), name)

    def open_scope():
        st = ExitStack()
        cur.insert(0, st)
        return st

    def close_scope(st):
        assert cur[0] is st
        S.barrier()
        st.close()
        cur.pop(0)

    x_all = din("x_all", [NPOS, D])
    x_own = din("x_own", [TOWN, D])
    w_in = din("w_in", [D, 4096])
    g_mix = din("g_mix", [1, D])
    g_ffn = din("g_ffn", [1, D])
    g_fin = din("g_fin", [1, D])
    lam_re = din("lam_re", [1, 2048])
    lam_im = din("lam_im", [1, 2048])
    log_dt = din("log_dt", [1, 32])
    b_re = din("b_re", [32, 64, 16])
    b_im = din("b_im", [32, 64, 16])
    c_re = din("c_re", [512, 64])
    c_im = din("c_im", [512, 64])
    d_skip = din("d_skip", [1, 512])
    glu_w = din("glu_w", [512, 512])
    glu_b = din("glu_b", [1, 512])
    w_pa = din("w_pa", [512, D])
    w_pb = din("w_pb", [512, D])
    w_o = din("w_o", [D, D])
    w_r = din("w_r", [D, 36])
    b_r = din("b_r", [1, 36])
    NE = 32 if moe else 1
    w1 = din("w1", [NE, D, 512])
    w3 = din("w3", [NE, D, 512])
    w2 = din("w2", [NE, 512, D])
    k_ident = din("k_ident", [128, 128])
    k_triltf = din("k_triltf", [128, 128])
    k_onesf = din("k_onesf", [128, 128])
    k_jrow = din("k_jrow", [1, 128])
    k_maskB = din("k_maskB", [128, 32])
    k_maskC = din("k_maskC", [128, 128])
    k_ecap = din("k_ecap", [128, 32])
    k_maskQ = din("k_maskQ", [128, 2])
    k_maskR = din("k_maskR", [128, 256])
    k_trige = din("k_trige", [128, 128], BF16)
    k_trilt = din("k_trilt", [128, 128], BF16)
    k_trile = din("k_trile", [128, 128], BF16)
    k_mask0 = din("k_mask0", [128, 512], BF16)
    k_maske = din("k_maske", [128, 8, 512], BF16)
    k_masko = din("k_masko", [128, 8, 512], BF16)
    k_sel = din("k_sel", [128, 2 * NSLOT])

    kt_scr = dscr("kt_scr", [512, NPOS], BF16)
    v_scr = dscr("v_scr", [8, 128, NB, 64], BF16)
    ssm_scr = dscr("ssm_scr", [4, 128, NPOS], BF16)
    h_scr = dscr("h_scr", [TOWN, D])
    xb_scr = dscr("xb_scr", [NXB, D])
    yb_scr = dscr("yb_scr", [NXB, D])
    out_own = dout("out_own", [TOWN, D])

    dbg_outs = {}

    def dbg(name, view, shape, dt=F32):
        if name not in debug:
            return
        t = dout("dbg_" + name, shape, dt)
        dbg_outs[name] = t
        dma("sp", t[:], view)

    bcreg = {}

    def bc_reg(e):
        if "r" not in bcreg:
            bcreg["r"] = e.to_reg(NXB - 1)
        return bcreg["r"]

    def mm(out, lhsT, rhs, start=True, stop=True):
        S.op("pe", lambda e: e.matmul(out.ap, lhsT.ap, rhs.ap, start=start, stop=stop),
             reads=[lhsT.buf, rhs.buf], writes=[out.buf])

    def tr(out, in_, ident_v):
        S.op("pe", lambda e: e.transpose(out.ap, in_.ap, ident_v.ap),
             reads=[in_.buf, ident_v.buf], writes=[out.buf])

    def act(out, in_, func, bias=None, scale=None, accum=None, extra_reads=()):
        kw = {}
        reads = [in_.buf] + list(extra_reads)
        writes = [out.buf]
        if bias is not None:
            if isinstance(bias, View):
                kw["bias"] = bias.ap
                reads.append(bias.buf)
            else:
                kw["bias"] = bias
        if scale is not None:
            if isinstance(scale, View):
                kw["scale"] = scale.ap
                reads.append(scale.buf)
            else:
                kw["scale"] = scale
        if accum is not None:
            kw["accum_out"] = accum.ap
            writes.append(accum.buf)
        S.op("act", lambda e: e.activation(out.ap, in_.ap, func, **kw), reads=reads, writes=writes)

    def tt(eng, out, in0, in1, op):
        S.op(eng, lambda e: e.tensor_tensor(out.ap, in0.ap, in1.ap, op),
             reads=[in0.buf, in1.buf], writes=[out.buf])

    def _sc(x, reads):
        if isinstance(x, View):
            reads.append(x.buf)
            return x.ap
        return x

    def ts(eng, out, in0, s1, op0, s2=None, op1=None):
        reads = [in0.buf]
        a1 = _sc(s1, reads)
        a2 = _sc(s2, reads)
        if op1 is None:
            S.op(eng, lambda e: e.tensor_scalar(out.ap, in0.ap, a1, None, op0), reads=reads, writes=[out.buf])
        else:
            S.op(eng, lambda e: e.tensor_scalar(out.ap, in0.ap, a1, a2, op0, op1), reads=reads,
                 writes=[out.buf])

    def stt(out, in0, scalar, in1, op0, op1):
        reads = [in0.buf, in1.buf]
        a = _sc(scalar, reads)
        S.op("dve", lambda e: e.scalar_tensor_tensor(out.ap, in0.ap, a, in1.ap, op0, op1),
             reads=reads, writes=[out.buf])

    def cp(eng, out, in_):
        if eng == "act":
            S.op("act", lambda e: e.copy(out.ap, in_.ap), reads=[in_.buf], writes=[out.buf])
        else:
            S.op(eng, lambda e: e.tensor_copy(out.ap, in_.ap), reads=[in_.buf], writes=[out.buf])

    def dma(q, out, in_, **kw):
        S.dma(q, lambda e: e.dma_start(out=out.ap, in_=in_.ap, **kw), reads=[in_.buf], writes=[out.buf])

    Pall_h = stack.enter_context(nc.psum_tensor("psall", [128, 4096], F32))

    class PB:
        def __init__(self, i0, nb):
            self.i0, self.nb = i0, nb
            self.buf = tuple(pbufs[i0:i0 + nb]) if nb > 1 else pbufs[i0]

        def __getitem__(self, k):
            return View(Pall_h[:, self.i0 * 512:(self.i0 + self.nb) * 512][k], self.buf)

    pbufs = [Buf("ps%d" % i) for i in range(8)]
    P = [PB(i, 1) for i in range(8)]
    PQ = PB(2, 4)
    P01 = PB(0, 2)
    P45 = PB(4, 2)
    P67 = PB(6, 2)

    def finish():
        final_bufs = [out_own.buf, kt_scr.buf, v_scr.buf, ssm_scr.buf, h_scr.buf, xb_scr.buf, yb_scr.buf] + [t.buf for t in dbg_outs.values()]
        with nc.Block() as block:
            @block.tensor
            def _(e):
                S.replay("pe", e)

            @block.scalar
            def _(e):
                S.replay("act", e)

            @block.vector
            def _(e):
                S.replay("dve", e)

            @block.gpsimd
            def _(e):
                S.replay("pool", e)

            @block.sync
            def _(e):
                S.replay("sp", e)
                S.final_waits("sp", e, final_bufs)
        stack.close()
        return nc

    ident = sb("ident", [128, 128])
    triltf = sb("triltf", [128, 128])
    onesf = sb("onesf", [128, 128])
    jrow = sb("jrow", [1, 128])
    maskB = sb("maskB", [128, 32])
    maskC = sb("maskC", [128, 128])
    ecap = sb("ecap", [128, 32])
    maskQ = sb("maskQ", [128, 2])
    maskR = sb("maskR", [128, 256])
    trige = sb("trige", [128, 128], BF16)
    trilt = sb("trilt", [128, 128], BF16)
    trile = sb("trile", [128, 128], BF16)
    sel = sb("sel", [128, 2 * NSLOT])
    epsc = sb("epsc", [128, 1])
    onec = sb("onec", [128, 1])
    grep_mix = sb("grep_mix", [128, D])
    grep_ffn = sb("grep_ffn", [128, D])
    grep_fin = sb("grep_fin", [128, D])
    brrep = sb("brrep", [128, 36])
    destall = sb("destall", [128, NOB, 2], I32)
    gwall = sb("gwall", [128, NOB, 2])
    scopeA = open_scope()
    for dst, src in ((ident, k_ident), (triltf, k_triltf), (onesf, k_onesf), (jrow, k_jrow), (maskB, k_maskB),
                     (maskC, k_maskC), (ecap, k_ecap), (maskQ, k_maskQ), (maskR, k_maskR), (trige, k_trige), (trilt, k_trilt), (trile, k_trile),
                     (sel, k_sel)):
        dma("sp", dst[:], src[:])

    dcol = sb("dcol", [128, 4])
    gbcol = sb("gbcol", [128, 4])
    T1re = sb("T1re", [128, 2048])
    T1im = sb("T1im", [128, 2048])
    T2re = sb("T2re", [128, 2048])
    T2im = sb("T2im", [128, 2048])
    are = sb("are", [128, 16])
    aim = sb("aim", [128, 16])
    Bp = sb("Bp", [128, 4, 2, 128])
    Cp = sb("Cp", [128, 4, 2, 128])
    Bq = sb("Bq", [128, 4, 2, 2, 128], BF16)
    Cq = sb("Cq", [128, 4, 2, 2, 128], BF16)
    s16 = [sb("s16_%d" % i, [128, 16]) for i in range(6)]
    scope0 = open_scope()
    rowtmp = sb("rowtmp", [1, 2048])

    def replicate_row(dst, src_dram, n):
        dma("sp", rowtmp[0:1, 0:n], src_dram[0:1, 0:n])
        for c0 in range(0, n, 512):
            c1 = min(n, c0 + 512)
            mm(P[0][:, 0:c1 - c0], onesf[0:1, :], rowtmp[0:1, c0:c1])
            cp("dve", dst[:, c0:c1], P[0][:, 0:c1 - c0])

    replicate_row(grep_mix, g_mix, D)
    replicate_row(grep_ffn, g_ffn, D)
    replicate_row(grep_fin, g_fin, D)
    replicate_row(brrep, b_r, 36)
    S.op("dve", lambda e: e.memset(onec[:].ap, 1.0), writes=[onec.buf])

    def row_to_col(dst, src_dram, nchunk):
        dma("sp", rowtmp[0:1, 0:nchunk * 128], src_dram[0:1, 0:nchunk * 128])
        for q in range(nchunk):
            mm(P[0][:, 2 * q:2 * q + 2], rowtmp[0:1, q * 128:(q + 1) * 128], onesf[0:1, 0:2])
        cp("dve", dst[:, 0:nchunk], P[0][:, 0:2 * nchunk].re("p (q t) -> p q t", t=2)[:, :, 0])

    row_to_col(dcol, d_skip, 4)
    row_to_col(gbcol, glu_b, 4)

    lrrow = sb("lrrow", [1, 2048])
    lirow = sb("lirow", [1, 2048])
    dtrow = sb("dtrow", [1, 32])
    rhorow = sb("rhorow", [1, 2048])
    throw = sb("throw", [1, 2048])
    dma("sp", lrrow[:], lam_re[:])
    dma("sp", lirow[:], lam_im[:])
    dma("sp", dtrow[:], log_dt[:])
    act(dtrow[:], dtrow[:], AF.Exp)
    dtb = dtrow[:].un(2).bc([1, 32, 64])
    tt("dve", rhorow[:].re("o (g p) -> o g p", p=64), lrrow[:].re("o (g p) -> o g p", p=64), dtb, ALU.mult)
    tt("dve", throw[:].re("o (g p) -> o g p", p=64), lirow[:].re("o (g p) -> o g p", p=64), dtb, ALU.mult)

    wkA = sb("wkA", [128, 2048])
    wkB = sb("wkB", [128, 2048])
    wkI = sb("wkI", [128, 2048], I32)
    wkM = sb("wkM", [128, 2048])

    def outer_tok(row):
        for k in range(4):
            mm(P[k][:, :], jrow[0:1, :], row[0:1, k * 512:(k + 1) * 512])

    def outer_feat(row):
        for gp in range(16):
            mm(P[gp // 4][:, (gp % 4) * 128:(gp % 4 + 1) * 128], row[0:1, gp * 128:(gp + 1) * 128], jrow[0:1, :])

    def sin_of(dst, shift):
        for k in range(4):
            ts("dve", wkA[:, k * 512:(k + 1) * 512], P[k][:, :], 1.0 / TWO_PI, ALU.mult, 0.5 + shift, ALU.add)
        cp("dve", wkI[:], wkA[:])
        cp("dve", wkB[:], wkI[:])
        for k in range(4):
            stt(wkA[:, k * 512:(k + 1) * 512], wkB[:, k * 512:(k + 1) * 512], -TWO_PI, P[k][:, :],
                ALU.mult, ALU.add)
        if shift != 0.0:
            ts("dve", wkA[:], wkA[:], shift * TWO_PI, ALU.add)
        ts("dve", wkB[:], wkA[:], math.pi, ALU.is_gt)
        stt(wkA[:], wkB[:], -TWO_PI, wkA[:], ALU.mult, ALU.add)
        ts("dve", wkB[:], wkA[:], -math.pi, ALU.is_lt)
        stt(wkA[:], wkB[:], TWO_PI, wkA[:], ALU.mult, ALU.add)
        ts("dve", wkA[:], wkA[:], 3.14159, ALU.min, -3.14159, ALU.max)
        act(dst, wkA[:], AF.Sin)

    def build_table(Tre, Tim, outer_fn, sign):
        outer_fn(rhorow)
        for k in range(4):
            act(wkM[:, k * 512:(k + 1) * 512], P[k][:, :], AF.Exp, scale=float(sign))
        outer_fn(throw)
        sin_of(Tim[:], 0.0)
        sin_of(Tre[:], 0.25)
        tt("dve", Tre[:], Tre[:], wkM[:], ALU.mult)
        stt(Tim[:], Tim[:], float(sign), wkM[:], ALU.mult, ALU.mult)

    build_table(T1re, T1im, outer_tok, -1)
    build_table(T2re, T2im, outer_feat, +1)

    lrc = sb("lrc", [128, 16])
    lic = sb("lic", [128, 16])
    for dst, row in ((lrc, lrrow), (lic, lirow)):
        for gp in range(16):
            mm(P[4][:, 2 * gp:2 * gp + 2], row[0:1, gp * 128:(gp + 1) * 128], onesf[0:1, 0:2])
        cp("dve", dst[:], P[4][:, 0:32].re("p (q t) -> p q t", t=2)[:, :, 0])
    T2re3 = T2re[:].re("p (g j) -> p g j", j=128)
    T2im3 = T2im[:].re("p (g j) -> p g j", j=128)
    cp("dve", are[:], T2re3[:, :, 1])
    cp("dve", aim[:], T2im3[:, :, 1])
    fre = sb("fre", [128, 16])
    fim = sb("fim", [128, 16])
    nr, den, t0, t1, t2, t3 = [s[:] for s in s16]
    ts("dve", nr, are[:], -1.0, ALU.add)
    tt("dve", t0, lrc[:], lrc[:], ALU.mult)
    tt("dve", t1, lic[:], lic[:], ALU.mult)
    tt("dve", den, t0, t1, ALU.add)
    S.op("dve", lambda e: e.reciprocal(den.ap, den.ap), reads=[den.buf], writes=[den.buf])
    tt("dve", t0, nr, lrc[:], ALU.mult)
    tt("dve", t1, aim[:], lic[:], ALU.mult)
    tt("dve", t2, t0, t1, ALU.add)
    tt("dve", fre[:], t2, den, ALU.mult)
    tt("dve", t0, aim[:], lrc[:], ALU.mult)
    tt("dve", t1, nr, lic[:], ALU.mult)
    tt("dve", t2, t0, t1, ALU.subtract)
    tt("dve", fim[:], t2, den, ALU.mult)

    Bsr = sb("Bsr", [128, 16, 16])
    Bsi = sb("Bsi", [128, 16, 16])
    for dst, src in ((Bsr, b_re), (Bsi, b_im)):
        v = src[:].re("(gp g2) p c -> g2 p gp c", g2=2)
        for g2 in range(2):
            dma("sp", dst[g2 * 64:(g2 + 1) * 64, :, :], v[g2])
    Bbr = sb("Bbr", [128, 16, 16])
    Bbi = sb("Bbi", [128, 16, 16])
    tB0 = sb("tB0", [128, 16, 16])
    tB1 = sb("tB1", [128, 16, 16])
    freb = fre[:].un(2).bc([128, 16, 16])
    fimb = fim[:].un(2).bc([128, 16, 16])
    tt("dve", tB0[:], Bsr[:], freb, ALU.mult)
    tt("dve", tB1[:], Bsi[:], fimb, ALU.mult)
    tt("dve", Bbr[:], tB0[:], tB1[:], ALU.subtract)
    tt("dve", tB0[:], Bsi[:], freb, ALU.mult)
    tt("dve", tB1[:], Bsr[:], fimb, ALU.mult)
    tt("dve", Bbi[:], tB0[:], tB1[:], ALU.add)
    dbg("Bbr", Bbr[:], [128, 16, 16])
    Y2 = sb("Y2", [128, 2, 16, 2, 16])
    mBb = maskB[:].re("p (a c) -> p a c", a=2).un(1).bc([128, 16, 2, 16])
    for pl, Bb in ((0, Bbr), (1, Bbi)):
        tt("dve", Y2[:, pl], Bb[:].un(2).bc([128, 16, 2, 16]), mBb, ALU.mult)
    for pl in range(2):
        for Q in range(4):
            tr(P[5][:, Q * 128:(Q + 1) * 128], Y2[:, pl, 4 * Q:4 * Q + 4].re("p a b c -> p (a b c)"), ident[:])
        cp("dve", Bp[:, :, pl, :], P[5][:, :].re("p (q n) -> p q n", n=128))
        for lh in range(2):
            ts("dve", Bq[:, :, lh, pl, :], Bp[:, :, pl, :], maskQ[:, lh:lh + 1], ALU.mult)
    Csr = sb("Csr", [128, 4, 64])
    Csi = sb("Csi", [128, 4, 64])
    dma("sp", Csr[:], c_re[:].re("(q r) p -> r q p", r=128))
    dma("sp", Csi[:], c_im[:].re("(q r) p -> r q p", r=128))
    Xc = sb("Xc", [128, 2, 4, 2, 64])
    mCb = maskC[:].re("r (a p) -> r a p", a=2).un(1).bc([128, 4, 2, 64])
    tt("dve", Xc[:, 0], Csr[:].un(2).bc([128, 4, 2, 64]), mCb, ALU.mult)
    for Q in range(4):
        stt(Xc[:, 1, Q], Csi[:, Q].un(1).bc([128, 2, 64]), -1.0, maskC[:].re("r (a p) -> r a p", a=2),
            ALU.mult, ALU.mult)
    for pl in range(2):
        for Q in range(4):
            tr(P[5][:, Q * 128:(Q + 1) * 128], Xc[:, pl, Q].re("r a p -> r (a p)"), ident[:])
        cp("dve", Cp[:, :, pl, :], P[5][:, :].re("p (q n) -> p q n", n=128))
        for lh in range(2):
            tt("dve", Cq[:, :, pl, lh, :], Cp[:, :, pl, :],
               maskR[:, lh * 128:(lh + 1) * 128].un(1).bc([128, 4, 128]), ALU.mult)

    close_scope(scope0)
    Wkvu = sb("Wkvu", [128, 8, 1536], BF16)
    for kc in range(8):
        dma("pool", Wkvu[:, kc, :], w_in[kc * 128:(kc + 1) * 128, 512:2048])
    Wg = sb("Wg", [128, 4, 512], BF16)
    for kc in range(4):
        dma("pool", Wg[:, kc, :], glu_w[kc * 128:(kc + 1) * 128, :])

    xblk = [sb("xblk%d" % i, [128, D]) for i in range(2)]
    junk = sb("junk", [128, D])
    ssq = sb("ssq", [128, 1])
    rstd = sb("rstd", [128, 1])
    xn = sb("xn", [128, D])
    xnT = sb("xnT", [128, 8, 128], BF16)
    ktb = sb("ktb", [128, 4, 128], BF16)
    vb = sb("vb", [128, 512], BF16)
    uT = sb("uT", [128, 4, 128], BF16)
    zt = sb("zt", [128, 16, 2, 128], BF16)
    tz = [sb("tz%d" % i, [128, 8, 128]) for i in range(4)]
    cg = sb("cg", [128, 8, 2, 128])
    xT = sb("xT", [128, 16, 2, 128], BF16)
    Gre = sb("Gre", [128, 16])
    Gim = sb("Gim", [128, 16])
    xlast = sb("xlast", [128, 16, 2])
    yv = sb("yv", [128, 4, 128])
    y2 = sb("y2", [128, 4, 128])
    y3 = sb("y3", [128, 4, 128])
    yg = sb("yg", [128, 4, 128])
    ygb = sb("ygb", [128, 4, 128], BF16)
    sgl = sb("sgl", [128, 4, 128])
    ssmT = sb("ssmT", [128, 4, 128], BF16)
    S.op("dve", lambda e: e.memset(Gre[:].ap, 0.0), writes=[Gre.buf])
    S.op("dve", lambda e: e.memset(Gim[:].ap, 0.0), writes=[Gim.buf])
    T1re3 = T1re[:].re("p (g n) -> p g n", n=128)
    T1im3 = T1im[:].re("p (g n) -> p g n", n=128)

    def rmsnorm(xv, grep, outv):
        act(junk[:], xv, AF.Square, accum=ssq[:])
        act(rstd[:], ssq[:], AF.Ln, scale=1.0 / D, bias=epsc[:])
        act(rstd[:], rstd[:], AF.Exp, scale=-0.5)
        stt(outv, xv, rstd[:], grep[:], ALU.mult, ALU.mult)

    S.op("dve", lambda e: e.memset(epsc[:].ap, 1e-6), writes=[epsc.buf])

    def transpose8(src, dstT, col0, ncol_total):
        for half in range(2):
            for k in range(4):
                kc = half * 4 + k
                tr(P[6 + half][:, k * 128:(k + 1) * 128], src[:, kc * 128:(kc + 1) * 128], ident[:])
            cp("act" if half == 0 else "dve", dstT[:, half * 4:half * 4 + 4, col0:col0 + 128],
               P[6 + half][:, :].re("p (k n) -> p k n", n=128))

    for blk in range(NB):
        xb_ = xblk[blk % 2]
        dma("sp", xb_[:], x_all[blk * 128:(blk + 1) * 128, :])
        rmsnorm(xb_[:], grep_mix, xn[:])
        transpose8(xn, xnT, 0, 128)
        for m in range(4):
            for kc in range(8):
                mm(P[0][:, m * 128:(m + 1) * 128], Wkvu[:, kc, m * 128:(m + 1) * 128], xnT[:, kc, :],
                   start=(kc == 0), stop=(kc == 7))
        cp("act", ktb[:], P[0][:, :].re("p (m n) -> p m n", n=128))
        dma("sp", kt_scr[:].re("(m q) n -> q m n", q=128)[:, :, blk * 128:(blk + 1) * 128], ktb[:])
        for kc in range(8):
            mm(P[2][:, :], xnT[:, kc, :], Wkvu[:, kc, 512:1024], start=(kc == 0), stop=(kc == 7))
        cp("act", vb[:], P[2][:, :])
        dma("sp", v_scr[:].re("h j b d -> j h b d")[:, :, blk, :], vb[:].re("j (h d) -> j h d", d=64))
        for m in range(4):
            for kc in range(8):
                mm(P[1][:, m * 128:(m + 1) * 128], Wkvu[:, kc, 1024 + m * 128:1024 + (m + 1) * 128],
                   xnT[:, kc, :], start=(kc == 0), stop=(kc == 7))
        cp("act", uT[:], P[1][:, :].re("p (m n) -> p m n", n=128))
        if blk == 1:
            dbg("uT", uT[:], [128, 4, 128], BF16)
        for hf in range(2):
            for b4 in range(4):
                Q, h = hf * 2 + b4 // 2, b4 % 2
                mm(P[2 + b4][:, :], uT[64 * h:64 * h + 64, Q, :],
                   Bq[64 * h:64 * h + 64, Q, :, :, :].re("k l a n -> k (l a n)"))
            def buv(pl):
                vs = []
                for b4 in range(4):
                    vs.append(P[2 + b4][:, :].re("p (g a n) -> p g a n", a=2, n=128)[:, :, pl, :])
                return vs
            bre = buv(0)
            bim = buv(1)
            gs = slice(hf * 8, hf * 8 + 8)
            for b4 in range(4):
                g2s = slice(hf * 8 + 2 * b4, hf * 8 + 2 * b4 + 2)
                l2s = slice(2 * b4, 2 * b4 + 2)
                tt("dve", tz[0][:, l2s, :], bre[b4], T1re3[:, g2s, :], ALU.mult)
                tt("dve", tz[1][:, l2s, :], bim[b4], T1im3[:, g2s, :], ALU.mult)
                tt("dve", tz[2][:, l2s, :], bre[b4], T1im3[:, g2s, :], ALU.mult)
                tt("dve", tz[3][:, l2s, :], bim[b4], T1re3[:, g2s, :], ALU.mult)
            tt("pool", zt[:, gs, 0, :], tz[0][:], tz[1][:], ALU.subtract)
            tt("pool", zt[:, gs, 1, :], tz[2][:], tz[3][:], ALU.add)
        for hf in range(2):
            gs = slice(hf * 8, hf * 8 + 8)
            for g8 in range(8):
                gp = hf * 8 + g8
                for pl in range(2):
                    idx = g8 * 2 + pl
                    mm(P[2 + idx // 4][:, (idx % 4) * 128:(idx % 4 + 1) * 128], zt[:, gp, pl, :], trile[:])
            for b4 in range(4):
                g2s = slice(hf * 8 + 2 * b4, hf * 8 + 2 * b4 + 2)
                l2s = slice(2 * b4, 2 * b4 + 2)
                pv = P[2 + b4][:, :].re("p (g a n) -> p g a n", a=2, n=128)
                tt("dve", cg[:, l2s, 0, :], pv[:, :, 0, :], Gre[:, g2s].un(2).bc([128, 2, 128]), ALU.add)
                tt("dve", cg[:, l2s, 1, :], pv[:, :, 1, :], Gim[:, g2s].un(2).bc([128, 2, 128]), ALU.add)
            tt("dve", tz[0][:], cg[:, :, 0, :], T2re3[:, gs, :], ALU.mult)
            tt("pool", tz[1][:], cg[:, :, 1, :], T2im3[:, gs, :], ALU.mult)
            tt("dve", tz[2][:], cg[:, :, 1, :], T2re3[:, gs, :], ALU.mult)
            tt("pool", tz[3][:], cg[:, :, 0, :], T2im3[:, gs, :], ALU.mult)
            tt("dve", xT[:, gs, 0, :], tz[0][:], tz[1][:], ALU.subtract)
            tt("pool", xT[:, gs, 1, :], tz[2][:], tz[3][:], ALU.add)
            tt("dve", xlast[:, gs, 0], tz[0][:, :, 127], tz[1][:, :, 127], ALU.subtract)
            tt("dve", xlast[:, gs, 1], tz[2][:, :, 127], tz[3][:, :, 127], ALU.add)
        tt("dve", s16[0][:], xlast[:, :, 0], are[:], ALU.mult)
        tt("dve", s16[1][:], xlast[:, :, 1], aim[:], ALU.mult)
        tt("dve", s16[2][:], xlast[:, :, 0], aim[:], ALU.mult)
        tt("dve", s16[3][:], xlast[:, :, 1], are[:], ALU.mult)
        tt("dve", Gre[:], s16[0][:], s16[1][:], ALU.subtract)
        tt("dve", Gim[:], s16[2][:], s16[3][:], ALU.add)
        for Q in range(4):
            for h in range(2):
                i = 0
                for lh in range(2):
                    gp = 4 * Q + 2 * h + lh
                    for pl in range(2):
                        mm(P[1][64 * h:64 * h + 64, Q * 128:(Q + 1) * 128], Cq[:, Q, pl, lh, 64 * h:64 * h + 64],
                           xT[:, gp, pl, :], start=(i == 0), stop=(i == 3))
                        i += 1
        for Q in range(4):
            stt(yv[:, Q, :], uT[:, Q, :], dcol[:, Q:Q + 1], P[1][:, Q * 128:(Q + 1) * 128], ALU.mult, ALU.add)
        if blk == 1:
            dbg("yv", yv[:], [128, 4, 128])
        tt("pool", y2[:], yv[:], yv[:], ALU.mult)
        ts("dve", y2[:], y2[:], 0.044715, ALU.mult, 1.0, ALU.add)
        tt("pool", y3[:], y2[:], yv[:], ALU.mult)
        act(y3[:], y3[:], AF.Sigmoid, scale=1.5957691216057308)
        tt("dve", yg[:], yv[:], y3[:], ALU.mult)
        cp("pool", ygb[:], yg[:])
        for co in range(4):
            for kc in range(4):
                mm(P[0][:, co * 128:(co + 1) * 128], Wg[:, kc, co * 128:(co + 1) * 128], ygb[:, kc, :],
                   start=(kc == 0), stop=(kc == 3))
        for co in range(4):
            act(sgl[:, co, :], P[0][:, co * 128:(co + 1) * 128], AF.Sigmoid, bias=gbcol[:, co:co + 1])
        tt("dve", ssmT[:], yg[:], sgl[:], ALU.mult)
        dma("sp", ssm_scr[:].re("q p n -> p q n")[:, :, blk * 128:(blk + 1) * 128], ssmT[:])

    close_scope(scopeA)
    if nphase < 2:
        while len(cur) > 1:
            cur[0].close()
            cur.pop(0)
        return finish()
    scope2 = open_scope()
    mask0 = sb("mask0", [128, 512], BF16)
    maske = sb("maske", [128, 8, 512], BF16)
    masko = sb("masko", [128, 8, 512], BF16)
    dma("sp", mask0[:], k_mask0[:])
    dma("sp", maske[:], k_maske[:])
    dma("sp", masko[:], k_masko[:])
    Wr = sb("Wr", [128, 8, 36])
    dma("sp", Wr[:], w_r[:].re("(kc p) n -> p kc n", p=128))
    Wpa = sb("Wpa", [64, 8, D], BF16)
    Wpb = sb("Wpb", [128, 4, D], BF16)
    Wo = sb("Wo", [128, 8, D], BF16)
    for h in range(8):
        dma("pool", Wpa[:, h, :], w_pa[h * 64:(h + 1) * 64, :])
    for kc in range(4):
        dma("pool", Wpb[:, kc, :], w_pb[kc * 128:(kc + 1) * 128, :])
    for kc in range(8):
        dma("pool", Wo[:, kc, :], w_o[kc * 128:(kc + 1) * 128, :])
    wring = [sb("wring%d" % i, [128, 8, 512], BF16) for i in range(2)]
    wri = [0]

    def load_wchunk(c0):
        t = wring[wri[0] % 2]
        wri[0] += 1
        for kc in range(8):
            dma("pool", t[:, kc, :], w_in[kc * 128:(kc + 1) * 128, c0:c0 + 512])
        return t

    xo = [sb("xo%d" % i, [128, D]) for i in range(2)]
    xn2 = sb("xn2", [128, D])
    junk2 = sb("junk2", [128, D], BF16)
    ssq2 = sb("ssq2", [128, 1])
    rstd2 = sb("rstd2", [128, 1])
    xoT = sb("xoT", [128, 8, 512], BF16)
    qT = sb("qT", [64, 8, 512], BF16)
    ktc = [[sb("ktc%d_%d" % (a, i), [64, 1024], BF16) for i in range(2)] for a in range(2)]
    vtc = [[sb("vtc%d_%d" % (a, i), [128, 8, 64], BF16) for i in range(2)] for a in range(2)]
    erw = [sb("erw%d" % i, [128, 1024], BF16) for i in range(2)]
    sprw = [sb("sprw%d" % i, [128, 1024], BF16) for i in range(2)]
    trw = sb("trw", [128, 1024], BF16)
    wrw = [sb("wrw%d" % i, [128, 1024], BF16) for i in range(2)]
    attnT = sb("attnT", [64, 8, 512], BF16)
    ssA = sb("ssA", [128, 4, 512], BF16)
    ssB = sb("ssB", [128, 4, 512], BF16)
    ssO = ssA
    sg1 = sb("sg1", [128, 512], BF16)
    sg2 = sb("sg2", [128, 512], BF16)
    mg1 = sb("mg1", [128, 512])
    mg2 = sb("mg2", [128, 512])
    mergedT = sb("mergedT", [128, 8, 512], BF16)
    hblk = [sb("hblk0", [128, D])] * 2
    hn = [xn2, xn2]
    hnT = sb("hnT", [128, 8, 128])
    lgt = sb("lgt", [128, 36])
    r1 = [sb("r1_%d" % i, [128, 1]) for i in range(12)]
    ohg = sb("ohg", [128, 4])
    eg = sb("eg", [128, 4])
    selv = sb("selv", [128, 8])
    m8 = sb("m8", [128, 8])
    oh1 = sb("oh1", [128, 8])
    oh2 = sb("oh2", [128, 8])
    M12 = sb("M12", [128, 64])
    pm = sb("pm", [128, 64])
    pmt = sb("pmt", [128, 64])
    base = sb("base", [128, 32])
    destf = sb("destf", [128, 2])
    S.op("dve", lambda e: e.memset(base[:].ap, 0.0), writes=[base.buf])

    def rmsnorm2(xv, grep, outv):
        act(junk2[:], xv, AF.Square, accum=ssq2[:])
        act(rstd2[:], ssq2[:], AF.Ln, scale=1.0 / D, bias=epsc[:])
        act(rstd2[:], rstd2[:], AF.Exp, scale=-0.5)
        stt(outv, xv, rstd2[:], grep[:], ALU.mult, ALU.mult)

    def mmacc(out, lhsT, rhs, start):
        S.op("pe", lambda e: e.matmul(out.ap, lhsT.ap, rhs.ap, start=start, stop=True, skip_group_check=True),
             reads=[lhsT.buf, rhs.buf], writes=[out.buf])

    for n in range(NSLOT):
        pmask = maske if n % 2 == 0 else masko
        for r in range(4):
            xb_ = xo[r % 2]
            dma("sp", xb_[:], x_own[(4 * n + r) * 128:(4 * n + r + 1) * 128, :])
            rmsnorm2(xb_[:], grep_mix, xn2[:])
            transpose8(xn2, xoT, r * 128, 512)
        wq = load_wchunk(0)
        for h in range(8):
            pb = P[4 + h % 4]
            for kc in range(8):
                mm(pb[0:64, :], wq[:, kc, h * 64:(h + 1) * 64], xoT[:, kc, :], start=(kc == 0), stop=(kc == 7))
            cp("act" if h % 2 == 0 else "dve", qT[:, h, :], pb[0:64, :])
        if n == 0:
            dbg("qT", qT[:], [64, 8, 512], BF16)
        nkb = 8 * n + 9
        kbs = [8 * n + 8 - s for s in range(nkb)]
        for hp in range(4):
            heads = (2 * hp, 2 * hp + 1)
            OB = (P[2], P[3])
            ZW = (P45, P67)
            cur_chunk = [None, None]
            cur_kt = [None, None]
            cur_vt = [None, None]
            nload = [0, 0]

            def get_kv(a, kb):
                h = heads[a]
                c = -1 if kb == 0 else (kb - 1) // 8
                if cur_chunk[a] != c:
                    i = nload[a] % 2
                    nload[a] += 1
                    kt_, vt_ = ktc[a][i], vtc[a][i]
                    if c < 0:
                        dma("sp", kt_[:, 0:128], kt_scr[h * 64:(h + 1) * 64, 0:128])
                        dma("sp", vt_[:, 0:1, :], v_scr[h, :, 0:1, :])
                    else:
                        dma("sp", kt_[:, :], kt_scr[h * 64:(h + 1) * 64, (8 * c + 1) * 128:(8 * c + 9) * 128])
                        dma("sp", vt_[:, :, :], v_scr[h, :, 8 * c + 1:8 * c + 9, :])
                    cur_chunk[a], cur_kt[a], cur_vt[a] = c, kt_, vt_
                lb = 0 if kb == 0 else (kb - 1) % 8
                return cur_kt[a][:, lb * 128:(lb + 1) * 128], cur_vt[a][:, lb, :]

            vsel = {}

            def front(s):
                kb = kbs[s]
                Z = ZW[s % 2]
                for a in range(2):
                    ktv, vtv = get_kv(a, kb)
                    vsel[(a, s)] = vtv
                    mm(Z[:, a * 512:(a + 1) * 512], ktv, qT[:, heads[a], :])
                e_ = erw[s % 2]
                act(e_[:], Z[:, :], AF.Exp, scale=0.125)
                e3 = e_[:].re("p (a n) -> p a n", a=2)
                if s < 8:
                    tt("dve", e3, e3, pmask[:, s, :].un(1).bc([128, 2, 512]), ALU.mult)
                elif kb == 0:
                    tt("dve", e3, e3, mask0[:].un(1).bc([128, 2, 512]), ALU.mult)
                act(sprw[s % 2][:], e_[:], AF.Ln, bias=onec[:])

            front(0)
            for s in range(nkb):
                sp_ = sprw[s % 2]
                for a in range(2):
                    mmacc(P01[:, a * 512:(a + 1) * 512], trige[:], sp_[:, a * 512:(a + 1) * 512], start=(s == 0))
                act(trw[:], P01[:, :], AF.Exp, scale=-1.0)
                if s + 1 < nkb:
                    front(s + 1)
                for a in range(2):
                    mmacc(P01[:, a * 512:(a + 1) * 512], trilt[:], sp_[:, a * 512:(a + 1) * 512], start=False)
                w_ = wrw[s % 2]
                tt("dve", w_[:], erw[s % 2][:], trw[:], ALU.mult)
                for a in range(2):
                    mmacc(OB[a][0:64, :], vsel[(a, s)], w_[:, a * 512:(a + 1) * 512], start=(s == 0))
            for a in range(2):
                cp("act" if a == 0 else "dve", attnT[:, heads[a], :], OB[a][0:64, :])
        if n == 0:
            dbg("attnT", attnT[:], [64, 8, 512], BF16)
        posA = (8 * n + 1) * 128
        posB = (8 * n + 5) * 128
        sv = ssm_scr[:].re("q p n -> p q n")
        dma("sp", ssA[:], sv[:, :, posA:posA + 512])
        dma("sp", ssB[:], sv[:, :, posB:posB + 512])
        ts("dve", ssO[:], ssA[:], sel[:, 2 * n:2 * n + 1], ALU.mult)
        stt(ssO[:], ssB[:], sel[:, 2 * n + 1:2 * n + 2], ssO[:], ALU.mult, ALU.add)
        for c2 in range(2):
            wga = load_wchunk(2048 + c2 * 512)
            wgb = load_wchunk(3072 + c2 * 512)
            for c4 in range(4):
                co = c2 * 4 + c4
                for kc in range(8):
                    mm(P[4][:, :], wga[:, kc, c4 * 128:(c4 + 1) * 128], xoT[:, kc, :], start=(kc == 0), stop=(kc == 7))
                for kc in range(8):
                    mm(P[5][:, :], wgb[:, kc, c4 * 128:(c4 + 1) * 128], xoT[:, kc, :], start=(kc == 0), stop=(kc == 7))
                for h in range(8):
                    mm(P[6][:, :], Wpa[:, h, co * 128:(co + 1) * 128], attnT[:, h, :], start=(h == 0), stop=(h == 7))
                for kc in range(4):
                    mm(P[7][:, :], Wpb[:, kc, co * 128:(co + 1) * 128], ssO[:, kc, :], start=(kc == 0), stop=(kc == 3))
                act(sg1[:], P[4][:, :], AF.Sigmoid)
                act(sg2[:], P[5][:, :], AF.Sigmoid)
                tt("dve", mg1[:], P[6][:, :], sg1[:], ALU.mult)
                tt("dve", mg2[:], P[7][:, :], sg2[:], ALU.mult)
                tt("pool", mergedT[:, co, :], mg1[:], mg2[:], ALU.add)
        if n == 0:
            dbg("mergedT", mergedT[:], [128, 8, 512], BF16)
        for r in range(4):
            ob = 4 * n + r
            xb_ = xo[r % 2]
            hb_ = hblk[r % 2]
            hn_ = hn[r % 2]
            dma("sp", xb_[:], x_own[ob * 128:(ob + 1) * 128, :])
            for half in range(2):
                for kc in range(8):
                    mm(P[half][:, :], mergedT[:, kc, r * 128:(r + 1) * 128], Wo[:, kc, half * 512:(half + 1) * 512],
                       start=(kc == 0), stop=(kc == 7))
                tt("dve", hb_[:, half * 512:(half + 1) * 512], P[half][:, :], xb_[:, half * 512:(half + 1) * 512],
                   ALU.add)
            dma("sp", h_scr[ob * 128:(ob + 1) * 128, :], hb_[:])
            rmsnorm2(hb_[:], grep_ffn, hn_[:])
            for half in range(2):
                for k in range(4):
                    kc = half * 4 + k
                    tr(P[6 + half][:, k * 128:(k + 1) * 128], hn_[:, kc * 128:(kc + 1) * 128], ident[:])
                cp("act" if half == 0 else "dve", hnT[:, half * 4:half * 4 + 4, :],
                   P[6 + half][:, :].re("p (k n) -> p k n", n=128))
            for kc in range(8):
                mm(P[2][:, 0:36], hnT[:, kc, :], Wr[:, kc, :], start=(kc == 0), stop=(kc == 7))
            tt("dve", lgt[:], P[2][:, 0:36], brrep[:], ALU.add)
            gmax, ngmax, gsum, ptop, dv, ex, g1, g2, p1loc, p2loc, ov1, ov2 = [t_[:] for t_ in r1]
            S.op("dve", lambda e: e.tensor_reduce(gmax.ap, lgt[:, 0:4].ap, AX.X, ALU.max),
                 reads=[lgt.buf], writes=[gmax.buf])
            ts("dve", ohg[:], lgt[:, 0:4], gmax, ALU.is_equal)
            ts("dve", ngmax, gmax, -1.0, ALU.mult)
            act(eg[:], lgt[:, 0:4], AF.Exp, bias=ngmax, accum=gsum)
            S.op("dve", lambda e: e.reciprocal(ptop.ap, gsum.ap), reads=[gsum.buf], writes=[ptop.buf])
            ts("dve", selv[:], lgt[:, 4:12], ohg[:, 0:1], ALU.mult)
            for g in range(1, 4):
                stt(selv[:], lgt[:, 4 + 8 * g:12 + 8 * g], ohg[:, g:g + 1], selv[:], ALU.mult, ALU.add)
            S.op("dve", lambda e: e.max(m8[:].ap, selv[:].ap), reads=[selv.buf], writes=[m8.buf])
            ts("dve", oh1[:], selv[:], m8[:, 0:1], ALU.is_equal)
            ts("dve", oh2[:], selv[:], m8[:, 1:2], ALU.is_equal)
            tt("dve", dv, m8[:, 1:2], m8[:, 0:1], ALU.subtract)
            act(ex, dv, AF.Exp)
            ts("dve", g1, ex, 1.0, ALU.add)
            S.op("dve", lambda e: e.reciprocal(g1.ap, g1.ap), reads=[g1.buf], writes=[g1.buf])
            tt("dve", g2, ex, g1, ALU.mult)
            tt("dve", gwall[:, ob, 0:1], g1, ptop, ALU.mult)
            tt("dve", gwall[:, ob, 1:2], g2, ptop, ALU.mult)
            tt("dve", M12[:, 0:32].re("p (g e) -> p g e", e=8), ohg[:].un(2).bc([128, 4, 8]),
               oh1[:].un(1).bc([128, 4, 8]), ALU.mult)
            tt("dve", M12[:, 32:64].re("p (g e) -> p g e", e=8), ohg[:].un(2).bc([128, 4, 8]),
               oh2[:].un(1).bc([128, 4, 8]), ALU.mult)
            mm(P[3][:, 0:64], triltf[:], M12[:])
            mm(P[3][:, 64:128], onesf[:], M12[:])
            tt("dve", pm[:, 0:32], P[3][:, 0:32], base[:], ALU.add)
            tt("dve", pm[:, 32:64], P[3][:, 32:64], base[:], ALU.add)
            tt("dve", pm[:, 32:64], P[3][:, 64:96], pm[:, 32:64], ALU.add)
            tt("dve", pmt[:], pm[:], M12[:], ALU.mult)
            S.op("dve", lambda e: e.tensor_reduce(p1loc.ap, pmt[:, 0:32].ap, AX.X, ALU.add),
                 reads=[pmt.buf], writes=[p1loc.buf])
            S.op("dve", lambda e: e.tensor_reduce(p2loc.ap, pmt[:, 32:64].ap, AX.X, ALU.add),
                 reads=[pmt.buf], writes=[p2loc.buf])
            tt("dve", pmt[:, 0:32], M12[:, 0:32], ecap[:], ALU.mult)
            tt("dve", pmt[:, 32:64], M12[:, 32:64], ecap[:], ALU.mult)
            S.op("dve", lambda e: e.tensor_reduce(ov1.ap, pmt[:, 0:32].ap, AX.X, ALU.add),
                 reads=[pmt.buf], writes=[ov1.buf])
            S.op("dve", lambda e: e.tensor_reduce(ov2.ap, pmt[:, 32:64].ap, AX.X, ALU.add),
                 reads=[pmt.buf], writes=[ov2.buf])
            tt("dve", destf[:, 0:1], ov1, p1loc, ALU.add)
            tt("dve", destf[:, 1:2], ov2, p2loc, ALU.add)
            ts("dve", ov1, p1loc, float(CAP) - 0.5, ALU.is_gt, float(4 * NXB), ALU.mult)
            ts("dve", ov2, p2loc, float(CAP) - 0.5, ALU.is_gt, float(4 * NXB), ALU.mult)
            tt("dve", destf[:, 0:1], destf[:, 0:1], ov1, ALU.add)
            tt("dve", destf[:, 1:2], destf[:, 1:2], ov2, ALU.add)
            cp("dve", destall[:, ob, :], destf[:])
            tt("dve", base[:], P[3][:, 64:96], base[:], ALU.add)
            tt("dve", base[:], P[3][:, 96:128], base[:], ALU.add)
            for k in range(2 if "noscat" not in debug else 0):
                dv_ = destall[:, ob, k:k + 1]
                S.dma("pool", (lambda dv_, hn_: (lambda e: e.indirect_dma_start(
                    out=xb_scr[:].ap, out_offset=bass.IndirectOffsetOnAxis(ap=dv_.ap, axis=0),
                    in_=hn_[:].ap, in_offset=None, bounds_check=bc_reg(e), oob_is_err=False)))(dv_, hn_),
                    reads=[hn_.buf, destall.buf], writes=[xb_scr.buf])
    dbg("destall", destall[:], [128, NOB, 2], I32)
    dbg("gwall", gwall[:], [128, NOB, 2])
    close_scope(scope2)
    if nphase < 3 or not moe:
        return finish()
    if nphase >= 3 and moe:
        scope3 = open_scope()
        W1 = [sb("W1_%d" % i, [128, 8, 512], BF16) for i in range(2)]
        W3 = [sb("W3_%d" % i, [128, 8, 512], BF16) for i in range(2)]
        W2 = [sb("W2_%d" % i, [128, 4, D], BF16) for i in range(2)]
        stg1 = sb("stg1", [128, 8, 512])
        stg3 = sb("stg3", [128, 8, 512])
        stg2 = sb("stg2", [128, 4, D])
        xg = [sb("xg%d" % i, [128, D]) for i in range(2)]
        xeT = sb("xeT", [128, 8, CAP], BF16)
        s1 = sb("s1", [128, CAP])
        hgT = sb("hgT", [128, 4, CAP], BF16)
        ybk = [sb("ybk%d" % i, [128, D]) for i in range(2)]

        def load_expert(ex_):
            dma("sp", stg1[:], w1[ex_].re("(kc p) f -> p kc f", p=128))
            dma("sp", stg3[:], w3[ex_].re("(kc p) f -> p kc f", p=128))
            dma("sp", stg2[:], w2[ex_].re("(fc p) n -> p fc n", p=128))

        def cast_expert(ex_):
            i = ex_ % 2
            cp("act", W1[i][:, 0:4, :], stg1[:, 0:4, :])
            cp("dve", W1[i][:, 4:8, :], stg1[:, 4:8, :])
            cp("act", W3[i][:, 0:4, :], stg3[:, 0:4, :])
            cp("dve", W3[i][:, 4:8, :], stg3[:, 4:8, :])
            cp("act", W2[i][:, 0:2, :], stg2[:, 0:2, :])
            cp("dve", W2[i][:, 2:4, :], stg2[:, 2:4, :])

        load_expert(0)
        cast_expert(0)
        for ex_ in range(32):
            w1_, w3_, w2_ = W1[ex_ % 2], W3[ex_ % 2], W2[ex_ % 2]
            if ex_ + 1 < 32:
                load_expert(ex_ + 1)
            for c in range(CAPB):
                xg_ = xg[c % 2]
                dma("sp", xg_[:], xb_scr[ex_ * CAP + c * 128:ex_ * CAP + (c + 1) * 128, :])
                transpose8(xg_, xeT, c * 128, CAP)
            for fc in range(4):
                for kc in range(8):
                    mm(P[0][:, 0:CAP], w1_[:, kc, fc * 128:(fc + 1) * 128], xeT[:, kc, :], start=(kc == 0), stop=(kc == 7))
                for kc in range(8):
                    mm(P[1][:, 0:CAP], w3_[:, kc, fc * 128:(fc + 1) * 128], xeT[:, kc, :], start=(kc == 0), stop=(kc == 7))
                act(s1[:], P[0][:, 0:CAP], AF.Silu)
                tt("dve", hgT[:, fc, :], s1[:], P[1][:, 0:CAP], ALU.mult)
            for c in range(CAPB):
                yb_ = ybk[c % 2]
                for half in range(2):
                    for fc in range(4):
                        mm(P[2 + half][:, :], hgT[:, fc, c * 128:(c + 1) * 128], w2_[:, fc, half * 512:(half + 1) * 512],
                           start=(fc == 0), stop=(fc == 3))
                    cp("act" if half == 0 else "dve", yb_[:, half * 512:(half + 1) * 512], P[2 + half][:, :])
                dma("sp", yb_scr[ex_ * CAP + c * 128:ex_ * CAP + (c + 1) * 128, :], yb_[:])
            if ex_ + 1 < 32:
                cast_expert(ex_ + 1)
        if nphase < 4:
            close_scope(scope3)
            return finish()
        yk = [sb("yk%d" % i, [128, D]) for i in range(2)]
        hb4 = [sb("hb4_%d" % i, [128, D]) for i in range(2)]
        oo = [sb("oo%d" % i, [128, D]) for i in range(2)]
        junk4 = sb("junk4", [128, D], BF16)
        ssq4 = sb("ssq4", [128, 1])
        rstd4 = sb("rstd4", [128, 1])
        for ob in range(NOB):
            hb_ = hb4[ob % 2]
            dma("sp", hb_[:], h_scr[ob * 128:(ob + 1) * 128, :])
            for k in range(2):
                yk_ = yk[k]
                S.op("pool", (lambda yk_: (lambda e: e.memset(yk_[:].ap, 0.0)))(yk_), writes=[yk_.buf])
                dv_ = destall[:, ob, k:k + 1]
                S.dma("pool", (lambda dv_, yk_: (lambda e: e.indirect_dma_start(
                    out=yk_[:].ap, out_offset=None, in_=yb_scr[:].ap,
                    in_offset=bass.IndirectOffsetOnAxis(ap=dv_.ap, axis=0),
                    bounds_check=bc_reg(e), oob_is_err=False)))(dv_, yk_),
                    reads=[yb_scr.buf, destall.buf], writes=[yk_.buf])
                stt(hb_[:], yk_[:], gwall[:, ob, k:k + 1], hb_[:], ALU.mult, ALU.add)
            oo_ = oo[ob % 2]
            act(junk4[:], hb_[:], AF.Square, accum=ssq4[:])
            act(rstd4[:], ssq4[:], AF.Ln, scale=1.0 / D, bias=epsc[:])
            act(rstd4[:], rstd4[:], AF.Exp, scale=-0.5)
            stt(oo_[:], hb_[:], rstd4[:], grep_fin[:], ALU.mult, ALU.mult)
            dma("sp", out_own[ob * 128:(ob + 1) * 128, :], oo_[:])
        close_scope(scope3)

    return finish()


def _consts(NSLOT, CAP):
    bf = ml_dtypes.bfloat16
    r = np.arange(128)[:, None]
    c = np.arange(128)[None, :]
    k = {}
    k["k_ident"] = (r == c).astype(np.float32)
    k["k_triltf"] = (r < c).astype(np.float32)
    k["k_onesf"] = np.ones((128, 128), np.float32)
    k["k_jrow"] = np.arange(128, dtype=np.float32)[None, :]
    mB = np.zeros((128, 2, 16), np.float32)
    for g2 in range(2):
        mB[g2 * 64:(g2 + 1) * 64, g2, :] = 1.0
    k["k_maskB"] = mB.reshape(128, 32)
    mC = np.zeros((128, 2, 64), np.float32)
    for rr in range(128):
        mC[rr, (rr // 16) % 2, :] = 1.0
    k["k_maskC"] = mC.reshape(128, 128)
    mQ = np.zeros((128, 2), np.float32)
    for rr in range(128):
        mQ[rr, (rr // 32) % 2] = 1.0
    k["k_maskQ"] = mQ
    mR = np.zeros((128, 2, 128), np.float32)
    for cc in range(128):
        mR[:, (cc // 32) % 2, cc] = 1.0
    k["k_maskR"] = mR.reshape(128, 256)
    k["k_ecap"] = np.tile((np.arange(32, dtype=np.float32) * CAP)[None, :], (128, 1))
    k["k_trige"] = (r >= c).astype(bf)
    k["k_trilt"] = (r < c).astype(bf)
    k["k_trile"] = (r <= c).astype(bf)
    m0 = np.ones((128, 512), np.float32)
    m0[:112, :] = 0.0
    k["k_mask0"] = m0.astype(bf)
    j = np.arange(128)[:, None]
    t = np.arange(512)[None, :]
    patA = np.zeros((128, 8, 512), np.float32)
    patB = np.zeros((128, 8, 512), np.float32)
    for s in range(8):
        if s < 4:
            rr = 3 - s
            patB[:, s, :] = (rr * 128 + j < t)
        else:
            rr = 7 - s
            patA[:, s, :] = (rr * 128 + j < t)
            patB[:, s, :] = 1.0
    return k, patA.astype(bf), patB.astype(bf)


def own_is_A(n, hf):
    return ((n + hf) % 2) == 0


def make_in_maps(inputs, NSLOT, CAP, n_cores=8, moe=True):
    x = np.asarray(inputs["x"], np.float32)
    NB = 1 + 8 * NSLOT
    NPOS = NB * 128
    meta = np.asarray(inputs["meta_tokens"], np.float32)
    k, patA, patB = _consts(NSLOT, CAP)
    f = lambda a: np.ascontiguousarray(np.asarray(a, np.float32))
    shared = dict(
        w_in=f(inputs["w_in"][0]), g_mix=f(inputs["norm_mix_g"][0:1]), g_ffn=f(inputs["norm_ffn_g"][0:1]),
        g_fin=f(inputs["norm_final_g"]).reshape(1, D),
        lam_re=f(inputs["ssm_lambda_re"][0]).reshape(1, 2048), lam_im=f(inputs["ssm_lambda_im"][0]).reshape(1, 2048),
        log_dt=f(inputs["ssm_log_dt"][0]).reshape(1, 32),
        b_re=f(inputs["ssm_b_re"][0]), b_im=f(inputs["ssm_b_im"][0]),
        c_re=f(inputs["ssm_c_re"][0]).reshape(512, 64), c_im=f(inputs["ssm_c_im"][0]).reshape(512, 64),
        d_skip=f(inputs["ssm_d"][0]).reshape(1, 512), glu_w=f(inputs["ssm_glu_w"][0]),
        glu_b=f(inputs["ssm_glu_b"][0]).reshape(1, 512),
        w_pa=f(inputs["w_branch_attn"][0]), w_pb=f(inputs["w_branch_ssm"][0]), w_o=f(inputs["w_out"][0]),
        w_r=f(np.concatenate([inputs["router_group_w"][0], inputs["router_expert_w"][0]], axis=1)),
        b_r=f(np.concatenate([inputs["router_group_b"][0], inputs["router_expert_b"][0]], axis=0)).reshape(1, 36),
        w1=f(inputs["expert_w1"][0][:(32 if moe else 1)]), w3=f(inputs["expert_w3"][0][:(32 if moe else 1)]),
        w2=f(inputs["expert_w2"][0][:(32 if moe else 1)]),
    )
    shared.update(k)
    maps = []
    for core in range(n_cores):
        b, hf = core // 2, core % 2
        xa = np.zeros((NPOS, D), np.float32)
        xa[112:128] = meta
        xa[128:] = x[b, :NPOS - 128]
        xo = np.zeros((512 * NSLOT, D), np.float32)
        sel = np.zeros((128, 2 * NSLOT), np.float32)
        for n in range(NSLOT):
            a = own_is_A(n, hf)
            r0 = (8 * n + (0 if a else 4)) * 128
            xo[n * 512:(n + 1) * 512] = x[b, r0:r0 + 512]
            sel[:, 2 * n] = 1.0 if a else 0.0
            sel[:, 2 * n + 1] = 0.0 if a else 1.0
        m = dict(shared)
        m["x_all"] = xa
        m["x_own"] = xo
        m["k_sel"] = sel
        m["k_maske"] = patA if own_is_A(0, hf) else patB
        m["k_masko"] = patA if own_is_A(1, hf) else patB
        maps.append(m)
    return maps


def assemble(results, NSLOT, B, SEQ):
    out = np.zeros((B, SEQ, D), np.float32)
    for core, r in enumerate(results):
        b, hf = core // 2, core % 2
        oo = np.asarray(r["out_own"], np.float32)
        for n in range(NSLOT):
            a = own_is_A(n, hf)
            r0 = (8 * n + (0 if a else 4)) * 128
            out[b, r0:r0 + 512] = oo[n * 512:(n + 1) * 512]
    return out


_NC_CACHE = {}


def kernel(**inputs):
    NSLOT, CAPB = 8, 3
    key = (NSLOT, CAPB)
    if key not in _NC_CACHE:
        _NC_CACHE[key] = build(NSLOT, CAPB)
    nc = _NC_CACHE[key]
    maps = make_in_maps(inputs, NSLOT, 128 * CAPB)
    res = run_bass_kernel_spmd(nc, maps, core_ids=list(range(8)))
    return assemble(res.results, NSLOT, 4, 8192)
```

```python
import math
from contextlib import ExitStack
import numpy as np
import ml_dtypes
import concourse.bass as bass
import concourse.mybir as mybir
from concourse.bass_utils import run_bass_kernel_spmd

F32 = mybir.dt.float32
BF16 = mybir.dt.bfloat16
I32 = mybir.dt.int32
AF = mybir.ActivationFunctionType
ALU = mybir.AluOpType
AX = mybir.AxisListType

SAME_ENGINE_SYNC = True
NDS = 20
D = 1024
TWO_PI = 2.0 * math.pi


class Buf:
    def __init__(self, name):
        self.name = name
        self.w = None
        self.r = {}


class View:
    def __init__(self, ap, buf):
        self.ap = ap
        self.buf = buf

    def __getitem__(self, k):
        return View(self.ap[k], self.buf)

    def re(self, pat, **kw):
        return View(self.ap.rearrange(pat, **kw), self.buf)

    def bc(self, shape):
        return View(self.ap.broadcast_to(list(shape)), self.buf)

    def un(self, axis):
        return View(self.ap.unsqueeze(axis), self.buf)


class T:
    def __init__(self, h, name):
        self.h = h
        self.buf = Buf(name)

    def __getitem__(self, k):
        return View(self.h[k], self.buf)


class Sched:
    ENG = ["pe", "act", "dve", "pool", "sp"]

    def __init__(self, nc, stack):
        self.nc = nc
        self.stack = stack
        self.epoch = {e: 0 for e in self.ENG}
        self.last_tok = {e: None for e in self.ENG}
        self.sem = {e: stack.enter_context(nc.semaphore("s_" + e)) for e in self.ENG}
        self.cnt = {e: 0 for e in self.ENG}
        self.ops = {e: [] for e in self.ENG}
        self.waited = {e: {} for e in self.ENG}
        self.dsem = {q: [stack.enter_context(nc.semaphore("d_%s%d" % (q, i))) for i in range(NDS)]
                     for q in ("sp", "pool")}
        self.dcnt = {q: [0] * NDS for q in ("sp", "pool")}
        self.drr = {"sp": 0, "pool": 0}

    @staticmethod
    def _flat(bufs):
        out = []
        for b in bufs:
            if isinstance(b, (tuple, list)):
                out.extend(b)
            else:
                out.append(b)
        return out

    def _deps(self, reads, writes):
        reads = self._flat(reads)
        writes = self._flat(writes)
        deps = []
        for b in reads:
            if b.w is not None:
                deps.append(b.w)
        for b in writes:
            if b.w is not None:
                deps.append(b.w)
            deps.extend(b.r.values())
        return deps

    def _waits(self, e, deps):
        need = {}
        for (key, sem, val) in deps:
            if val <= 0:
                continue
            if key[0] == "E" and key[1] == e and (e == "pe" or not SAME_ENGINE_SYNC):
                continue
            if self.waited[e].get(key, 0) >= val:
                continue
            if key not in need or need[key][1] < val:
                need[key] = (sem, val)
        for key, (sem, val) in need.items():
            self.waited[e][key] = val
        return list(need.values())

    def _record(self, tok, reads, writes):
        reads = self._flat(reads)
        writes = self._flat(writes)
        for b in reads:
            old = b.r.get(tok[0])
            if old is None or old[2] < tok[2]:
                b.r[tok[0]] = tok
        for b in writes:
            b.w = tok
            b.r = {}

    def op(self, e, fn, reads=(), writes=()):
        waits = self._waits(e, self._deps(reads, writes))
        if self.cnt[e] >= 16000:
            self.epoch[e] += 1
            self.sem[e] = self.stack.enter_context(self.nc.semaphore("s_%s_%d" % (e, self.epoch[e])))
            self.cnt[e] = 0
        self.cnt[e] += 1
        tok = (("E", e, self.epoch[e]), self.sem[e], self.cnt[e])
        self.last_tok[e] = tok
        self.ops[e].append((waits, fn, self.sem[e], 1))
        self._record(tok, reads, writes)

    def dma(self, q, fn, reads=(), writes=()):
        deps = self._deps(reads, writes)
        i = self.drr[q]
        self.drr[q] = (i + 1) % NDS
        sem = self.dsem[q][i]
        key = ("D", q, i)
        prev = self.dcnt[q][i]
        deps.append((key, sem, prev))
        waits = self._waits(q, deps)
        self.dcnt[q][i] = prev + 16
        tok = (key, sem, prev + 16)
        self.ops[q].append((waits, fn, sem, 16))
        self._record(tok, reads, writes)

    def barrier(self):
        toks = [self.last_tok[e] for e in self.ENG if self.last_tok[e] is not None]
        for q in ("sp", "pool"):
            for i in range(NDS):
                if self.dcnt[q][i] > 0:
                    toks.append((("D", q, i), self.dsem[q][i], self.dcnt[q][i]))
        for e in self.ENG:
            waits = self._waits(e, toks)
            if waits:
                self.ops[e].append((waits, None, None, 0))

    def replay(self, e, eng):
        for waits, fn, sem, inc in self.ops[e]:
            for (s, v) in waits:
                eng.wait_ge(s, v)
            if fn is not None:
                fn(eng).then_inc(sem, inc)

    def final_waits(self, e, eng, bufs):
        deps = []
        for b in self._flat(bufs):
            if b.w is not None:
                deps.append(b.w)
        for (s, v) in self._waits(e, deps):
            eng.wait_ge(s, v)


def build(NSLOT, CAPB, debug=(), moe=True, nphase=4):
    NB = 1 + 8 * NSLOT
    NPOS = NB * 128
    TOWN = 512 * NSLOT
    NOB = 4 * NSLOT
    CAP = 128 * CAPB
    NXB = 32 * CAP
    nc = bass.Bass("TRN2", target_bir_lowering=False)
    stack = ExitStack()
    S = Sched(nc, stack)

    def din(name, shape, dt=F32):
        return T(nc.dram_tensor(name, list(shape), dt, kind="ExternalInput").ap(), name)

    def dscr(name, shape, dt=F32):
        kind = "ExternalOutput" if "scr" in debug else "Internal"
        return T(nc.dram_tensor(name, list(shape), dt, kind=kind).ap(), name)

    def dout(name, shape, dt=F32):
        return T(nc.dram_tensor(name, list(shape), dt, kind="ExternalOutput").ap(), name)

    cur = [stack]

    def sb(name, shape, dt=F32):
        return T(cur[0].enter_context(nc.sbuf_tensor(name, list(shape), dt)), name)

    def open_scope():
        st = ExitStack()
        cur.insert(0, st)
        return st

    def close_scope(st):
        assert cur[0] is st
        S.barrier()
        st.close()
        cur.pop(0)

    x_all = din("x_all", [NPOS, D])
    x_own = din("x_own", [TOWN, D])
    w_in = din("w_in", [D, 4096])
    g_mix = din("g_mix", [1, D])
    g_ffn = din("g_ffn", [1, D])
    g_fin = din("g_fin", [1, D])
    lam_re = din("lam_re", [1, 2048])
    lam_im = din("lam_im", [1, 2048])
    log_dt = din("log_dt", [1, 32])
    b_re = din("b_re", [32, 64, 16])
    b_im = din("b_im", [32, 64, 16])
    c_re = din("c_re", [512, 64])
    c_im = din("c_im", [512, 64])
    d_skip = din("d_skip", [1, 512])
    glu_w = din("glu_w", [512, 512])
    glu_b = din("glu_b", [1, 512])
    w_pa = din("w_pa", [512, D])
    w_pb = din("w_pb", [512, D])
    w_o = din("w_o", [D, D])
    w_r = din("w_r", [D, 36])
    b_r = din("b_r", [1, 36])
    NE = 32 if moe else 1
    w1 = din("w1", [NE, D, 512])
    w3 = din("w3", [NE, D, 512])
    w2 = din("w2", [NE, 512, D])
    k_ident = din("k_ident", [128, 128])
    k_triltf = din("k_triltf", [128, 128])
    k_onesf = din("k_onesf", [128, 128])
    k_jrow = din("k_jrow", [1, 128])
    k_maskB = din("k_maskB", [128, 32])
    k_maskC = din("k_maskC", [128, 128])
    k_ecap = din("k_ecap", [128, 32])
    k_maskQ = din("k_maskQ", [128, 2])
    k_maskR = din("k_maskR", [128, 256])
    k_trige = din("k_trige", [128, 128], BF16)
    k_trilt = din("k_trilt", [128, 128], BF16)
    k_trile = din("k_trile", [128, 128], BF16)
    k_mask0 = din("k_mask0", [128, 512], BF16)
    k_maske = din("k_maske", [128, 8, 512], BF16)
    k_masko = din("k_masko", [128, 8, 512], BF16)
    k_sel = din("k_sel", [128, 2 * NSLOT])

    kt_scr = dscr("kt_scr", [512, NPOS], BF16)
    v_scr = dscr("v_scr", [8, 128, NB, 64], BF16)
    ssm_scr = dscr("ssm_scr", [4, 128, NPOS], BF16)
    h_scr = dscr("h_scr", [TOWN, D])
    xb_scr = dscr("xb_scr", [NXB, D])
    yb_scr = dscr("yb_scr", [NXB, D])
    out_own = dout("out_own", [TOWN, D])

    dbg_outs = {}

    def dbg(name, view, shape, dt=F32):
        if name not in debug:
            return
        t = dout("dbg_" + name, shape, dt)
        dbg_outs[name] = t
        dma("sp", t[:], view)

    bcreg = {}

    def bc_reg(e):
        if "r" not in bcreg:
            bcreg["r"] = e.to_reg(NXB - 1)
        return bcreg["r"]

    def mm(out, lhsT, rhs, start=True, stop=True):
        S.op("pe", lambda e: e.matmul(out.ap, lhsT.ap, rhs.ap, start=start, stop=stop),
             reads=[lhsT.buf, rhs.buf], writes=[out.buf])

    def tr(out, in_, ident_v):
        S.op("pe", lambda e: e.transpose(out.ap, in_.ap, ident_v.ap),
             reads=[in_.buf, ident_v.buf], writes=[out.buf])

    def act(out, in_, func, bias=None, scale=None, accum=None, extra_reads=()):
        kw = {}
        reads = [in_.buf] + list(extra_reads)
        writes = [out.buf]
        if bias is not None:
            if isinstance(bias, View):
                kw["bias"] = bias.ap
                reads.append(bias.buf)
            else:
                kw["bias"] = bias
        if scale is not None:
            if isinstance(scale, View):
                kw["scale"] = scale.ap
                reads.append(scale.buf)
            else:
                kw["scale"] = scale
        if accum is not None:
            kw["accum_out"] = accum.ap
            writes.append(accum.buf)
        S.op("act", lambda e: e.activation(out.ap, in_.ap, func, **kw), reads=reads, writes=writes)

    def tt(eng, out, in0, in1, op):
        S.op(eng, lambda e: e.tensor_tensor(out.ap, in0.ap, in1.ap, op),
             reads=[in0.buf, in1.buf], writes=[out.buf])

    def _sc(x, reads):
        if isinstance(x, View):
            reads.append(x.buf)
            return x.ap
        return x

    def ts(eng, out, in0, s1, op0, s2=None, op1=None):
        reads = [in0.buf]
        a1 = _sc(s1, reads)
        a2 = _sc(s2, reads)
        if op1 is None:
            S.op(eng, lambda e: e.tensor_scalar(out.ap, in0.ap, a1, None, op0), reads=reads, writes=[out.buf])
        else:
            S.op(eng, lambda e: e.tensor_scalar(out.ap, in0.ap, a1, a2, op0, op1), reads=reads,
                 writes=[out.buf])

    def stt(out, in0, scalar, in1, op0, op1):
        reads = [in0.buf, in1.buf]
        a = _sc(scalar, reads)
        S.op("dve", lambda e: e.scalar_tensor_tensor(out.ap, in0.ap, a, in1.ap, op0, op1),
             reads=reads, writes=[out.buf])

    def cp(eng, out, in_):
        if eng == "act":
            S.op("act", lambda e: e.copy(out.ap, in_.ap), reads=[in_.buf], writes=[out.buf])
        else:
            S.op(eng, lambda e: e.tensor_copy(out.ap, in_.ap), reads=[in_.buf], writes=[out.buf])

    def dma(q, out, in_, **kw):
        S.dma(q, lambda e: e.dma_start(out=out.ap, in_=in_.ap, **kw), reads=[in_.buf], writes=[out.buf])

    Pall_h = stack.enter_context(nc.psum_tensor("psall", [128, 4096], F32))

    class PB:
        def __init__(self, i0, nb):
            self.i0, self.nb = i0, nb
            self.buf = tuple(pbufs[i0:i0 + nb]) if nb > 1 else pbufs[i0]

        def __getitem__(self, k):
            return View(Pall_h[:, self.i0 * 512:(self.i0 + self.nb) * 512][k], self.buf)

    pbufs = [Buf("ps%d" % i) for i in range(8)]
    P = [PB(i, 1) for i in range(8)]
    PQ = PB(2, 4)
    P01 = PB(0, 2)
    P45 = PB(4, 2)
    P67 = PB(6, 2)

    def finish():
        final_bufs = [out_own.buf, kt_scr.buf, v_scr.buf, ssm_scr.buf, h_scr.buf, xb_scr.buf, yb_scr.buf] + [t.buf for t in dbg_outs.values()]
        with nc.Block() as block:
            @block.tensor
            def _(e):
                S.replay("pe", e)

            @block.scalar
            def _(e):
                S.replay("act", e)

            @block.vector
            def _(e):
                S.replay("dve", e)

            @block.gpsimd
            def _(e):
                S.replay("pool", e)

            @block.sync
            def _(e):
                S.replay("sp", e)
                S.final_waits("sp", e, final_bufs)
        stack.close()
        return nc

    ident = sb("ident", [128, 128])
    triltf = sb("triltf", [128, 128])
    onesf = sb("onesf", [128, 128])
    jrow = sb("jrow", [1, 128])
    maskB = sb("maskB", [128, 32])
    maskC = sb("maskC", [128, 128])
    ecap = sb("ecap", [128, 32])
    maskQ = sb("maskQ", [128, 2])
    maskR = sb("maskR", [128, 256])
    trige = sb("trige", [128, 128], BF16)
    trilt = sb("trilt", [128, 128], BF16)
    trile = sb("trile", [128, 128], BF16)
    sel = sb("sel", [128, 2 * NSLOT])
    epsc = sb("epsc", [128, 1])
    onec = sb("onec", [128, 1])
    grep_mix = sb("grep_mix", [128, D])
    grep_ffn = sb("grep_ffn", [128, D])
    grep_fin = sb("grep_fin", [128, D])
    brrep = sb("brrep", [128, 36])
    destall = sb("destall", [128, NOB, 2], I32)
    gwall = sb("gwall", [128, NOB, 2])
    scopeA = open_scope()
    for dst, src in ((ident, k_ident), (triltf, k_triltf), (onesf, k_onesf), (jrow, k_jrow), (maskB, k_maskB),
                     (maskC, k_maskC), (ecap, k_ecap), (maskQ, k_maskQ), (maskR, k_maskR), (trige, k_trige), (trilt, k_trilt), (trile, k_trile),
                     (sel, k_sel)):
        dma("sp", dst[:], src[:])

    dcol = sb("dcol", [128, 4])
    gbcol = sb("gbcol", [128, 4])
    T1re = sb("T1re", [128, 2048])
    T1im = sb("T1im", [128, 2048])
    T2re = sb("T2re", [128, 2048])
    T2im = sb("T2im", [128, 2048])
    are = sb("are", [128, 16])
    aim = sb("aim", [128, 16])
    Bp = sb("Bp", [128, 4, 2, 128])
    Cp = sb("Cp", [128, 4, 2, 128])
    Bq = sb("Bq", [128, 4, 2, 2, 128], BF16)
    Cq = sb("Cq", [128, 4, 2, 2, 128], BF16)
    s16 = [sb("s16_%d" % i, [128, 16]) for i in range(6)]
    scope0 = open_scope()
    rowtmp = sb("rowtmp", [1, 2048])

    def replicate_row(dst, src_dram, n):
        dma("sp", rowtmp[0:1, 0:n], src_dram[0:1, 0:n])
        for c0 in range(0, n, 512):
            c1 = min(n, c0 + 512)
            mm(P[0][:, 0:c1 - c0], onesf[0:1, :], rowtmp[0:1, c0:c1])
            cp("dve", dst[:, c0:c1], P[0][:, 0:c1 - c0])

    replicate_row(grep_mix, g_mix, D)
    replicate_row(grep_ffn, g_ffn, D)
    replicate_row(grep_fin, g_fin, D)
    replicate_row(brrep, b_r, 36)
    S.op("dve", lambda e: e.memset(onec[:].ap, 1.0), writes=[onec.buf])

    def row_to_col(dst, src_dram, nchunk):
        dma("sp", rowtmp[0:1, 0:nchunk * 128], src_dram[0:1, 0:nchunk * 128])
        for q in range(nchunk):
            mm(P[0][:, 2 * q:2 * q + 2], rowtmp[0:1, q * 128:(q + 1) * 128], onesf[0:1, 0:2])
        cp("dve", dst[:, 0:nchunk], P[0][:, 0:2 * nchunk].re("p (q t) -> p q t", t=2)[:, :, 0])

    row_to_col(dcol, d_skip, 4)
    row_to_col(gbcol, glu_b, 4)

    lrrow = sb("lrrow", [1, 2048])
    lirow = sb("lirow", [1, 2048])
    dtrow = sb("dtrow", [1, 32])
    rhorow = sb("rhorow", [1, 2048])
    throw = sb("throw", [1, 2048])
    dma("sp", lrrow[:], lam_re[:])
    dma("sp", lirow[:], lam_im[:])
    dma("sp", dtrow[:], log_dt[:])
    act(dtrow[:], dtrow[:], AF.Exp)
    dtb = dtrow[:].un(2).bc([1, 32, 64])
    tt("dve", rhorow[:].re("o (g p) -> o g p", p=64), lrrow[:].re("o (g p) -> o g p", p=64), dtb, ALU.mult)
    tt("dve", throw[:].re("o (g p) -> o g p", p=64), lirow[:].re("o (g p) -> o g p", p=64), dtb, ALU.mult)

    wkA = sb("wkA", [128, 2048])
    wkB = sb("wkB", [128, 2048])
    wkI = sb("wkI", [128, 2048], I32)
    wkM = sb("wkM", [128, 2048])

    def outer_tok(row):
        for k in range(4):
            mm(P[k][:, :], jrow[0:1, :], row[0:1, k * 512:(k + 1) * 512])

    def outer_feat(row):
        for gp in range(16):
            mm(P[gp // 4][:, (gp % 4) * 128:(gp % 4 + 1) * 128], row[0:1, gp * 128:(gp + 1) * 128], jrow[0:1, :])

    def sin_of(dst, shift):
        for k in range(4):
            ts("dve", wkA[:, k * 512:(k + 1) * 512], P[k][:, :], 1.0 / TWO_PI, ALU.mult, 0.5 + shift, ALU.add)
        cp("dve", wkI[:], wkA[:])
        cp("dve", wkB[:], wkI[:])
        for k in range(4):
            stt(wkA[:, k * 512:(k + 1) * 512], wkB[:, k * 512:(k + 1) * 512], -TWO_PI, P[k][:, :],
                ALU.mult, ALU.add)
        if shift != 0.0:
            ts("dve", wkA[:], wkA[:], shift * TWO_PI, ALU.add)
        ts("dve", wkB[:], wkA[:], math.pi, ALU.is_gt)
        stt(wkA[:], wkB[:], -TWO_PI, wkA[:], ALU.mult, ALU.add)
        ts("dve", wkB[:], wkA[:], -math.pi, ALU.is_lt)
        stt(wkA[:], wkB[:], TWO_PI, wkA[:], ALU.mult, ALU.add)
        ts("dve", wkA[:], wkA[:], 3.14159, ALU.min, -3.14159, ALU.max)
        act(dst, wkA[:], AF.Sin)

    def build_table(Tre, Tim, outer_fn, sign):
        outer_fn(rhorow)
        for k in range(4):
            act(wkM[:, k * 512:(k + 1) * 512], P[k][:, :], AF.Exp, scale=float(sign))
        outer_fn(throw)
        sin_of(Tim[:], 0.0)
        sin_of(Tre[:], 0.25)
        tt("dve", Tre[:], Tre[:], wkM[:], ALU.mult)
        stt(Tim[:], Tim[:], float(sign), wkM[:], ALU.mult, ALU.mult)

    build_table(T1re, T1im, outer_tok, -1)
    build_table(T2re, T2im, outer_feat, +1)

    lrc = sb("lrc", [128, 16])
    lic = sb("lic", [128, 16])
    for dst, row in ((lrc, lrrow), (lic, lirow)):
        for gp in range(16):
            mm(P[4][:, 2 * gp:2 * gp + 2], row[0:1, gp * 128:(gp + 1) * 128], onesf[0:1, 0:2])
        cp("dve", dst[:], P[4][:, 0:32].re("p (q t) -> p q t", t=2)[:, :, 0])
    T2re3 = T2re[:].re("p (g j) -> p g j", j=128)
    T2im3 = T2im[:].re("p (g j) -> p g j", j=128)
    cp("dve", are[:], T2re3[:, :, 1])
    cp("dve", aim[:], T2im3[:, :, 1])
    fre = sb("fre", [128, 16])
    fim = sb("fim", [128, 16])
    nr, den, t0, t1, t2, t3 = [s[:] for s in s16]
    ts("dve", nr, are[:], -1.0, ALU.add)
    tt("dve", t0, lrc[:], lrc[:], ALU.mult)
    tt("dve", t1, lic[:], lic[:], ALU.mult)
    tt("dve", den, t0, t1, ALU.add)
    S.op("dve", lambda e: e.reciprocal(den.ap, den.ap), reads=[den.buf], writes=[den.buf])
    tt("dve", t0, nr, lrc[:], ALU.mult)
    tt("dve", t1, aim[:], lic[:], ALU.mult)
    tt("dve", t2, t0, t1, ALU.add)
    tt("dve", fre[:], t2, den, ALU.mult)
    tt("dve", t0, aim[:], lrc[:], ALU.mult)
    tt("dve", t1, nr, lic[:], ALU.mult)
    tt("dve", t2, t0, t1, ALU.subtract)
    tt("dve", fim[:], t2, den, ALU.mult)

    Bsr = sb("Bsr", [128, 16, 16])
    Bsi = sb("Bsi", [128, 16, 16])
    for dst, src in ((Bsr, b_re), (Bsi, b_im)):
        v = src[:].re("(gp g2) p c -> g2 p gp c", g2=2)
        for g2 in range(2):
            dma("sp", dst[g2 * 64:(g2 + 1) * 64, :, :], v[g2])
    Bbr = sb("Bbr", [128, 16, 16])
    Bbi = sb("Bbi", [128, 16, 16])
    tB0 = sb("tB0", [128, 16, 16])
    tB1 = sb("tB1", [128, 16, 16])
    freb = fre[:].un(2).bc([128, 16, 16])
    fimb = fim[:].un(2).bc([128, 16, 16])
    tt("dve", tB0[:], Bsr[:], freb, ALU.mult)
    tt("dve", tB1[:], Bsi[:], fimb, ALU.mult)
    tt("dve", Bbr[:], tB0[:], tB1[:], ALU.subtract)
    tt("dve", tB0[:], Bsi[:], freb, ALU.mult)
    tt("dve", tB1[:], Bsr[:], fimb, ALU.mult)
    tt("dve", Bbi[:], tB0[:], tB1[:], ALU.add)
    dbg("Bbr", Bbr[:], [128, 16, 16])
    Y2 = sb("Y2", [128, 2, 16, 2, 16])
    mBb = maskB[:].re("p (a c) -> p a c", a=2).un(1).bc([128, 16, 2, 16])
    for pl, Bb in ((0, Bbr), (1, Bbi)):
        tt("dve", Y2[:, pl], Bb[:].un(2).bc([128, 16, 2, 16]), mBb, ALU.mult)
    for pl in range(2):
        for Q in range(4):
            tr(P[5][:, Q * 128:(Q + 1) * 128], Y2[:, pl, 4 * Q:4 * Q + 4].re("p a b c -> p (a b c)"), ident[:])
        cp("dve", Bp[:, :, pl, :], P[5][:, :].re("p (q n) -> p q n", n=128))
        for lh in range(2):
            ts("dve", Bq[:, :, lh, pl, :], Bp[:, :, pl, :], maskQ[:, lh:lh + 1], ALU.mult)
    Csr = sb("Csr", [128, 4, 64])
    Csi = sb("Csi", [128, 4, 64])
    dma("sp", Csr[:], c_re[:].re("(q r) p -> r q p", r=128))
    dma("sp", Csi[:], c_im[:].re("(q r) p -> r q p", r=128))
    Xc = sb("Xc", [128, 2, 4, 2, 64])
    mCb = maskC[:].re("r (a p) -> r a p", a=2).un(1).bc([128, 4, 2, 64])
    tt("dve", Xc[:, 0], Csr[:].un(2).bc([128, 4, 2, 64]), mCb, ALU.mult)
    for Q in range(4):
        stt(Xc[:, 1, Q], Csi[:, Q].un(1).bc([128, 2, 64]), -1.0, maskC[:].re("r (a p) -> r a p", a=2),
            ALU.mult, ALU.mult)
    for pl in range(2):
        for Q in range(4):
            tr(P[5][:, Q * 128:(Q + 1) * 128], Xc[:, pl, Q].re("r a p -> r (a p)"), ident[:])
        cp("dve", Cp[:, :, pl, :], P[5][:, :].re("p (q n) -> p q n", n=128))
        for lh in range(2):
            tt("dve", Cq[:, :, pl, lh, :], Cp[:, :, pl, :],
               maskR[:, lh * 128:(lh + 1) * 128].un(1).bc([128, 4, 128]), ALU.mult)

    close_scope(scope0)
    Wkvu = sb("Wkvu", [128, 8, 1536], BF16)
    for kc in range(8):
        dma("pool", Wkvu[:, kc, :], w_in[kc * 128:(kc + 1) * 128, 512:2048])
    Wg = sb("Wg", [128, 4, 512], BF16)
    for kc in range(4):
        dma("pool", Wg[:, kc, :], glu_w[kc * 128:(kc + 1) * 128, :])

    xblk = [sb("xblk%d" % i, [128, D]) for i in range(2)]
    junk = sb("junk", [128, D])
    ssq = sb("ssq", [128, 1])
    rstd = sb("rstd", [128, 1])
    xn = sb("xn", [128, D])
    xnT = sb("xnT", [128, 8, 128], BF16)
    ktb = sb("ktb", [128, 4, 128], BF16)
    vb = sb("vb", [128, 512], BF16)
    uT = sb("uT", [128, 4, 128], BF16)
    zt = sb("zt", [128, 16, 2, 128], BF16)
    tz = [sb("tz%d" % i, [128, 8, 128]) for i in range(4)]
    cg = sb("cg", [128, 8, 2, 128])
    xT = sb("xT", [128, 16, 2, 128], BF16)
    Gre = sb("Gre", [128, 16])
    Gim = sb("Gim", [128, 16])
    xlast = sb("xlast", [128, 16, 2])
    yv = sb("yv", [128, 4, 128])
    y2 = sb("y2", [128, 4, 128])
    y3 = sb("y3", [128, 4, 128])
    yg = sb("yg", [128, 4, 128])
    ygb = sb("ygb", [128, 4, 128], BF16)
    sgl = sb("sgl", [128, 4, 128])
    ssmT = sb("ssmT", [128, 4, 128], BF16)
    S.op("dve", lambda e: e.memset(Gre[:].ap, 0.0), writes=[Gre.buf])
    S.op("dve", lambda e: e.memset(Gim[:].ap, 0.0), writes=[Gim.buf])
    T1re3 = T1re[:].re("p (g n) -> p g n", n=128)
    T1im3 = T1im[:].re("p (g n) -> p g n", n=128)

    def rmsnorm(xv, grep, outv):
        act(junk[:], xv, AF.Square, accum=ssq[:])
        act(rstd[:], ssq[:], AF.Ln, scale=1.0 / D, bias=epsc[:])
        act(rstd[:], rstd[:], AF.Exp, scale=-0.5)
        stt(outv, xv, rstd[:], grep[:], ALU.mult, ALU.mult)

    S.op("dve", lambda e: e.memset(epsc[:].ap, 1e-6), writes=[epsc.buf])

    def transpose8(src, dstT, col0, ncol_total):
        for half in range(2):
            for k in range(4):
                kc = half * 4 + k
                tr(P[6 + half][:, k * 128:(k + 1) * 128], src[:, kc * 128:(kc + 1) * 128], ident[:])
            cp("act" if half == 0 else "dve", dstT[:, half * 4:half * 4 + 4, col0:col0 + 128],
               P[6 + half][:, :].re("p (k n) -> p k n", n=128))

    for blk in range(NB):
        xb_ = xblk[blk % 2]
        dma("sp", xb_[:], x_all[blk * 128:(blk + 1) * 128, :])
        rmsnorm(xb_[:], grep_mix, xn[:])
        transpose8(xn, xnT, 0, 128)
        for m in range(4):
            for kc in range(8):
                mm(P[0][:, m * 128:(m + 1) * 128], Wkvu[:, kc, m * 128:(m + 1) * 128], xnT[:, kc, :],
                   start=(kc == 0), stop=(kc == 7))
        cp("act", ktb[:], P[0][:, :].re("p (m n) -> p m n", n=128))
        dma("sp", kt_scr[:].re("(m q) n -> q m n", q=128)[:, :, blk * 128:(blk + 1) * 128], ktb[:])
        for kc in range(8):
            mm(P[2][:, :], xnT[:, kc, :], Wkvu[:, kc, 512:1024], start=(kc == 0), stop=(kc == 7))
        cp("act", vb[:], P[2][:, :])
        dma("sp", v_scr[:].re("h j b d -> j h b d")[:, :, blk, :], vb[:].re("j (h d) -> j h d", d=64))
        for m in range(4):
            for kc in range(8):
                mm(P[1][:, m * 128:(m + 1) * 128], Wkvu[:, kc, 1024 + m * 128:1024 + (m + 1) * 128],
                   xnT[:, kc, :], start=(kc == 0), stop=(kc == 7))
        cp("act", uT[:], P[1][:, :].re("p (m n) -> p m n", n=128))
        if blk == 1:
            dbg("uT", uT[:], [128, 4, 128], BF16)
        for hf in range(2):
            for b4 in range(4):
                Q, h = hf * 2 + b4 // 2, b4 % 2
                mm(P[2 + b4][:, :], uT[64 * h:64 * h + 64, Q, :],
                   Bq[64 * h:64 * h + 64, Q, :, :, :].re("k l a n -> k (l a n)"))
            def buv(pl):
                vs = []
                for b4 in range(4):
                    vs.append(P[2 + b4][:, :].re("p (g a n) -> p g a n", a=2, n=128)[:, :, pl, :])
                return vs
            bre = buv(0)
            bim = buv(1)
            gs = slice(hf * 8, hf * 8 + 8)
            for b4 in range(4):
                g2s = slice(hf * 8 + 2 * b4, hf * 8 + 2 * b4 + 2)
                l2s = slice(2 * b4, 2 * b4 + 2)
                tt("dve", tz[0][:, l2s, :], bre[b4], T1re3[:, g2s, :], ALU.mult)
                tt("dve", tz[1][:, l2s, :], bim[b4], T1im3[:, g2s, :], ALU.mult)
                tt("dve", tz[2][:, l2s, :], bre[b4], T1im3[:, g2s, :], ALU.mult)
                tt("dve", tz[3][:, l2s, :], bim[b4], T1re3[:, g2s, :], ALU.mult)
            tt("pool", zt[:, gs, 0, :], tz[0][:], tz[1][:], ALU.subtract)
            tt("pool", zt[:, gs, 1, :], tz[2][:], tz[3][:], ALU.add)
        for hf in range(2):
            gs = slice(hf * 8, hf * 8 + 8)
            for g8 in range(8):
                gp = hf * 8 + g8
                for pl in range(2):
                    idx = g8 * 2 + pl
                    mm(P[2 + idx // 4][:, (idx % 4) * 128:(idx % 4 + 1) * 128], zt[:, gp, pl, :], trile[:])
            for b4 in range(4):
                g2s = slice(hf * 8 + 2 * b4, hf * 8 + 2 * b4 + 2)
                l2s = slice(2 * b4, 2 * b4 + 2)
                pv = P[2 + b4][:, :].re("p (g a n) -> p g a n", a=2, n=128)
                tt("dve", cg[:, l2s, 0, :], pv[:, :, 0, :], Gre[:, g2s].un(2).bc([128, 2, 128]), ALU.add)
                tt("dve", cg[:, l2s, 1, :], pv[:, :, 1, :], Gim[:, g2s].un(2).bc([128, 2, 128]), ALU.add)
            tt("dve", tz[0][:], cg[:, :, 0, :], T2re3[:, gs, :], ALU.mult)
            tt("pool", tz[1][:], cg[:, :, 1, :], T2im3[:, gs, :], ALU.mult)
            tt("dve", tz[2][:], cg[:, :, 1, :], T2re3[:, gs, :], ALU.mult)
            tt("pool", tz[3][:], cg[:, :, 0, :], T2im3[:, gs, :], ALU.mult)
            tt("dve", xT[:, gs, 0, :], tz[0][:], tz[1][:], ALU.subtract)
            tt("pool", xT[:, gs, 1, :], tz[2][:], tz[3][:], ALU.add)
            tt("dve", xlast[:, gs, 0], tz[0][:, :, 127], tz[1][:, :, 127], ALU.subtract)
            tt("dve", xlast[:, gs, 1], tz[2][:, :, 127], tz[3][:, :, 127], ALU.add)
        tt("dve", s16[0][:], xlast[:, :, 0], are[:], ALU.mult)
        tt("dve", s16[1][:], xlast[:, :, 1], aim[:], ALU.mult)
        tt("dve", s16[2][:], xlast[:, :, 0], aim[:], ALU.mult)
        tt("dve", s16[3][:], xlast[:, :, 1], are[:], ALU.mult)
        tt("dve", Gre[:], s16[0][:], s16[1][:], ALU.subtract)
        tt("dve", Gim[:], s16[2][:], s16[3][:], ALU.add)
        for Q in range(4):
            for h in range(2):
                i = 0
                for lh in range(2):
                    gp = 4 * Q + 2 * h + lh
                    for pl in range(2):
                        mm(P[1][64 * h:64 * h + 64, Q * 128:(Q + 1) * 128], Cq[:, Q, pl, lh, 64 * h:64 * h + 64],
                           xT[:, gp, pl, :], start=(i == 0), stop=(i == 3))
                        i += 1
        for Q in range(4):
            stt(yv[:, Q, :], uT[:, Q, :], dcol[:, Q:Q + 1], P[1][:, Q * 128:(Q + 1) * 128], ALU.mult, ALU.add)
        if blk == 1:
            dbg("yv", yv[:], [128, 4, 128])
        tt("pool", y2[:], yv[:], yv[:], ALU.mult)
        ts("dve", y2[:], y2[:], 0.044715, ALU.mult, 1.0, ALU.add)
        tt("pool", y3[:], y2[:], yv[:], ALU.mult)
        act(y3[:], y3[:], AF.Sigmoid, scale=1.5957691216057308)
        tt("dve", yg[:], yv[:], y3[:], ALU.mult)
        cp("pool", ygb[:], yg[:])
        for co in range(4):
            for kc in range(4):
                mm(P[0][:, co * 128:(co + 1) * 128], Wg[:, kc, co * 128:(co + 1) * 128], ygb[:, kc, :],
                   start=(kc == 0), stop=(kc == 3))
        for co in range(4):
            act(sgl[:, co, :], P[0][:, co * 128:(co + 1) * 128], AF.Sigmoid, bias=gbcol[:, co:co + 1])
        tt("dve", ssmT[:], yg[:], sgl[:], ALU.mult)
        dma("sp", ssm_scr[:].re("q p n -> p q n")[:, :, blk * 128:(blk + 1) * 128], ssmT[:])

    close_scope(scopeA)
    if nphase < 2:
        while len(cur) > 1:
            cur[0].close()
            cur.pop(0)
        return finish()
    scope2 = open_scope()
    mask0 = sb("mask0", [128, 512], BF16)
    maske = sb("maske", [128, 8, 512], BF16)
    masko = sb("masko", [128, 8, 512], BF16)
    dma("sp", mask0[:], k_mask0[:])
    dma("sp", maske[:], k_maske[:])
    dma("sp", masko[:], k_masko[:])
    Wr = sb("Wr", [128, 8, 36])
    dma("sp", Wr[:], w_r[:].re("(kc p) n -> p kc n", p=128))
    Wpa = sb("Wpa", [64, 8, D], BF16)
    Wpb = sb("Wpb", [128, 4, D], BF16)
    Wo = sb("Wo", [128, 8, D], BF16)
    for h in range(8):
        dma("pool", Wpa[:, h, :], w_pa[h * 64:(h + 1) * 64, :])
    for kc in range(4):
        dma("pool", Wpb[:, kc, :], w_pb[kc * 128:(kc + 1) * 128, :])
    for kc in range(8):
        dma("pool", Wo[:, kc, :], w_o[kc * 128:(kc + 1) * 128, :])
    wring = [sb("wring%d" % i, [128, 8, 512], BF16) for i in range(2)]
    wri = [0]

    def load_wchunk(c0):
        t = wring[wri[0] % 2]
        wri[0] += 1
        for kc in range(8):
            dma("pool", t[:, kc, :], w_in[kc * 128:(kc + 1) * 128, c0:c0 + 512])
        return t

    xo = [sb("xo%d" % i, [128, D]) for i in range(2)]
    xn2 = sb("xn2", [128, D])
    junk2 = sb("junk2", [128, D], BF16)
    ssq2 = sb("ssq2", [128, 1])
    rstd2 = sb("rstd2", [128, 1])
    xoT = sb("xoT", [128, 8, 512], BF16)
    qT = sb("qT", [64, 8, 512], BF16)
    ktc = [[sb("ktc%d_%d" % (a, i), [64, 1024], BF16) for i in range(2)] for a in range(2)]
    vtc = [[sb("vtc%d_%d" % (a, i), [128, 8, 64], BF16) for i in range(2)] for a in range(2)]
    erw = [sb("erw%d" % i, [128, 1024], BF16) for i in range(2)]
    sprw = [sb("sprw%d" % i, [128, 1024], BF16) for i in range(2)]
    trw = sb("trw", [128, 1024], BF16)
    wrw = [sb("wrw%d" % i, [128, 1024], BF16) for i in range(2)]
    attnT = sb("attnT", [64, 8, 512], BF16)
    ssA = sb("ssA", [128, 4, 512], BF16)
    ssB = sb("ssB", [128, 4, 512], BF16)
    ssO = ssA
    sg1 = sb("sg1", [128, 512], BF16)
    sg2 = sb("sg2", [128, 512], BF16)
    mg1 = sb("mg1", [128, 512])
    mg2 = sb("mg2", [128, 512])
    mergedT = sb("mergedT", [128, 8, 512], BF16)
    hblk = [sb("hblk0", [128, D])] * 2
    hn = [xn2, xn2]
    hnT = sb("hnT", [128, 8, 128])
    lgt = sb("lgt", [128, 36])
    r1 = [sb("r1_%d" % i, [128, 1]) for i in range(12)]
    ohg = sb("ohg", [128, 4])
    eg = sb("eg", [128, 4])
    selv = sb("selv", [128, 8])
    m8 = sb("m8", [128, 8])
    oh1 = sb("oh1", [128, 8])
    oh2 = sb("oh2", [128, 8])
    M12 = sb("M12", [128, 64])
    pm = sb("pm", [128, 64])
    pmt = sb("pmt", [128, 64])
    base = sb("base", [128, 32])
    destf = sb("destf", [128, 2])
    S.op("dve", lambda e: e.memset(base[:].ap, 0.0), writes=[base.buf])

    def rmsnorm2(xv, grep, outv):
        act(junk2[:], xv, AF.Square, accum=ssq2[:])
        act(rstd2[:], ssq2[:], AF.Ln, scale=1.0 / D, bias=epsc[:])
        act(rstd2[:], rstd2[:], AF.Exp, scale=-0.5)
        stt(outv, xv, rstd2[:], grep[:], ALU.mult, ALU.mult)

    def mmacc(out, lhsT, rhs, start):
        S.op("pe", lambda e: e.matmul(out.ap, lhsT.ap, rhs.ap, start=start, stop=True, skip_group_check=True),
             reads=[lhsT.buf, rhs.buf], writes=[out.buf])

    for n in range(NSLOT):
        pmask = maske if n % 2 == 0 else masko
        for r in range(4):
            xb_ = xo[r % 2]
            dma("sp", xb_[:], x_own[(4 * n + r) * 128:(4 * n + r + 1) * 128, :])
            rmsnorm2(xb_[:], grep_mix, xn2[:])
            transpose8(xn2, xoT, r * 128, 512)
        wq = load_wchunk(0)
        for h in range(8):
            pb = P[4 + h % 4]
            for kc in range(8):
                mm(pb[0:64, :], wq[:, kc, h * 64:(h + 1) * 64], xoT[:, kc, :], start=(kc == 0), stop=(kc == 7))
            cp("act" if h % 2 == 0 else "dve", qT[:, h, :], pb[0:64, :])
        if n == 0:
            dbg("qT", qT[:], [64, 8, 512], BF16)
        nkb = 8 * n + 9
        kbs = [8 * n + 8 - s for s in range(nkb)]
        for hp in range(4):
            heads = (2 * hp, 2 * hp + 1)
            OB = (P[2], P[3])
            ZW = (P45, P67)
            cur_chunk = [None, None]
            cur_kt = [None, None]
            cur_vt = [None, None]
            nload = [0, 0]

            def get_kv(a, kb):
                h = heads[a]
                c = -1 if kb == 0 else (kb - 1) // 8
                if cur_chunk[a] != c:
                    i = nload[a] % 2
                    nload[a] += 1
                    kt_, vt_ = ktc[a][i], vtc[a][i]
                    if c < 0:
                        dma("sp", kt_[:, 0:128], kt_scr[h * 64:(h + 1) * 64, 0:128])
                        dma("sp", vt_[:, 0:1, :], v_scr[h, :, 0:1, :])
                    else:
                        dma("sp", kt_[:, :], kt_scr[h * 64:(h + 1) * 64, (8 * c + 1) * 128:(8 * c + 9) * 128])
                        dma("sp", vt_[:, :, :], v_scr[h, :, 8 * c + 1:8 * c + 9, :])
                    cur_chunk[a], cur_kt[a], cur_vt[a] = c, kt_, vt_
                lb = 0 if kb == 0 else (kb - 1) % 8
                return cur_kt[a][:, lb * 128:(lb + 1) * 128], cur_vt[a][:, lb, :]

            vsel = {}

            def front(s):
                kb = kbs[s]
                Z = ZW[s % 2]
                for a in range(2):
                    ktv, vtv = get_kv(a, kb)
                    vsel[(a, s)] = vtv
                    mm(Z[:, a * 512:(a + 1) * 512], ktv, qT[:, heads[a], :])
                e_ = erw[s % 2]
                act(e_[:], Z[:, :], AF.Exp, scale=0.125)
                e3 = e_[:].re("p (a n) -> p a n", a=2)
                if s < 8:
                    tt("dve", e3, e3, pmask[:, s, :].un(1).bc([128, 2, 512]), ALU.mult)
                elif kb == 0:
                    tt("dve", e3, e3, mask0[:].un(1).bc([128, 2, 512]), ALU.mult)
                act(sprw[s % 2][:], e_[:], AF.Ln, bias=onec[:])

            front(0)
            for s in range(nkb):
                sp_ = sprw[s % 2]
                for a in range(2):
                    mmacc(P01[:, a * 512:(a + 1) * 512], trige[:], sp_[:, a * 512:(a + 1) * 512], start=(s == 0))
                act(trw[:], P01[:, :], AF.Exp, scale=-1.0)
                if s + 1 < nkb:
                    front(s + 1)
                for a in range(2):
                    mmacc(P01[:, a * 512:(a + 1) * 512], trilt[:], sp_[:, a * 512:(a + 1) * 512], start=False)
                w_ = wrw[s % 2]
                tt("dve", w_[:], erw[s % 2][:], trw[:], ALU.mult)
                for a in range(2):
                    mmacc(OB[a][0:64, :], vsel[(a, s)], w_[:, a * 512:(a + 1) * 512], start=(s == 0))
            for a in range(2):
                cp("act" if a == 0 else "dve", attnT[:, heads[a], :], OB[a][0:64, :])
        if n == 0:
            dbg("attnT", attnT[:], [64, 8, 512], BF16)
        posA = (8 * n + 1) * 128
        posB = (8 * n + 5) * 128
        sv = ssm_scr[:].re("q p n -> p q n")
        dma("sp", ssA[:], sv[:, :, posA:posA + 512])
        dma("sp", ssB[:], sv[:, :, posB:posB + 512])
        ts("dve", ssO[:], ssA[:], sel[:, 2 * n:2 * n + 1], ALU.mult)
        stt(ssO[:], ssB[:], sel[:, 2 * n + 1:2 * n + 2], ssO[:], ALU.mult, ALU.add)
        for c2 in range(2):
            wga = load_wchunk(2048 + c2 * 512)
            wgb = load_wchunk(3072 + c2 * 512)
            for c4 in range(4):
                co = c2 * 4 + c4
                for kc in range(8):
                    mm(P[4][:, :], wga[:, kc, c4 * 128:(c4 + 1) * 128], xoT[:, kc, :], start=(kc == 0), stop=(kc == 7))
                for kc in range(8):
                    mm(P[5][:, :], wgb[:, kc, c4 * 128:(c4 + 1) * 128], xoT[:, kc, :], start=(kc == 0), stop=(kc == 7))
                for h in range(8):
                    mm(P[6][:, :], Wpa[:, h, co * 128:(co + 1) * 128], attnT[:, h, :], start=(h == 0), stop=(h == 7))
                for kc in range(4):
                    mm(P[7][:, :], Wpb[:, kc, co * 128:(co + 1) * 128], ssO[:, kc, :], start=(kc == 0), stop=(kc == 3))
                act(sg1[:], P[4][:, :], AF.Sigmoid)
                act(sg2[:], P[5][:, :], AF.Sigmoid)
                tt("dve", mg1[:], P[6][:, :], sg1[:], ALU.mult)
                tt("dve", mg2[:], P[7][:, :], sg2[:], ALU.mult)
                tt("pool", mergedT[:, co, :], mg1[:], mg2[:], ALU.add)
        if n == 0:
            dbg("mergedT", mergedT[:], [128, 8, 512], BF16)
        for r in range(4):
            ob = 4 * n + r
            xb_ = xo[r % 2]
            hb_ = hblk[r % 2]
            hn_ = hn[r % 2]
            dma("sp", xb_[:], x_own[ob * 128:(ob + 1) * 128, :])
            for half in range(2):
                for kc in range(8):
                    mm(P[half][:, :], mergedT[:, kc, r * 128:(r + 1) * 128], Wo[:, kc, half * 512:(half + 1) * 512],
                       start=(kc == 0), stop=(kc == 7))
                tt("dve", hb_[:, half * 512:(half + 1) * 512], P[half][:, :], xb_[:, half * 512:(half + 1) * 512],
                   ALU.add)
            dma("sp", h_scr[ob * 128:(ob + 1) * 128, :], hb_[:])
            rmsnorm2(hb_[:], grep_ffn, hn_[:])
            for half in range(2):
                for k in range(4):
                    kc = half * 4 + k
                    tr(P[6 + half][:, k * 128:(k + 1) * 128], hn_[:, kc * 128:(kc + 1) * 128], ident[:])
                cp("act" if half == 0 else "dve", hnT[:, half * 4:half * 4 + 4, :],
                   P[6 + half][:, :].re("p (k n) -> p k n", n=128))
            for kc in range(8):
                mm(P[2][:, 0:36], hnT[:, kc, :], Wr[:, kc, :], start=(kc == 0), stop=(kc == 7))
            tt("dve", lgt[:], P[2][:, 0:36], brrep[:], ALU.add)
            gmax, ngmax, gsum, ptop, dv, ex, g1, g2, p1loc, p2loc, ov1, ov2 = [t_[:] for t_ in r1]
            S.op("dve", lambda e: e.tensor_reduce(gmax.ap, lgt[:, 0:4].ap, AX.X, ALU.max),
                 reads=[lgt.buf], writes=[gmax.buf])
            ts("dve", ohg[:], lgt[:, 0:4], gmax, ALU.is_equal)
            ts("dve", ngmax, gmax, -1.0, ALU.mult)
            act(eg[:], lgt[:, 0:4], AF.Exp, bias=ngmax, accum=gsum)
            S.op("dve", lambda e: e.reciprocal(ptop.ap, gsum.ap), reads=[gsum.buf], writes=[ptop.buf])
            ts("dve", selv[:], lgt[:, 4:12], ohg[:, 0:1], ALU.mult)
            for g in range(1, 4):
                stt(selv[:], lgt[:, 4 + 8 * g:12 + 8 * g], ohg[:, g:g + 1], selv[:], ALU.mult, ALU.add)
            S.op("dve", lambda e: e.max(m8[:].ap, selv[:].ap), reads=[selv.buf], writes=[m8.buf])
            ts("dve", oh1[:], selv[:], m8[:, 0:1], ALU.is_equal)
            ts("dve", oh2[:], selv[:], m8[:, 1:2], ALU.is_equal)
            tt("dve", dv, m8[:, 1:2], m8[:, 0:1], ALU.subtract)
            act(ex, dv, AF.Exp)
            ts("dve", g1, ex, 1.0, ALU.add)
            S.op("dve", lambda e: e.reciprocal(g1.ap, g1.ap), reads=[g1.buf], writes=[g1.buf])
            tt("dve", g2, ex, g1, ALU.mult)
            tt("dve", gwall[:, ob, 0:1], g1, ptop, ALU.mult)
            tt("dve", gwall[:, ob, 1:2], g2, ptop, ALU.mult)
            tt("dve", M12[:, 0:32].re("p (g e) -> p g e", e=8), ohg[:].un(2).bc([128, 4, 8]),
               oh1[:].un(1).bc([128, 4, 8]), ALU.mult)
            tt("dve", M12[:, 32:64].re("p (g e) -> p g e", e=8), ohg[:].un(2).bc([128, 4, 8]),
               oh2[:].un(1).bc([128, 4, 8]), ALU.mult)
            mm(P[3][:, 0:64], triltf[:], M12[:])
            mm(P[3][:, 64:128], onesf[:], M12[:])
            tt("dve", pm[:, 0:32], P[3][:, 0:32], base[:], ALU.add)
            tt("dve", pm[:, 32:64], P[3][:, 32:64], base[:], ALU.add)
            tt("dve", pm[:, 32:64], P[3][:, 64:96], pm[:, 32:64], ALU.add)
            tt("dve", pmt[:], pm[:], M12[:], ALU.mult)
            S.op("dve", lambda e: e.tensor_reduce(p1loc.ap, pmt[:, 0:32].ap, AX.X, ALU.add),
                 reads=[pmt.buf], writes=[p1loc.buf])
            S.op("dve", lambda e: e.tensor_reduce(p2loc.ap, pmt[:, 32:64].ap, AX.X, ALU.add),
                 reads=[pmt.buf], writes=[p2loc.buf])
            tt("dve", pmt[:, 0:32], M12[:, 0:32], ecap[:], ALU.mult)
            tt("dve", pmt[:, 32:64], M12[:, 32:64], ecap[:], ALU.mult)
            S.op("dve", lambda e: e.tensor_reduce(ov1.ap, pmt[:, 0:32].ap, AX.X, ALU.add),
                 reads=[pmt.buf], writes=[ov1.buf])
            S.op("dve", lambda e: e.tensor_reduce(ov2.ap, pmt[:, 32:64].ap, AX.X, ALU.add),
                 reads=[pmt.buf], writes=[ov2.buf])
            tt("dve", destf[:, 0:1], ov1, p1loc, ALU.add)
            tt("dve", destf[:, 1:2], ov2, p2loc, ALU.add)
            ts("dve", ov1, p1loc, float(CAP) - 0.5, ALU.is_gt, float(4 * NXB), ALU.mult)
            ts("dve", ov2, p2loc, float(CAP) - 0.5, ALU.is_gt, float(4 * NXB), ALU.mult)
            tt("dve", destf[:, 0:1], destf[:, 0:1], ov1, ALU.add)
            tt("dve", destf[:, 1:2], destf[:, 1:2], ov2, ALU.add)
            cp("dve", destall[:, ob, :], destf[:])
            tt("dve", base[:], P[3][:, 64:96], base[:], ALU.add)
            tt("dve", base[:], P[3][:, 96:128], base[:], ALU.add)
            for k in range(2 if "noscat" not in debug else 0):
                dv_ = destall[:, ob, k:k + 1]
                S.dma("pool", (lambda dv_, hn_: (lambda e: e.indirect_dma_start(
                    out=xb_scr[:].ap, out_offset=bass.IndirectOffsetOnAxis(ap=dv_.ap, axis=0),
                    in_=hn_[:].ap, in_offset=None, bounds_check=bc_reg(e), oob_is_err=False)))(dv_, hn_),
                    reads=[hn_.buf, destall.buf], writes=[xb_scr.buf])
    dbg("destall", destall[:], [128, NOB, 2], I32)
    dbg("gwall", gwall[:], [128, NOB, 2])
    close_scope(scope2)
    if nphase < 3 or not moe:
        return finish()
    if nphase >= 3 and moe:
        scope3 = open_scope()
        W1 = [sb("W1_%d" % i, [128, 8, 512], BF16) for i in range(2)]
        W3 = [sb("W3_%d" % i, [128, 8, 512], BF16) for i in range(2)]
        W2 = [sb("W2_%d" % i, [128, 4, D], BF16) for i in range(2)]
        stg1 = sb("stg1", [128, 8, 512])
        stg3 = sb("stg3", [128, 8, 512])
        stg2 = sb("stg2", [128, 4, D])
        xg = [sb("xg%d" % i, [128, D]) for i in range(2)]
        xeT = sb("xeT", [128, 8, CAP], BF16)
        s1 = sb("s1", [128, CAP])
        hgT = sb("hgT", [128, 4, CAP], BF16)
        ybk = [sb("ybk%d" % i, [128, D]) for i in range(2)]

        def load_expert(ex_):
            dma("sp", stg1[:], w1[ex_].re("(p kc) f -> p kc f", p=128))
            dma("sp", stg3[:], w3[ex_].re("(p kc) f -> p kc f", p=128))
            dma("sp", stg2[:], w2[ex_].re("(p fc) n -> p fc n", p=128))

        def cast_expert(ex_):
            i = ex_ % 2
            cp("act", W1[i][:, 0:4, :], stg1[:, 0:4, :])
            cp("dve", W1[i][:, 4:8, :], stg1[:, 4:8, :])
            cp("act", W3[i][:, 0:4, :], stg3[:, 0:4, :])
            cp("dve", W3[i][:, 4:8, :], stg3[:, 4:8, :])
            cp("act", W2[i][:, 0:2, :], stg2[:, 0:2, :])
            cp("dve", W2[i][:, 2:4, :], stg2[:, 2:4, :])

        load_expert(0)
        cast_expert(0)
        for ex_ in range(32):
            w1_, w3_, w2_ = W1[ex_ % 2], W3[ex_ % 2], W2[ex_ % 2]
            if ex_ + 1 < 32:
                load_expert(ex_ + 1)
            for c in range(CAPB):
                xg_ = xg[c % 2]
                dma("sp", xg_[:], xb_scr[ex_ * CAP + c * 128:ex_ * CAP + (c + 1) * 128, :])
                xg3 = xg_[:].re("t (p k) -> t k p", k=8)
                for half in range(2):
                    for k in range(4):
                        kc = half * 4 + k
                        tr(P[6 + half][:, k * 128:(k + 1) * 128], xg3[:, kc, :], ident[:])
                    cp("act" if half == 0 else "dve", xeT[:, half * 4:half * 4 + 4, c * 128:(c + 1) * 128],
                       P[6 + half][:, :].re("p (k n) -> p k n", n=128))
            for fc in range(4):
                for kc in range(8):
                    mm(P[0][:, 0:CAP], w1_[:, kc, :].re("p (m f) -> p f m", f=4)[:, fc, :], xeT[:, kc, :],
                       start=(kc == 0), stop=(kc == 7))
                for kc in range(8):
                    mm(P[1][:, 0:CAP], w3_[:, kc, :].re("p (m f) -> p f m", f=4)[:, fc, :], xeT[:, kc, :],
                       start=(kc == 0), stop=(kc == 7))
                act(s1[:], P[0][:, 0:CAP], AF.Silu)
                tt("dve", hgT[:, fc, :], s1[:], P[1][:, 0:CAP], ALU.mult)
            for c in range(CAPB):
                yb_ = ybk[c % 2]
                for half in range(2):
                    for fc in range(4):
                        mm(P[2 + half][:, :], hgT[:, fc, c * 128:(c + 1) * 128], w2_[:, fc, half * 512:(half + 1) * 512],
                           start=(fc == 0), stop=(fc == 3))
                    cp("act" if half == 0 else "dve", yb_[:, half * 512:(half + 1) * 512], P[2 + half][:, :])
                dma("sp", yb_scr[ex_ * CAP + c * 128:ex_ * CAP + (c + 1) * 128, :], yb_[:])
            if ex_ + 1 < 32:
                cast_expert(ex_ + 1)
        if nphase < 4:
            close_scope(scope3)
            return finish()
        yk = [sb("yk%d" % i, [128, D]) for i in range(2)]
        hb4 = [sb("hb4_%d" % i, [128, D]) for i in range(2)]
        oo = [sb("oo%d" % i, [128, D]) for i in range(2)]
        junk4 = sb("junk4", [128, D], BF16)
        ssq4 = sb("ssq4", [128, 1])
        rstd4 = sb("rstd4", [128, 1])
        for ob in range(NOB):
            hb_ = hb4[ob % 2]
            dma("sp", hb_[:], h_scr[ob * 128:(ob + 1) * 128, :])
            for k in range(2):
                yk_ = yk[k]
                S.op("pool", (lambda yk_: (lambda e: e.memset(yk_[:].ap, 0.0)))(yk_), writes=[yk_.buf])
                dv_ = destall[:, ob, k:k + 1]
                S.dma("pool", (lambda dv_, yk_: (lambda e: e.indirect_dma_start(
                    out=yk_[:].ap, out_offset=None, in_=yb_scr[:].ap,
                    in_offset=bass.IndirectOffsetOnAxis(ap=dv_.ap, axis=0),
                    bounds_check=bc_reg(e), oob_is_err=False)))(dv_, yk_),
                    reads=[yb_scr.buf, destall.buf], writes=[yk_.buf])
                stt(hb_[:], yk_[:], gwall[:, ob, k:k + 1], hb_[:], ALU.mult, ALU.add)
            oo_ = oo[ob % 2]
            act(junk4[:], hb_[:], AF.Square, accum=ssq4[:])
            act(rstd4[:], ssq4[:], AF.Ln, scale=1.0 / D, bias=epsc[:])
            act(rstd4[:], rstd4[:], AF.Exp, scale=-0.5)
            stt(oo_[:], hb_[:], rstd4[:], grep_fin[:], ALU.mult, ALU.mult)
            dma("sp", out_own[ob * 128:(ob + 1) * 128, :], oo_[:])
        close_scope(scope3)

    return finish()


def _consts(NSLOT, CAP):
    bf = ml_dtypes.bfloat16
    r = np.arange(128)[:, None]
    c = np.arange(128)[None, :]
    k = {}
    k["k_ident"] = (r == c).astype(np.float32)
    k["k_triltf"] = (r < c).astype(np.float32)
    k["k_onesf"] = np.ones((128, 128), np.float32)
    k["k_jrow"] = np.arange(128, dtype=np.float32)[None, :]
    mB = np.zeros((128, 2, 16), np.float32)
    for g2 in range(2):
        mB[g2 * 64:(g2 + 1) * 64, g2, :] = 1.0
    k["k_maskB"] = mB.reshape(128, 32)
    mC = np.zeros((128, 2, 64), np.float32)
    for rr in range(128):
        mC[rr, (rr // 16) % 2, :] = 1.0
    k["k_maskC"] = mC.reshape(128, 128)
    mQ = np.zeros((128, 2), np.float32)
    for rr in range(128):
        mQ[rr, (rr // 32) % 2] = 1.0
    k["k_maskQ"] = mQ
    mR = np.zeros((128, 2, 128), np.float32)
    for cc in range(128):
        mR[:, (cc // 32) % 2, cc] = 1.0
    k["k_maskR"] = mR.reshape(128, 256)
    k["k_ecap"] = np.tile((np.arange(32, dtype=np.float32) * CAP)[None, :], (128, 1))
    k["k_trige"] = (r >= c).astype(bf)
    k["k_trilt"] = (r < c).astype(bf)
    k["k_trile"] = (r <= c).astype(bf)
    m0 = np.ones((128, 512), np.float32)
    m0[:112, :] = 0.0
    k["k_mask0"] = m0.astype(bf)
    j = np.arange(128)[:, None]
    t = np.arange(512)[None, :]
    patA = np.zeros((128, 8, 512), np.float32)
    patB = np.zeros((128, 8, 512), np.float32)
    for s in range(8):
        if s < 4:
            rr = 3 - s
            patB[:, s, :] = (rr * 128 + j < t)
        else:
            rr = 7 - s
            patA[:, s, :] = (rr * 128 + j < t)
            patB[:, s, :] = 1.0
    return k, patA.astype(bf), patB.astype(bf)


def own_is_A(n, hf):
    return ((n + hf) % 2) == 0


def make_in_maps(inputs, NSLOT, CAP, n_cores=8, moe=True):
    x = np.asarray(inputs["x"], np.float32)
    NB = 1 + 8 * NSLOT
    NPOS = NB * 128
    meta = np.asarray(inputs["meta_tokens"], np.float32)
    k, patA, patB = _consts(NSLOT, CAP)
    f = lambda a: np.ascontiguousarray(np.asarray(a, np.float32))
    shared = dict(
        w_in=f(inputs["w_in"][0]), g_mix=f(inputs["norm_mix_g"][0:1]), g_ffn=f(inputs["norm_ffn_g"][0:1]),
        g_fin=f(inputs["norm_final_g"]).reshape(1, D),
        lam_re=f(inputs["ssm_lambda_re"][0]).reshape(1, 2048), lam_im=f(inputs["ssm_lambda_im"][0]).reshape(1, 2048),
        log_dt=f(inputs["ssm_log_dt"][0]).reshape(1, 32),
        b_re=f(inputs["ssm_b_re"][0]), b_im=f(inputs["ssm_b_im"][0]),
        c_re=f(inputs["ssm_c_re"][0]).reshape(512, 64), c_im=f(inputs["ssm_c_im"][0]).reshape(512, 64),
        d_skip=f(inputs["ssm_d"][0]).reshape(1, 512), glu_w=f(inputs["ssm_glu_w"][0]),
        glu_b=f(inputs["ssm_glu_b"][0]).reshape(1, 512),
        w_pa=f(inputs["w_branch_attn"][0]), w_pb=f(inputs["w_branch_ssm"][0]), w_o=f(inputs["w_out"][0]),
        w_r=f(np.concatenate([inputs["router_group_w"][0], inputs["router_expert_w"][0]], axis=1)),
        b_r=f(np.concatenate([inputs["router_group_b"][0], inputs["router_expert_b"][0]], axis=0)).reshape(1, 36),
        w1=f(inputs["expert_w1"][0][:(32 if moe else 1)]), w3=f(inputs["expert_w3"][0][:(32 if moe else 1)]),
        w2=f(inputs["expert_w2"][0][:(32 if moe else 1)]),
    )
    shared.update(k)
    maps = []
    for core in range(n_cores):
        b, hf = core // 2, core % 2
        xa = np.zeros((NPOS, D), np.float32)
        xa[112:128] = meta
        xa[128:] = x[b, :NPOS - 128]
        xo = np.zeros((512 * NSLOT, D), np.float32)
        sel = np.zeros((128, 2 * NSLOT), np.float32)
        for n in range(NSLOT):
            a = own_is_A(n, hf)
            r0 = (8 * n + (0 if a else 4)) * 128
            xo[n * 512:(n + 1) * 512] = x[b, r0:r0 + 512]
            sel[:, 2 * n] = 1.0 if a else 0.0
            sel[:, 2 * n + 1] = 0.0 if a else 1.0
        m = dict(shared)
        m["x_all"] = xa
        m["x_own"] = xo
        m["k_sel"] = sel
        m["k_maske"] = patA if own_is_A(0, hf) else patB
        m["k_masko"] = patA if own_is_A(1, hf) else patB
        maps.append(m)
    return maps


def assemble(results, NSLOT, B, SEQ):
    out = np.zeros((B, SEQ, D), np.float32)
    for core, r in enumerate(results):
        b, hf = core // 2, core % 2
        oo = np.asarray(r["out_own"], np.float32)
        for n in range(NSLOT):
            a = own_is_A(n, hf)
            r0 = (8 * n + (0 if a else 4)) * 128
            out[b, r0:r0 + 512] = oo[n * 512:(n + 1) * 512]
    return out


_NC_CACHE = {}


def kernel(**inputs):
    NSLOT, CAPB = 8, 3
    key = (NSLOT, CAPB)
    if key not in _NC_CACHE:
        _NC_CACHE[key] = build(NSLOT, CAPB)
    nc = _NC_CACHE[key]
    maps = make_in_maps(inputs, NSLOT, 128 * CAPB)
    res = run_bass_kernel_spmd(nc, maps, core_ids=list(range(8)))
    return assemble(res.results, NSLOT, 4, 8192)
```

```python
import math
from contextlib import ExitStack
import numpy as np
import ml_dtypes
import concourse.bass as bass
import concourse.mybir as mybir
from concourse.bass_utils import run_bass_kernel_spmd

F32 = mybir.dt.float32
BF16 = mybir.dt.bfloat16
I32 = mybir.dt.int32
AF = mybir.ActivationFunctionType
ALU = mybir.AluOpType
AX = mybir.AxisListType

SAME_ENGINE_SYNC = True
NDS = 20
D = 1024
TWO_PI = 2.0 * math.pi


class Buf:
    def __init__(self, name):
        self.name = name
        self.w = None
        self.r = {}


class View:
    def __init__(self, ap, buf):
        self.ap = ap
        self.buf = buf

    def __getitem__(self, k):
        return View(self.ap[k], self.buf)

    def re(self, pat, **kw):
        return View(self.ap.rearrange(pat, **kw), self.buf)

    def bc(self, shape):
        return View(self.ap.broadcast_to(list(shape)), self.buf)

    def un(self, axis):
        return View(self.ap.unsqueeze(axis), self.buf)


class T:
    def __init__(self, h, name):
        self.h = h
        self.buf = Buf(name)

    def __getitem__(self, k):
        return View(self.h[k], self.buf)


class Sched:
    ENG = ["pe", "act", "dve", "pool", "sp"]

    def __init__(self, nc, stack):
        self.nc = nc
        self.stack = stack
        self.epoch = {e: 0 for e in self.ENG}
        self.last_tok = {e: None for e in self.ENG}
        self.sem = {e: stack.enter_context(nc.semaphore("s_" + e)) for e in self.ENG}
        self.cnt = {e: 0 for e in self.ENG}
        self.ops = {e: [] for e in self.ENG}
        self.waited = {e: {} for e in self.ENG}
        self.dsem = {q: [stack.enter_context(nc.semaphore("d_%s%d" % (q, i))) for i in range(NDS)]
                     for q in ("sp", "pool")}
        self.dcnt = {q: [0] * NDS for q in ("sp", "pool")}
        self.drr = {"sp": 0, "pool": 0}

    @staticmethod
    def _flat(bufs):
        out = []
        for b in bufs:
            if isinstance(b, (tuple, list)):
                out.extend(b)
            else:
                out.append(b)
        return out

    def _deps(self, reads, writes):
        reads = self._flat(reads)
        writes = self._flat(writes)
        deps = []
        for b in reads:
            if b.w is not None:
                deps.append(b.w)
        for b in writes:
            if b.w is not None:
                deps.append(b.w)
            deps.extend(b.r.values())
        return deps

    def _waits(self, e, deps):
        need = {}
        for (key, sem, val) in deps:
            if val <= 0:
                continue
            if key[0] == "E" and key[1] == e and (e == "pe" or not SAME_ENGINE_SYNC):
                continue
            if self.waited[e].get(key, 0) >= val:
                continue
            if key not in need or need[key][1] < val:
                need[key] = (sem, val)
        for key, (sem, val) in need.items():
            self.waited[e][key] = val
        return list(need.values())

    def _record(self, tok, reads, writes):
        reads = self._flat(reads)
        writes = self._flat(writes)
        for b in reads:
            old = b.r.get(tok[0])
            if old is None or old[2] < tok[2]:
                b.r[tok[0]] = tok
        for b in writes:
            b.w = tok
            b.r = {}

    def op(self, e, fn, reads=(), writes=()):
        waits = self._waits(e, self._deps(reads, writes))
        if self.cnt[e] >= 16000:
            self.epoch[e] += 1
            self.sem[e] = self.stack.enter_context(self.nc.semaphore("s_%s_%d" % (e, self.epoch[e])))
            self.cnt[e] = 0
        self.cnt[e] += 1
        tok = (("E", e, self.epoch[e]), self.sem[e], self.cnt[e])
        self.last_tok[e] = tok
        self.ops[e].append((waits, fn, self.sem[e], 1))
        self._record(tok, reads, writes)

    def dma(self, q, fn, reads=(), writes=()):
        deps = self._deps(reads, writes)
        i = self.drr[q]
        self.drr[q] = (i + 1) % NDS
        sem = self.dsem[q][i]
        key = ("D", q, i)
        prev = self.dcnt[q][i]
        deps.append((key, sem, prev))
        waits = self._waits(q, deps)
        self.dcnt[q][i] = prev + 16
        tok = (key, sem, prev + 16)
        self.ops[q].append((waits, fn, sem, 16))
        self._record(tok, reads, writes)

    def barrier(self):
        toks = [self.last_tok[e] for e in self.ENG if self.last_tok[e] is not None]
        for q in ("sp", "pool"):
            for i in range(NDS):
                if self.dcnt[q][i] > 0:
                    toks.append((("D", q, i), self.dsem[q][i], self.dcnt[q][i]))
        for e in self.ENG:
            waits = self._waits(e, toks)
            if waits:
                self.ops[e].append((waits, None, None, 0))

    def replay(self, e, eng):
        for waits, fn, sem, inc in self.ops[e]:
            for (s, v) in waits:
                eng.wait_ge(s, v)
            if fn is not None:
                fn(eng).then_inc(sem, inc)

    def final_waits(self, e, eng, bufs):
        deps = []
        for b in self._flat(bufs):
            if b.w is not None:
                deps.append(b.w)
        for (s, v) in self._waits(e, deps):
            eng.wait_ge(s, v)


def build(NSLOT, CAPB, debug=(), moe=True, nphase=4):
    NB = 1 + 8 * NSLOT
    NPOS = NB * 128
    TOWN = 512 * NSLOT
    NOB = 4 * NSLOT
    CAP = 128 * CAPB
    NXB = 32 * CAP
    nc = bass.Bass("TRN2", target_bir_lowering=False)
    stack = ExitStack()
    S = Sched(nc, stack)

    def din(name, shape, dt=F32):
        return T(nc.dram_tensor(name, list(shape), dt, kind="ExternalInput").ap(), name)

    def dscr(name, shape, dt=F32):
        kind = "ExternalOutput" if "scr" in debug else "Internal"
        return T(nc.dram_tensor(name, list(shape), dt, kind=kind).ap(), name)

    def dout(name, shape, dt=F32):
        return T(nc.dram_tensor(name, list(shape), dt, kind="ExternalOutput").ap(), name)

    cur = [stack]

    def sb(name, shape, dt=F32):
        return T(cur[0].enter_context(nc.sbuf_tensor(name, list(shape), dt)), name)

    def open_scope():
        st = ExitStack()
        cur.insert(0, st)
        return st

    def close_scope(st):
        assert cur[0] is st
        S.barrier()
        st.close()
        cur.pop(0)

    x_all = din("x_all", [NPOS, D])
    x_own = din("x_own", [TOWN, D])
    w_in = din("w_in", [D, 4096])
    g_mix = din("g_mix", [1, D])
    g_ffn = din("g_ffn", [1, D])
    g_fin = din("g_fin", [1, D])
    lam_re = din("lam_re", [1, 2048])
    lam_im = din("lam_im", [1, 2048])
    log_dt = din("log_dt", [1, 32])
    b_re = din("b_re", [32, 64, 16])
    b_im = din("b_im", [32, 64, 16])
    c_re = din("c_re", [512, 64])
    c_im = din("c_im", [512, 64])
    d_skip = din("d_skip", [1, 512])
    glu_w = din("glu_w", [512, 512])
    glu_b = din("glu_b", [1, 512])
    w_pa = din("w_pa", [512, D])
    w_pb = din("w_pb", [512, D])
    w_o = din("w_o", [D, D])
    w_r = din("w_r", [D, 36])
    b_r = din("b_r", [1, 36])
    NE = 32 if moe else 1
    w1 = din("w1", [NE, D, 512])
    w3 = din("w3", [NE, D, 512])
    w2 = din("w2", [NE, 512, D])
    k_ident = din("k_ident", [128, 128])
    k_triltf = din("k_triltf", [128, 128])
    k_onesf = din("k_onesf", [128, 128])
    k_jrow = din("k_jrow", [1, 128])
    k_maskB = din("k_maskB", [128, 32])
    k_maskC = din("k_maskC", [128, 128])
    k_ecap = din("k_ecap", [128, 32])
    k_maskQ = din("k_maskQ", [128, 2])
    k_maskR = din("k_maskR", [128, 256])
    k_trige = din("k_trige", [128, 128], BF16)
    k_trilt = din("k_trilt", [128, 128], BF16)
    k_trile = din("k_trile", [128, 128], BF16)
    k_mask0 = din("k_mask0", [128, 512], BF16)
    k_maske = din("k_maske", [128, 8, 512], BF16)
    k_masko = din("k_masko", [128, 8, 512], BF16)
    k_sel = din("k_sel", [128, 2 * NSLOT])

    kt_scr = dscr("kt_scr", [512, NPOS], BF16)
    v_scr = dscr("v_scr", [8, 128, NB, 64], BF16)
    ssm_scr = dscr("ssm_scr", [4, 128, NPOS], BF16)
    h_scr = dscr("h_scr", [TOWN, D])
    xb_scr = dscr("xb_scr", [NXB, D])
    yb_scr = dscr("yb_scr", [NXB, D])
    out_own = dout("out_own", [TOWN, D])

    dbg_outs = {}

    def dbg(name, view, shape, dt=F32):
        if name not in debug:
            return
        t = dout("dbg_" + name, shape, dt)
        dbg_outs[name] = t
        dma("sp", t[:], view)

    bcreg = {}

    def bc_reg(e):
        if "r" not in bcreg:
            bcreg["r"] = e.to_reg(NXB - 1)
        return bcreg["r"]

    def mm(out, lhsT, rhs, start=True, stop=True):
        S.op("pe", lambda e: e.matmul(out.ap, lhsT.ap, rhs.ap, start=start, stop=stop),
             reads=[lhsT.buf, rhs.buf], writes=[out.buf])

    def tr(out, in_, ident_v):
        S.op("pe", lambda e: e.transpose(out.ap, in_.ap, ident_v.ap),
             reads=[in_.buf, ident_v.buf], writes=[out.buf])

    def act(out, in_, func, bias=None, scale=None, accum=None, extra_reads=()):
        kw = {}
        reads = [in_.buf] + list(extra_reads)
        writes = [out.buf]
        if bias is not None:
            if isinstance(bias, View):
                kw["bias"] = bias.ap
                reads.append(bias.buf)
            else:
                kw["bias"] = bias
        if scale is not None:
            if isinstance(scale, View):
                kw["scale"] = scale.ap
                reads.append(scale.buf)
            else:
                kw["scale"] = scale
        if accum is not None:
            kw["accum_out"] = accum.ap
            writes.append(accum.buf)
        S.op("act", lambda e: e.activation(out.ap, in_.ap, func, **kw), reads=reads, writes=writes)

    def tt(eng, out, in0, in1, op):
        S.op(eng, lambda e: e.tensor_tensor(out.ap, in0.ap, in1.ap, op),
             reads=[in0.buf, in1.buf], writes=[out.buf])

    def _sc(x, reads):
        if isinstance(x, View):
            reads.append(x.buf)
            return x.ap
        return x

    def ts(eng, out, in0, s1, op0, s2=None, op1=None):
        reads = [in0.buf]
        a1 = _sc(s1, reads)
        a2 = _sc(s2, reads)
        if op1 is None:
            S.op(eng, lambda e: e.tensor_scalar(out.ap, in0.ap, a1, None, op0), reads=reads, writes=[out.buf])
        else:
            S.op(eng, lambda e: e.tensor_scalar(out.ap, in0.ap, a1, a2, op0, op1), reads=reads,
                 writes=[out.buf])

    def stt(out, in0, scalar, in1, op0, op1):
        reads = [in0.buf, in1.buf]
        a = _sc(scalar, reads)
        S.op("dve", lambda e: e.scalar_tensor_tensor(out.ap, in0.ap, a, in1.ap, op0, op1),
             reads=reads, writes=[out.buf])

    def cp(eng, out, in_):
        if eng == "act":
            S.op("act", lambda e: e.copy(out.ap, in_.ap), reads=[in_.buf], writes=[out.buf])
        else:
            S.op(eng, lambda e: e.tensor_copy(out.ap, in_.ap), reads=[in_.buf], writes=[out.buf])

    def dma(q, out, in_, **kw):
        S.dma(q, lambda e: e.dma_start(out=out.ap, in_=in_.ap, **kw), reads=[in_.buf], writes=[out.buf])

    Pall_h = stack.enter_context(nc.psum_tensor("psall", [128, 4096], F32))

    class PB:
        def __init__(self, i0, nb):
            self.i0, self.nb = i0, nb
            self.buf = tuple(pbufs[i0:i0 + nb]) if nb > 1 else pbufs[i0]

        def __getitem__(self, k):
            return View(Pall_h[:, self.i0 * 512:(self.i0 + self.nb) * 512][k], self.buf)

    pbufs = [Buf("ps%d" % i) for i in range(8)]
    P = [PB(i, 1) for i in range(8)]
    PQ = PB(2, 4)
    P01 = PB(0, 2)
    P45 = PB(4, 2)
    P67 = PB(6, 2)

    def finish():
        final_bufs = [out_own.buf, kt_scr.buf, v_scr.buf, ssm_scr.buf, h_scr.buf, xb_scr.buf, yb_scr.buf] + [t.buf for t in dbg_outs.values()]
        with nc.Block() as block:
            @block.tensor
            def _(e):
                S.replay("pe", e)

            @block.scalar
            def _(e):
                S.replay("act", e)

            @block.vector
            def _(e):
                S.replay("dve", e)

            @block.gpsimd
            def _(e):
                S.replay("pool", e)

            @block.sync
            def _(e):
                S.replay("sp", e)
                S.final_waits("sp", e, final_bufs)
        stack.close()
        return nc

    ident = sb("ident", [128, 128])
    triltf = sb("triltf", [128, 128])
    onesf = sb("onesf", [128, 128])
    jrow = sb("jrow", [1, 128])
    maskB = sb("maskB", [128, 32])
    maskC = sb("maskC", [128, 128])
    ecap = sb("ecap", [128, 32])
    maskQ = sb("maskQ", [128, 2])
    maskR = sb("maskR", [128, 256])
    trige = sb("trige", [128, 128], BF16)
    trilt = sb("trilt", [128, 128], BF16)
    trile = sb("trile", [128, 128], BF16)
    sel = sb("sel", [128, 2 * NSLOT])
    epsc = sb("epsc", [128, 1])
    onec = sb("onec", [128, 1])
    grep_mix = sb("grep_mix", [128, D])
    grep_ffn = sb("grep_ffn", [128, D])
    grep_fin = sb("grep_fin", [128, D])
    brrep = sb("brrep", [128, 36])
    destall = sb("destall", [128, NOB, 2], I32)
    gwall = sb("gwall", [128, NOB, 2])
    scopeA = open_scope()
    for dst, src in ((ident, k_ident), (triltf, k_triltf), (onesf, k_onesf), (jrow, k_jrow), (maskB, k_maskB),
                     (maskC, k_maskC), (ecap, k_ecap), (maskQ, k_maskQ), (maskR, k_maskR), (trige, k_trige), (trilt, k_trilt), (trile, k_trile),
                     (sel, k_sel)):
        dma("sp", dst[:], src[:])

    dcol = sb("dcol", [128, 4])
    gbcol = sb("gbcol", [128, 4])
    T1re = sb("T1re", [128, 2048])
    T1im = sb("T1im", [128, 2048])
    T2re = sb("T2re", [128, 2048])
    T2im = sb("T2im", [128, 2048])
    are = sb("are", [128, 16])
    aim = sb("aim", [128, 16])
    Bp = sb("Bp", [128, 4, 2, 128])
    Cp = sb("Cp", [128, 4, 2, 128])
    Bq = sb("Bq", [128, 4, 2, 2, 128], BF16)
    Cq = sb("Cq", [128, 4, 2, 2, 128], BF16)
    s16 = [sb("s16_%d" % i, [128, 16]) for i in range(6)]
    scope0 = open_scope()
    rowtmp = sb("rowtmp", [1, 2048])

    def replicate_row(dst, src_dram, n):
        dma("sp", rowtmp[0:1, 0:n], src_dram[0:1, 0:n])
        for c0 in range(0, n, 512):
            c1 = min(n, c0 + 512)
            mm(P[0][:, 0:c1 - c0], onesf[0:1, :], rowtmp[0:1, c0:c1])
            cp("dve", dst[:, c0:c1], P[0][:, 0:c1 - c0])

    replicate_row(grep_mix, g_mix, D)
    replicate_row(grep_ffn, g_ffn, D)
    replicate_row(grep_fin, g_fin, D)
    replicate_row(brrep, b_r, 36)
    S.op("dve", lambda e: e.memset(onec[:].ap, 1.0), writes=[onec.buf])

    def row_to_col(dst, src_dram, nchunk):
        dma("sp", rowtmp[0:1, 0:nchunk * 128], src_dram[0:1, 0:nchunk * 128])
        for q in range(nchunk):
            mm(P[0][:, 2 * q:2 * q + 2], rowtmp[0:1, q * 128:(q + 1) * 128], onesf[0:1, 0:2])
        cp("dve", dst[:, 0:nchunk], P[0][:, 0:2 * nchunk].re("p (q t) -> p q t", t=2)[:, :, 0])

    row_to_col(dcol, d_skip, 4)
    row_to_col(gbcol, glu_b, 4)

    lrrow = sb("lrrow", [1, 2048])
    lirow = sb("lirow", [1, 2048])
    dtrow = sb("dtrow", [1, 32])
    rhorow = sb("rhorow", [1, 2048])
    throw = sb("throw", [1, 2048])
    dma("sp", lrrow[:], lam_re[:])
    dma("sp", lirow[:], lam_im[:])
    dma("sp", dtrow[:], log_dt[:])
    act(dtrow[:], dtrow[:], AF.Exp)
    dtb = dtrow[:].un(2).bc([1, 32, 64])
    tt("dve", rhorow[:].re("o (g p) -> o g p", p=64), lrrow[:].re("o (g p) -> o g p", p=64), dtb, ALU.mult)
    tt("dve", throw[:].re("o (g p) -> o g p", p=64), lirow[:].re("o (g p) -> o g p", p=64), dtb, ALU.mult)

    wkA = sb("wkA", [128, 2048])
    wkB = sb("wkB", [128, 2048])
    wkI = sb("wkI", [128, 2048], I32)
    wkM = sb("wkM", [128, 2048])

    def outer_tok(row):
        for k in range(4):
            mm(P[k][:, :], jrow[0:1, :], row[0:1, k * 512:(k + 1) * 512])

    def outer_feat(row):
        for gp in range(16):
            mm(P[gp // 4][:, (gp % 4) * 128:(gp % 4 + 1) * 128], row[0:1, gp * 128:(gp + 1) * 128], jrow[0:1, :])

    def sin_of(dst, shift):
        for k in range(4):
            ts("dve", wkA[:, k * 512:(k + 1) * 512], P[k][:, :], 1.0 / TWO_PI, ALU.mult, 0.5 + shift, ALU.add)
        cp("dve", wkI[:], wkA[:])
        cp("dve", wkB[:], wkI[:])
        for k in range(4):
            stt(wkA[:, k * 512:(k + 1) * 512], wkB[:, k * 512:(k + 1) * 512], -TWO_PI, P[k][:, :],
                ALU.mult, ALU.add)
        if shift != 0.0:
            ts("dve", wkA[:], wkA[:], shift * TWO_PI, ALU.add)
        ts("dve", wkB[:], wkA[:], math.pi, ALU.is_gt)
        stt(wkA[:], wkB[:], -TWO_PI, wkA[:], ALU.mult, ALU.add)
        ts("dve", wkB[:], wkA[:], -math.pi, ALU.is_lt)
        stt(wkA[:], wkB[:], TWO_PI, wkA[:], ALU.mult, ALU.add)
        ts("dve", wkA[:], wkA[:], 3.14159, ALU.min, -3.14159, ALU.max)
        act(dst, wkA[:], AF.Sin)

    def build_table(Tre, Tim, outer_fn, sign):
        outer_fn(rhorow)
        for k in range(4):
            act(wkM[:, k * 512:(k + 1) * 512], P[k][:, :], AF.Exp, scale=float(sign))
        outer_fn(throw)
        sin_of(Tim[:], 0.0)
        sin_of(Tre[:], 0.25)
        tt("dve", Tre[:], Tre[:], wkM[:], ALU.mult)
        stt(Tim[:], Tim[:], float(sign), wkM[:], ALU.mult, ALU.mult)

    build_table(T1re, T1im, outer_tok, -1)
    build_table(T2re, T2im, outer_feat, +1)

    lrc = sb("lrc", [128, 16])
    lic = sb("lic", [128, 16])
    for dst, row in ((lrc, lrrow), (lic, lirow)):
        for gp in range(16):
            mm(P[4][:, 2 * gp:2 * gp + 2], row[0:1, gp * 128:(gp + 1) * 128], onesf[0:1, 0:2])
        cp("dve", dst[:], P[4][:, 0:32].re("p (q t) -> p q t", t=2)[:, :, 0])
    T2re3 = T2re[:].re("p (g j) -> p g j", j=128)
    T2im3 = T2im[:].re("p (g j) -> p g j", j=128)
    cp("dve", are[:], T2re3[:, :, 1])
    cp("dve", aim[:], T2im3[:, :, 1])
    fre = sb("fre", [128, 16])
    fim = sb("fim", [128, 16])
    nr, den, t0, t1, t2, t3 = [s[:] for s in s16]
    ts("dve", nr, are[:], -1.0, ALU.add)
    tt("dve", t0, lrc[:], lrc[:], ALU.mult)
    tt("dve", t1, lic[:], lic[:], ALU.mult)
    tt("dve", den, t0, t1, ALU.add)
    S.op("dve", lambda e: e.reciprocal(den.ap, den.ap), reads=[den.buf], writes=[den.buf])
    tt("dve", t0, nr, lrc[:], ALU.mult)
    tt("dve", t1, aim[:], lic[:], ALU.mult)
    tt("dve", t2, t0, t1, ALU.add)
    tt("dve", fre[:], t2, den, ALU.mult)
    tt("dve", t0, aim[:], lrc[:], ALU.mult)
    tt("dve", t1, nr, lic[:], ALU.mult)
    tt("dve", t2, t0, t1, ALU.subtract)
    tt("dve", fim[:], t2, den, ALU.mult)

    Bsr = sb("Bsr", [128, 16, 16])
    Bsi = sb("Bsi", [128, 16, 16])
    for dst, src in ((Bsr, b_re), (Bsi, b_im)):
        v = src[:].re("(gp g2) p c -> g2 p gp c", g2=2)
        for g2 in range(2):
            dma("sp", dst[g2 * 64:(g2 + 1) * 64, :, :], v[g2])
    Bbr = sb("Bbr", [128, 16, 16])
    Bbi = sb("Bbi", [128, 16, 16])
    tB0 = sb("tB0", [128, 16, 16])
    tB1 = sb("tB1", [128, 16, 16])
    freb = fre[:].un(2).bc([128, 16, 16])
    fimb = fim[:].un(2).bc([128, 16, 16])
    tt("dve", tB0[:], Bsr[:], freb, ALU.mult)
    tt("dve", tB1[:], Bsi[:], fimb, ALU.mult)
    tt("dve", Bbr[:], tB0[:], tB1[:], ALU.subtract)
    tt("dve", tB0[:], Bsi[:], freb, ALU.mult)
    tt("dve", tB1[:], Bsr[:], fimb, ALU.mult)
    tt("dve", Bbi[:], tB0[:], tB1[:], ALU.add)
    dbg("Bbr", Bbr[:], [128, 16, 16])
    Y2 = sb("Y2", [128, 2, 16, 2, 16])
    mBb = maskB[:].re("p (a c) -> p a c", a=2).un(1).bc([128, 16, 2, 16])
    for pl, Bb in ((0, Bbr), (1, Bbi)):
        tt("dve", Y2[:, pl], Bb[:].un(2).bc([128, 16, 2, 16]), mBb, ALU.mult)
    for pl in range(2):
        for Q in range(4):
            tr(P[5][:, Q * 128:(Q + 1) * 128], Y2[:, pl, 4 * Q:4 * Q + 4].re("p a b c -> p (a b c)"), ident[:])
        cp("dve", Bp[:, :, pl, :], P[5][:, :].re("p (q n) -> p q n", n=128))
        for lh in range(2):
            ts("dve", Bq[:, :, lh, pl, :], Bp[:, :, pl, :], maskQ[:, lh:lh + 1], ALU.mult)
    Csr = sb("Csr", [128, 4, 64])
    Csi = sb("Csi", [128, 4, 64])
    dma("sp", Csr[:], c_re[:].re("(q r) p -> r q p", r=128))
    dma("sp", Csi[:], c_im[:].re("(q r) p -> r q p", r=128))
    Xc = sb("Xc", [128, 2, 4, 2, 64])
    mCb = maskC[:].re("r (a p) -> r a p", a=2).un(1).bc([128, 4, 2, 64])
    tt("dve", Xc[:, 0], Csr[:].un(2).bc([128, 4, 2, 64]), mCb, ALU.mult)
    for Q in range(4):
        stt(Xc[:, 1, Q], Csi[:, Q].un(1).bc([128, 2, 64]), -1.0, maskC[:].re("r (a p) -> r a p", a=2),
            ALU.mult, ALU.mult)
    for pl in range(2):
        for Q in range(4):
            tr(P[5][:, Q * 128:(Q + 1) * 128], Xc[:, pl, Q].re("r a p -> r (a p)"), ident[:])
        cp("dve", Cp[:, :, pl, :], P[5][:, :].re("p (q n) -> p q n", n=128))
        for lh in range(2):
            tt("dve", Cq[:, :, pl, lh, :], Cp[:, :, pl, :],
               maskR[:, lh * 128:(lh + 1) * 128].un(1).bc([128, 4, 128]), ALU.mult)

    close_scope(scope0)
    Wkvu = sb("Wkvu", [128, 8, 1536], BF16)
    for kc in range(8):
        dma("pool", Wkvu[:, kc, :], w_in[kc * 128:(kc + 1) * 128, 512:2048])
    Wg = sb("Wg", [128, 4, 512], BF16)
    for kc in range(4):
        dma("pool", Wg[:, kc, :], glu_w[kc * 128:(kc + 1) * 128, :])

    xblk = [sb("xblk%d" % i, [128, D]) for i in range(2)]
    junk = sb("junk", [128, D])
    ssq = sb("ssq", [128, 1])
    rstd = sb("rstd", [128, 1])
    xn = sb("xn", [128, D])
    xnT = sb("xnT", [128, 8, 128], BF16)
    ktb = sb("ktb", [128, 4, 128], BF16)
    vb = sb("vb", [128, 512], BF16)
    uT = sb("uT", [128, 4, 128], BF16)
    zt = sb("zt", [128, 16, 2, 128], BF16)
    tz = [sb("tz%d" % i, [128, 8, 128]) for i in range(4)]
    cg = sb("cg", [128, 8, 2, 128])
    xT = sb("xT", [128, 16, 2, 128], BF16)
    Gre = sb("Gre", [128, 16])
    Gim = sb("Gim", [128, 16])
    xlast = sb("xlast", [128, 16, 2])
    yv = sb("yv", [128, 4, 128])
    y2 = sb("y2", [128, 4, 128])
    y3 = sb("y3", [128, 4, 128])
    yg = sb("yg", [128, 4, 128])
    ygb = sb("ygb", [128, 4, 128], BF16)
    sgl = sb("sgl", [128, 4, 128])
    ssmT = sb("ssmT", [128, 4, 128], BF16)
    S.op("dve", lambda e: e.memset(Gre[:].ap, 0.0), writes=[Gre.buf])
    S.op("dve", lambda e: e.memset(Gim[:].ap, 0.0), writes=[Gim.buf])
    T1re3 = T1re[:].re("p (g n) -> p g n", n=128)
    T1im3 = T1im[:].re("p (g n) -> p g n", n=128)

    def rmsnorm(xv, grep, outv):
        act(junk[:], xv, AF.Square, accum=ssq[:])
        act(rstd[:], ssq[:], AF.Ln, scale=1.0 / D, bias=epsc[:])
        act(rstd[:], rstd[:], AF.Exp, scale=-0.5)
        stt(outv, xv, rstd[:], grep[:], ALU.mult, ALU.mult)

    S.op("dve", lambda e: e.memset(epsc[:].ap, 1e-6), writes=[epsc.buf])

    def transpose8(src, dstT, col0, ncol_total):
        for half in range(2):
            for k in range(4):
                kc = half * 4 + k
                tr(P[6 + half][:, k * 128:(k + 1) * 128], src[:, kc * 128:(kc + 1) * 128], ident[:])
            cp("act" if half == 0 else "dve", dstT[:, half * 4:half * 4 + 4, col0:col0 + 128],
               P[6 + half][:, :].re("p (k n) -> p k n", n=128))

    for blk in range(NB):
        xb_ = xblk[blk % 2]
        dma("sp", xb_[:], x_all[blk * 128:(blk + 1) * 128, :])
        rmsnorm(xb_[:], grep_mix, xn[:])
        transpose8(xn, xnT, 0, 128)
        for m in range(4):
            for kc in range(8):
                mm(P[0][:, m * 128:(m + 1) * 128], Wkvu[:, kc, m * 128:(m + 1) * 128], xnT[:, kc, :],
                   start=(kc == 0), stop=(kc == 7))
        cp("act", ktb[:], P[0][:, :].re("p (m n) -> p m n", n=128))
        dma("sp", kt_scr[:].re("(m q) n -> q m n", q=128)[:, :, blk * 128:(blk + 1) * 128], ktb[:])
        for kc in range(8):
            mm(P[2][:, :], xnT[:, kc, :], Wkvu[:, kc, 512:1024], start=(kc == 0), stop=(kc == 7))
        cp("act", vb[:], P[2][:, :])
        dma("sp", v_scr[:].re("h j b d -> j h b d")[:, :, blk, :], vb[:].re("j (h d) -> j h d", d=64))
        for m in range(4):
            for kc in range(8):
                mm(P[1][:, m * 128:(m + 1) * 128], Wkvu[:, kc, 1024 + m * 128:1024 + (m + 1) * 128],
                   xnT[:, kc, :], start=(kc == 0), stop=(kc == 7))
        cp("act", uT[:], P[1][:, :].re("p (m n) -> p m n", n=128))
        if blk == 1:
            dbg("uT", uT[:], [128, 4, 128], BF16)
        for hf in range(2):
            for b4 in range(4):
                Q, h = hf * 2 + b4 // 2, b4 % 2
                mm(P[2 + b4][:, :], uT[64 * h:64 * h + 64, Q, :],
                   Bq[64 * h:64 * h + 64, Q, :, :, :].re("k l a n -> k (l a n)"))
            def buv(pl):
                vs = []
                for b4 in range(4):
                    vs.append(P[2 + b4][:, :].re("p (g a n) -> p g a n", a=2, n=128)[:, :, pl, :])
                return vs
            bre = buv(0)
            bim = buv(1)
            gs = slice(hf * 8, hf * 8 + 8)
            for b4 in range(4):
                g2s = slice(hf * 8 + 2 * b4, hf * 8 + 2 * b4 + 2)
                l2s = slice(2 * b4, 2 * b4 + 2)
                tt("dve", tz[0][:, l2s, :], bre[b4], T1re3[:, g2s, :], ALU.mult)
                tt("dve", tz[1][:, l2s, :], bim[b4], T1im3[:, g2s, :], ALU.mult)
                tt("dve", tz[2][:, l2s, :], bre[b4], T1im3[:, g2s, :], ALU.mult)
                tt("dve", tz[3][:, l2s, :], bim[b4], T1re3[:, g2s, :], ALU.mult)
            tt("dve", zt[:, gs, 0, :], tz[0][:], tz[1][:], ALU.subtract)
            tt("dve", zt[:, gs, 1, :], tz[2][:], tz[3][:], ALU.add)
        for hf in range(2):
            gs = slice(hf * 8, hf * 8 + 8)
            for g8 in range(8):
                gp = hf * 8 + g8
                for pl in range(2):
                    idx = g8 * 2 + pl
                    mm(P[2 + idx // 4][:, (idx % 4) * 128:(idx % 4 + 1) * 128], zt[:, gp, pl, :], trile[:])
            for b4 in range(4):
                g2s = slice(hf * 8 + 2 * b4, hf * 8 + 2 * b4 + 2)
                l2s = slice(2 * b4, 2 * b4 + 2)
                pv = P[2 + b4][:, :].re("p (g a n) -> p g a n", a=2, n=128)
                tt("dve", cg[:, l2s, 0, :], pv[:, :, 0, :], Gre[:, g2s].un(2).bc([128, 2, 128]), ALU.add)
                tt("dve", cg[:, l2s, 1, :], pv[:, :, 1, :], Gim[:, g2s].un(2).bc([128, 2, 128]), ALU.add)
            tt("dve", tz[0][:], cg[:, :, 0, :], T2re3[:, gs, :], ALU.mult)
            tt("dve", tz[1][:], cg[:, :, 1, :], T2im3[:, gs, :], ALU.mult)
            tt("dve", tz[2][:], cg[:, :, 1, :], T2re3[:, gs, :], ALU.mult)
            tt("dve", tz[3][:], cg[:, :, 0, :], T2im3[:, gs, :], ALU.mult)
            tt("dve", xT[:, gs, 0, :], tz[0][:], tz[1][:], ALU.subtract)
            tt("dve", xT[:, gs, 1, :], tz[2][:], tz[3][:], ALU.add)
            tt("dve", xlast[:, gs, 0], tz[0][:, :, 127], tz[1][:, :, 127], ALU.subtract)
            tt("dve", xlast[:, gs, 1], tz[2][:, :, 127], tz[3][:, :, 127], ALU.add)
        tt("dve", s16[0][:], xlast[:, :, 0], are[:], ALU.mult)
        tt("dve", s16[1][:], xlast[:, :, 1], aim[:], ALU.mult)
        tt("dve", s16[2][:], xlast[:, :, 0], aim[:], ALU.mult)
        tt("dve", s16[3][:], xlast[:, :, 1], are[:], ALU.mult)
        tt("dve", Gre[:], s16[0][:], s16[1][:], ALU.subtract)
        tt("dve", Gim[:], s16[2][:], s16[3][:], ALU.add)
        for Q in range(4):
            for h in range(2):
                i = 0
                for lh in range(2):
                    gp = 4 * Q + 2 * h + lh
                    for pl in range(2):
                        mm(P[1][64 * h:64 * h + 64, Q * 128:(Q + 1) * 128], Cq[:, Q, pl, lh, 64 * h:64 * h + 64],
                           xT[:, gp, pl, :], start=(i == 0), stop=(i == 3))
                        i += 1
        for Q in range(4):
            stt(yv[:, Q, :], uT[:, Q, :], dcol[:, Q:Q + 1], P[1][:, Q * 128:(Q + 1) * 128], ALU.mult, ALU.add)
        if blk == 1:
            dbg("yv", yv[:], [128, 4, 128])
        tt("dve", y2[:], yv[:], yv[:], ALU.mult)
        ts("dve", y2[:], y2[:], 0.044715, ALU.mult, 1.0, ALU.add)
        tt("dve", y3[:], y2[:], yv[:], ALU.mult)
        act(y3[:], y3[:], AF.Sigmoid, scale=1.5957691216057308)
        tt("dve", yg[:], yv[:], y3[:], ALU.mult)
        cp("act", ygb[:], yg[:])
        for co in range(4):
            for kc in range(4):
                mm(P[0][:, co * 128:(co + 1) * 128], Wg[:, kc, co * 128:(co + 1) * 128], ygb[:, kc, :],
                   start=(kc == 0), stop=(kc == 3))
        for co in range(4):
            act(sgl[:, co, :], P[0][:, co * 128:(co + 1) * 128], AF.Sigmoid, bias=gbcol[:, co:co + 1])
        tt("dve", ssmT[:], yg[:], sgl[:], ALU.mult)
        dma("sp", ssm_scr[:].re("q p n -> p q n")[:, :, blk * 128:(blk + 1) * 128], ssmT[:])

    close_scope(scopeA)
    if nphase < 2:
        while len(cur) > 1:
            cur[0].close()
            cur.pop(0)
        return finish()
    scope2 = open_scope()
    mask0 = sb("mask0", [128, 512], BF16)
    maske = sb("maske", [128, 8, 512], BF16)
    masko = sb("masko", [128, 8, 512], BF16)
    dma("sp", mask0[:], k_mask0[:])
    dma("sp", maske[:], k_maske[:])
    dma("sp", masko[:], k_masko[:])
    Wr = sb("Wr", [128, 8, 36])
    dma("sp", Wr[:], w_r[:].re("(kc p) n -> p kc n", p=128))
    Wpa = sb("Wpa", [64, 8, D], BF16)
    Wpb = sb("Wpb", [128, 4, D], BF16)
    Wo = sb("Wo", [128, 8, D], BF16)
    for h in range(8):
        dma("pool", Wpa[:, h, :], w_pa[h * 64:(h + 1) * 64, :])
    for kc in range(4):
        dma("pool", Wpb[:, kc, :], w_pb[kc * 128:(kc + 1) * 128, :])
    for kc in range(8):
        dma("pool", Wo[:, kc, :], w_o[kc * 128:(kc + 1) * 128, :])
    wring = [sb("wring%d" % i, [128, 8, 512], BF16) for i in range(2)]
    wri = [0]

    def load_wchunk(c0):
        t = wring[wri[0] % 2]
        wri[0] += 1
        for kc in range(8):
            dma("pool", t[:, kc, :], w_in[kc * 128:(kc + 1) * 128, c0:c0 + 512])
        return t

    xo = [sb("xo%d" % i, [128, D]) for i in range(2)]
    xn2 = sb("xn2", [128, D])
    junk2 = sb("junk2", [128, D], BF16)
    ssq2 = sb("ssq2", [128, 1])
    rstd2 = sb("rstd2", [128, 1])
    xoT = sb("xoT", [128, 8, 512], BF16)
    qT = sb("qT", [64, 8, 512], BF16)
    ktc = [[sb("ktc%d_%d" % (a, i), [64, 1024], BF16) for i in range(2)] for a in range(2)]
    vtc = [[sb("vtc%d_%d" % (a, i), [128, 8, 64], BF16) for i in range(2)] for a in range(2)]
    erw = [sb("erw%d" % i, [128, 1024], BF16) for i in range(2)]
    sprw = [sb("sprw%d" % i, [128, 1024], BF16) for i in range(2)]
    trw = sb("trw", [128, 1024], BF16)
    wrw = [sb("wrw%d" % i, [128, 1024], BF16) for i in range(2)]
    attnT = sb("attnT", [64, 8, 512], BF16)
    ssA = sb("ssA", [128, 4, 512], BF16)
    ssB = sb("ssB", [128, 4, 512], BF16)
    ssO = ssA
    sg1 = sb("sg1", [128, 512], BF16)
    sg2 = sb("sg2", [128, 512], BF16)
    mg1 = sb("mg1", [128, 512])
    mg2 = sb("mg2", [128, 512])
    mergedT = sb("mergedT", [128, 8, 512], BF16)
    hblk = [sb("hblk0", [128, D])] * 2
    hn = [xn2, xn2]
    hnT = sb("hnT", [128, 8, 128])
    lgt = sb("lgt", [128, 36])
    r1 = [sb("r1_%d" % i, [128, 1]) for i in range(12)]
    ohg = sb("ohg", [128, 4])
    eg = sb("eg", [128, 4])
    selv = sb("selv", [128, 8])
    m8 = sb("m8", [128, 8])
    oh1 = sb("oh1", [128, 8])
    oh2 = sb("oh2", [128, 8])
    M12 = sb("M12", [128, 64])
    pm = sb("pm", [128, 64])
    pmt = sb("pmt", [128, 64])
    base = sb("base", [128, 32])
    destf = sb("destf", [128, 2])
    S.op("dve", lambda e: e.memset(base[:].ap, 0.0), writes=[base.buf])

    def rmsnorm2(xv, grep, outv):
        act(junk2[:], xv, AF.Square, accum=ssq2[:])
        act(rstd2[:], ssq2[:], AF.Ln, scale=1.0 / D, bias=epsc[:])
        act(rstd2[:], rstd2[:], AF.Exp, scale=-0.5)
        stt(outv, xv, rstd2[:], grep[:], ALU.mult, ALU.mult)

    def mmacc(out, lhsT, rhs, start):
        S.op("pe", lambda e: e.matmul(out.ap, lhsT.ap, rhs.ap, start=start, stop=True, skip_group_check=True),
             reads=[lhsT.buf, rhs.buf], writes=[out.buf])

    for n in range(NSLOT):
        pmask = maske if n % 2 == 0 else masko
        for r in range(4):
            xb_ = xo[r % 2]
            dma("sp", xb_[:], x_own[(4 * n + r) * 128:(4 * n + r + 1) * 128, :])
            rmsnorm2(xb_[:], grep_mix, xn2[:])
            transpose8(xn2, xoT, r * 128, 512)
        wq = load_wchunk(0)
        for h in range(8):
            pb = P[4 + h % 4]
            for kc in range(8):
                mm(pb[0:64, :], wq[:, kc, h * 64:(h + 1) * 64], xoT[:, kc, :], start=(kc == 0), stop=(kc == 7))
            cp("act" if h % 2 == 0 else "dve", qT[:, h, :], pb[0:64, :])
        if n == 0:
            dbg("qT", qT[:], [64, 8, 512], BF16)
        nkb = 8 * n + 9
        kbs = [8 * n + 8 - s for s in range(nkb)]
        for hp in range(4):
            heads = (2 * hp, 2 * hp + 1)
            OB = (P[2], P[3])
            ZW = (P45, P67)
            cur_chunk = [None, None]
            cur_kt = [None, None]
            cur_vt = [None, None]
            nload = [0, 0]

            def get_kv(a, kb):
                h = heads[a]
                c = -1 if kb == 0 else (kb - 1) // 8
                if cur_chunk[a] != c:
                    i = nload[a] % 2
                    nload[a] += 1
                    kt_, vt_ = ktc[a][i], vtc[a][i]
                    if c < 0:
                        dma("sp", kt_[:, 0:128], kt_scr[h * 64:(h + 1) * 64, 0:128])
                        dma("sp", vt_[:, 0:1, :], v_scr[h, :, 0:1, :])
                    else:
                        dma("sp", kt_[:, :], kt_scr[h * 64:(h + 1) * 64, (8 * c + 1) * 128:(8 * c + 9) * 128])
                        dma("sp", vt_[:, :, :], v_scr[h, :, 8 * c + 1:8 * c + 9, :])
                    cur_chunk[a], cur_kt[a], cur_vt[a] = c, kt_, vt_
                lb = 0 if kb == 0 else (kb - 1) % 8
                return cur_kt[a][:, lb * 128:(lb + 1) * 128], cur_vt[a][:, lb, :]

            vsel = {}

            def front(s):
                kb = kbs[s]
                Z = ZW[s % 2]
                for a in range(2):
                    ktv, vtv = get_kv(a, kb)
                    vsel[(a, s)] = vtv
                    mm(Z[:, a * 512:(a + 1) * 512], ktv, qT[:, heads[a], :])
                e_ = erw[s % 2]
                act(e_[:], Z[:, :], AF.Exp, scale=0.125)
                e3 = e_[:].re("p (a n) -> p a n", a=2)
                if s < 8:
                    tt("dve", e3, e3, pmask[:, s, :].un(1).bc([128, 2, 512]), ALU.mult)
                elif kb == 0:
                    tt("dve", e3, e3, mask0[:].un(1).bc([128, 2, 512]), ALU.mult)
                act(sprw[s % 2][:], e_[:], AF.Ln, bias=onec[:])

            front(0)
            for s in range(nkb):
                sp_ = sprw[s % 2]
                for a in range(2):
                    mmacc(P01[:, a * 512:(a + 1) * 512], trige[:], sp_[:, a * 512:(a + 1) * 512], start=(s == 0))
                act(trw[:], P01[:, :], AF.Exp, scale=-1.0)
                if s + 1 < nkb:
                    front(s + 1)
                for a in range(2):
                    mmacc(P01[:, a * 512:(a + 1) * 512], trilt[:], sp_[:, a * 512:(a + 1) * 512], start=False)
                w_ = wrw[s % 2]
                tt("dve", w_[:], erw[s % 2][:], trw[:], ALU.mult)
                for a in range(2):
                    mmacc(OB[a][0:64, :], vsel[(a, s)], w_[:, a * 512:(a + 1) * 512], start=(s == 0))
            for a in range(2):
                cp("act" if a == 0 else "dve", attnT[:, heads[a], :], OB[a][0:64, :])
        if n == 0:
            dbg("attnT", attnT[:], [64, 8, 512], BF16)
        posA = (8 * n + 1) * 128
        posB = (8 * n + 5) * 128
        sv = ssm_scr[:].re("q p n -> p q n")
        dma("sp", ssA[:], sv[:, :, posA:posA + 512])
        dma("sp", ssB[:], sv[:, :, posB:posB + 512])
        ts("dve", ssO[:], ssA[:], sel[:, 2 * n:2 * n + 1], ALU.mult)
        stt(ssO[:], ssB[:], sel[:, 2 * n + 1:2 * n + 2], ssO[:], ALU.mult, ALU.add)
        for c2 in range(2):
            wga = load_wchunk(2048 + c2 * 512)
            wgb = load_wchunk(3072 + c2 * 512)
            for c4 in range(4):
                co = c2 * 4 + c4
                for kc in range(8):
                    mm(P[4][:, :], wga[:, kc, c4 * 128:(c4 + 1) * 128], xoT[:, kc, :], start=(kc == 0), stop=(kc == 7))
                for kc in range(8):
                    mm(P[5][:, :], wgb[:, kc, c4 * 128:(c4 + 1) * 128], xoT[:, kc, :], start=(kc == 0), stop=(kc == 7))
                for h in range(8):
                    mm(P[6][:, :], Wpa[:, h, co * 128:(co + 1) * 128], attnT[:, h, :], start=(h == 0), stop=(h == 7))
                for kc in range(4):
                    mm(P[7][:, :], Wpb[:, kc, co * 128:(co + 1) * 128], ssO[:, kc, :], start=(kc == 0), stop=(kc == 3))
                act(sg1[:], P[4][:, :], AF.Sigmoid)
                act(sg2[:], P[5][:, :], AF.Sigmoid)
                tt("dve", mg1[:], P[6][:, :], sg1[:], ALU.mult)
                tt("dve", mg2[:], P[7][:, :], sg2[:], ALU.mult)
                tt("dve", mergedT[:, co, :], mg1[:], mg2[:], ALU.add)
        if n == 0:
            dbg("mergedT", mergedT[:], [128, 8, 512], BF16)
        for r in range(4):
            ob = 4 * n + r
            xb_ = xo[r % 2]
            hb_ = hblk[r % 2]
            hn_ = hn[r % 2]
            dma("sp", xb_[:], x_own[ob * 128:(ob + 1) * 128, :])
            for half in range(2):
                for kc in range(8):
                    mm(P[half][:, :], mergedT[:, kc, r * 128:(r + 1) * 128], Wo[:, kc, half * 512:(half + 1) * 512],
                       start=(kc == 0), stop=(kc == 7))
                tt("dve", hb_[:, half * 512:(half + 1) * 512], P[half][:, :], xb_[:, half * 512:(half + 1) * 512],
                   ALU.add)
            dma("sp", h_scr[ob * 128:(ob + 1) * 128, :], hb_[:])
            rmsnorm2(hb_[:], grep_ffn, hn_[:])
            for half in range(2):
                for k in range(4):
                    kc = half * 4 + k
                    tr(P[6 + half][:, k * 128:(k + 1) * 128], hn_[:, kc * 128:(kc + 1) * 128], ident[:])
                cp("act" if half == 0 else "dve", hnT[:, half * 4:half * 4 + 4, :],
                   P[6 + half][:, :].re("p (k n) -> p k n", n=128))
            for kc in range(8):
                mm(P[2][:, 0:36], hnT[:, kc, :], Wr[:, kc, :], start=(kc == 0), stop=(kc == 7))
            tt("dve", lgt[:], P[2][:, 0:36], brrep[:], ALU.add)
            gmax, ngmax, gsum, ptop, dv, ex, g1, g2, p1loc, p2loc, ov1, ov2 = [t_[:] for t_ in r1]
            S.op("dve", lambda e: e.tensor_reduce(gmax.ap, lgt[:, 0:4].ap, AX.X, ALU.max),
                 reads=[lgt.buf], writes=[gmax.buf])
            ts("dve", ohg[:], lgt[:, 0:4], gmax, ALU.is_equal)
            ts("dve", ngmax, gmax, -1.0, ALU.mult)
            act(eg[:], lgt[:, 0:4], AF.Exp, bias=ngmax, accum=gsum)
            S.op("dve", lambda e: e.reciprocal(ptop.ap, gsum.ap), reads=[gsum.buf], writes=[ptop.buf])
            ts("dve", selv[:], lgt[:, 4:12], ohg[:, 0:1], ALU.mult)
            for g in range(1, 4):
                stt(selv[:], lgt[:, 4 + 8 * g:12 + 8 * g], ohg[:, g:g + 1], selv[:], ALU.mult, ALU.add)
            S.op("dve", lambda e: e.max(m8[:].ap, selv[:].ap), reads=[selv.buf], writes=[m8.buf])
            ts("dve", oh1[:], selv[:], m8[:, 0:1], ALU.is_equal)
            ts("dve", oh2[:], selv[:], m8[:, 1:2], ALU.is_equal)
            tt("dve", dv, m8[:, 1:2], m8[:, 0:1], ALU.subtract)
            act(ex, dv, AF.Exp)
            ts("dve", g1, ex, 1.0, ALU.add)
            S.op("dve", lambda e: e.reciprocal(g1.ap, g1.ap), reads=[g1.buf], writes=[g1.buf])
            tt("dve", g2, ex, g1, ALU.mult)
            tt("dve", gwall[:, ob, 0:1], g1, ptop, ALU.mult)
            tt("dve", gwall[:, ob, 1:2], g2, ptop, ALU.mult)
            tt("dve", M12[:, 0:32].re("p (g e) -> p g e", e=8), ohg[:].un(2).bc([128, 4, 8]),
               oh1[:].un(1).bc([128, 4, 8]), ALU.mult)
            tt("dve", M12[:, 32:64].re("p (g e) -> p g e", e=8), ohg[:].un(2).bc([128, 4, 8]),
               oh2[:].un(1).bc([128, 4, 8]), ALU.mult)
            mm(P[3][:, 0:64], triltf[:], M12[:])
            mm(P[3][:, 64:128], onesf[:], M12[:])
            tt("dve", pm[:, 0:32], P[3][:, 0:32], base[:], ALU.add)
            tt("dve", pm[:, 32:64], P[3][:, 32:64], base[:], ALU.add)
            tt("dve", pm[:, 32:64], P[3][:, 64:96], pm[:, 32:64], ALU.add)
            tt("dve", pmt[:], pm[:], M12[:], ALU.mult)
            S.op("dve", lambda e: e.tensor_reduce(p1loc.ap, pmt[:, 0:32].ap, AX.X, ALU.add),
                 reads=[pmt.buf], writes=[p1loc.buf])
            S.op("dve", lambda e: e.tensor_reduce(p2loc.ap, pmt[:, 32:64].ap, AX.X, ALU.add),
                 reads=[pmt.buf], writes=[p2loc.buf])
            tt("dve", pmt[:, 0:32], M12[:, 0:32], ecap[:], ALU.mult)
            tt("dve", pmt[:, 32:64], M12[:, 32:64], ecap[:], ALU.mult)
            S.op("dve", lambda e: e.tensor_reduce(ov1.ap, pmt[:, 0:32].ap, AX.X, ALU.add),
                 reads=[pmt.buf], writes=[ov1.buf])
            S.op("dve", lambda e: e.tensor_reduce(ov2.ap, pmt[:, 32:64].ap, AX.X, ALU.add),
                 reads=[pmt.buf], writes=[ov2.buf])
            tt("dve", destf[:, 0:1], ov1, p1loc, ALU.add)
            tt("dve", destf[:, 1:2], ov2, p2loc, ALU.add)
            ts("dve", ov1, p1loc, float(CAP) - 0.5, ALU.is_gt, float(4 * NXB), ALU.mult)
            ts("dve", ov2, p2loc, float(CAP) - 0.5, ALU.is_gt, float(4 * NXB), ALU.mult)
            tt("dve", destf[:, 0:1], destf[:, 0:1], ov1, ALU.add)
            tt("dve", destf[:, 1:2], destf[:, 1:2], ov2, ALU.add)
            cp("dve", destall[:, ob, :], destf[:])
            tt("dve", base[:], P[3][:, 64:96], base[:], ALU.add)
            tt("dve", base[:], P[3][:, 96:128], base[:], ALU.add)
            for k in range(2 if "noscat" not in debug else 0):
                dv_ = destall[:, ob, k:k + 1]
                S.dma("pool", (lambda dv_, hn_: (lambda e: e.indirect_dma_start(
                    out=xb_scr[:].ap, out_offset=bass.IndirectOffsetOnAxis(ap=dv_.ap, axis=0),
                    in_=hn_[:].ap, in_offset=None, bounds_check=bc_reg(e), oob_is_err=False)))(dv_, hn_),
                    reads=[hn_.buf, destall.buf], writes=[xb_scr.buf])
    dbg("destall", destall[:], [128, NOB, 2], I32)
    dbg("gwall", gwall[:], [128, NOB, 2])
    close_scope(scope2)
    if nphase < 3 or not moe:
        return finish()
    if nphase >= 3 and moe:
        scope3 = open_scope()
        W1 = [sb("W1_%d" % i, [128, 8, 512], BF16) for i in range(2)]
        W3 = [sb("W3_%d" % i, [128, 8, 512], BF16) for i in range(2)]
        W2 = [sb("W2_%d" % i, [128, 4, D], BF16) for i in range(2)]
        stg1 = sb("stg1", [128, 8, 512])
        stg3 = sb("stg3", [128, 8, 512])
        stg2 = sb("stg2", [128, 4, D])
        xg = [sb("xg%d" % i, [128, D]) for i in range(2)]
        xeT = sb("xeT", [128, 8, CAP], BF16)
        s1 = sb("s1", [128, CAP])
        hgT = sb("hgT", [128, 4, CAP], BF16)
        ybk = [sb("ybk%d" % i, [128, D]) for i in range(2)]

        def load_expert(ex_):
            dma("sp", stg1[:], w1[ex_].re("(p kc) f -> p kc f", p=128))
            dma("sp", stg3[:], w3[ex_].re("(p kc) f -> p kc f", p=128))
            dma("sp", stg2[:], w2[ex_].re("(p fc) n -> p fc n", p=128))

        def cast_expert(ex_):
            i = ex_ % 2
            cp("act", W1[i][:, 0:4, :], stg1[:, 0:4, :])
            cp("dve", W1[i][:, 4:8, :], stg1[:, 4:8, :])
            cp("act", W3[i][:, 0:4, :], stg3[:, 0:4, :])
            cp("dve", W3[i][:, 4:8, :], stg3[:, 4:8, :])
            cp("act", W2[i][:, 0:2, :], stg2[:, 0:2, :])
            cp("dve", W2[i][:, 2:4, :], stg2[:, 2:4, :])

        load_expert(0)
        cast_expert(0)
        for ex_ in range(32):
            w1_, w3_, w2_ = W1[ex_ % 2], W3[ex_ % 2], W2[ex_ % 2]
            if ex_ + 1 < 32:
                load_expert(ex_ + 1)
            for c in range(CAPB):
                xg_ = xg[c % 2]
                dma("sp", xg_[:], xb_scr[ex_ * CAP + c * 128:ex_ * CAP + (c + 1) * 128, :])
                xg3 = xg_[:].re("t (p k) -> t k p", k=8)
                for half in range(2):
                    for k in range(4):
                        kc = half * 4 + k
                        tr(P[6 + half][:, k * 128:(k + 1) * 128], xg3[:, kc, :], ident[:])
                    cp("act" if half == 0 else "dve", xeT[:, half * 4:half * 4 + 4, c * 128:(c + 1) * 128],
                       P[6 + half][:, :].re("p (k n) -> p k n", n=128))
            for fc in range(4):
                for kc in range(8):
                    mm(P[0][:, 0:CAP], w1_[:, kc, :].re("p (m f) -> p f m", f=4)[:, fc, :], xeT[:, kc, :],
                       start=(kc == 0), stop=(kc == 7))
                for kc in range(8):
                    mm(P[1][:, 0:CAP], w3_[:, kc, :].re("p (m f) -> p f m", f=4)[:, fc, :], xeT[:, kc, :],
                       start=(kc == 0), stop=(kc == 7))
                act(s1[:], P[0][:, 0:CAP], AF.Silu)
                tt("dve", hgT[:, fc, :], s1[:], P[1][:, 0:CAP], ALU.mult)
            for c in range(CAPB):
                yb_ = ybk[c % 2]
                for half in range(2):
                    for fc in range(4):
                        mm(P[2 + half][:, :], hgT[:, fc, c * 128:(c + 1) * 128], w2_[:, fc, half * 512:(half + 1) * 512],
                           start=(fc == 0), stop=(fc == 3))
                    cp("act" if half == 0 else "dve", yb_[:, half * 512:(half + 1) * 512], P[2 + half][:, :])
                dma("sp", yb_scr[ex_ * CAP + c * 128:ex_ * CAP + (c + 1) * 128, :], yb_[:])
            if ex_ + 1 < 32:
                cast_expert(ex_ + 1)
        if nphase < 4:
            close_scope(scope3)
            return finish()
        yk = [sb("yk%d" % i, [128, D]) for i in range(2)]
        hb4 = [sb("hb4_%d" % i, [128, D]) for i in range(2)]
        oo = [sb("oo%d" % i, [128, D]) for i in range(2)]
        junk4 = sb("junk4", [128, D], BF16)
        ssq4 = sb("ssq4", [128, 1])
        rstd4 = sb("rstd4", [128, 1])
        for ob in range(NOB):
            hb_ = hb4[ob % 2]
            dma("sp", hb_[:], h_scr[ob * 128:(ob + 1) * 128, :])
            for k in range(2):
                yk_ = yk[k]
                S.op("pool", (lambda yk_: (lambda e: e.memset(yk_[:].ap, 0.0)))(yk_), writes=[yk_.buf])
                dv_ = destall[:, ob, k:k + 1]
                S.dma("pool", (lambda dv_, yk_: (lambda e: e.indirect_dma_start(
                    out=yk_[:].ap, out_offset=None, in_=yb_scr[:].ap,
                    in_offset=bass.IndirectOffsetOnAxis(ap=dv_.ap, axis=0),
                    bounds_check=bc_reg(e), oob_is_err=False)))(dv_, yk_),
                    reads=[yb_scr.buf, destall.buf], writes=[yk_.buf])
                stt(hb_[:], yk_[:], gwall[:, ob, k:k + 1], hb_[:], ALU.mult, ALU.add)
            oo_ = oo[ob % 2]
            act(junk4[:], hb_[:], AF.Square, accum=ssq4[:])
            act(rstd4[:], ssq4[:], AF.Ln, scale=1.0 / D, bias=epsc[:])
            act(rstd4[:], rstd4[:], AF.Exp, scale=-0.5)
            stt(oo_[:], hb_[:], rstd4[:], grep_fin[:], ALU.mult, ALU.mult)
            dma("sp", out_own[ob * 128:(ob + 1) * 128, :], oo_[:])
        close_scope(scope3)

    return finish()


def _consts(NSLOT, CAP):
    bf = ml_dtypes.bfloat16
    r = np.arange(128)[:, None]
    c = np.arange(128)[None, :]
    k = {}
    k["k_ident"] = (r == c).astype(np.float32)
    k["k_triltf"] = (r < c).astype(np.float32)
    k["k_onesf"] = np.ones((128, 128), np.float32)
    k["k_jrow"] = np.arange(128, dtype=np.float32)[None, :]
    mB = np.zeros((128, 2, 16), np.float32)
    for g2 in range(2):
        mB[g2 * 64:(g2 + 1) * 64, g2, :] = 1.0
    k["k_maskB"] = mB.reshape(128, 32)
    mC = np.zeros((128, 2, 64), np.float32)
    for rr in range(128):
        mC[rr, (rr // 16) % 2, :] = 1.0
    k["k_maskC"] = mC.reshape(128, 128)
    mQ = np.zeros((128, 2), np.float32)
    for rr in range(128):
        mQ[rr, (rr // 32) % 2] = 1.0
    k["k_maskQ"] = mQ
    mR = np.zeros((128, 2, 128), np.float32)
    for cc in range(128):
        mR[:, (cc // 32) % 2, cc] = 1.0
    k["k_maskR"] = mR.reshape(128, 256)
    k["k_ecap"] = np.tile((np.arange(32, dtype=np.float32) * CAP)[None, :], (128, 1))
    k["k_trige"] = (r >= c).astype(bf)
    k["k_trilt"] = (r < c).astype(bf)
    k["k_trile"] = (r <= c).astype(bf)
    m0 = np.ones((128, 512), np.float32)
    m0[:112, :] = 0.0
    k["k_mask0"] = m0.astype(bf)
    j = np.arange(128)[:, None]
    t = np.arange(512)[None, :]
    patA = np.zeros((128, 8, 512), np.float32)
    patB = np.zeros((128, 8, 512), np.float32)
    for s in range(8):
        if s < 4:
            rr = 3 - s
            patB[:, s, :] = (rr * 128 + j < t)
        else:
            rr = 7 - s
            patA[:, s, :] = (rr * 128 + j < t)
            patB[:, s, :] = 1.0
    return k, patA.astype(bf), patB.astype(bf)


def own_is_A(n, hf):
    return ((n + hf) % 2) == 0


def make_in_maps(inputs, NSLOT, CAP, n_cores=8, moe=True):
    x = np.asarray(inputs["x"], np.float32)
    NB = 1 + 8 * NSLOT
    NPOS = NB * 128
    meta = np.asarray(inputs["meta_tokens"], np.float32)
    k, patA, patB = _consts(NSLOT, CAP)
    f = lambda a: np.ascontiguousarray(np.asarray(a, np.float32))
    shared = dict(
        w_in=f(inputs["w_in"][0]), g_mix=f(inputs["norm_mix_g"][0:1]), g_ffn=f(inputs["norm_ffn_g"][0:1]),
        g_fin=f(inputs["norm_final_g"]).reshape(1, D),
        lam_re=f(inputs["ssm_lambda_re"][0]).reshape(1, 2048), lam_im=f(inputs["ssm_lambda_im"][0]).reshape(1, 2048),
        log_dt=f(inputs["ssm_log_dt"][0]).reshape(1, 32),
        b_re=f(inputs["ssm_b_re"][0]), b_im=f(inputs["ssm_b_im"][0]),
        c_re=f(inputs["ssm_c_re"][0]).reshape(512, 64), c_im=f(inputs["ssm_c_im"][0]).reshape(512, 64),
        d_skip=f(inputs["ssm_d"][0]).reshape(1, 512), glu_w=f(inputs["ssm_glu_w"][0]),
        glu_b=f(inputs["ssm_glu_b"][0]).reshape(1, 512),
        w_pa=f(inputs["w_branch_attn"][0]), w_pb=f(inputs["w_branch_ssm"][0]), w_o=f(inputs["w_out"][0]),
        w_r=f(np.concatenate([inputs["router_group_w"][0], inputs["router_expert_w"][0]], axis=1)),
        b_r=f(np.concatenate([inputs["router_group_b"][0], inputs["router_expert_b"][0]], axis=0)).reshape(1, 36),
        w1=f(inputs["expert_w1"][0][:(32 if moe else 1)]), w3=f(inputs["expert_w3"][0][:(32 if moe else 1)]),
        w2=f(inputs["expert_w2"][0][:(32 if moe else 1)]),
    )
    shared.update(k)
    maps = []
    for core in range(n_cores):
        b, hf = core // 2, core % 2
        xa = np.zeros((NPOS, D), np.float32)
        xa[112:128] = meta
        xa[128:] = x[b, :NPOS - 128]
        xo = np.zeros((512 * NSLOT, D), np.float32)
        sel = np.zeros((128, 2 * NSLOT), np.float32)
        for n in range(NSLOT):
            a = own_is_A(n, hf)
            r0 = (8 * n + (0 if a else 4)) * 128
            xo[n * 512:(n + 1) * 512] = x[b, r0:r0 + 512]
            sel[:, 2 * n] = 1.0 if a else 0.0
            sel[:, 2 * n + 1] = 0.0 if a else 1.0
        m = dict(shared)
        m["x_all"] = xa
        m["x_own"] = xo
        m["k_sel"] = sel
        m["k_maske"] = patA if own_is_A(0, hf) else patB
        m["k_masko"] = patA if own_is_A(1, hf) else patB
        maps.append(m)
    return maps


def assemble(results, NSLOT, B, SEQ):
    out = np.zeros((B, SEQ, D), np.float32)
    for core, r in enumerate(results):
        b, hf = core // 2, core % 2
        oo = np.asarray(r["out_own"], np.float32)
        for n in range(NSLOT):
            a = own_is_A(n, hf)
            r0 = (8 * n + (0 if a else 4)) * 128
            out[b, r0:r0 + 512] = oo[n * 512:(n + 1) * 512]
    return out


_NC_CACHE = {}


def kernel(**inputs):
    NSLOT, CAPB = 8, 3
    key = (NSLOT, CAPB)
    if key not in _NC_CACHE:
        _NC_CACHE[key] = build(NSLOT, CAPB)
    nc = _NC_CACHE[key]
    maps = make_in_maps(inputs, NSLOT, 128 * CAPB)
    res = run_bass_kernel_spmd(nc, maps, core_ids=list(range(8)))
    return assemble(res.results, NSLOT, 4, 8192)
```

```python
import math
from contextlib import ExitStack
import numpy as np
import ml_dtypes
import concourse.bass as bass
import concourse.mybir as mybir
from concourse.bass_utils import run_bass_kernel_spmd

F32 = mybir.dt.float32
BF16 = mybir.dt.bfloat16
I32 = mybir.dt.int32
AF = mybir.ActivationFunctionType
ALU = mybir.AluOpType
AX = mybir.AxisListType

SAME_ENGINE_SYNC = True
NDS = 20
D = 1024
TWO_PI = 2.0 * math.pi


class Buf:
    def __init__(self, name):
        self.name = name
        self.w = None
        self.r = {}


class View:
    def __init__(self, ap, buf):
        self.ap = ap
        self.buf = buf

    def __getitem__(self, k):
        return View(self.ap[k], self.buf)

    def re(self, pat, **kw):
        return View(self.ap.rearrange(pat, **kw), self.buf)

    def bc(self, shape):
        return View(self.ap.broadcast_to(list(shape)), self.buf)

    def un(self, axis):
        return View(self.ap.unsqueeze(axis), self.buf)


class T:
    def __init__(self, h, name):
        self.h = h
        self.buf = Buf(name)

    def __getitem__(self, k):
        return View(self.h[k], self.buf)


class Sched:
    ENG = ["pe", "act", "dve", "pool", "sp"]

    def __init__(self, nc, stack):
        self.nc = nc
        self.stack = stack
        self.epoch = {e: 0 for e in self.ENG}
        self.last_tok = {e: None for e in self.ENG}
        self.sem = {e: stack.enter_context(nc.semaphore("s_" + e)) for e in self.ENG}
        self.cnt = {e: 0 for e in self.ENG}
        self.ops = {e: [] for e in self.ENG}
        self.waited = {e: {} for e in self.ENG}
        self.dsem = {q: [stack.enter_context(nc.semaphore("d_%s%d" % (q, i))) for i in range(NDS)]
                     for q in ("sp", "pool")}
        self.dcnt = {q: [0] * NDS for q in ("sp", "pool")}
        self.drr = {"sp": 0, "pool": 0}

    @staticmethod
    def _flat(bufs):
        out = []
        for b in bufs:
            if isinstance(b, (tuple, list)):
                out.extend(b)
            else:
                out.append(b)
        return out

    def _deps(self, reads, writes):
        reads = self._flat(reads)
        writes = self._flat(writes)
        deps = []
        for b in reads:
            if b.w is not None:
                deps.append(b.w)
        for b in writes:
            if b.w is not None:
                deps.append(b.w)
            deps.extend(b.r.values())
        return deps

    def _waits(self, e, deps):
        need = {}
        for (key, sem, val) in deps:
            if val <= 0:
                continue
            if key[0] == "E" and key[1] == e and (e == "pe" or not SAME_ENGINE_SYNC):
                continue
            if self.waited[e].get(key, 0) >= val:
                continue
            if key not in need or need[key][1] < val:
                need[key] = (sem, val)
        for key, (sem, val) in need.items():
            self.waited[e][key] = val
        return list(need.values())

    def _record(self, tok, reads, writes):
        reads = self._flat(reads)
        writes = self._flat(writes)
        for b in reads:
            old = b.r.get(tok[0])
            if old is None or old[2] < tok[2]:
                b.r[tok[0]] = tok
        for b in writes:
            b.w = tok
            b.r = {}

    def op(self, e, fn, reads=(), writes=()):
        waits = self._waits(e, self._deps(reads, writes))
        if self.cnt[e] >= 16000:
            self.epoch[e] += 1
            self.sem[e] = self.stack.enter_context(self.nc.semaphore("s_%s_%d" % (e, self.epoch[e])))
            self.cnt[e] = 0
        self.cnt[e] += 1
        tok = (("E", e, self.epoch[e]), self.sem[e], self.cnt[e])
        self.last_tok[e] = tok
        self.ops[e].append((waits, fn, self.sem[e], 1))
        self._record(tok, reads, writes)

    def dma(self, q, fn, reads=(), writes=()):
        deps = self._deps(reads, writes)
        i = self.drr[q]
        self.drr[q] = (i + 1) % NDS
        sem = self.dsem[q][i]
        key = ("D", q, i)
        prev = self.dcnt[q][i]
        deps.append((key, sem, prev))
        waits = self._waits(q, deps)
        self.dcnt[q][i] = prev + 16
        tok = (key, sem, prev + 16)
        self.ops[q].append((waits, fn, sem, 16))
        self._record(tok, reads, writes)

    def barrier(self):
        toks = [self.last_tok[e] for e in self.ENG if self.last_tok[e] is not None]
        for q in ("sp", "pool"):
            for i in range(NDS):
                if self.dcnt[q][i] > 0:
                    toks.append((("D", q, i), self.dsem[q][i], self.dcnt[q][i]))
        for e in self.ENG:
            waits = self._waits(e, toks)
            if waits:
                self.ops[e].append((waits, None, None, 0))

    def replay(self, e, eng):
        for waits, fn, sem, inc in self.ops[e]:
            for (s, v) in waits:
                eng.wait_ge(s, v)
            if fn is not None:
                fn(eng).then_inc(sem, inc)

    def final_waits(self, e, eng, bufs):
        deps = []
        for b in self._flat(bufs):
            if b.w is not None:
                deps.append(b.w)
        for (s, v) in self._waits(e, deps):
            eng.wait_ge(s, v)


def build(NSLOT, CAPB, debug=(), moe=True, nphase=4):
    NB = 1 + 8 * NSLOT
    NPOS = NB * 128
    TOWN = 512 * NSLOT
    NOB = 4 * NSLOT
    CAP = 128 * CAPB
    NXB = 32 * CAP
    nc = bass.Bass("TRN2", target_bir_lowering=False)
    stack = ExitStack()
    S = Sched(nc, stack)

    def din(name, shape, dt=F32):
        return T(nc.dram_tensor(name, list(shape), dt, kind="ExternalInput").ap(), name)

    def dscr(name, shape, dt=F32):
        kind = "ExternalOutput" if "scr" in debug else "Internal"
        return T(nc.dram_tensor(name, list(shape), dt, kind=kind).ap(), name)

    def dout(name, shape, dt=F32):
        return T(nc.dram_tensor(name, list(shape), dt, kind="ExternalOutput").ap(), name)

    cur = [stack]

    def sb(name, shape, dt=F32):
        return T(cur[0].enter_context(nc.sbuf_tensor(name, list(shape), dt)), name)

    def open_scope():
        st = ExitStack()
        cur.insert(0, st)
        return st

    def close_scope(st):
        assert cur[0] is st
        S.barrier()
        st.close()
        cur.pop(0)

    x_all = din("x_all", [NPOS, D])
    x_own = din("x_own", [TOWN, D])
    w_in = din("w_in", [D, 4096])
    g_mix = din("g_mix", [1, D])
    g_ffn = din("g_ffn", [1, D])
    g_fin = din("g_fin", [1, D])
    lam_re = din("lam_re", [1, 2048])
    lam_im = din("lam_im", [1, 2048])
    log_dt = din("log_dt", [1, 32])
    b_re = din("b_re", [32, 64, 16])
    b_im = din("b_im", [32, 64, 16])
    c_re = din("c_re", [512, 64])
    c_im = din("c_im", [512, 64])
    d_skip = din("d_skip", [1, 512])
    glu_w = din("glu_w", [512, 512])
    glu_b = din("glu_b", [1, 512])
    w_pa = din("w_pa", [512, D])
    w_pb = din("w_pb", [512, D])
    w_o = din("w_o", [D, D])
    w_r = din("w_r", [D, 36])
    b_r = din("b_r", [1, 36])
    NE = 32 if moe else 1
    w1 = din("w1", [NE, D, 512])
    w3 = din("w3", [NE, D, 512])
    w2 = din("w2", [NE, 512, D])
    k_ident = din("k_ident", [128, 128])
    k_triltf = din("k_triltf", [128, 128])
    k_onesf = din("k_onesf", [128, 128])
    k_jrow = din("k_jrow", [1, 128])
    k_maskB = din("k_maskB", [128, 32])
    k_maskC = din("k_maskC", [128, 128])
    k_ecap = din("k_ecap", [128, 32])
    k_maskQ = din("k_maskQ", [128, 2])
    k_maskR = din("k_maskR", [128, 256])
    k_trige = din("k_trige", [128, 128], BF16)
    k_trilt = din("k_trilt", [128, 128], BF16)
    k_trile = din("k_trile", [128, 128], BF16)
    k_mask0 = din("k_mask0", [128, 512], BF16)
    k_maske = din("k_maske", [128, 8, 512], BF16)
    k_masko = din("k_masko", [128, 8, 512], BF16)
    k_sel = din("k_sel", [128, 2 * NSLOT])

    kt_scr = dscr("kt_scr", [512, NPOS], BF16)
    v_scr = dscr("v_scr", [8, 128, NB, 64], BF16)
    ssm_scr = dscr("ssm_scr", [4, 128, NPOS], BF16)
    h_scr = dscr("h_scr", [TOWN, D])
    xb_scr = dscr("xb_scr", [NXB, D])
    yb_scr = dscr("yb_scr", [NXB, D])
    out_own = dout("out_own", [TOWN, D])

    dbg_outs = {}

    def dbg(name, view, shape, dt=F32):
        if name not in debug:
            return
        t = dout("dbg_" + name, shape, dt)
        dbg_outs[name] = t
        dma("sp", t[:], view)

    bcreg = {}

    def bc_reg(e):
        if "r" not in bcreg:
            bcreg["r"] = e.to_reg(NXB - 1)
        return bcreg["r"]

    def mm(out, lhsT, rhs, start=True, stop=True):
        S.op("pe", lambda e: e.matmul(out.ap, lhsT.ap, rhs.ap, start=start, stop=stop),
             reads=[lhsT.buf, rhs.buf], writes=[out.buf])

    def tr(out, in_, ident_v):
        S.op("pe", lambda e: e.transpose(out.ap, in_.ap, ident_v.ap),
             reads=[in_.buf, ident_v.buf], writes=[out.buf])

    def act(out, in_, func, bias=None, scale=None, accum=None, extra_reads=()):
        kw = {}
        reads = [in_.buf] + list(extra_reads)
        writes = [out.buf]
        if bias is not None:
            if isinstance(bias, View):
                kw["bias"] = bias.ap
                reads.append(bias.buf)
            else:
                kw["bias"] = bias
        if scale is not None:
            if isinstance(scale, View):
                kw["scale"] = scale.ap
                reads.append(scale.buf)
            else:
                kw["scale"] = scale
        if accum is not None:
            kw["accum_out"] = accum.ap
            writes.append(accum.buf)
        S.op("act", lambda e: e.activation(out.ap, in_.ap, func, **kw), reads=reads, writes=writes)

    def tt(eng, out, in0, in1, op):
        S.op(eng, lambda e: e.tensor_tensor(out.ap, in0.ap, in1.ap, op),
             reads=[in0.buf, in1.buf], writes=[out.buf])

    def _sc(x, reads):
        if isinstance(x, View):
            reads.append(x.buf)
            return x.ap
        return x

    def ts(eng, out, in0, s1, op0, s2=None, op1=None):
        reads = [in0.buf]
        a1 = _sc(s1, reads)
        a2 = _sc(s2, reads)
        if op1 is None:
            S.op(eng, lambda e: e.tensor_scalar(out.ap, in0.ap, a1, None, op0), reads=reads, writes=[out.buf])
        else:
            S.op(eng, lambda e: e.tensor_scalar(out.ap, in0.ap, a1, a2, op0, op1), reads=reads,
                 writes=[out.buf])

    def stt(out, in0, scalar, in1, op0, op1):
        reads = [in0.buf, in1.buf]
        a = _sc(scalar, reads)
        S.op("dve", lambda e: e.scalar_tensor_tensor(out.ap, in0.ap, a, in1.ap, op0, op1),
             reads=reads, writes=[out.buf])

    def cp(eng, out, in_):
        if eng == "act":
            S.op("act", lambda e: e.copy(out.ap, in_.ap), reads=[in_.buf], writes=[out.buf])
        else:
            S.op(eng, lambda e: e.tensor_copy(out.ap, in_.ap), reads=[in_.buf], writes=[out.buf])

    def dma(q, out, in_, **kw):
        S.dma(q, lambda e: e.dma_start(out=out.ap, in_=in_.ap, **kw), reads=[in_.buf], writes=[out.buf])

    Pall_h = stack.enter_context(nc.psum_tensor("psall", [128, 4096], F32))

    class PB:
        def __init__(self, i0, nb):
            self.i0, self.nb = i0, nb
            self.buf = tuple(pbufs[i0:i0 + nb]) if nb > 1 else pbufs[i0]

        def __getitem__(self, k):
            return View(Pall_h[:, self.i0 * 512:(self.i0 + self.nb) * 512][k], self.buf)

    pbufs = [Buf("ps%d" % i) for i in range(8)]
    P = [PB(i, 1) for i in range(8)]
    PQ = PB(2, 4)
    P01 = PB(0, 2)
    P45 = PB(4, 2)
    P67 = PB(6, 2)

    def finish():
        final_bufs = [out_own.buf, kt_scr.buf, v_scr.buf, ssm_scr.buf, h_scr.buf, xb_scr.buf, yb_scr.buf] + [t.buf for t in dbg_outs.values()]
        with nc.Block() as block:
            @block.tensor
            def _(e):
                S.replay("pe", e)

            @block.scalar
            def _(e):
                S.replay("act", e)

            @block.vector
            def _(e):
                S.replay("dve", e)

            @block.gpsimd
            def _(e):
                S.replay("pool", e)

            @block.sync
            def _(e):
                S.replay("sp", e)
                S.final_waits("sp", e, final_bufs)
        stack.close()
        return nc

    ident = sb("ident", [128, 128])
    triltf = sb("triltf", [128, 128])
    onesf = sb("onesf", [128, 128])
    jrow = sb("jrow", [1, 128])
    maskB = sb("maskB", [128, 32])
    maskC = sb("maskC", [128, 128])
    ecap = sb("ecap", [128, 32])
    maskQ = sb("maskQ", [128, 2])
    maskR = sb("maskR", [128, 256])
    trige = sb("trige", [128, 128], BF16)
    trilt = sb("trilt", [128, 128], BF16)
    trile = sb("trile", [128, 128], BF16)
    sel = sb("sel", [128, 2 * NSLOT])
    epsc = sb("epsc", [128, 1])
    onec = sb("onec", [128, 1])
    grep_mix = sb("grep_mix", [128, D])
    grep_ffn = sb("grep_ffn", [128, D])
    grep_fin = sb("grep_fin", [128, D])
    brrep = sb("brrep", [128, 36])
    destall = sb("destall", [128, NOB, 2], I32)
    gwall = sb("gwall", [128, NOB, 2])
    scopeA = open_scope()
    for dst, src in ((ident, k_ident), (triltf, k_triltf), (onesf, k_onesf), (jrow, k_jrow), (maskB, k_maskB),
                     (maskC, k_maskC), (ecap, k_ecap), (maskQ, k_maskQ), (maskR, k_maskR), (trige, k_trige), (trilt, k_trilt), (trile, k_trile),
                     (sel, k_sel)):
        dma("sp", dst[:], src[:])

    dcol = sb("dcol", [128, 4])
    gbcol = sb("gbcol", [128, 4])
    T1re = sb("T1re", [128, 2048])
    T1im = sb("T1im", [128, 2048])
    T2re = sb("T2re", [128, 2048])
    T2im = sb("T2im", [128, 2048])
    are = sb("are", [128, 16])
    aim = sb("aim", [128, 16])
    Bp = sb("Bp", [128, 4, 2, 128])
    Cp = sb("Cp", [128, 4, 2, 128])
    Bq = sb("Bq", [128, 4, 2, 2, 128], BF16)
    Cq = sb("Cq", [128, 4, 2, 2, 128], BF16)
    s16 = [sb("s16_%d" % i, [128, 16]) for i in range(6)]
    scope0 = open_scope()
    rowtmp = sb("rowtmp", [1, 2048])

    def replicate_row(dst, src_dram, n):
        dma("sp", rowtmp[0:1, 0:n], src_dram[0:1, 0:n])
        for c0 in range(0, n, 512):
            c1 = min(n, c0 + 512)
            mm(P[0][:, 0:c1 - c0], onesf[0:1, :], rowtmp[0:1, c0:c1])
            cp("dve", dst[:, c0:c1], P[0][:, 0:c1 - c0])

    replicate_row(grep_mix, g_mix, D)
    replicate_row(grep_ffn, g_ffn, D)
    replicate_row(grep_fin, g_fin, D)
    replicate_row(brrep, b_r, 36)
    S.op("dve", lambda e: e.memset(onec[:].ap, 1.0), writes=[onec.buf])

    def row_to_col(dst, src_dram, nchunk):
        dma("sp", rowtmp[0:1, 0:nchunk * 128], src_dram[0:1, 0:nchunk * 128])
        for q in range(nchunk):
            mm(P[0][:, 2 * q:2 * q + 2], rowtmp[0:1, q * 128:(q + 1) * 128], onesf[0:1, 0:2])
        cp("dve", dst[:, 0:nchunk], P[0][:, 0:2 * nchunk].re("p (q t) -> p q t", t=2)[:, :, 0])

    row_to_col(dcol, d_skip, 4)
    row_to_col(gbcol, glu_b, 4)

    lrrow = sb("lrrow", [1, 2048])
    lirow = sb("lirow", [1, 2048])
    dtrow = sb("dtrow", [1, 32])
    rhorow = sb("rhorow", [1, 2048])
    throw = sb("throw", [1, 2048])
    dma("sp", lrrow[:], lam_re[:])
    dma("sp", lirow[:], lam_im[:])
    dma("sp", dtrow[:], log_dt[:])
    act(dtrow[:], dtrow[:], AF.Exp)
    dtb = dtrow[:].un(2).bc([1, 32, 64])
    tt("dve", rhorow[:].re("o (g p) -> o g p", p=64), lrrow[:].re("o (g p) -> o g p", p=64), dtb, ALU.mult)
    tt("dve", throw[:].re("o (g p) -> o g p", p=64), lirow[:].re("o (g p) -> o g p", p=64), dtb, ALU.mult)

    wkA = sb("wkA", [128, 2048])
    wkB = sb("wkB", [128, 2048])
    wkI = sb("wkI", [128, 2048], I32)
    wkM = sb("wkM", [128, 2048])

    def outer_tok(row):
        for k in range(4):
            mm(P[k][:, :], jrow[0:1, :], row[0:1, k * 512:(k + 1) * 512])

    def outer_feat(row):
        for gp in range(16):
            mm(P[gp // 4][:, (gp % 4) * 128:(gp % 4 + 1) * 128], row[0:1, gp * 128:(gp + 1) * 128], jrow[0:1, :])

    def sin_of(dst, shift):
        for k in range(4):
            ts("dve", wkA[:, k * 512:(k + 1) * 512], P[k][:, :], 1.0 / TWO_PI, ALU.mult, 0.5 + shift, ALU.add)
        cp("dve", wkI[:], wkA[:])
        cp("dve", wkB[:], wkI[:])
        for k in range(4):
            stt(wkA[:, k * 512:(k + 1) * 512], wkB[:, k * 512:(k + 1) * 512], -TWO_PI, P[k][:, :],
                ALU.mult, ALU.add)
        if shift != 0.0:
            ts("dve", wkA[:], wkA[:], shift * TWO_PI, ALU.add)
        ts("dve", wkB[:], wkA[:], math.pi, ALU.is_gt)
        stt(wkA[:], wkB[:], -TWO_PI, wkA[:], ALU.mult, ALU.add)
        ts("dve", wkB[:], wkA[:], -math.pi, ALU.is_lt)
        stt(wkA[:], wkB[:], TWO_PI, wkA[:], ALU.mult, ALU.add)
        ts("dve", wkA[:], wkA[:], 3.14159, ALU.min, -3.14159, ALU.max)
        act(dst, wkA[:], AF.Sin)

    def build_table(Tre, Tim, outer_fn, sign):
        outer_fn(rhorow)
        for k in range(4):
            act(wkM[:, k * 512:(k + 1) * 512], P[k][:, :], AF.Exp, scale=float(sign))
        outer_fn(throw)
        sin_of(Tim[:], 0.0)
        sin_of(Tre[:], 0.25)
        tt("dve", Tre[:], Tre[:], wkM[:], ALU.mult)
        stt(Tim[:], Tim[:], float(sign), wkM[:], ALU.mult, ALU.mult)

    build_table(T1re, T1im, outer_tok, -1)
    build_table(T2re, T2im, outer_feat, +1)

    lrc = sb("lrc", [128, 16])
    lic = sb("lic", [128, 16])
    for dst, row in ((lrc, lrrow), (lic, lirow)):
        for gp in range(16):
            mm(P[4][:, 2 * gp:2 * gp + 2], row[0:1, gp * 128:(gp + 1) * 128], onesf[0:1, 0:2])
        cp("dve", dst[:], P[4][:, 0:32].re("p (q t) -> p q t", t=2)[:, :, 0])
    T2re3 = T2re[:].re("p (g j) -> p g j", j=128)
    T2im3 = T2im[:].re("p (g j) -> p g j", j=128)
    cp("dve", are[:], T2re3[:, :, 1])
    cp("dve", aim[:], T2im3[:, :, 1])
    fre = sb("fre", [128, 16])
    fim = sb("fim", [128, 16])
    nr, den, t0, t1, t2, t3 = [s[:] for s in s16]
    ts("dve", nr, are[:], -1.0, ALU.add)
    tt("dve", t0, lrc[:], lrc[:], ALU.mult)
    tt("dve", t1, lic[:], lic[:], ALU.mult)
    tt("dve", den, t0, t1, ALU.add)
    S.op("dve", lambda e: e.reciprocal(den.ap, den.ap), reads=[den.buf], writes=[den.buf])
    tt("dve", t0, nr, lrc[:], ALU.mult)
    tt("dve", t1, aim[:], lic[:], ALU.mult)
    tt("dve", t2, t0, t1, ALU.add)
    tt("dve", fre[:], t2, den, ALU.mult)
    tt("dve", t0, aim[:], lrc[:], ALU.mult)
    tt("dve", t1, nr, lic[:], ALU.mult)
    tt("dve", t2, t0, t1, ALU.subtract)
    tt("dve", fim[:], t2, den, ALU.mult)

    Bsr = sb("Bsr", [128, 16, 16])
    Bsi = sb("Bsi", [128, 16, 16])
    for dst, src in ((Bsr, b_re), (Bsi, b_im)):
        v = src[:].re("(gp g2) p c -> g2 p gp c", g2=2)
        for g2 in range(2):
            dma("sp", dst[g2 * 64:(g2 + 1) * 64, :, :], v[g2])
    Bbr = sb("Bbr", [128, 16, 16])
    Bbi = sb("Bbi", [128, 16, 16])
    tB0 = sb("tB0", [128, 16, 16])
    tB1 = sb("tB1", [128, 16, 16])
    freb = fre[:].un(2).bc([128, 16, 16])
    fimb = fim[:].un(2).bc([128, 16, 16])
    tt("dve", tB0[:], Bsr[:], freb, ALU.mult)
    tt("dve", tB1[:], Bsi[:], fimb, ALU.mult)
    tt("dve", Bbr[:], tB0[:], tB1[:], ALU.subtract)
    tt("dve", tB0[:], Bsi[:], freb, ALU.mult)
    tt("dve", tB1[:], Bsr[:], fimb, ALU.mult)
    tt("dve", Bbi[:], tB0[:], tB1[:], ALU.add)
    dbg("Bbr", Bbr[:], [128, 16, 16])
    Y2 = sb("Y2", [128, 2, 16, 2, 16])
    mBb = maskB[:].re("p (a c) -> p a c", a=2).un(1).bc([128, 16, 2, 16])
    for pl, Bb in ((0, Bbr), (1, Bbi)):
        tt("dve", Y2[:, pl], Bb[:].un(2).bc([128, 16, 2, 16]), mBb, ALU.mult)
    for pl in range(2):
        for Q in range(4):
            tr(P[5][:, Q * 128:(Q + 1) * 128], Y2[:, pl, 4 * Q:4 * Q + 4].re("p a b c -> p (a b c)"), ident[:])
        cp("dve", Bp[:, :, pl, :], P[5][:, :].re("p (q n) -> p q n", n=128))
        for lh in range(2):
            ts("dve", Bq[:, :, lh, pl, :], Bp[:, :, pl, :], maskQ[:, lh:lh + 1], ALU.mult)
    Csr = sb("Csr", [128, 4, 64])
    Csi = sb("Csi", [128, 4, 64])
    dma("sp", Csr[:], c_re[:].re("(q r) p -> r q p", r=128))
    dma("sp", Csi[:], c_im[:].re("(q r) p -> r q p", r=128))
    Xc = sb("Xc", [128, 2, 4, 2, 64])
    mCb = maskC[:].re("r (a p) -> r a p", a=2).un(1).bc([128, 4, 2, 64])
    tt("dve", Xc[:, 0], Csr[:].un(2).bc([128, 4, 2, 64]), mCb, ALU.mult)
    for Q in range(4):
        stt(Xc[:, 1, Q], Csi[:, Q].un(1).bc([128, 2, 64]), -1.0, maskC[:].re("r (a p) -> r a p", a=2),
            ALU.mult, ALU.mult)
    for pl in range(2):
        for Q in range(4):
            tr(P[5][:, Q * 128:(Q + 1) * 128], Xc[:, pl, Q].re("r a p -> r (a p)"), ident[:])
        cp("dve", Cp[:, :, pl, :], P[5][:, :].re("p (q n) -> p q n", n=128))
        for lh in range(2):
            tt("dve", Cq[:, :, pl, lh, :], Cp[:, :, pl, :],
               maskR[:, lh * 128:(lh + 1) * 128].un(1).bc([128, 4, 128]), ALU.mult)

    close_scope(scope0)
    Wkvu = sb("Wkvu", [128, 8, 1536], BF16)
    for kc in range(8):
        dma("pool", Wkvu[:, kc, :], w_in[kc * 128:(kc + 1) * 128, 512:2048])
    Wg = sb("Wg", [128, 4, 512], BF16)
    for kc in range(4):
        dma("pool", Wg[:, kc, :], glu_w[kc * 128:(kc + 1) * 128, :])

    xblk = [sb("xblk%d" % i, [128, D]) for i in range(2)]
    junk = sb("junk", [128, D])
    ssq = sb("ssq", [128, 1])
    rstd = sb("rstd", [128, 1])
    xn = sb("xn", [128, D])
    xnT = sb("xnT", [128, 8, 128], BF16)
    ktb = sb("ktb", [128, 4, 128], BF16)
    vb = sb("vb", [128, 512], BF16)
    uT = sb("uT", [128, 4, 128], BF16)
    zt = sb("zt", [128, 16, 2, 128], BF16)
    tz = [sb("tz%d" % i, [128, 8, 128]) for i in range(4)]
    cg = sb("cg", [128, 8, 2, 128])
    xT = sb("xT", [128, 16, 2, 128], BF16)
    Gre = sb("Gre", [128, 16])
    Gim = sb("Gim", [128, 16])
    xlast = sb("xlast", [128, 16, 2])
    yv = sb("yv", [128, 4, 128])
    y2 = sb("y2", [128, 4, 128])
    y3 = sb("y3", [128, 4, 128])
    yg = sb("yg", [128, 4, 128])
    ygb = sb("ygb", [128, 4, 128], BF16)
    sgl = sb("sgl", [128, 4, 128])
    ssmT = sb("ssmT", [128, 4, 128], BF16)
    S.op("dve", lambda e: e.memset(Gre[:].ap, 0.0), writes=[Gre.buf])
    S.op("dve", lambda e: e.memset(Gim[:].ap, 0.0), writes=[Gim.buf])
    T1re3 = T1re[:].re("p (g n) -> p g n", n=128)
    T1im3 = T1im[:].re("p (g n) -> p g n", n=128)

    def rmsnorm(xv, grep, outv):
        act(junk[:], xv, AF.Square, accum=ssq[:])
        act(rstd[:], ssq[:], AF.Ln, scale=1.0 / D, bias=epsc[:])
        act(rstd[:], rstd[:], AF.Exp, scale=-0.5)
        stt(outv, xv, rstd[:], grep[:], ALU.mult, ALU.mult)

    S.op("dve", lambda e: e.memset(epsc[:].ap, 1e-6), writes=[epsc.buf])

    def transpose8(src, dstT, col0, ncol_total):
        for half in range(2):
            for k in range(4):
                kc = half * 4 + k
                tr(P[6 + half][:, k * 128:(k + 1) * 128], src[:, kc * 128:(kc + 1) * 128], ident[:])
            cp("act" if half == 0 else "dve", dstT[:, half * 4:half * 4 + 4, col0:col0 + 128],
               P[6 + half][:, :].re("p (k n) -> p k n", n=128))

    for blk in range(NB):
        xb_ = xblk[blk % 2]
        dma("sp", xb_[:], x_all[blk * 128:(blk + 1) * 128, :])
        rmsnorm(xb_[:], grep_mix, xn[:])
        transpose8(xn, xnT, 0, 128)
        for m in range(4):
            for kc in range(8):
                mm(P[0][:, m * 128:(m + 1) * 128], Wkvu[:, kc, m * 128:(m + 1) * 128], xnT[:, kc, :],
                   start=(kc == 0), stop=(kc == 7))
        cp("act", ktb[:], P[0][:, :].re("p (m n) -> p m n", n=128))
        dma("sp", kt_scr[:].re("(m q) n -> q m n", q=128)[:, :, blk * 128:(blk + 1) * 128], ktb[:])
        for kc in range(8):
            mm(P[2][:, :], xnT[:, kc, :], Wkvu[:, kc, 512:1024], start=(kc == 0), stop=(kc == 7))
        cp("act", vb[:], P[2][:, :])
        dma("sp", v_scr[:].re("h j b d -> j h b d")[:, :, blk, :], vb[:].re("j (h d) -> j h d", d=64))
        for m in range(4):
            for kc in range(8):
                mm(P[1][:, m * 128:(m + 1) * 128], Wkvu[:, kc, 1024 + m * 128:1024 + (m + 1) * 128],
                   xnT[:, kc, :], start=(kc == 0), stop=(kc == 7))
        cp("act", uT[:], P[1][:, :].re("p (m n) -> p m n", n=128))
        if blk == 1:
            dbg("uT", uT[:], [128, 4, 128], BF16)
        for hf in range(2):
            for b4 in range(4):
                Q, h = hf * 2 + b4 // 2, b4 % 2
                mm(P[2 + b4][:, :], uT[64 * h:64 * h + 64, Q, :],
                   Bq[64 * h:64 * h + 64, Q, :, :, :].re("k l a n -> k (l a n)"))
            def buv(pl):
                vs = []
                for b4 in range(4):
                    vs.append(P[2 + b4][:, :].re("p (g a n) -> p g a n", a=2, n=128)[:, :, pl, :])
                return vs
            gs = slice(hf * 8, hf * 8 + 8)
            pvw = PQ[:, :].re("p (g a n) -> p g a n", a=2, n=128)
            tt("dve", tz[0][:], pvw[:, :, 0, :], T1re3[:, gs, :], ALU.mult)
            tt("dve", tz[1][:], pvw[:, :, 1, :], T1im3[:, gs, :], ALU.mult)
            tt("dve", tz[2][:], pvw[:, :, 0, :], T1im3[:, gs, :], ALU.mult)
            tt("dve", tz[3][:], pvw[:, :, 1, :], T1re3[:, gs, :], ALU.mult)
            tt("dve", zt[:, gs, 0, :], tz[0][:], tz[1][:], ALU.subtract)
            tt("dve", zt[:, gs, 1, :], tz[2][:], tz[3][:], ALU.add)
        for hf in range(2):
            gs = slice(hf * 8, hf * 8 + 8)
            for g8 in range(8):
                gp = hf * 8 + g8
                for pl in range(2):
                    idx = g8 * 2 + pl
                    mm(P[2 + idx // 4][:, (idx % 4) * 128:(idx % 4 + 1) * 128], zt[:, gp, pl, :], trile[:])
            pvw = PQ[:, :].re("p (g a n) -> p g a n", a=2, n=128)
            tt("dve", cg[:, :, 0, :], pvw[:, :, 0, :], Gre[:, gs].un(2).bc([128, 8, 128]), ALU.add)
            tt("dve", cg[:, :, 1, :], pvw[:, :, 1, :], Gim[:, gs].un(2).bc([128, 8, 128]), ALU.add)
            tt("dve", tz[0][:], cg[:, :, 0, :], T2re3[:, gs, :], ALU.mult)
            tt("dve", tz[1][:], cg[:, :, 1, :], T2im3[:, gs, :], ALU.mult)
            tt("dve", tz[2][:], cg[:, :, 1, :], T2re3[:, gs, :], ALU.mult)
            tt("dve", tz[3][:], cg[:, :, 0, :], T2im3[:, gs, :], ALU.mult)
            tt("dve", xT[:, gs, 0, :], tz[0][:], tz[1][:], ALU.subtract)
            tt("dve", xT[:, gs, 1, :], tz[2][:], tz[3][:], ALU.add)
            tt("dve", xlast[:, gs, 0], tz[0][:, :, 127], tz[1][:, :, 127], ALU.subtract)
            tt("dve", xlast[:, gs, 1], tz[2][:, :, 127], tz[3][:, :, 127], ALU.add)
        tt("dve", s16[0][:], xlast[:, :, 0], are[:], ALU.mult)
        tt("dve", s16[1][:], xlast[:, :, 1], aim[:], ALU.mult)
        tt("dve", s16[2][:], xlast[:, :, 0], aim[:], ALU.mult)
        tt("dve", s16[3][:], xlast[:, :, 1], are[:], ALU.mult)
        tt("dve", Gre[:], s16[0][:], s16[1][:], ALU.subtract)
        tt("dve", Gim[:], s16[2][:], s16[3][:], ALU.add)
        for Q in range(4):
            for h in range(2):
                i = 0
                for lh in range(2):
                    gp = 4 * Q + 2 * h + lh
                    for pl in range(2):
                        mm(P[1][64 * h:64 * h + 64, Q * 128:(Q + 1) * 128], Cq[:, Q, pl, lh, 64 * h:64 * h + 64],
                           xT[:, gp, pl, :], start=(i == 0), stop=(i == 3))
                        i += 1
        for Q in range(4):
            stt(yv[:, Q, :], uT[:, Q, :], dcol[:, Q:Q + 1], P[1][:, Q * 128:(Q + 1) * 128], ALU.mult, ALU.add)
        if blk == 1:
            dbg("yv", yv[:], [128, 4, 128])
        tt("dve", y2[:], yv[:], yv[:], ALU.mult)
        ts("dve", y2[:], y2[:], 0.044715, ALU.mult, 1.0, ALU.add)
        tt("dve", y3[:], y2[:], yv[:], ALU.mult)
        act(y3[:], y3[:], AF.Sigmoid, scale=1.5957691216057308)
        tt("dve", yg[:], yv[:], y3[:], ALU.mult)
        cp("act", ygb[:], yg[:])
        for co in range(4):
            for kc in range(4):
                mm(P[0][:, co * 128:(co + 1) * 128], Wg[:, kc, co * 128:(co + 1) * 128], ygb[:, kc, :],
                   start=(kc == 0), stop=(kc == 3))
        for co in range(4):
            act(sgl[:, co, :], P[0][:, co * 128:(co + 1) * 128], AF.Sigmoid, bias=gbcol[:, co:co + 1])
        tt("dve", ssmT[:], yg[:], sgl[:], ALU.mult)
        dma("sp", ssm_scr[:].re("q p n -> p q n")[:, :, blk * 128:(blk + 1) * 128], ssmT[:])

    close_scope(scopeA)
    if nphase < 2:
        while len(cur) > 1:
            cur[0].close()
            cur.pop(0)
        return finish()
    scope2 = open_scope()
    mask0 = sb("mask0", [128, 512], BF16)
    maske = sb("maske", [128, 8, 512], BF16)
    masko = sb("masko", [128, 8, 512], BF16)
    dma("sp", mask0[:], k_mask0[:])
    dma("sp", maske[:], k_maske[:])
    dma("sp", masko[:], k_masko[:])
    Wr = sb("Wr", [128, 8, 36])
    dma("sp", Wr[:], w_r[:].re("(kc p) n -> p kc n", p=128))
    Wpa = sb("Wpa", [64, 8, D], BF16)
    Wpb = sb("Wpb", [128, 4, D], BF16)
    Wo = sb("Wo", [128, 8, D], BF16)
    for h in range(8):
        dma("pool", Wpa[:, h, :], w_pa[h * 64:(h + 1) * 64, :])
    for kc in range(4):
        dma("pool", Wpb[:, kc, :], w_pb[kc * 128:(kc + 1) * 128, :])
    for kc in range(8):
        dma("pool", Wo[:, kc, :], w_o[kc * 128:(kc + 1) * 128, :])
    wring = [sb("wring%d" % i, [128, 8, 512], BF16) for i in range(2)]
    wri = [0]

    def load_wchunk(c0):
        t = wring[wri[0] % 2]
        wri[0] += 1
        for kc in range(8):
            dma("pool", t[:, kc, :], w_in[kc * 128:(kc + 1) * 128, c0:c0 + 512])
        return t

    xo = [sb("xo%d" % i, [128, D]) for i in range(2)]
    xn2 = sb("xn2", [128, D])
    junk2 = sb("junk2", [128, D], BF16)
    ssq2 = sb("ssq2", [128, 1])
    rstd2 = sb("rstd2", [128, 1])
    xoT = sb("xoT", [128, 8, 512], BF16)
    qT = sb("qT", [64, 8, 512], BF16)
    ktc = [[sb("ktc%d_%d" % (a, i), [64, 1024], BF16) for i in range(2)] for a in range(2)]
    vtc = [[sb("vtc%d_%d" % (a, i), [128, 8, 64], BF16) for i in range(2)] for a in range(2)]
    erw = [sb("erw%d" % i, [128, 1024], BF16) for i in range(2)]
    sprw = [sb("sprw%d" % i, [128, 1024], BF16) for i in range(2)]
    trw = sb("trw", [128, 1024], BF16)
    wrw = [sb("wrw%d" % i, [128, 1024], BF16) for i in range(2)]
    attnT = sb("attnT", [64, 8, 512], BF16)
    ssA = sb("ssA", [128, 4, 512], BF16)
    ssB = sb("ssB", [128, 4, 512], BF16)
    ssO = ssA
    sg1 = sb("sg1", [128, 512], BF16)
    sg2 = sb("sg2", [128, 512], BF16)
    mg1 = sb("mg1", [128, 512])
    mg2 = sb("mg2", [128, 512])
    mergedT = sb("mergedT", [128, 8, 512], BF16)
    hblk = [sb("hblk0", [128, D])] * 2
    hn = [xn2, xn2]
    hnT = sb("hnT", [128, 8, 128])
    lgt = sb("lgt", [128, 36])
    r1 = [sb("r1_%d" % i, [128, 1]) for i in range(12)]
    ohg = sb("ohg", [128, 4])
    eg = sb("eg", [128, 4])
    selv = sb("selv", [128, 8])
    m8 = sb("m8", [128, 8])
    oh1 = sb("oh1", [128, 8])
    oh2 = sb("oh2", [128, 8])
    M12 = sb("M12", [128, 64])
    pm = sb("pm", [128, 64])
    pmt = sb("pmt", [128, 64])
    base = sb("base", [128, 32])
    destf = sb("destf", [128, 2])
    S.op("dve", lambda e: e.memset(base[:].ap, 0.0), writes=[base.buf])

    def rmsnorm2(xv, grep, outv):
        act(junk2[:], xv, AF.Square, accum=ssq2[:])
        act(rstd2[:], ssq2[:], AF.Ln, scale=1.0 / D, bias=epsc[:])
        act(rstd2[:], rstd2[:], AF.Exp, scale=-0.5)
        stt(outv, xv, rstd2[:], grep[:], ALU.mult, ALU.mult)

    def mmacc(out, lhsT, rhs, start):
        S.op("pe", lambda e: e.matmul(out.ap, lhsT.ap, rhs.ap, start=start, stop=True, skip_group_check=True),
             reads=[lhsT.buf, rhs.buf], writes=[out.buf])

    for n in range(NSLOT):
        pmask = maske if n % 2 == 0 else masko
        for r in range(4):
            xb_ = xo[r % 2]
            dma("sp", xb_[:], x_own[(4 * n + r) * 128:(4 * n + r + 1) * 128, :])
            rmsnorm2(xb_[:], grep_mix, xn2[:])
            transpose8(xn2, xoT, r * 128, 512)
        wq = load_wchunk(0)
        for h in range(8):
            pb = P[4 + h % 4]
            for kc in range(8):
                mm(pb[0:64, :], wq[:, kc, h * 64:(h + 1) * 64], xoT[:, kc, :], start=(kc == 0), stop=(kc == 7))
            cp("act" if h % 2 == 0 else "dve", qT[:, h, :], pb[0:64, :])
        if n == 0:
            dbg("qT", qT[:], [64, 8, 512], BF16)
        nkb = 8 * n + 9
        kbs = [8 * n + 8 - s for s in range(nkb)]
        for hp in range(4):
            heads = (2 * hp, 2 * hp + 1)
            OB = (P[2], P[3])
            ZW = (P45, P67)
            cur_chunk = [None, None]
            cur_kt = [None, None]
            cur_vt = [None, None]
            nload = [0, 0]

            def get_kv(a, kb):
                h = heads[a]
                c = -1 if kb == 0 else (kb - 1) // 8
                if cur_chunk[a] != c:
                    i = nload[a] % 2
                    nload[a] += 1
                    kt_, vt_ = ktc[a][i], vtc[a][i]
                    if c < 0:
                        dma("sp", kt_[:, 0:128], kt_scr[h * 64:(h + 1) * 64, 0:128])
                        dma("sp", vt_[:, 0:1, :], v_scr[h, :, 0:1, :])
                    else:
                        dma("sp", kt_[:, :], kt_scr[h * 64:(h + 1) * 64, (8 * c + 1) * 128:(8 * c + 9) * 128])
                        dma("sp", vt_[:, :, :], v_scr[h, :, 8 * c + 1:8 * c + 9, :])
                    cur_chunk[a], cur_kt[a], cur_vt[a] = c, kt_, vt_
                lb = 0 if kb == 0 else (kb - 1) % 8
                return cur_kt[a][:, lb * 128:(lb + 1) * 128], cur_vt[a][:, lb, :]

            vsel = {}

            def front(s):
                kb = kbs[s]
                Z = ZW[s % 2]
                for a in range(2):
                    ktv, vtv = get_kv(a, kb)
                    vsel[(a, s)] = vtv
                    mm(Z[:, a * 512:(a + 1) * 512], ktv, qT[:, heads[a], :])
                e_ = erw[s % 2]
                act(e_[:], Z[:, :], AF.Exp, scale=0.125)
                e3 = e_[:].re("p (a n) -> p a n", a=2)
                if s < 8:
                    tt("dve", e3, e3, pmask[:, s, :].un(1).bc([128, 2, 512]), ALU.mult)
                elif kb == 0:
                    tt("dve", e3, e3, mask0[:].un(1).bc([128, 2, 512]), ALU.mult)
                act(sprw[s % 2][:], e_[:], AF.Ln, bias=onec[:])

            front(0)
            for s in range(nkb):
                sp_ = sprw[s % 2]
                for a in range(2):
                    mmacc(P01[:, a * 512:(a + 1) * 512], trige[:], sp_[:, a * 512:(a + 1) * 512], start=(s == 0))
                act(trw[:], P01[:, :], AF.Exp, scale=-1.0)
                if s + 1 < nkb:
                    front(s + 1)
                for a in range(2):
                    mmacc(P01[:, a * 512:(a + 1) * 512], trilt[:], sp_[:, a * 512:(a + 1) * 512], start=False)
                w_ = wrw[s % 2]
                tt("dve", w_[:], erw[s % 2][:], trw[:], ALU.mult)
                for a in range(2):
                    mmacc(OB[a][0:64, :], vsel[(a, s)], w_[:, a * 512:(a + 1) * 512], start=(s == 0))
            for a in range(2):
                cp("act" if a == 0 else "dve", attnT[:, heads[a], :], OB[a][0:64, :])
        if n == 0:
            dbg("attnT", attnT[:], [64, 8, 512], BF16)
        posA = (8 * n + 1) * 128
        posB = (8 * n + 5) * 128
        sv = ssm_scr[:].re("q p n -> p q n")
        dma("sp", ssA[:], sv[:, :, posA:posA + 512])
        dma("sp", ssB[:], sv[:, :, posB:posB + 512])
        ts("dve", ssO[:], ssA[:], sel[:, 2 * n:2 * n + 1], ALU.mult)
        stt(ssO[:], ssB[:], sel[:, 2 * n + 1:2 * n + 2], ssO[:], ALU.mult, ALU.add)
        for c2 in range(2):
            wga = load_wchunk(2048 + c2 * 512)
            wgb = load_wchunk(3072 + c2 * 512)
            for c4 in range(4):
                co = c2 * 4 + c4
                for kc in range(8):
                    mm(P[4][:, :], wga[:, kc, c4 * 128:(c4 + 1) * 128], xoT[:, kc, :], start=(kc == 0), stop=(kc == 7))
                for kc in range(8):
                    mm(P[5][:, :], wgb[:, kc, c4 * 128:(c4 + 1) * 128], xoT[:, kc, :], start=(kc == 0), stop=(kc == 7))
                for h in range(8):
                    mm(P[6][:, :], Wpa[:, h, co * 128:(co + 1) * 128], attnT[:, h, :], start=(h == 0), stop=(h == 7))
                for kc in range(4):
                    mm(P[7][:, :], Wpb[:, kc, co * 128:(co + 1) * 128], ssO[:, kc, :], start=(kc == 0), stop=(kc == 3))
                act(sg1[:], P[4][:, :], AF.Sigmoid)
                act(sg2[:], P[5][:, :], AF.Sigmoid)
                tt("dve", mg1[:], P[6][:, :], sg1[:], ALU.mult)
                tt("dve", mg2[:], P[7][:, :], sg2[:], ALU.mult)
                tt("dve", mergedT[:, co, :], mg1[:], mg2[:], ALU.add)
        if n == 0:
            dbg("mergedT", mergedT[:], [128, 8, 512], BF16)
        for r in range(4):
            ob = 4 * n + r
            xb_ = xo[r % 2]
            hb_ = hblk[r % 2]
            hn_ = hn[r % 2]
            dma("sp", xb_[:], x_own[ob * 128:(ob + 1) * 128, :])
            for half in range(2):
                for kc in range(8):
                    mm(P[half][:, :], mergedT[:, kc, r * 128:(r + 1) * 128], Wo[:, kc, half * 512:(half + 1) * 512],
                       start=(kc == 0), stop=(kc == 7))
                tt("dve", hb_[:, half * 512:(half + 1) * 512], P[half][:, :], xb_[:, half * 512:(half + 1) * 512],
                   ALU.add)
            dma("sp", h_scr[ob * 128:(ob + 1) * 128, :], hb_[:])
            rmsnorm2(hb_[:], grep_ffn, hn_[:])
            for half in range(2):
                for k in range(4):
                    kc = half * 4 + k
                    tr(P[6 + half][:, k * 128:(k + 1) * 128], hn_[:, kc * 128:(kc + 1) * 128], ident[:])
                cp("act" if half == 0 else "dve", hnT[:, half * 4:half * 4 + 4, :],
                   P[6 + half][:, :].re("p (k n) -> p k n", n=128))
            for kc in range(8):
                mm(P[2][:, 0:36], hnT[:, kc, :], Wr[:, kc, :], start=(kc == 0), stop=(kc == 7))
            tt("dve", lgt[:], P[2][:, 0:36], brrep[:], ALU.add)
            gmax, ngmax, gsum, ptop, dv, ex, g1, g2, p1loc, p2loc, ov1, ov2 = [t_[:] for t_ in r1]
            S.op("dve", lambda e: e.tensor_reduce(gmax.ap, lgt[:, 0:4].ap, AX.X, ALU.max),
                 reads=[lgt.buf], writes=[gmax.buf])
            ts("dve", ohg[:], lgt[:, 0:4], gmax, ALU.is_equal)
            ts("dve", ngmax, gmax, -1.0, ALU.mult)
            act(eg[:], lgt[:, 0:4], AF.Exp, bias=ngmax, accum=gsum)
            S.op("dve", lambda e: e.reciprocal(ptop.ap, gsum.ap), reads=[gsum.buf], writes=[ptop.buf])
            ts("dve", selv[:], lgt[:, 4:12], ohg[:, 0:1], ALU.mult)
            for g in range(1, 4):
                stt(selv[:], lgt[:, 4 + 8 * g:12 + 8 * g], ohg[:, g:g + 1], selv[:], ALU.mult, ALU.add)
            S.op("dve", lambda e: e.max(m8[:].ap, selv[:].ap), reads=[selv.buf], writes=[m8.buf])
            ts("dve", oh1[:], selv[:], m8[:, 0:1], ALU.is_equal)
            ts("dve", oh2[:], selv[:], m8[:, 1:2], ALU.is_equal)
            tt("dve", dv, m8[:, 1:2], m8[:, 0:1], ALU.subtract)
            act(ex, dv, AF.Exp)
            ts("dve", g1, ex, 1.0, ALU.add)
            S.op("dve", lambda e: e.reciprocal(g1.ap, g1.ap), reads=[g1.buf], writes=[g1.buf])
            tt("dve", g2, ex, g1, ALU.mult)
            tt("dve", gwall[:, ob, 0:1], g1, ptop, ALU.mult)
            tt("dve", gwall[:, ob, 1:2], g2, ptop, ALU.mult)
            tt("dve", M12[:, 0:32].re("p (g e) -> p g e", e=8), ohg[:].un(2).bc([128, 4, 8]),
               oh1[:].un(1).bc([128, 4, 8]), ALU.mult)
            tt("dve", M12[:, 32:64].re("p (g e) -> p g e", e=8), ohg[:].un(2).bc([128, 4, 8]),
               oh2[:].un(1).bc([128, 4, 8]), ALU.mult)
            mm(P[3][:, 0:64], triltf[:], M12[:])
            mm(P[3][:, 64:128], onesf[:], M12[:])
            tt("dve", pm[:, 0:32], P[3][:, 0:32], base[:], ALU.add)
            tt("dve", pm[:, 32:64], P[3][:, 32:64], base[:], ALU.add)
            tt("dve", pm[:, 32:64], P[3][:, 64:96], pm[:, 32:64], ALU.add)
            tt("dve", pmt[:], pm[:], M12[:], ALU.mult)
            S.op("dve", lambda e: e.tensor_reduce(p1loc.ap, pmt[:, 0:32].ap, AX.X, ALU.add),
                 reads=[pmt.buf], writes=[p1loc.buf])
            S.op("dve", lambda e: e.tensor_reduce(p2loc.ap, pmt[:, 32:64].ap, AX.X, ALU.add),
                 reads=[pmt.buf], writes=[p2loc.buf])
            tt("dve", pmt[:, 0:32], M12[:, 0:32], ecap[:], ALU.mult)
            tt("dve", pmt[:, 32:64], M12[:, 32:64], ecap[:], ALU.mult)
            S.op("dve", lambda e: e.tensor_reduce(ov1.ap, pmt[:, 0:32].ap, AX.X, ALU.add),
                 reads=[pmt.buf], writes=[ov1.buf])
            S.op("dve", lambda e: e.tensor_reduce(ov2.ap, pmt[:, 32:64].ap, AX.X, ALU.add),
                 reads=[pmt.buf], writes=[ov2.buf])
            tt("dve", destf[:, 0:1], ov1, p1loc, ALU.add)
            tt("dve", destf[:, 1:2], ov2, p2loc, ALU.add)
            ts("dve", ov1, p1loc, float(CAP) - 0.5, ALU.is_gt, float(4 * NXB), ALU.mult)
            ts("dve", ov2, p2loc, float(CAP) - 0.5, ALU.is_gt, float(4 * NXB), ALU.mult)
            tt("dve", destf[:, 0:1], destf[:, 0:1], ov1, ALU.add)
            tt("dve", destf[:, 1:2], destf[:, 1:2], ov2, ALU.add)
            cp("dve", destall[:, ob, :], destf[:])
            tt("dve", base[:], P[3][:, 64:96], base[:], ALU.add)
            tt("dve", base[:], P[3][:, 96:128], base[:], ALU.add)
            for k in range(2 if "noscat" not in debug else 0):
                dv_ = destall[:, ob, k:k + 1]
                S.dma("pool", (lambda dv_, hn_: (lambda e: e.indirect_dma_start(
                    out=xb_scr[:].ap, out_offset=bass.IndirectOffsetOnAxis(ap=dv_.ap, axis=0),
                    in_=hn_[:].ap, in_offset=None, bounds_check=bc_reg(e), oob_is_err=False)))(dv_, hn_),
                    reads=[hn_.buf, destall.buf], writes=[xb_scr.buf])
    dbg("destall", destall[:], [128, NOB, 2], I32)
    dbg("gwall", gwall[:], [128, NOB, 2])
    close_scope(scope2)
    if nphase < 3 or not moe:
        return finish()
    if nphase >= 3 and moe:
        scope3 = open_scope()
        W1 = [sb("W1_%d" % i, [128, 8, 512], BF16) for i in range(2)]
        W3 = [sb("W3_%d" % i, [128, 8, 512], BF16) for i in range(2)]
        W2 = [sb("W2_%d" % i, [128, 4, D], BF16) for i in range(2)]
        stg1 = sb("stg1", [128, 8, 512])
        stg3 = sb("stg3", [128, 8, 512])
        stg2 = sb("stg2", [128, 4, D])
        xg = [sb("xg%d" % i, [128, D]) for i in range(2)]
        xeT = sb("xeT", [128, 8, CAP], BF16)
        s1 = sb("s1", [128, CAP])
        hgT = sb("hgT", [128, 4, CAP], BF16)
        ybk = [sb("ybk%d" % i, [128, D]) for i in range(2)]

        def load_expert(ex_):
            dma("sp", stg1[:], w1[ex_].re("(p kc) f -> p kc f", p=128))
            dma("sp", stg3[:], w3[ex_].re("(p kc) f -> p kc f", p=128))
            dma("sp", stg2[:], w2[ex_].re("(p fc) n -> p fc n", p=128))

        def cast_expert(ex_):
            i = ex_ % 2
            cp("act", W1[i][:, 0:4, :], stg1[:, 0:4, :])
            cp("dve", W1[i][:, 4:8, :], stg1[:, 4:8, :])
            cp("act", W3[i][:, 0:4, :], stg3[:, 0:4, :])
            cp("dve", W3[i][:, 4:8, :], stg3[:, 4:8, :])
            cp("act", W2[i][:, 0:2, :], stg2[:, 0:2, :])
            cp("dve", W2[i][:, 2:4, :], stg2[:, 2:4, :])

        load_expert(0)
        cast_expert(0)
        for ex_ in range(32):
            w1_, w3_, w2_ = W1[ex_ % 2], W3[ex_ % 2], W2[ex_ % 2]
            if ex_ + 1 < 32:
                load_expert(ex_ + 1)
            for c in range(CAPB):
                xg_ = xg[c % 2]
                dma("sp", xg_[:], xb_scr[ex_ * CAP + c * 128:ex_ * CAP + (c + 1) * 128, :])
                xg3 = xg_[:].re("t (p k) -> t k p", k=8)
                for half in range(2):
                    for k in range(4):
                        kc = half * 4 + k
                        tr(P[6 + half][:, k * 128:(k + 1) * 128], xg3[:, kc, :], ident[:])
                    cp("act" if half == 0 else "dve", xeT[:, half * 4:half * 4 + 4, c * 128:(c + 1) * 128],
                       P[6 + half][:, :].re("p (k n) -> p k n", n=128))
            for fc in range(4):
                for kc in range(8):
                    mm(P[0][:, 0:CAP], w1_[:, kc, :].re("p (m f) -> p f m", f=4)[:, fc, :], xeT[:, kc, :],
                       start=(kc == 0), stop=(kc == 7))
                for kc in range(8):
                    mm(P[1][:, 0:CAP], w3_[:, kc, :].re("p (m f) -> p f m", f=4)[:, fc, :], xeT[:, kc, :],
                       start=(kc == 0), stop=(kc == 7))
                act(s1[:], P[0][:, 0:CAP], AF.Silu)
                tt("dve", hgT[:, fc, :], s1[:], P[1][:, 0:CAP], ALU.mult)
            for c in range(CAPB):
                yb_ = ybk[c % 2]
                for half in range(2):
                    for fc in range(4):
                        mm(P[2 + half][:, :], hgT[:, fc, c * 128:(c + 1) * 128], w2_[:, fc, half * 512:(half + 1) * 512],
                           start=(fc == 0), stop=(fc == 3))
                    cp("act" if half == 0 else "dve", yb_[:, half * 512:(half + 1) * 512], P[2 + half][:, :])
                dma("sp", yb_scr[ex_ * CAP + c * 128:ex_ * CAP + (c + 1) * 128, :], yb_[:])
            if ex_ + 1 < 32:
                cast_expert(ex_ + 1)
        if nphase < 4:
            close_scope(scope3)
            return finish()
        yk = [sb("yk%d" % i, [128, D]) for i in range(2)]
        hb4 = [sb("hb4_%d" % i, [128, D]) for i in range(2)]
        oo = [sb("oo%d" % i, [128, D]) for i in range(2)]
        junk4 = sb("junk4", [128, D], BF16)
        ssq4 = sb("ssq4", [128, 1])
        rstd4 = sb("rstd4", [128, 1])
        for ob in range(NOB):
            hb_ = hb4[ob % 2]
            dma("sp", hb_[:], h_scr[ob * 128:(ob + 1) * 128, :])
            for k in range(2):
                yk_ = yk[k]
                S.op("pool", (lambda yk_: (lambda e: e.memset(yk_[:].ap, 0.0)))(yk_), writes=[yk_.buf])
                dv_ = destall[:, ob, k:k + 1]
                S.dma("pool", (lambda dv_, yk_: (lambda e: e.indirect_dma_start(
                    out=yk_[:].ap, out_offset=None, in_=yb_scr[:].ap,
                    in_offset=bass.IndirectOffsetOnAxis(ap=dv_.ap, axis=0),
                    bounds_check=bc_reg(e), oob_is_err=False)))(dv_, yk_),
                    reads=[yb_scr.buf, destall.buf], writes=[yk_.buf])
                stt(hb_[:], yk_[:], gwall[:, ob, k:k + 1], hb_[:], ALU.mult, ALU.add)
            oo_ = oo[ob % 2]
            act(junk4[:], hb_[:], AF.Square, accum=ssq4[:])
            act(rstd4[:], ssq4[:], AF.Ln, scale=1.0 / D, bias=epsc[:])
            act(rstd4[:], rstd4[:], AF.Exp, scale=-0.5)
            stt(oo_[:], hb_[:], rstd4[:], grep_fin[:], ALU.mult, ALU.mult)
            dma("sp", out_own[ob * 128:(ob + 1) * 128, :], oo_[:])
        close_scope(scope3)

    return finish()


def _consts(NSLOT, CAP):
    bf = ml_dtypes.bfloat16
    r = np.arange(128)[:, None]
    c = np.arange(128)[None, :]
    k = {}
    k["k_ident"] = (r == c).astype(np.float32)
    k["k_triltf"] = (r < c).astype(np.float32)
    k["k_onesf"] = np.ones((128, 128), np.float32)
    k["k_jrow"] = np.arange(128, dtype=np.float32)[None, :]
    mB = np.zeros((128, 2, 16), np.float32)
    for g2 in range(2):
        mB[g2 * 64:(g2 + 1) * 64, g2, :] = 1.0
    k["k_maskB"] = mB.reshape(128, 32)
    mC = np.zeros((128, 2, 64), np.float32)
    for rr in range(128):
        mC[rr, (rr // 16) % 2, :] = 1.0
    k["k_maskC"] = mC.reshape(128, 128)
    mQ = np.zeros((128, 2), np.float32)
    for rr in range(128):
        mQ[rr, (rr // 32) % 2] = 1.0
    k["k_maskQ"] = mQ
    mR = np.zeros((128, 2, 128), np.float32)
    for cc in range(128):
        mR[:, (cc // 32) % 2, cc] = 1.0
    k["k_maskR"] = mR.reshape(128, 256)
    k["k_ecap"] = np.tile((np.arange(32, dtype=np.float32) * CAP)[None, :], (128, 1))
    k["k_trige"] = (r >= c).astype(bf)
    k["k_trilt"] = (r < c).astype(bf)
    k["k_trile"] = (r <= c).astype(bf)
    m0 = np.ones((128, 512), np.float32)
    m0[:112, :] = 0.0
    k["k_mask0"] = m0.astype(bf)
    j = np.arange(128)[:, None]
    t = np.arange(512)[None, :]
    patA = np.zeros((128, 8, 512), np.float32)
    patB = np.zeros((128, 8, 512), np.float32)
    for s in range(8):
        if s < 4:
            rr = 3 - s
            patB[:, s, :] = (rr * 128 + j < t)
        else:
            rr = 7 - s
            patA[:, s, :] = (rr * 128 + j < t)
            patB[:, s, :] = 1.0
    return k, patA.astype(bf), patB.astype(bf)


def own_is_A(n, hf):
    return ((n + hf) % 2) == 0


def make_in_maps(inputs, NSLOT, CAP, n_cores=8, moe=True):
    x = np.asarray(inputs["x"], np.float32)
    NB = 1 + 8 * NSLOT
    NPOS = NB * 128
    meta = np.asarray(inputs["meta_tokens"], np.float32)
    k, patA, patB = _consts(NSLOT, CAP)
    f = lambda a: np.ascontiguousarray(np.asarray(a, np.float32))
    shared = dict(
        w_in=f(inputs["w_in"][0]), g_mix=f(inputs["norm_mix_g"][0:1]), g_ffn=f(inputs["norm_ffn_g"][0:1]),
        g_fin=f(inputs["norm_final_g"]).reshape(1, D),
        lam_re=f(inputs["ssm_lambda_re"][0]).reshape(1, 2048), lam_im=f(inputs["ssm_lambda_im"][0]).reshape(1, 2048),
        log_dt=f(inputs["ssm_log_dt"][0]).reshape(1, 32),
        b_re=f(inputs["ssm_b_re"][0]), b_im=f(inputs["ssm_b_im"][0]),
        c_re=f(inputs["ssm_c_re"][0]).reshape(512, 64), c_im=f(inputs["ssm_c_im"][0]).reshape(512, 64),
        d_skip=f(inputs["ssm_d"][0]).reshape(1, 512), glu_w=f(inputs["ssm_glu_w"][0]),
        glu_b=f(inputs["ssm_glu_b"][0]).reshape(1, 512),
        w_pa=f(inputs["w_branch_attn"][0]), w_pb=f(inputs["w_branch_ssm"][0]), w_o=f(inputs["w_out"][0]),
        w_r=f(np.concatenate([inputs["router_group_w"][0], inputs["router_expert_w"][0]], axis=1)),
        b_r=f(np.concatenate([inputs["router_group_b"][0], inputs["router_expert_b"][0]], axis=0)).reshape(1, 36),
        w1=f(inputs["expert_w1"][0][:(32 if moe else 1)]), w3=f(inputs["expert_w3"][0][:(32 if moe else 1)]),
        w2=f(inputs["expert_w2"][0][:(32 if moe else 1)]),
    )
    shared.update(k)
    maps = []
    for core in range(n_cores):
        b, hf = core // 2, core % 2
        xa = np.zeros((NPOS, D), np.float32)
        xa[112:128] = meta
        xa[128:] = x[b, :NPOS - 128]
        xo = np.zeros((512 * NSLOT, D), np.float32)
        sel = np.zeros((128, 2 * NSLOT), np.float32)
        for n in range(NSLOT):
            a = own_is_A(n, hf)
            r0 = (8 * n + (0 if a else 4)) * 128
            xo[n * 512:(n + 1) * 512] = x[b, r0:r0 + 512]
            sel[:, 2 * n] = 1.0 if a else 0.0
            sel[:, 2 * n + 1] = 0.0 if a else 1.0
        m = dict(shared)
        m["x_all"] = xa
        m["x_own"] = xo
        m["k_sel"] = sel
        m["k_maske"] = patA if own_is_A(0, hf) else patB
        m["k_masko"] = patA if own_is_A(1, hf) else patB
        maps.append(m)
    return maps


def assemble(results, NSLOT, B, SEQ):
    out = np.zeros((B, SEQ, D), np.float32)
    for core, r in enumerate(results):
        b, hf = core // 2, core % 2
        oo = np.asarray(r["out_own"], np.float32)
        for n in range(NSLOT):
            a = own_is_A(n, hf)
            r0 = (8 * n + (0 if a else 4)) * 128
            out[b, r0:r0 + 512] = oo[n * 512:(n + 1) * 512]
    return out


_NC_CACHE = {}


def kernel(**inputs):
    NSLOT, CAPB = 8, 3
    key = (NSLOT, CAPB)
    if key not in _NC_CACHE:
        _NC_CACHE[key] = build(NSLOT, CAPB)
    nc = _NC_CACHE[key]
    maps = make_in_maps(inputs, NSLOT, 128 * CAPB)
    res = run_bass_kernel_spmd(nc, maps, core_ids=list(range(8)))
    return assemble(res.results, NSLOT, 4, 8192)
```
